# Optimizing a Trainium2 kernel written in Bass

```python
import jax
import jax.numpy as jnp
from jax import lax
import numpy as np

D_MODEL = 1024
BATCH = 4
SEQ = 8192
DEPTH = 1

GRID_W = 64
CTX_LEN = 256
N_MOD = 6
EPS = 1e-6
ROPE_BASE = 10000.0
Q_BLOCK = 128
RET_CHUNK = 128

MLA_HEADS = 8
MLA_NOPE = 64
MLA_ROPE = 32
MLA_QK = MLA_NOPE + MLA_ROPE
MLA_V = 64
Q_LORA = 256
KV_LORA = 128
RET_HEADS = 4
RET_DK = 64
RET_DV = 128
MIX_WIDTH = MLA_HEADS * MLA_V + RET_HEADS * RET_DV

OFF_Q = 0
OFF_KV = OFF_Q + Q_LORA
OFF_PE = OFF_KV + KV_LORA
OFF_RQ = OFF_PE + MLA_ROPE
OFF_RK = OFF_RQ + RET_HEADS * RET_DK
OFF_RV = OFF_RK + RET_HEADS * RET_DK
OFF_RG = OFF_RV + RET_HEADS * RET_DV
D_IN = OFF_RG + RET_HEADS * RET_DV

N_EXPERTS = 32
TOP_K = 4
D_FF = D_MODEL
SWIGLU_LIMIT = 7.0
SWIGLU_ALPHA = 1.702
MOE_BLOCK = 128

kernel_name = 'hybrid_mla_retention_moe_prefix_dit_layer'


def rms_norm(x, w):
    xf = x.astype(jnp.float32)
    y = xf * lax.rsqrt(jnp.mean(xf * xf, axis=-1, keepdims=True) + EPS)
    return (y * w.astype(jnp.float32)).astype(x.dtype)


def modulate(h, shift, scale):
    return h * (1 + scale) + shift


def axial_rope_tables(L, dim):
    rows = L // GRID_W
    nf = dim // 4
    inv = jnp.power(ROPE_BASE, -jnp.arange(nf, dtype=jnp.float32) / nf)
    row = jnp.repeat(jnp.arange(rows, dtype=jnp.float32), GRID_W)
    col = jnp.tile(jnp.arange(GRID_W, dtype=jnp.float32), rows)
    pos = jnp.stack([row, col], axis=-1)
    ang = pos[:, :, None] * inv
    ang = jnp.broadcast_to(ang[:, :, None, :], (L, 2, 2, nf)).reshape(L, dim)
    return jnp.cos(ang), jnp.sin(ang)


def apply_axial_rope(x, cos, sin):
    d = x.shape[-1]
    xa = x.reshape(x.shape[:-1] + (2, 2, d // 4))
    rot = jnp.stack([-xa[..., 1, :], xa[..., 0, :]], axis=-2).reshape(x.shape)
    return (x.astype(jnp.float32) * cos + rot.astype(jnp.float32) * sin).astype(x.dtype)


def merge_heads(o):
    B, H, L, dv = o.shape
    return o.transpose(0, 2, 1, 3).reshape(B, L, H * dv)


def mla_q(p, g_q_lora, w_q_up, g_q_head, rope):
    B, L, _ = p.shape
    c_q = rms_norm(p[..., OFF_Q:OFF_Q + Q_LORA], g_q_lora)
    q = (c_q @ w_q_up).reshape(B, L, MLA_HEADS, MLA_QK)
    q = rms_norm(q, g_q_head).transpose(0, 2, 1, 3)
    if rope is not None:
        q = jnp.concatenate([q[..., :MLA_NOPE], apply_axial_rope(q[..., MLA_NOPE:], *rope)], axis=-1)
    return q


def mla_kv(p, g_kv_lora, w_kv_up, g_k_head, rope):
    B, L, _ = p.shape
    c_kv = rms_norm(p[..., OFF_KV:OFF_KV + KV_LORA], g_kv_lora)
    k_pe = p[..., OFF_PE:OFF_PE + MLA_ROPE]
    kv = (c_kv @ w_kv_up).reshape(B, L, MLA_HEADS, MLA_NOPE + MLA_V)
    k_nope, v = kv[..., :MLA_NOPE], kv[..., MLA_NOPE:]
    k_pe = jnp.broadcast_to(k_pe[:, :, None, :], (B, L, MLA_HEADS, MLA_ROPE))
    k = rms_norm(jnp.concatenate([k_nope, k_pe], axis=-1), g_k_head).transpose(0, 2, 1, 3)
    if rope is not None:
        k = jnp.concatenate([k[..., :MLA_NOPE], apply_axial_rope(k[..., MLA_NOPE:], *rope)], axis=-1)
    return k, v.transpose(0, 2, 1, 3)


def attend_blocks(q, k, v):
    B, H, Lq, dq = q.shape
    nb = Lq // Q_BLOCK
    scale = dq ** -0.5
    qb = q.reshape(B, H, nb, Q_BLOCK, dq).transpose(2, 0, 1, 3, 4)

    def one_block(qi):
        s = jnp.einsum('bhqd,bhkd->bhqk', qi, k).astype(jnp.float32) * scale
        pr = jax.nn.softmax(s, axis=-1).astype(v.dtype)
        return jnp.einsum('bhqk,bhkd->bhqd', pr, v)

    o = lax.map(one_block, qb)
    return o.transpose(1, 2, 0, 3, 4).reshape(B, H, Lq, v.shape[-1])


def ret_q(p, rope):
    B, L, _ = p.shape
    q = p[..., OFF_RQ:OFF_RK].reshape(B, L, RET_HEADS, RET_DK).transpose(0, 2, 1, 3)
    return q if rope is None else apply_axial_rope(q, *rope)


def ret_kv(p, rope):
    B, L, _ = p.shape
    k = p[..., OFF_RK:OFF_RV].reshape(B, L, RET_HEADS, RET_DK).transpose(0, 2, 1, 3) * (RET_DK ** -0.5)
    v = p[..., OFF_RV:OFF_RG].reshape(B, L, RET_HEADS, RET_DV).transpose(0, 2, 1, 3)
    if rope is not None:
        k = apply_axial_rope(k, *rope)
    return k, v


def retention_state(k, v, log_gamma, reverse):
    L = k.shape[2]
    j = jnp.arange(L, dtype=jnp.float32)
    expo = j if reverse else (L - 1) - j
    w = jnp.exp(expo[None, :] * log_gamma[:, None])
    return jnp.einsum('bhld,bhle->bhde', k.astype(jnp.float32) * w[None, :, :, None],
                      v.astype(jnp.float32))


def retention_scan(q, k, v, log_gamma, s0):
    B, H, L, DK = q.shape
    DV = v.shape[-1]
    C = RET_CHUNK
    n = L // C
    to_chunks = lambda t: t.astype(jnp.float32).reshape(B, H, n, C, t.shape[-1]).transpose(2, 0, 1, 3, 4)
    i = jnp.arange(C, dtype=jnp.float32)
    diff = i[:, None] - i[None, :]
    causal = diff >= 0
    decay_mask = jnp.where(causal, jnp.exp(jnp.where(causal, diff, 0.0)[None] * log_gamma[:, None, None]), 0.0)
    q_decay = jnp.exp((i + 1.0)[None, :] * log_gamma[:, None])[..., None]
    k_decay = jnp.exp(((C - 1.0) - i)[None, :] * log_gamma[:, None])[..., None]
    chunk_decay = jnp.exp(C * log_gamma)[:, None, None]

    def step(s, inp):
        qc, kc, vc = inp
        inner = jnp.einsum('bhid,bhjd->bhij', qc, kc) * decay_mask
        o = jnp.einsum('bhij,bhje->bhie', inner, vc) + jnp.einsum('bhid,bhde->bhie', qc * q_decay, s)
        s_new = s * chunk_decay + jnp.einsum('bhjd,bhje->bhde', kc * k_decay, vc)
        return s_new, o

    _, o = lax.scan(step, s0.astype(jnp.float32), (to_chunks(q), to_chunks(k), to_chunks(v)))
    return o.transpose(1, 2, 0, 3, 4).reshape(B, H, L, DV)


def retention_bidir(q, k, v, log_gamma, s0_f, s0_b):
    flip = lambda t: jnp.flip(t, axis=2)
    o_f = retention_scan(q, k, v, log_gamma[0], s0_f)
    o_b = flip(retention_scan(flip(q), flip(k), flip(v), log_gamma[1], s0_b))
    return o_f + o_b


def ret_output(o, p, g_ret_out):
    B, H, L, DV = o.shape
    o = rms_norm(o.transpose(0, 2, 1, 3), g_ret_out.reshape(H, DV)).reshape(B, L, H * DV)
    gate = p[..., OFF_RG:D_IN].astype(jnp.float32)
    return (o * jax.nn.silu(gate)).astype(p.dtype)


def moe_ffn(h, w_router, b_router, w1, b1, w2, b2):
    T, D = h.shape
    N = T * TOP_K
    logits = (h @ w_router).astype(jnp.float32) + b_router.astype(jnp.float32)
    top_val, top_idx = lax.top_k(logits, TOP_K)
    gates = jax.nn.softmax(top_val, axis=-1)
    flat_e = top_idx.reshape(N).astype(jnp.int32)
    flat_g = gates.reshape(N)
    ar = jnp.arange(N, dtype=jnp.int32)
    flat_tok = ar // TOP_K
    order = jnp.argsort(flat_e)
    sorted_e = flat_e[order]
    counts = jax.ops.segment_sum(jnp.ones((N,), jnp.int32), flat_e, num_segments=N_EXPERTS)
    padded = ((counts + MOE_BLOCK - 1) // MOE_BLOCK) * MOE_BLOCK
    pad_end = jnp.cumsum(padded)
    pad_start = pad_end - padded
    start = jnp.cumsum(counts) - counts
    dest = pad_start[sorted_e] + (ar - start[sorted_e])
    n_blocks = -(-N // MOE_BLOCK) + N_EXPERTS
    n_pad = n_blocks * MOE_BLOCK
    buf_tok = jnp.full((n_pad,), T, jnp.int32).at[dest].set(flat_tok[order])
    buf_g = jnp.zeros((n_pad,), jnp.float32).at[dest].set(flat_g[order])
    block_e = jnp.minimum(jnp.searchsorted(pad_end, jnp.arange(n_blocks, dtype=jnp.int32) * MOE_BLOCK,
                                           side='right'), N_EXPERTS - 1).astype(jnp.int32)
    h_pad = jnp.concatenate([h, jnp.zeros((1, D), h.dtype)], axis=0)

    def expert_block(args):
        tok, g, e = args
        xb = h_pad[tok]
        u = (xb @ w1[e] + b1[e]).astype(jnp.float32)
        glu = jnp.minimum(u[:, 0::2], SWIGLU_LIMIT)
        lin = jnp.clip(u[:, 1::2], -SWIGLU_LIMIT, SWIGLU_LIMIT)
        act = glu * jax.nn.sigmoid(SWIGLU_ALPHA * glu) * (lin + 1.0)
        y = act.astype(xb.dtype) @ w2[e] + b2[e]
        return y.astype(jnp.float32) * g[:, None]

    ys = lax.map(expert_block, (buf_tok.reshape(n_blocks, MOE_BLOCK),
                                buf_g.reshape(n_blocks, MOE_BLOCK), block_e))
    out = jnp.zeros((T + 1, D), jnp.float32).at[buf_tok].add(ys.reshape(n_pad, D))[:T]
    return out.astype(h.dtype)


def setup_inputs(seed: int = 0) -> dict:
    key = jax.random.key(seed)
    ks = jax.random.split(key, 24)
    f32 = jnp.float32

    def nrm(k, shape, scale):
        return jax.random.normal(k, shape, f32) * scale

    def gain(k, shape):
        return 1.0 + 0.1 * jax.random.normal(k, shape, f32)

    dec0 = jnp.log(jnp.power(2.0, 5.0 + jnp.arange(RET_HEADS, dtype=f32)) - 1.0)
    return {
        'x': nrm(ks[0], (BATCH, SEQ, D_MODEL), 1.0),
        'c': nrm(ks[1], (BATCH, D_MODEL), 1.0),
        'ctx': nrm(ks[2], (BATCH, CTX_LEN, D_MODEL), 1.0),
        'c_ctx': nrm(ks[3], (D_MODEL,), 1.0),
        'g_attn': gain(ks[4], (DEPTH, D_MODEL)),
        'g_ffn': gain(ks[5], (DEPTH, D_MODEL)),
        'w_ada': nrm(ks[6], (DEPTH, D_MODEL, N_MOD * D_MODEL), 0.5 * D_MODEL ** -0.5),
        'b_ada': nrm(ks[7], (DEPTH, N_MOD * D_MODEL), 0.02),
        'w_in': nrm(ks[8], (DEPTH, D_MODEL, D_IN), D_MODEL ** -0.5),
        'g_q_lora': gain(ks[9], (DEPTH, Q_LORA)),
        'w_q_up': nrm(ks[10], (DEPTH, Q_LORA, MLA_HEADS * MLA_QK), Q_LORA ** -0.5),
        'g_q_head': gain(ks[11], (DEPTH, MLA_QK)),
        'g_kv_lora': gain(ks[12], (DEPTH, KV_LORA)),
        'w_kv_up': nrm(ks[13], (DEPTH, KV_LORA, MLA_HEADS * (MLA_NOPE + MLA_V)), KV_LORA ** -0.5),
        'g_k_head': gain(ks[14], (DEPTH, MLA_QK)),
        'ret_decay_logit': dec0 + nrm(ks[15], (DEPTH, 2, RET_HEADS), 0.1),
        'g_ret_out': gain(ks[16], (DEPTH, RET_HEADS * RET_DV)),
        'w_out': nrm(ks[17], (DEPTH, MIX_WIDTH, D_MODEL), MIX_WIDTH ** -0.5),
        'w_router': nrm(ks[18], (DEPTH, D_MODEL, N_EXPERTS), D_MODEL ** -0.5),
        'b_router': nrm(ks[19], (DEPTH, N_EXPERTS), 0.01),
        'w_mlp1': nrm(ks[20], (DEPTH, N_EXPERTS, D_MODEL, 2 * D_FF), D_MODEL ** -0.5),
        'b_mlp1': nrm(ks[21], (DEPTH, N_EXPERTS, 2 * D_FF), 0.02),
        'w_mlp2': nrm(ks[22], (DEPTH, N_EXPERTS, D_FF, D_MODEL), D_FF ** -0.5),
        'b_mlp2': nrm(ks[23], (DEPTH, N_EXPERTS, D_MODEL), 0.02),
    }


def reference(x, c, ctx, c_ctx, g_attn, g_ffn, w_ada, b_ada, w_in, g_q_lora, w_q_up, g_q_head,
              g_kv_lora, w_kv_up, g_k_head, ret_decay_logit, g_ret_out, w_out,
              w_router, b_router, w_mlp1, b_mlp1, w_mlp2, b_mlp2):
    B, L, D = x.shape
    Lc = ctx.shape[1]
    rope_mla = axial_rope_tables(L, MLA_ROPE)
    rope_ret = axial_rope_tables(L, RET_DK)
    for l in range(DEPTH):
        last = l == DEPTH - 1
        mod = (jax.nn.silu(c) @ w_ada[l] + b_ada[l]).reshape(B, N_MOD, 1, D)
        mod_c = (jax.nn.silu(c_ctx) @ w_ada[l] + b_ada[l]).reshape(N_MOD, 1, D)

        h = modulate(rms_norm(x, g_attn[l]), mod[:, 0], mod[:, 1])
        hc = modulate(rms_norm(ctx, g_attn[l]), mod_c[0], mod_c[1])
        p = h @ w_in[l]
        pc = hc @ w_in[l]

        k_c, v_c = mla_kv(pc, g_kv_lora[l], w_kv_up[l], g_k_head[l], None)
        k_x, v_x = mla_kv(p, g_kv_lora[l], w_kv_up[l], g_k_head[l], rope_mla)
        q_x = mla_q(p, g_q_lora[l], w_q_up[l], g_q_head[l], rope_mla)
        o_mla = attend_blocks(q_x, jnp.concatenate([k_c, k_x], axis=2), jnp.concatenate([v_c, v_x], axis=2))

        log_gamma = jax.nn.log_sigmoid(ret_decay_logit[l].astype(jnp.float32))
        kr_c, vr_c = ret_kv(pc, None)
        s0_f = retention_state(kr_c, vr_c, log_gamma[0], reverse=False)
        s0_b = retention_state(kr_c, vr_c, log_gamma[1], reverse=True)
        kr_x, vr_x = ret_kv(p, rope_ret)
        o_ret = retention_bidir(ret_q(p, rope_ret), kr_x, vr_x, log_gamma, s0_f, s0_b)

        mix = jnp.concatenate([merge_heads(o_mla), ret_output(o_ret, p, g_ret_out[l])], axis=-1)
        x = x + mod[:, 2] * (mix @ w_out[l])
        if not last:
            o_mla_c = attend_blocks(mla_q(pc, g_q_lora[l], w_q_up[l], g_q_head[l], None), k_c, v_c)
            s_zero = jnp.zeros((B, RET_HEADS, RET_DK, RET_DV), jnp.float32)
            o_ret_c = retention_bidir(ret_q(pc, None), kr_c, vr_c, log_gamma, s_zero, s_zero)
            mix_c = jnp.concatenate([merge_heads(o_mla_c), ret_output(o_ret_c, pc, g_ret_out[l])], axis=-1)
            ctx = ctx + mod_c[2] * (mix_c @ w_out[l])

        hf = modulate(rms_norm(x, g_ffn[l]), mod[:, 3], mod[:, 4]).reshape(B * L, D)
        moe_w = (w_router[l], b_router[l], w_mlp1[l], b_mlp1[l], w_mlp2[l], b_mlp2[l])
        if last:
            x = x + mod[:, 5] * moe_ffn(hf, *moe_w).reshape(B, L, D)
        else:
            hcf = modulate(rms_norm(ctx, g_ffn[l]), mod_c[3], mod_c[4]).reshape(B * Lc, D)
            y = moe_ffn(jnp.concatenate([hf, hcf], axis=0), *moe_w)
            x = x + mod[:, 5] * y[:B * L].reshape(B, L, D)
            ctx = ctx + mod_c[5] * y[B * L:].reshape(B, Lc, D)
    return x
```

```python
import contextlib
import numpy as np
import concourse.bass as bass
import concourse.mybir as mybir
from concourse.bass_utils import run_bass_kernel_spmd

F32 = mybir.dt.float32
I32 = mybir.dt.int32
BF16 = mybir.dt.bfloat16
AF = mybir.ActivationFunctionType
ALU = mybir.AluOpType
AX = mybir.AxisListType

D = 1024
NOWN = 4096
NKEY = 8448
NCORES = 8
EPS = 1e-6
BIG = 1.0e6
DEBUG = False
RUN_RET = True
RUN_MLA = True
RUN_REST = True

OFF_Q, OFF_KV, OFF_PE, OFF_RQ, OFF_RK, OFF_RV, OFF_RG = 0, 256, 384, 416, 672, 928, 1440
C_CQ = 0
C_CKV = 256
C_PE = 384
C_PESW = 480
C_RET = 576
NCOL = C_RET + 4 * 512
RQ, RQS, RK, RKS, RGt, RVt = 0, 64, 128, 192, 256, 384


def _swap_idx(dim):
    nf = dim // 4
    idx = np.arange(dim).reshape(2, 2, nf)
    return idx[:, ::-1, :].reshape(dim)


def _rope_tables(pos_row, pos_col, dim):
    nf = dim // 4
    inv = np.power(np.float32(10000.0), -np.arange(nf, dtype=np.float32) / np.float32(nf)).astype(np.float32)
    pos = np.stack([pos_row, pos_col], axis=-1).astype(np.float32)
    ang = pos[:, :, None] * inv
    ang = np.broadcast_to(ang[:, :, None, :], (pos.shape[0], 2, 2, nf)).reshape(pos.shape[0], dim)
    cos = np.cos(ang).astype(np.float32)
    sin = np.sin(ang).astype(np.float32)
    sgn = np.ones((2, 2, nf), np.float32)
    sgn[:, 0, :] = -1.0
    return cos, sin * sgn.reshape(dim)


class KB:
    def __init__(self, nc, es):
        self.nc = nc
        self.es = es
        self.eng = {'pe': nc.tensor, 'act': nc.scalar, 'dve': nc.vector, 'pool': nc.gpsimd, 'sp': nc.sync}
        self.sems = {}
        self.cnt = {}
        for e in ('pe', 'act', 'dve', 'pool'):
            self.sems[e] = es.enter_context(nc.semaphore("c_" + e))
            self.cnt[e] = 0
        self.seen = {e: {} for e in self.eng}
        self.lastw = {}
        self.rds = {}
        self.dcnt = {}
        self.freed = []
        self.uniq = 0
        self.iq = []
        self.bregs = {}

    def _dsem(self, sem):
        if sem not in self.sems:
            if self.freed:
                h, c = self.freed.pop()
            else:
                h, c = self.es.enter_context(self.nc.semaphore("s_" + sem)), 0
            self.sems[sem] = h
            self.dcnt[sem] = c

    def _waits(self, engine, reads, writes):
        need = {}
        for k in list(reads) + list(writes):
            ev = self.lastw.get(k)
            if ev is not None:
                s, v, e = ev
                if not (e == engine and engine == 'pe'):
                    need[s] = max(need.get(s, 0), v)
        for k in writes:
            for (s, v, e) in self.rds.get(k, ()):
                if e == engine:
                    continue
                need[s] = max(need.get(s, 0), v)
        eng = self.eng[engine]
        for s, v in need.items():
            if self.seen[engine].get(s, 0) >= v:
                continue
            eng.wait_ge(self.sems[s], v)
            self.seen[engine][s] = v

    def _record(self, ev, reads, writes):
        for k in reads:
            self.rds.setdefault(k, []).append(ev)
        for k in writes:
            self.lastw[k] = ev
            self.rds[k] = []

    def op(self, engine, fn, reads=(), writes=()):
        self._waits(engine, reads, writes)
        ins = fn(self.eng[engine])
        self.cnt[engine] += 1
        ins.then_inc(self.sems[engine], 1)
        self._record((engine, self.cnt[engine], engine), reads, writes)

    def dma(self, out, in_, reads=(), writes=(), sem='d0', queue='sp'):
        if len(sem) == 2 and sem[0] == 'c' and sem[1].isdigit():
            self.uniq += 1
            sem = '%s_%d' % (sem, self.uniq)
        self._dsem(sem)
        self._waits(queue, reads, writes)
        self.eng[queue].dma_start(out=out, in_=in_).then_inc(self.sems[sem], 16)
        self.dcnt[sem] += 16
        self._record((sem, self.dcnt[sem], 'dma'), reads, writes)

    def idma(self, out, out_off, in_, in_off, bounds, reads=(), writes=(), sem='i0'):
        self._dsem(sem)
        self._waits('pool', reads, writes)
        oo = bass.IndirectOffsetOnAxis(ap=out_off, axis=0) if out_off is not None else None
        io = bass.IndirectOffsetOnAxis(ap=in_off, axis=0) if in_off is not None else None
        if bounds not in self.bregs:
            r = self.nc.gpsimd.alloc_register("bc%d" % bounds)
            self.nc.gpsimd.reg_mov(r, bounds)
            self.bregs[bounds] = r
        self.nc.gpsimd.indirect_dma_start(out=out, out_offset=oo, in_=in_, in_offset=io, bounds_check=self.bregs[bounds],
                                          oob_is_err=False).then_inc(self.sems[sem], 16)
        self.dcnt[sem] += 16
        self._record((sem, self.dcnt[sem], 'dma'), reads, writes)
        self.iq.append((sem, self.dcnt[sem]))
        if len(self.iq) > 24:
            need = {}
            while len(self.iq) > 8:
                s_, v_ = self.iq.pop(0)
                need[s_] = max(need.get(s_, 0), v_)
            for s_, v_ in need.items():
                if s_ in self.sems and self.seen['pool'].get(s_, 0) < v_:
                    self.eng['pool'].wait_ge(self.sems[s_], v_)
                    self.seen['pool'][s_] = v_

    def barrier(self):
        for e in self.eng:
            eng = self.eng[e]
            for s in ('pe', 'act', 'dve', 'pool'):
                if s != e and self.cnt[s] > self.seen[e].get(s, 0):
                    eng.wait_ge(self.sems[s], self.cnt[s])
                    self.seen[e][s] = self.cnt[s]
            for s, v in self.dcnt.items():
                if v > self.seen[e].get(s, 0):
                    eng.wait_ge(self.sems[s], v)
                    self.seen[e][s] = v
        self.lastw = {}
        self.rds = {}
        self.iq = []
        for s in list(self.dcnt):
            self.freed.append((self.sems.pop(s), self.dcnt.pop(s)))
            for e in self.seen:
                self.seen[e].pop(s, None)


def build_nc():
    nc = bass.Bass("TRN2", target_bir_lowering=False)
    es = contextlib.ExitStack()

    def din(name, shape, dt=F32):
        return nc.dram_tensor(name, list(shape), dt, kind="ExternalInput").ap()

    def dscr(name, shape, dt):
        return nc.dram_tensor(name, list(shape), dt).ap()

    xall = din("xall", [NKEY, D])
    cvec = din("cvec", [128, 8, 2])
    w_ada = din("w_ada", [D, 6 * D])
    b_ada = din("b_ada", [128, 48])
    gvec = din("gvec", [128, 16])
    w_ext = din("w_ext", [D, NCOL])
    wq_ext = din("wq_ext", [256, 8 * 2 * 96])
    wkv_k = din("wkv_k", [128, 8 * 64])
    wkv_v = din("wkv_v", [128, 8 * 64])
    smallv = din("smallv", [128, 16])
    lgin = din("lgin", [128, 8])
    kcs = din("kcs", [2, 32, NKEY])
    qcs = din("qcs", [2, 32, NOWN])
    rkcs = din("rkcs", [2, 64, NKEY])
    rqcs = din("rqcs", [2, 64, NOWN])
    utab = din("utab", [3, 128, 512])
    dgtab = din("dgtab", [4, 3, 128, 512])
    cwtab = din("cwtab", [128, 5 * 8 * 32])
    flagv = din("flagv", [128, 2])
    w_out = din("w_out", [D, D])
    w_router = din("w_router", [D, 32])
    br_bc = din("br_bc", [128, 32])
    w1 = din("w1", [32, D, 2 * D])
    w2 = din("w2", [32, D, D])
    b2 = din("b2", [32, D])
    identf = din("identf", [128, 128])
    ustrict = din("ustrict", [128, 128])
    tri32 = din("tri32", [32, 64])
    routetab = din("routetab", [128, 161])
    B1R = din("B1R", [32, 2 * D])
    y = nc.dram_tensor("y", [NOWN, D], F32, kind="ExternalOutput").ap()

    XT = dscr("XT", [128, 8, NKEY], BF16)
    WP = dscr("WP", [128, 8, NCOL], BF16)
    WPC = dscr("WPC", [128, 8, NCOL], BF16)
    MIXT = dscr("MIXT", [D, NOWN], BF16)
    X1 = dscr("X1", [NOWN, D], F32)
    HF = dscr("HF", [NOWN, D], BF16)
    XS = dscr("XS", [160 * 128, D], BF16)
    YS = dscr("YS", [160 * 128, D], F32)
    W1R = dscr("W1R", [32 * 128, 8 * 2048], BF16)
    W2R = dscr("W2R", [32 * 128, 8 * 1024], BF16)
    B2G = dscr("B2G", [32, D], F32)
    dbg = {}
    if DEBUG:
        dbg['mixt'] = nc.dram_tensor("dbg_mixt", [D, NOWN], BF16, kind="ExternalOutput").ap()
        dbg['x1'] = nc.dram_tensor("dbg_x1", [NOWN, D], F32, kind="ExternalOutput").ap()
        dbg['mod'] = nc.dram_tensor("dbg_mod", [128, 96], F32, kind="ExternalOutput").ap()

    kb = KB(nc, es)
    op, dma = kb.op, kb.dma

    def sb(name, shape, dt=F32, stack=None):
        return (stack or es).enter_context(nc.sbuf_tensor(name, list(shape), dt))

    def ps(name, shape, dt=F32, stack=None):
        return (stack or es).enter_context(nc.psum_tensor(name, list(shape), dt))

    ident = sb("ident", [128, 128], F32)
    identb = sb("identb", [128, 128], BF16)
    onesb = sb("onesb", [128, 128], BF16)
    onesf = sb("onesf", [128, 128], F32)
    modv = sb("modv", [128, 96], F32)
    sv = sb("sv", [128, 16], F32)
    gv = sb("gv", [128, 16], F32)
    lg = sb("lg", [128, 16], F32)
    flg = sb("flg", [128, 2], F32)
    rs1 = sb("rs1", [128, 16], F32)
    dma(ident[:], identf[:, :], writes=['ident'], sem='c0')
    dma(sv[:], smallv[:, :], writes=['sv'], sem='c0')
    dma(gv[:], gvec[:, :], writes=['gv'], sem='c0')
    dma(lg[:, 0:8], lgin[:, :], writes=['lg'], sem='c0')
    dma(flg[:], flagv[:, :], writes=['flg'], sem='c0')
    op('dve', lambda e: e.tensor_copy(out=identb[:], in_=ident[:]), reads=['ident'], writes=['identb'])
    op('pool', lambda e: e.memset(onesb[:], 1.0), writes=['onesb'])
    op('pool', lambda e: e.memset(onesf[:], 1.0), writes=['onesf'])
    op('act', lambda e: e.activation(out=lg[:, 12:16], in_=lg[:, 0:4], func=AF.Exp, scale=-1.0), reads=['lg'], writes=['lgs'])
    op('act', lambda e: e.activation(out=lg[:, 8:12], in_=lg[:, 4:8], func=AF.Exp, scale=-1.0), reads=['lg'], writes=['lgs2'])
    op('act', lambda e: e.activation(out=lg[:, 12:16], in_=lg[:, 12:16], func=AF.Ln, bias=1.0), reads=['lgs'], writes=['lgs'])
    op('act', lambda e: e.activation(out=lg[:, 8:12], in_=lg[:, 8:12], func=AF.Ln, bias=1.0), reads=['lgs2'], writes=['lgs2'])
    op('dve', lambda e: e.tensor_scalar(out=lg[:, 0:4], in0=lg[:, 12:16], scalar1=-1.0, scalar2=None, op0=ALU.mult), reads=['lgs', 'lg'], writes=['lgf'])
    op('dve', lambda e: e.tensor_scalar(out=lg[:, 4:8], in0=lg[:, 8:12], scalar1=-1.0, scalar2=None, op0=ALU.mult), reads=['lgs2', 'lg'], writes=['lgb'])
    op('dve', lambda e: e.tensor_scalar(out=lg[:, 12:16], in0=lg[:, 0:4], scalar1=flg[:, 0:1], scalar2=None, op0=ALU.mult), reads=['lgf', 'flg', 'lgs'], writes=['lgt'])
    op('dve', lambda e: e.scalar_tensor_tensor(out=lg[:, 8:12], in0=lg[:, 4:8], scalar=flg[:, 1:2], in1=lg[:, 12:16], op0=ALU.mult, op1=ALU.add), reads=['lgb', 'lgt', 'lgs2'], writes=['lgo'])

    with contextlib.ExitStack() as st:
        cv = sb("cv", [128, 8, 2], F32, st)
        sg = sb("sgm", [128, 8, 2], F32, st)
        wa = [sb("wa%d" % i, [128, 8, 1024], F32, st) for i in range(2)]
        bad = sb("bad", [128, 48], F32, st)
        mps = ps("mps", [128, 96], F32, st)
        dma(cv[:], cvec[:, :, :], writes=['cv'], sem='c0')
        dma(bad[:], b_ada[:, :], writes=['bad'], sem='c0')
        op('act', lambda e: e.activation(out=sg[:], in_=cv[:], func=AF.Sigmoid), reads=['cv'], writes=['sg'])
        op('dve', lambda e: e.tensor_tensor(out=cv[:], in0=cv[:], in1=sg[:], op=ALU.mult), reads=['cv', 'sg'], writes=['cv'])
        wav = w_ada.rearrange("(kc p) n -> p kc n", p=128)
        for mc in range(6):
            w = wa[mc % 2]
            wk = 'wa%d' % (mc % 2)
            for kc in range(8):
                dma(w[:, kc, :], wav[:, kc, mc * 1024:(mc + 1) * 1024], writes=[wk], sem=wk)
            for fc in range(8):
                for kc in range(8):
                    op('pe', lambda e, fc=fc, kc=kc, w=w, mc=mc: e.matmul(
                        mps[:, (mc * 8 + fc) * 2:(mc * 8 + fc) * 2 + 2], lhsT=w[:, kc, fc * 128:(fc + 1) * 128],
                        rhs=cv[:, kc, :], start=(kc == 0), stop=(kc == 7)),
                        reads=[wk, 'cv'], writes=['mps'])
        mv3 = modv[:].rearrange("p (m j) -> p m j", j=2)
        op('dve', lambda e: e.tensor_tensor(out=mv3, in0=mps[:].rearrange("p (m j) -> p m j", j=2),
                                            in1=bad[:].unsqueeze(2).to_broadcast([128, 48, 2]), op=ALU.add),
           reads=['mps', 'bad'], writes=['modv'])
        for j in range(2):
            op('dve', lambda e, j=j: e.scalar_tensor_tensor(out=rs1[:, j * 8:(j + 1) * 8], in0=mv3[:, 8:16, j], scalar=1.0,
                                                          in1=gv[:, 0:8], op0=ALU.add, op1=ALU.mult),
               reads=['modv', 'gv'], writes=['rs1'])
        if DEBUG:
            dma(dbg['mod'][:, :], modv[:], reads=['modv'], sem='dbg')
        kb.barrier()

    g2g_bc = sb("g2g_bc", [128, 1024], F32)
    g2gs_bc = sb("g2gs_bc", [128, 1024], F32)
    mv3 = modv[:].rearrange("p (m j) -> p m j", j=2)
    with contextlib.ExitStack() as st:
        dg0 = [sb("dg0%d" % i, [128, 128], F32, st) for i in range(2)]
        b2t = sb("b2t", [32, 1024], F32, st)
        gps = ps("gps", [128, 1024], F32, st)
        for kc in range(8):
            d_, dk = dg0[kc % 2], 'dg0%d' % (kc % 2)
            op('dve', lambda e, d_=d_, kc=kc: e.tensor_scalar(out=d_[:], in0=ident[:], scalar1=mv3[:, 40 + kc, 0:1], scalar2=None, op0=ALU.mult),
               reads=['ident', 'modv'], writes=[dk])
            op('pe', lambda e, d_=d_, kc=kc: e.matmul(gps[:, kc * 128:(kc + 1) * 128], lhsT=onesf[:], rhs=d_[:], start=True, stop=True), reads=['onesf', dk], writes=['gps'])
        op('act', lambda e: e.copy(out=g2g_bc[:], in_=gps[:, :]), reads=['gps'], writes=['g2g_bc'])
        op('act', lambda e: e.mul(out=g2gs_bc[:], in_=gps[:, :], mul=float(1.0 / 1.702)), reads=['gps'], writes=['g2gs_bc'])
        dma(b2t[:], b2[:, :], writes=['b2t'], sem='c0')
        op('dve', lambda e: e.tensor_tensor(out=b2t[:], in0=b2t[:], in1=g2g_bc[0:32, :], op=ALU.mult), reads=['b2t', 'g2g_bc'], writes=['b2t'])
        dma(B2G[:, :], b2t[:], reads=['b2t'], writes=['B2G'], sem='c0')
        kb.barrier()

    mv3 = modv[:].rearrange("p (m j) -> p m j", j=2)
    FCH = [(C_CQ, 128), (C_CQ + 128, 128), (C_CKV, 128), (C_PE, 96), (C_PESW, 96)]
    for h in range(4):
        b0 = C_RET + h * 512
        FCH += [(b0 + RQ, 64), (b0 + RQS, 64), (b0 + RK, 64), (b0 + RKS, 64), (b0 + RGt, 128)]
    NF = len(FCH)
    FIDX = {c0: i for i, (c0, _) in enumerate(FCH)}
    pbf = sb("pbf", [128, NF, 2], F32)
    pbv = sb("pbv", [128, 2, 512], F32)

    def mm(out, lhsT, rhs, start=True, stop=True):
        return lambda e: e.matmul(out, lhsT=lhsT, rhs=rhs, start=start, stop=stop)

    with contextlib.ExitStack() as st:
        wr = [sb("wr%d" % i, [128, 8, 576], F32, st) for i in range(2)]
        wb = [sb("wb%d" % i, [128, 8, 576], BF16, st) for i in range(2)]
        wc = [sb("wc%d" % i, [128, 8, 576], BF16, st) for i in range(2)]
        shb = sb("shb", [128, 2, 8, 128], F32, st)
        bps = ps("bps", [128, NF * 2], F32, st)
        vps = ps("vps", [128, 2, 512], F32, st)
        for j in range(2):
            for kc in range(8):
                op('dve', lambda e, j=j, kc=kc: e.tensor_copy(out=shb[:, j, kc, :], in_=mv3[:, kc, j:j + 1].to_broadcast([128, 128])),
                   reads=['modv'], writes=['shb'])
        wev = w_ext.rearrange("(kc p) n -> p kc n", p=128)
        pieces = [(0, 576)] + [(C_RET + h * 512, 512) for h in range(4)]
        for pi, (c0, w) in enumerate(pieces):
            r = wr[pi % 2]
            rk = 'wr%d' % (pi % 2)
            for kc in range(8):
                dma(r[:, kc, :w], wev[:, kc, c0:c0 + w], writes=[rk], sem=rk)
            for fi, (fc0, M) in enumerate(FCH):
                if not (c0 <= fc0 < c0 + w):
                    continue
                for kc in range(8):
                    op('pe', mm(bps[0:M, fi * 2:fi * 2 + 2], r[:, kc, fc0 - c0:fc0 - c0 + M], mv3[:, kc, :], kc == 0, kc == 7),
                       reads=[rk, 'modv'], writes=['bps'])
            if pi >= 1:
                h = pi - 1
                for j in range(2):
                    for kc in range(8):
                        op('pe', mm(vps[:, j, h * 128:(h + 1) * 128], shb[:, j, kc, :], r[:, kc, RVt:RVt + 128], kc == 0, kc == 7),
                           reads=[rk, 'shb'], writes=['vps'])
            wbk, wck = 'wb%d' % (pi % 2), 'wc%d' % (pi % 2)
            for kc in range(8):
                op('dve', lambda e, kc=kc, r=r, w=w, pi=pi: e.tensor_scalar(out=wb[pi % 2][:, kc, :w], in0=r[:, kc, :w], scalar1=rs1[:, kc:kc + 1],
                                                                    scalar2=None, op0=ALU.mult), reads=[rk, 'rs1'], writes=[wbk])
                op('act', lambda e, kc=kc, r=r, w=w, pi=pi: e.activation(out=wc[pi % 2][:, kc, :w], in_=r[:, kc, :w], func=AF.Identity, scale=rs1[:, 8 + kc:9 + kc]),
                   reads=[rk, 'rs1'], writes=[wck])
            dma(WP[:, :, c0:c0 + w], wb[pi % 2][:, :, :w], reads=[wbk], writes=['WP'], sem='wpo%d' % (pi % 2))
            dma(WPC[:, :, c0:c0 + w], wc[pi % 2][:, :, :w], reads=[wck], writes=['WPC'], sem='wpc%d' % (pi % 2))
        op('dve', lambda e: e.tensor_copy(out=pbf[:].rearrange("p f j -> p (f j)"), in_=bps[:]), reads=['bps'], writes=['pbf'])
        op('dve', lambda e: e.tensor_copy(out=pbv[:], in_=vps[:]), reads=['vps'], writes=['pbv'])
        kb.barrier()

    GROUPS = [(0, 2)] + [(2 + 4 * g, 4) for g in range(16)]
    with contextlib.ExitStack() as st:
        xt = [sb("xt%d" % i, [128, 1024], F32, st) for i in range(3)]
        sqj = sb("sqj", [128, 1024], F32, st)
        ssq = [sb("ssq%d" % i, [128, 4], F32, st) for i in range(3)]
        xh = [sb("xh%d" % i, [128, 1024], BF16, st) for i in range(2)]
        xTb = [sb("xTb%d" % i, [128, 8, 512], BF16, st) for i in range(2)]
        tps = [ps("tps%d" % i, [128, 8, 128], BF16, st) for i in range(2)]
        tiles = [(gi, t0, nt, t) for gi, (t0, nt) in enumerate(GROUPS) for t in range(nt)]

        def p1_a(ti):
            gi, t0, nt, t = tiles[ti]
            tile = t0 + t
            a, bh = ti % 3, ti % 2
            xk, hk, pk = 'xt%d' % a, 'xh%d' % bh, 'tps%d' % bh
            dma(xt[a][:], xall[tile * 128:(tile + 1) * 128, :], writes=[xk], sem=xk)
            op('act', lambda e, a=a: e.activation(out=sqj[:], in_=xt[a][:], func=AF.Square, accum_out=ssq[a][:, 0:1]),
               reads=[xk], writes=['sqj', 'ss%d' % a])
            op('act', lambda e, a=a: e.activation(out=ssq[a][:, 1:2], in_=ssq[a][:, 0:1], func=AF.Sqrt, scale=1.0 / 1024, bias=EPS),
               reads=['ss%d' % a], writes=['sr%d' % a])
            op('dve', lambda e, a=a: e.reciprocal(out=ssq[a][:, 2:3], in_=ssq[a][:, 1:2]), reads=['sr%d' % a], writes=['rc%d' % a])
            op('dve', lambda e, a=a, bh=bh: e.tensor_scalar(out=xh[bh][:], in0=xt[a][:], scalar1=ssq[a][:, 2:3], scalar2=None, op0=ALU.mult),
               reads=[xk, 'rc%d' % a], writes=[hk])
            for kc in range(8):
                op('pe', lambda e, kc=kc, bh=bh: e.transpose(out=tps[bh][:, kc, :], in_=xh[bh][:, kc * 128:(kc + 1) * 128], identity=identb[:]),
                   reads=[hk, 'identb'], writes=[pk])

        def p1_b(ti):
            gi, t0, nt, t = tiles[ti]
            bh = ti % 2
            xb_ = xTb[gi % 2]
            xbk_ = 'xTb%d' % (gi % 2)
            op('dve', lambda e, bh=bh, t=t, xb_=xb_: e.tensor_copy(out=xb_[:, :, t * 128:(t + 1) * 128], in_=tps[bh][:]), reads=['tps%d' % bh], writes=[xbk_])
            if t == nt - 1:
                dma(XT[:, :, t0 * 128:(t0 + nt) * 128], xb_[:, :, :nt * 128], reads=[xbk_], writes=['XT'], sem='xto%d' % (gi % 2))

        p1_a(0)
        for ti in range(len(tiles)):
            if ti + 1 < len(tiles):
                p1_a(ti + 1)
            p1_b(ti)
        kb.barrier()

    if RUN_RET:
      with contextlib.ExitStack() as st:
        wsl = sb("wsl", [128, 8, 512], BF16, st)
        wslc = sb("wslc", [128, 8, 512], BF16, st)
        kT = sb("kT", [128, NKEY], BF16, st)
        Vr = sb("Vr", [128, 66, 128], BF16, st)
        qT = sb("qT", [128, NOWN], BF16, st)
        qTv = sb("qTv", [128, 3, NOWN], BF16, st)
        sgT = sb("sgT", [128, NOWN], BF16, st)
        xb = [sb("xb%d" % i, [128, 8, 512], BF16, st) for i in range(2)]
        tabc = [sb("tabc%d" % i, [64, 512], F32, st) for i in range(2)]
        tabs = [sb("tabs%d" % i, [64, 512], F32, st) for i in range(2)]
        ta = [sb("ta%d" % i, [64, 512], F32, st) for i in range(2)]
        tb = [sb("tb%d" % i, [64, 512], F32, st) for i in range(2)]
        uts = sb("uts", [128, 3, 512], F32, st)
        UT = sb("UT", [128, 3, 512], F32, st)
        dgs = sb("dgs", [128, 4, 3, 512], F32, st)
        bts = sb("bts", [128, 5, 256], F32, st)
        Bh = sb("Bh", [128, 5, 256], F32, st)
        mk = [sb("mk%d" % i, [128, 512], F32, st) for i in range(3)]
        mkx = sb("mkx", [128, 512], F32, st)
        Am = [sb("Am%d" % i, [128, 512], BF16, st) for i in range(3)]
        osb = sb("osb", [128, 512], F32, st)
        osq = sb("osq", [128, 512], BF16, st)
        orr = sb("orr", [128, 512], F32, st)
        omx = [sb("omx%d" % i, [128, 512], BF16, st) for i in range(2)]
        pk_ps = [ps("pk%d" % i, [64, 512], F32, st) for i in range(2)]
        pg_ps = ps("pg", [128, 512], F32, st)
        st_ps = [ps("stp%d" % i, [128, 512], F32, st) for i in range(3)]
        o_ps = ps("ops", [128, 512], F32, st)
        ss_ps = ps("ssp", [128, 512], F32, st)
        for i in range(3):
            dma(uts[:, i, :], utab[i, :, :], writes=['uts'], sem='c1')
        op('pool', lambda e: e.memset(kT[64:128, :], 0.0), writes=['kTz'])
        op('pool', lambda e: e.memset(qT[64:128, :], 0.0), writes=['qTz'])
        XSz = XS.rearrange("(a p r) n -> a p (r n)", p=64, r=4)
        for a in range(160 * 128 // 256):
            dma(XSz[a], kT[64:128, 0:4096], reads=['kTz'], writes=['XS'], sem='xsz')
        op('pool', lambda e: e.memset(qTv[64:128, :, :], 0.0), writes=['qTvz'])
        for r_ in range(4):
            for i in range(3):
                dma(dgs[:, r_, i, :], dgtab[r_, i, :, :], writes=['dgs'], sem='c1')
        dma(bts[:].rearrange("p c n -> p (c n)"), cwtab[:, :], writes=['bts'], sem='c1')
        LGC = [0, 8, 0, 4, 4]
        for h in range(4):
            b0 = C_RET + h * 512
            dma(wsl[:], WP[:, :, b0:b0 + 512], writes=['wsl'], sem='wsl')
            dma(wslc[:], WPC[:, :, b0:b0 + 512], writes=['wslc'], sem='wslc')
            for cl in range(5):
                op('act', lambda e, cl=cl, h=h: e.activation(out=Bh[:, cl, :], in_=bts[:, cl, :], func=AF.Exp, scale=lg[:, LGC[cl] + h:LGC[cl] + h + 1]),
                   reads=['bts', 'lgf', 'lgb', 'lgo'], writes=['Bh'])
            op('dve', lambda e: e.tensor_scalar(out=Bh[:], in0=Bh[:], scalar1=0.125, scalar2=None, op0=ALU.mult), reads=['Bh'], writes=['Bh'])
            for ti_, lc in enumerate((0, 4, 8)):
                op('act', lambda e, ti_=ti_, lc=lc, h=h: e.activation(out=UT[:, ti_, :], in_=uts[:, ti_, :], func=AF.Exp, scale=lg[:, lc + h:lc + h + 1]),
                   reads=['uts', 'lgf', 'lgb', 'lgo'], writes=['UT'])
            fq, fqs, fk, fks, fg = (FIDX[b0 + RQ], FIDX[b0 + RQS], FIDX[b0 + RK], FIDX[b0 + RKS], FIDX[b0 + RGt])
            for gi, (t0, nt) in enumerate(GROUPS):
                nb = nt * 128
                tok0 = t0 * 128
                j = 1 if gi == 0 else 0
                W = wslc if gi == 0 else wsl
                Wk = 'wslc' if gi == 0 else 'wsl'
                xbb = xb[gi % 2]
                xk = 'xb%d' % (gi % 2)
                dma(xbb[:, :, :nb], XT[:, :, tok0:tok0 + nb], reads=['XT'], writes=[xk], sem=xk)
                tc_, ts_ = tabc[gi % 2], tabs[gi % 2]
                tk = 'tab%d' % (gi % 2)
                dma(tc_[:, :nb], rkcs[0, :, tok0:tok0 + nb], writes=[tk + 'c'], sem=tk + 'c')
                dma(ts_[:, :nb], rkcs[1, :, tok0:tok0 + nb], writes=[tk + 's'], sem=tk + 's')

                def rope_proj(c_a, c_b, f_a, f_b, dst, dkey, q0v=None, gi=gi, nb=nb, W=W, Wk=Wk, xbb=xbb, xk=xk, tc_=tc_, ts_=ts_, tk=tk, j=j):
                    for kc in range(8):
                        op('pe', mm(pk_ps[0][:, :nb], W[:, kc, c_a:c_a + 64], xbb[:, kc, :nb], kc == 0, kc == 7), reads=[Wk, xk], writes=['pk0'])
                    for kc in range(8):
                        op('pe', mm(pk_ps[1][:, :nb], W[:, kc, c_b:c_b + 64], xbb[:, kc, :nb], kc == 0, kc == 7), reads=[Wk, xk], writes=['pk1'])
                    a_, b_ = ta[gi % 2], tb[gi % 2]
                    op('dve', lambda e: e.scalar_tensor_tensor(out=a_[:, :nb], in0=pk_ps[0][:, :nb], scalar=pbf[0:64, f_a, j:j + 1], in1=tc_[:, :nb],
                                                               op0=ALU.add, op1=ALU.mult), reads=['pk0', 'pbf', tk + 'c'], writes=['ta%d' % (gi % 2)])
                    op('dve', lambda e: e.scalar_tensor_tensor(out=b_[:, :nb], in0=pk_ps[1][:, :nb], scalar=pbf[0:64, f_b, j:j + 1], in1=ts_[:, :nb],
                                                               op0=ALU.add, op1=ALU.mult), reads=['pk1', 'pbf', tk + 's'], writes=['tb%d' % (gi % 2)])
                    if q0v is None:
                        op('pool', lambda e: e.tensor_tensor(out=dst, in0=a_[:, :nb], in1=b_[:, :nb], op=ALU.add),
                           reads=['ta%d' % (gi % 2), 'tb%d' % (gi % 2)], writes=[dkey])
                    else:
                        op('dve', lambda e: e.tensor_tensor(out=a_[:, :nb], in0=a_[:, :nb], in1=b_[:, :nb], op=ALU.add),
                           reads=['ta%d' % (gi % 2), 'tb%d' % (gi % 2)], writes=['ta%d' % (gi % 2)])
                        op('pool', lambda e: e.tensor_copy(out=dst, in_=a_[:, :nb]), reads=['ta%d' % (gi % 2)], writes=[dkey])
                        for v in range(3):
                            op('dve', lambda e, v=v: e.tensor_tensor(out=qTv[0:64, v, q0v:q0v + 512], in0=a_[:, :nb], in1=UT[0:64, v, :], op=ALU.mult),
                               reads=['ta%d' % (gi % 2), 'UT'], writes=['qTv'])

                rope_proj(RK, RKS, fk, fks, kT[0:64, tok0:tok0 + nb], 'kT')
                for t in range(nt):
                    for kc in range(8):
                        op('pe', mm(pg_ps[:, t * 128:(t + 1) * 128], xbb[:, kc, t * 128:(t + 1) * 128], W[:, kc, RVt:RVt + 128], kc == 0, kc == 7),
                           reads=[Wk, xk], writes=['pg'])
                op('dve', lambda e, t0=t0, nt=nt, j=j, h=h: e.tensor_tensor(
                    out=Vr[:, t0:t0 + nt, :], in0=pg_ps[:, :nt * 128].rearrange("p (t c) -> p t c", c=128),
                    in1=pbv[:, j, h * 128:(h + 1) * 128].unsqueeze(1).to_broadcast([128, nt, 128]), op=ALU.add),
                    reads=['pg', 'pbv'], writes=['Vr'])
                if gi >= 9:
                    q0 = (gi - 9) * 512
                    rope_proj(RQ, RQS, fq, fqs, qT[0:64, q0:q0 + 512], 'qT', q0v=q0)
                    for kc in range(8):
                        op('pe', mm(pg_ps[:, :], W[:, kc, RGt:RGt + 128], xbb[:, kc, :], kc == 0, kc == 7), reads=[Wk, xk], writes=['pg'])
                    op('act', lambda e, q0=q0, fg=fg: e.activation(out=sgT[:, q0:q0 + 512], in_=pg_ps[:, :], func=AF.Silu, bias=pbf[:, fg, 0:1]),
                       reads=['pg', 'pbf'], writes=['sgT'])
            ui = 0
            rfin = []
            for qb in range(8):
                units = [(0, kt, kt, 0) for kt in range(2)]
                units += [(1, kt, 2 + kt, 2) for kt in range(32)]
                for kt in range(32):
                    if kt < 4 * qb:
                        units.append((2, kt, 34 + kt, 0))
                    elif kt >= 4 * qb + 4:
                        units.append((3, kt, 34 + kt, 1))
                    else:
                        units.append((-1, kt, 34 + kt, kt - 4 * qb))
                units += [(4, kt, kt, 1) for kt in range(2)]
                LA = 2
                pend = []
                for n in range(len(units) + LA):
                    if n < len(units):
                        (cl, kt, ktile, tidx) = units[n]
                        sp_, m_, a_ = st_ps[ui % 3], mk[ui % 3], Am[ui % 3]
                        spk, mkk, ak = 'stp%d' % (ui % 3), 'mk%d' % (ui % 3), 'Am%d' % (ui % 3)
                        qop = qTv[:, tidx, qb * 512:(qb + 1) * 512] if cl >= 0 else qT[:, qb * 512:(qb + 1) * 512]
                        op('pe', mm(sp_[:, :], kT[:, ktile * 128:(ktile + 1) * 128], qop), reads=['kT', 'qT', 'qTv', 'kTz', 'qTz', 'qTvz'], writes=[spk])
                        if cl >= 0:
                            if ui % 2 == 0:
                                op('dve', lambda e, a_=a_, sp_=sp_, cl=cl, qb=qb, kt=kt: e.tensor_scalar(
                                    out=a_[:], in0=sp_[:, :], scalar1=Bh[:, cl, qb * 32 + kt:qb * 32 + kt + 1], scalar2=None, op0=ALU.mult),
                                    reads=[spk, 'Bh'], writes=[ak])
                            else:
                                op('act', lambda e, a_=a_, sp_=sp_, cl=cl, qb=qb, kt=kt: e.activation(
                                    out=a_[:], in_=sp_[:, :], func=AF.Identity, scale=Bh[:, cl, qb * 32 + kt:qb * 32 + kt + 1]),
                                    reads=[spk, 'Bh'], writes=[ak])
                        else:
                            op('act', lambda e, m_=m_, tidx=tidx, h=h: e.activation(out=m_[:], in_=dgs[:, tidx, 0, :], func=AF.Exp, scale=lg[:, h:h + 1]),
                               reads=['dgs', 'lgf'], writes=[mkk])
                            op('act', lambda e, tidx=tidx, h=h: e.activation(out=mkx[:], in_=dgs[:, tidx, 1, :], func=AF.Exp, scale=lg[:, 4 + h:5 + h]),
                               reads=['dgs', 'lgb'], writes=['mkx'])
                            op('pool', lambda e, m_=m_: e.tensor_tensor(out=m_[:], in0=m_[:], in1=mkx[:], op=ALU.add), reads=[mkk, 'mkx'], writes=[mkk])
                            op('pool', lambda e, m_=m_, tidx=tidx: e.tensor_tensor(out=m_[:], in0=m_[:], in1=dgs[:, tidx, 2, :], op=ALU.add),
                               reads=[mkk, 'dgs'], writes=[mkk])
                            op('dve', lambda e, a_=a_, sp_=sp_, m_=m_: e.scalar_tensor_tensor(out=a_[:], in0=sp_[:, :], scalar=0.125, in1=m_[:],
                                                                                             op0=ALU.mult, op1=ALU.mult), reads=[spk, mkk], writes=[ak])
                        pend.append((n, ktile, a_, ak))
                        ui += 1
                        if rfin and n % 6 == 5:
                            rfin.pop(0)()
                    if n >= LA:
                        (n0, ktile0, a0, ak0) = pend.pop(0)
                        op('pe', mm(o_ps[:, :], Vr[:, ktile0, :], a0[:], n0 == 0, n0 == len(units) - 1), reads=['Vr', ak0], writes=['ops'])
                op('act', lambda e: e.copy(out=osb[:], in_=o_ps[:, :]), reads=['ops'], writes=['osb'])

                def fin_steps(h=h, qb=qb):
                    mx = omx[qb % 2]
                    mxk = 'omx%d' % (qb % 2)
                    return [
                        lambda: op('dve', lambda e: e.tensor_tensor(out=osq[:], in0=osb[:], in1=osb[:], op=ALU.mult), reads=['osb'], writes=['osq']),
                        lambda: op('pe', mm(ss_ps[:, :], onesb[:], osq[:]), reads=['onesb', 'osq'], writes=['ssp']),
                        lambda: op('act', lambda e: e.activation(out=orr[:], in_=ss_ps[:, :], func=AF.Sqrt, scale=1.0 / 128, bias=EPS), reads=['ssp'], writes=['orr']),
                        lambda: op('dve', lambda e: e.reciprocal(out=orr[:], in_=orr[:]), reads=['orr'], writes=['orr']),
                        lambda: op('dve', lambda e: e.scalar_tensor_tensor(out=osb[:], in0=osb[:], scalar=sv[:, 7 + h:8 + h], in1=orr[:], op0=ALU.mult, op1=ALU.mult),
                                   reads=['osb', 'orr', 'sv'], writes=['osb']),
                        lambda: (op('dve', lambda e: e.tensor_tensor(out=mx[:], in0=osb[:], in1=sgT[:, qb * 512:(qb + 1) * 512], op=ALU.mult), reads=['osb', 'sgT'], writes=[mxk]),
                                 dma(MIXT[512 + h * 128:512 + (h + 1) * 128, qb * 512:(qb + 1) * 512], mx[:], reads=[mxk], writes=['MIXT'], sem=mxk)),
                    ]
                rfin.extend(fin_steps())
                if qb == 7:
                    while rfin:
                        rfin.pop(0)()
        kb.barrier()
    if RUN_MLA:
      with contextlib.ExitStack() as st:
        ckvT = sb("ckvT", [128, NKEY], BF16, st)
        KT = [sb("KT%d" % i, [96, NKEY], BF16, st) for i in range(2)]
        cqT = sb("cqT", [128, 2, NOWN], BF16, st)
        sspe = sb("sspe", [128, 66], F32, st)
        wqb = sb("wqb", [128, 2, 1536], BF16, st)
        wkb = sb("wkb", [128, 1024], BF16, st)
        fkv, fpe, fpesw, fq0, fq1 = FIDX[C_CKV], FIDX[C_PE], FIDX[C_PESW], FIDX[C_CQ], FIDX[C_CQ + 128]
        with contextlib.ExitStack() as s2:
            wm = sb("wm", [128, 8, 576], BF16, s2)
            wmc = sb("wmc", [128, 8, 576], BF16, s2)
            wqr = sb("wqr", [128, 2, 1536], F32, s2)
            wkr = sb("wkr", [128, 1024], F32, s2)
            xb = [sb("mxb%d" % i, [128, 8, 512], BF16, s2) for i in range(2)]
            pkv = sb("pkv", [128, 512], F32, s2)
            sqv = sb("sqv", [128, 512], BF16, s2)
            srt = sb("srt", [128, 512], F32, s2)
            rawpe = sb("rawpe", [96, 512], F32, s2)
            rawsw = sb("rawsw", [96, 512], F32, s2)
            sqpe = sb("sqpe", [96, 512], BF16, s2)
            tcm = [sb("tcm%d" % i, [96, 512], F32, s2) for i in range(2)]
            tsm = [sb("tsm%d" % i, [96, 512], F32, s2) for i in range(2)]
            pa_ = sb("pa_", [96, 512], F32, s2)
            pb_ = sb("pb_", [96, 512], F32, s2)
            pq = sb("pq", [128, 2, 512], F32, s2)
            sq2 = sb("sq2", [128, 2, 512], BF16, s2)
            pA = ps("pA", [128, 512], F32, s2)
            pB = ps("pB", [96, 512], F32, s2)
            pC = ps("pC", [96, 512], F32, s2)
            ssb = ps("ssb", [128, 512], F32, s2)
            pss = ps("pss", [128, 66], F32, s2)
            pQ = [ps("pQ%d" % i, [128, 512], F32, s2) for i in range(2)]
            dma(wm[:], WP[:, :, 0:576], writes=['wm'], sem='c2')
            dma(wmc[:], WPC[:, :, 0:576], writes=['wmc'], sem='c2')
            dma(wqr[:], wq_ext.rearrange("(c p) n -> p c n", p=128), writes=['wqr'], sem='c2')
            dma(wkr[:, 0:512], wkv_k[:, :], writes=['wkr'], sem='c2')
            dma(wkr[:, 512:1024], wkv_v[:, :], writes=['wkr'], sem='c2')
            for c in range(2):
                op('dve', lambda e, c=c: e.tensor_scalar(out=wqb[:, c, :], in0=wqr[:, c, :], scalar1=sv[:, c:c + 1], scalar2=None, op0=ALU.mult),
                   reads=['wqr', 'sv'], writes=['wqb'])
            op('dve', lambda e: e.tensor_scalar(out=wkb[:], in0=wkr[:], scalar1=sv[:, 2:3], scalar2=None, op0=ALU.mult),
               reads=['wkr', 'sv'], writes=['wkb'])
            for gi, (t0, nt) in enumerate(GROUPS):
                nb, tok0 = nt * 128, t0 * 128
                j = 1 if gi == 0 else 0
                W, Wk = (wmc, 'wmc') if gi == 0 else (wm, 'wm')
                xbb, xk = xb[gi % 2], 'mxb%d' % (gi % 2)
                dma(xbb[:, :, :nb], XT[:, :, tok0:tok0 + nb], reads=['XT'], writes=[xk], sem=xk)
                tc_, ts_, tk = tcm[gi % 2], tsm[gi % 2], 'mtab%d' % (gi % 2)
                dma(tc_[64:96, :nb], kcs[0, :, tok0:tok0 + nb], writes=[tk + 'c'], sem=tk + 'c')
                dma(ts_[64:96, :nb], kcs[1, :, tok0:tok0 + nb], writes=[tk + 's'], sem=tk + 's')
                for kc in range(8):
                    op('pe', mm(pA[:, :nb], W[:, kc, C_CKV:C_CKV + 128], xbb[:, kc, :nb], kc == 0, kc == 7), reads=[Wk, xk], writes=['pA'])
                op('act', lambda e, nb=nb, j=j: e.activation(out=pkv[:, :nb], in_=pA[:, :nb], func=AF.Identity, bias=pbf[:, fkv, j:j + 1]),
                   reads=['pA', 'pbf'], writes=['pkv'])
                op('pool', lambda e, nb=nb: e.tensor_tensor(out=sqv[:, :nb], in0=pkv[:, :nb], in1=pkv[:, :nb], op=ALU.mult), reads=['pkv'], writes=['sqv'])
                op('pe', mm(ssb[:, :nb], onesb[:], sqv[:, :nb]), reads=['onesb', 'sqv'], writes=['ssb'])
                op('act', lambda e, nb=nb: e.activation(out=srt[:, :nb], in_=ssb[:, :nb], func=AF.Sqrt, scale=1.0 / 128, bias=EPS), reads=['ssb'], writes=['srt'])
                op('dve', lambda e, nb=nb: e.reciprocal(out=srt[:, :nb], in_=srt[:, :nb]), reads=['srt'], writes=['srt'])
                op('dve', lambda e, nb=nb, tok0=tok0: e.tensor_tensor(out=ckvT[:, tok0:tok0 + nb], in0=pkv[:, :nb], in1=srt[:, :nb], op=ALU.mult),
                   reads=['pkv', 'srt'], writes=['ckvT'])
                for kc in range(8):
                    op('pe', mm(pB[:, :nb], W[:, kc, C_PE:C_PE + 96], xbb[:, kc, :nb], kc == 0, kc == 7), reads=[Wk, xk], writes=['pB'])
                for kc in range(8):
                    op('pe', mm(pC[:, :nb], W[:, kc, C_PESW:C_PESW + 96], xbb[:, kc, :nb], kc == 0, kc == 7), reads=[Wk, xk], writes=['pC'])
                op('act', lambda e, nb=nb, j=j: e.activation(out=rawpe[64:96, :nb], in_=pB[64:96, :nb], func=AF.Identity, bias=pbf[64:96, fpe, j:j + 1]),
                   reads=['pB', 'pbf'], writes=['rawpe'])
                op('act', lambda e, nb=nb, j=j: e.activation(out=rawsw[64:96, :nb], in_=pC[64:96, :nb], func=AF.Identity, bias=pbf[64:96, fpesw, j:j + 1]),
                   reads=['pC', 'pbf'], writes=['rawsw'])
                op('pool', lambda e, nb=nb: e.tensor_tensor(out=sqpe[64:96, :nb], in0=rawpe[64:96, :nb], in1=rawpe[64:96, :nb], op=ALU.mult),
                   reads=['rawpe'], writes=['sqpe'])
                for t in range(nt):
                    op('pe', mm(pss[:, t0 + t:t0 + t + 1], sqpe[64:96, t * 128:(t + 1) * 128], onesb[64:96, 0:1]), reads=['sqpe', 'onesb'], writes=['pss'])
                op('dve', lambda e, nb=nb, tc_=tc_: e.scalar_tensor_tensor(out=pa_[64:96, :nb], in0=rawpe[64:96, :nb], scalar=sv[64:96, 5:6], in1=tc_[64:96, :nb],
                                                                      op0=ALU.mult, op1=ALU.mult), reads=['rawpe', 'sv', tk + 'c'], writes=['pa_'])
                op('dve', lambda e, nb=nb, ts_=ts_: e.scalar_tensor_tensor(out=pb_[64:96, :nb], in0=rawsw[64:96, :nb], scalar=sv[64:96, 6:7], in1=ts_[64:96, :nb],
                                                                      op0=ALU.mult, op1=ALU.mult), reads=['rawsw', 'sv', tk + 's'], writes=['pb_'])
                op('pool', lambda e, nb=nb, tok0=tok0: e.tensor_tensor(out=KT[0][64:96, tok0:tok0 + nb], in0=pa_[64:96, :nb], in1=pb_[64:96, :nb], op=ALU.add),
                   reads=['pa_', 'pb_'], writes=['KT0pe'])
                op('pool', lambda e, nb=nb, tok0=tok0: e.tensor_copy(out=KT[1][64:96, tok0:tok0 + nb], in_=KT[0][64:96, tok0:tok0 + nb]),
                   reads=['KT0pe'], writes=['KT1pe'])
                if gi >= 9:
                    q0 = (gi - 9) * 512
                    for c in range(2):
                        for kc in range(8):
                            op('pe', mm(pQ[c][:, :], W[:, kc, C_CQ + c * 128:C_CQ + (c + 1) * 128], xbb[:, kc, :], kc == 0, kc == 7),
                               reads=[Wk, xk], writes=['pQ%d' % c])
                        op('act', lambda e, c=c: e.activation(out=pq[:, c, :], in_=pQ[c][:, :], func=AF.Identity, bias=pbf[:, fq0 + c, 0:1]),
                           reads=['pQ%d' % c, 'pbf'], writes=['pq%d' % c])
                        op('pool', lambda e, c=c: e.tensor_tensor(out=sq2[:, c, :], in0=pq[:, c, :], in1=pq[:, c, :], op=ALU.mult),
                           reads=['pq%d' % c], writes=['sq2%d' % c])
                    for c in range(2):
                        op('pe', mm(ssb[:, :], onesb[:], sq2[:, c, :], c == 0, c == 1), reads=['onesb', 'sq2%d' % c], writes=['ssb'])
                    op('act', lambda e: e.activation(out=srt[:, :], in_=ssb[:, :], func=AF.Sqrt, scale=1.0 / 256, bias=EPS), reads=['ssb'], writes=['srt'])
                    op('dve', lambda e: e.reciprocal(out=srt[:, :], in_=srt[:, :]), reads=['srt'], writes=['srt'])
                    for c in range(2):
                        op('dve', lambda e, c=c, q0=q0: e.tensor_tensor(out=cqT[:, c, q0:q0 + 512], in0=pq[:, c, :], in1=srt[:, :], op=ALU.mult),
                           reads=['pq%d' % c, 'srt'], writes=['cqT'])
            op('dve', lambda e: e.tensor_copy(out=sspe[:], in_=pss[:, :]), reads=['pss'], writes=['sspe'])
            kb.barrier()
        with contextlib.ExitStack() as s2:
            QT = [sb("QT%d" % i, [96, NOWN], BF16, s2) for i in range(2)]
            Vh = [sb("Vh%d" % i, [128, 66, 65], BF16, s2) for i in range(2)]
            skh = [sb("skh%d" % i, [128, 66], F32, s2) for i in range(2)]
            sqk = [sb("sqk%d" % i, [64, 512], BF16, s2) for i in range(2)]
            qraw = sb("qraw", [96, 512], F32, s2)
            sqq = sb("sqq", [96, 512], BF16, s2)
            rq = sb("rq", [96, 512], F32, s2)
            qc_ = [sb("qc%d" % i, [96, 512], F32, s2) for i in range(2)]
            qs_ = [sb("qs%d" % i, [96, 512], F32, s2) for i in range(2)]
            qa_ = sb("qa_", [96, 512], F32, s2)
            qb_ = sb("qb_", [96, 512], F32, s2)
            pT = [sb("pT%d" % i, [128, 512], BF16, s2) for i in range(3)]
            ot = sb("ot", [65, 512], F32, s2)
            rec = sb("rec", [65, 512], F32, s2)
            mixh = [sb("mixh%d" % i, [64, 512], BF16, s2) for i in range(2)]
            kn_ps = ps("knp", [64, 512], F32, s2)
            bcp = ps("bcp", [64, 512], F32, s2)
            pvs = ps("pvs", [128, 512], F32, s2)
            pV = pvs[:, 0:256].rearrange("p (t c) -> p t c", c=64)
            pss2 = pvs[:, 256:322]
            qp = ps("qp", [96, 512], F32, s2)
            qsp = ps("qsp", [96, 512], F32, s2)
            stp = [ps("mst%d" % i, [128, 512], F32, s2) for i in range(2)]
            o_ps = ps("mo", [65, 512], F32, s2)
            for i in range(2):
                op('pool', lambda e, i=i: e.memset(Vh[i][:, :, 64:65], 1.0), writes=['Vh%d' % i])
            cs1 = [sb("cs1%d" % i, [128, 2048], F32, s2) for i in range(2)]
            cc1 = [sb("cc1%d" % i, [128, 2, 1024], BF16, s2) for i in range(2)]
            cs2 = [sb("cs2%d" % i, [128, 1024], F32, s2) for i in range(2)]
            cc2 = [sb("cc2%d" % i, [128, 1024], BF16, s2) for i in range(2)]
            cast_ld = [0]
            cast_dn = [0]

            def cast_load(n):
                ex, kc = n // 8, n % 8
                i4 = n % 2
                dma(cs1[i4][:], w1[ex, kc * 128:(kc + 1) * 128, :], writes=['cs1%d' % i4], sem='cs1%d' % i4)
                dma(cs2[i4][:], w2[ex, kc * 128:(kc + 1) * 128, :], writes=['cs2%d' % i4], sem='cs2%d' % i4)

            def cast_do(n):
                ex, kc = n // 8, n % 8
                i4 = n % 2
                a_, ak, b_, bk = cs1[i4], 'cs1%d' % i4, cc1[i4], 'cc1%d' % i4
                c_, ck, d_, dk = cs2[i4], 'cs2%d' % i4, cc2[i4], 'cc2%d' % i4
                op('dve', lambda e: e.tensor_copy(out=b_[:], in_=a_[:].rearrange("p (f g) -> p g f", g=2)), reads=[ak], writes=[bk])
                dma(W1R[ex * 128:(ex + 1) * 128, kc * 2048:(kc + 1) * 2048], b_[:].rearrange("p g f -> p (g f)"), reads=[bk], writes=['W1R'], sem=bk)
                op('dve', lambda e: e.tensor_tensor(out=d_[:], in0=c_[:], in1=g2gs_bc[:], op=ALU.mult), reads=[ck, 'g2gs_bc'], writes=[dk])
                dma(W2R[ex * 128:(ex + 1) * 128, kc * 1024:(kc + 1) * 1024], d_[:], reads=[dk], writes=['W2R'], sem=dk)

            def cast_tick(flush=False):
                if cast_dn[0] < cast_ld[0] and (flush or cast_dn[0] < cast_ld[0] - 0):
                    pass
                if cast_ld[0] < 256:
                    cast_load(cast_ld[0])
                    cast_ld[0] += 1
                    if cast_dn[0] < cast_ld[0] - 1:
                        cast_do(cast_dn[0])
                        cast_dn[0] += 1
                elif cast_dn[0] < 256:
                    cast_do(cast_dn[0])
                    cast_dn[0] += 1

            ui = [0]
            deferred = []

            def gen_steps(h):
                hb = h % 2
                KTh, ktk = KT[hb], 'KT%d' % hb
                vk, sk_k, qtk = 'Vh%d' % hb, 'skh%d' % hb, 'QT%d' % hb
                sk_ = skh[hb]
                steps = []

                def kstep_a(gi, t0, nt):
                    nb, tok0 = nt * 128, t0 * 128
                    kp, kpk = kn_ps, 'knp'
                    sq_, sqkk = sqk[gi % 2], 'sqk%d' % (gi % 2)
                    op('pe', mm(kp[:, :nb], wkb[:, h * 64:(h + 1) * 64], ckvT[:, tok0:tok0 + nb]), reads=['wkb', 'ckvT'], writes=[kpk])
                    for t in range(nt):
                        op('pe', mm(pV[:, t, :], ckvT[:, tok0 + t * 128:tok0 + (t + 1) * 128], wkb[:, 512 + h * 64:512 + (h + 1) * 64]), reads=['ckvT', 'wkb'], writes=['pV'])

                def kstep_b(gi, t0, nt):
                    nb, tok0 = nt * 128, t0 * 128
                    kp, kpk = kn_ps, 'knp'
                    sq_, sqkk = sqk[gi % 2], 'sqk%d' % (gi % 2)
                    op('act', lambda e: e.activation(out=KTh[0:64, tok0:tok0 + nb], in_=kp[:, :nb], func=AF.Identity, scale=sv[0:64, 5:6]), reads=[kpk, 'sv'], writes=[ktk])
                    op('act', lambda e: e.activation(out=sq_[:, :nb], in_=kp[:, :nb], func=AF.Square), reads=[kpk], writes=[sqkk])
                    op('dve', lambda e: e.tensor_copy(out=Vh[hb][:, t0:t0 + nt, 0:64], in_=pV[:, 0:nt, :]), reads=['pV'], writes=[vk])

                def kstep_c(gi, t0, nt):
                    sq_, sqkk = sqk[gi % 2], 'sqk%d' % (gi % 2)
                    for t in range(nt):
                        op('pe', mm(pss2[:, t0 + t:t0 + t + 1], sq_[0:64, t * 128:(t + 1) * 128], onesb[0:64, 0:1]), reads=[sqkk, 'onesb'], writes=['pss2'])

                for gi, (t0, nt) in enumerate(GROUPS):
                    steps.append(lambda gi=gi, t0=t0, nt=nt: kstep_a(gi, t0, nt))
                    steps.append(lambda gi=gi, t0=t0, nt=nt: kstep_b(gi, t0, nt))
                    steps.append(lambda gi=gi, t0=t0, nt=nt: kstep_c(gi, t0, nt))

                steps.append(lambda: op('dve', lambda e: e.tensor_tensor(out=sk_[:], in0=pss2[:, :], in1=sspe[:], op=ALU.add), reads=['pss2', 'sspe'], writes=[sk_k]))
                steps.append(lambda: op('act', lambda e: e.activation(out=sk_[:], in_=sk_[:], func=AF.Sqrt, scale=1.0 / 96, bias=EPS), reads=[sk_k], writes=[sk_k]))

                def scale_c():
                    op('dve', lambda e: e.reciprocal(out=sk_[:], in_=sk_[:]), reads=[sk_k], writes=[sk_k])
                    op('dve', lambda e: e.tensor_scalar(out=sk_[:], in0=sk_[:], scalar1=float(96 ** -0.5), scalar2=None, op0=ALU.mult), reads=[sk_k], writes=[sk_k])
                steps.append(scale_c)

                def q_a(qb):
                    q0 = qb * 512
                    tq = qb % 2
                    dma(qc_[tq][64:96, :], qcs[0, :, q0:q0 + 512], writes=['qc%d' % tq], sem='qtabc%d' % tq)
                    dma(qs_[tq][64:96, :], qcs[1, :, q0:q0 + 512], writes=['qs%d' % tq], sem='qtabs%d' % tq)
                    for c in range(2):
                        op('pe', mm(qp[:, :], wqb[:, c, (h * 2) * 96:(h * 2) * 96 + 96], cqT[:, c, q0:q0 + 512], c == 0, c == 1), reads=['wqb', 'cqT'], writes=['qp'])
                    for c in range(2):
                        op('pe', mm(qsp[:, :], wqb[:, c, (h * 2 + 1) * 96:(h * 2 + 1) * 96 + 96], cqT[:, c, q0:q0 + 512], c == 0, c == 1), reads=['wqb', 'cqT'], writes=['qsp'])

                def q_b(qb):
                    tq = qb % 2
                    op('act', lambda e: e.copy(out=qraw[:], in_=qp[:, :]), reads=['qp'], writes=['qraw'])
                    op('dve', lambda e: e.scalar_tensor_tensor(out=qb_[64:96, :], in0=qsp[64:96, :], scalar=sv[64:96, 4:5], in1=qs_[tq][64:96, :],
                                                               op0=ALU.mult, op1=ALU.mult), reads=['qsp', 'sv', 'qs%d' % tq], writes=['qb_'])

                def q_c(qb):
                    tq = qb % 2
                    op('pool', lambda e: e.tensor_tensor(out=sqq[:], in0=qraw[:], in1=qraw[:], op=ALU.mult), reads=['qraw'], writes=['sqq'])
                    op('dve', lambda e: e.scalar_tensor_tensor(out=qa_[64:96, :], in0=qraw[64:96, :], scalar=sv[64:96, 3:4], in1=qc_[tq][64:96, :],
                                                               op0=ALU.mult, op1=ALU.mult), reads=['qraw', 'sv', 'qc%d' % tq], writes=['qa_'])

                def q_d(qb):
                    op('pe', mm(qp[:, :], onesb[0:96, 0:96], sqq[:]), reads=['onesb', 'sqq', 'qraw'], writes=['qp'])
                    op('pool', lambda e: e.tensor_tensor(out=qa_[64:96, :], in0=qa_[64:96, :], in1=qb_[64:96, :], op=ALU.add), reads=['qa_', 'qb_'], writes=['qa_'])

                def q_e(qb):
                    op('act', lambda e: e.activation(out=rq[:], in_=qp[:, :], func=AF.Sqrt, scale=1.0 / 96, bias=EPS), reads=['qp'], writes=['rq'])

                def q_f(qb):
                    op('dve', lambda e: e.reciprocal(out=rq[:], in_=rq[:]), reads=['rq'], writes=['rq'])

                def q_g(qb):
                    q0 = qb * 512
                    op('dve', lambda e: e.scalar_tensor_tensor(out=QT[hb][0:64, q0:q0 + 512], in0=qraw[0:64, :], scalar=sv[0:64, 3:4], in1=rq[0:64, :],
                                                               op0=ALU.mult, op1=ALU.mult), reads=['qraw', 'sv', 'rq'], writes=[qtk])
                    op('dve', lambda e: e.tensor_tensor(out=QT[hb][64:96, q0:q0 + 512], in0=qa_[64:96, :], in1=rq[64:96, :], op=ALU.mult),
                       reads=['qa_', 'rq'], writes=[qtk])

                for qb in range(8):
                    for f_ in (q_a, q_b, q_c, q_d, q_e, q_f, q_g):
                        steps.append(lambda qb=qb, f_=f_: f_(qb))
                return steps

            def attend(h, nxt):
                hb = h % 2
                KTh, ktk = KT[hb], 'KT%d' % hb
                vk, sk_k, qtk = 'Vh%d' % hb, 'skh%d' % hb, 'QT%d' % hb
                sk_ = skh[hb]
                every = max(1, (8 * 66) // (len(nxt) + 1)) if nxt else 0
                ucount = 0
                for qb in range(8):
                    q0 = qb * 512
                    pend = []
                    for kt in range(66 + 1):
                        if kt < 66:
                            u = ui[0]
                            sp_, spk = stp[u % 2], 'mst%d' % (u % 2)
                            p_, pk = pT[u % 3], 'pT%d' % (u % 3)
                            op('pe', mm(sp_[:, :], KTh[0:96, kt * 128:(kt + 1) * 128], QT[hb][0:96, q0:q0 + 512]), reads=[ktk, 'KT%dpe' % hb, qtk], writes=[spk])
                            op('act', lambda e, p_=p_, sp_=sp_, kt=kt: e.activation(out=p_[:], in_=sp_[:, :], func=AF.Exp, scale=sk_[:, kt:kt + 1]),
                               reads=[spk, sk_k], writes=[pk])
                            pend.append((kt, p_, pk))
                            ui[0] += 1
                            ucount += 1
                            if nxt and ucount % every == 0:
                                nxt.pop(0)()
                            if ucount % 16 == 8:
                                cast_tick()
                            if kt == 8 and deferred:
                                deferred.pop(0)()
                        if kt >= 1:
                            (k0, p0, pk0) = pend.pop(0)
                            op('pe', mm(o_ps[:, :], Vh[hb][:, k0, 0:65], p0[:], k0 == 0, k0 == 65), reads=[vk, pk0], writes=['mo'])
                    op('dve', lambda e: e.tensor_copy(out=ot[:], in_=o_ps[:, :]), reads=['mo'], writes=['ot'])
                    op('dve', lambda e: e.reciprocal(out=rec[64:65, :], in_=ot[64:65, :]), reads=['ot'], writes=['rec'])

                    def fin(h=h, qb=qb, q0=q0):
                        mh, mhk = mixh[qb % 2], 'mixh%d' % (qb % 2)
                        op('pe', mm(bcp[0:64, :], onesf[64:65, 0:64], rec[64:65, :]), reads=['onesf', 'rec'], writes=['bcp'])
                        op('dve', lambda e, mh=mh: e.tensor_tensor(out=mh[:], in0=ot[0:64, :], in1=bcp[0:64, :], op=ALU.mult), reads=['ot', 'bcp'], writes=[mhk])
                        dma(MIXT[h * 64:(h + 1) * 64, q0:q0 + 512], mh[:], reads=[mhk], writes=['MIXT'], sem=mhk)
                    deferred.append(fin)
                while nxt:
                    nxt.pop(0)()
                if h == 7:
                    while deferred:
                        deferred.pop(0)()

            for st_ in gen_steps(0):
                st_()
            for h in range(8):
                attend(h, gen_steps(h + 1) if h + 1 < 8 else [])
            while cast_dn[0] < 256:
                cast_tick()
            kb.barrier()
    if RUN_REST:
      NBLK = 160
      NPAD = NBLK * 128
      GK = sb("GK", [128, 32, 4], F32)
      DSTi = sb("DSTi", [128, 128], I32)
      IDXW = sb("IDXW", [128, NBLK], I32)
      IDXB = sb("IDXB", [128, NBLK], I32)
      XOWN = 256 + NOWN
      with contextlib.ExitStack() as st:
        g1_bc = sb("g1_bc", [128, 1024], F32, st)
        g2s_bc = sb("g2s_bc", [128, 1024], F32, st)
        sh2_bc = sb("sh2_bc", [128, 1024], F32, st)
        g2sv = sb("g2sv", [128, 8], F32, st)
        dg_ = [sb("dg_%d" % i, [128, 128], F32, st) for i in range(2)]
        Wo = sb("Wo", [128, 8, 1024], BF16, st)
        Wr = sb("Wr", [128, 8, 32], BF16, st)
        Wrf = sb("Wrf", [128, 8, 32], F32, st)
        brb = sb("brb", [128, 32], F32, st)
        OHall = sb("OHall", [128, 32, 4, 32], BF16, st)
        Rall = sb("Rall", [128, 32, 32], F32, st)
        CUM = sb("CUM", [128, 32], F32, st)
        ustr = sb("ustr", [128, 128], BF16, st)
        ustrf = sb("ustrf", [128, 128], F32, st)
        tri = sb("tri", [32, 64], F32, st)
        rtab = sb("rtab", [128, NBLK + 1], F32, st)
        yps = [ps("yps%d" % i, [128, 1024], F32, st) for i in range(2)]
        tp4 = [ps("tp4%d" % i, [128, 8, 128], BF16, st) for i in range(2)]
        lgp = ps("lgp", [128, 32], F32, st)
        rkp = ps("rkp", [128, 64], F32, st)
        with contextlib.ExitStack() as s2:
            wof = sb("wof", [128, 8, 1024], F32, s2)
            dma(wof[:], w_out.rearrange("(c p) n -> p c n", p=128), writes=['wof'], sem='c3')
            for c in range(8):
                op('pool' if c % 2 else 'dve', lambda e, c=c: e.tensor_copy(out=Wo[:, c, :], in_=wof[:, c, :]), reads=['wof'], writes=['Wo'])
            dma(Wrf[:], w_router.rearrange("(c p) n -> p c n", p=128), writes=['Wrf'], sem='c3')
            op('dve', lambda e: e.tensor_copy(out=Wr[:], in_=Wrf[:]), reads=['Wrf'], writes=['Wr'])
            dma(brb[:], br_bc[:, :], writes=['brb'], sem='c3')
            dma(ustrf[:], ustrict[:, :], writes=['ustrf'], sem='c3')
            op('dve', lambda e: e.tensor_copy(out=ustr[:], in_=ustrf[:]), reads=['ustrf'], writes=['ustr'])
            dma(tri[:], tri32[:, :], writes=['tri'], sem='c3')
            dma(rtab[:], routetab[:, :], writes=['rtab'], sem='c3')
            op('pool', lambda e: e.memset(CUM[:], 0.0), writes=['CUM'])
            op('dve', lambda e: e.scalar_tensor_tensor(out=g2sv[:], in0=mv3[:, 32:40, 0], scalar=1.0, in1=gv[:, 8:16], op0=ALU.add, op1=ALU.mult),
               reads=['modv', 'gv'], writes=['g2sv'])
            di = 0
            for (dst, dkey, vec) in ((g1_bc, 'g1_bc', lambda kc: mv3[:, 16 + kc, 0:1]), (g2s_bc, 'g2s_bc', lambda kc: g2sv[:, kc:kc + 1]),
                                     (sh2_bc, 'sh2_bc', lambda kc: mv3[:, 24 + kc, 0:1])):
                for kc in range(8):
                    d_, dk = dg_[di % 2], 'dg_%d' % (di % 2)
                    op('dve', lambda e, d_=d_, vec=vec, kc=kc: e.tensor_scalar(out=d_[:], in0=ident[:], scalar1=vec(kc), scalar2=None, op0=ALU.mult),
                       reads=['ident', 'modv', 'g2sv'], writes=[dk])
                    op('pe', mm(yps[0][:, kc * 128:(kc + 1) * 128], onesf[:], d_[:]), reads=['onesf', dk], writes=['yps0'])
                    di += 1
                op('act', lambda e, dst=dst: e.copy(out=dst[:], in_=yps[0][:, :]), reads=['yps0'], writes=[dkey])
            kb.barrier()
        mt = [sb("mt%d" % i, [128, 8, 128], BF16, st) for i in range(2)]
        xo = [sb("xo%d" % i, [128, 1024], F32, st) for i in range(2)]
        x1t = [sb("x1t%d" % i, [128, 1024], F32, st) for i in range(2)]
        tmp4 = sb("tmp4", [128, 1024], F32, st)
        sq4 = sb("sq4", [128, 1024], F32, st)
        st4 = [sb("st4%d" % i, [128, 4], F32, st) for i in range(2)]
        hfb = [sb("hfb%d" % i, [128, 1024], BF16, st) for i in range(2)]
        hT = [sb("hT%d" % i, [128, 8, 128], BF16, st) for i in range(2)]
        lgt = [sb("lgt%d" % i, [128, 32], F32, st) for i in range(2)]
        mx8 = [sb("mx8%d" % i, [128, 8], F32, st) for i in range(2)]
        ex4 = [sb("ex4%d" % i, [128, 4], F32, st) for i in range(2)]
        Mb = [sb("Mb%d" % i, [128, 32], BF16, st) for i in range(2)]
        sm4 = [sb("sm4%d" % i, [128, 4], F32, st) for i in range(2)]
        MIXv = MIXT.rearrange("(c p) n -> p c n", p=128)
        def K(t):
            i2 = t % 2
            return i2, t * 128, (lambda nm: '%s%d' % (nm, i2))

        def S0(t):
            i2, tok, k = K(t)
            dma(mt[i2][:], MIXv[:, :, tok:tok + 128], reads=['MIXT'], writes=[k('mt')], sem=k('mt'))
            dma(xo[i2][:], xall[XOWN + tok:XOWN + tok + 128, :], writes=[k('xo')], sem=k('xo'))

        def S1(t):
            i2, tok, k = K(t)
            for n2 in range(2):
                for c in range(8):
                    op('pe', mm(yps[i2][:, n2 * 512:(n2 + 1) * 512], mt[i2][:, c, :], Wo[:, c, n2 * 512:(n2 + 1) * 512], c == 0, c == 7),
                       reads=[k('mt'), 'Wo'], writes=[k('yps')])
            op('dve', lambda e: e.tensor_tensor(out=tmp4[:], in0=yps[i2][:, :], in1=g1_bc[:], op=ALU.mult), reads=[k('yps'), 'g1_bc'], writes=['tmp4'])
            op('dve', lambda e: e.tensor_tensor(out=x1t[i2][:], in0=tmp4[:], in1=xo[i2][:], op=ALU.add), reads=['tmp4', k('xo')], writes=[k('x1t')])
            dma(X1[tok:tok + 128, :], x1t[i2][:], reads=[k('x1t')], writes=['X1'], sem=k('x1o'))
            op('act', lambda e: e.activation(out=sq4[:], in_=x1t[i2][:], func=AF.Square, accum_out=st4[i2][:, 0:1]), reads=[k('x1t')], writes=['sq4', k('s4a')])

        def S2(t):
            i2, tok, k = K(t)
            op('act', lambda e: e.activation(out=st4[i2][:, 1:2], in_=st4[i2][:, 0:1], func=AF.Sqrt, scale=1.0 / 1024, bias=EPS), reads=[k('s4a')], writes=[k('s4b')])
            op('dve', lambda e: e.reciprocal(out=st4[i2][:, 2:3], in_=st4[i2][:, 1:2]), reads=[k('s4b')], writes=[k('s4c')])
            op('dve', lambda e: e.scalar_tensor_tensor(out=tmp4[:], in0=x1t[i2][:], scalar=st4[i2][:, 2:3], in1=g2s_bc[:], op0=ALU.mult, op1=ALU.mult),
               reads=[k('x1t'), k('s4c'), 'g2s_bc'], writes=['tmp4'])
            op('dve', lambda e: e.tensor_tensor(out=hfb[i2][:], in0=tmp4[:], in1=sh2_bc[:], op=ALU.add), reads=['tmp4', 'sh2_bc'], writes=[k('hfb')])
            dma(HF[tok:tok + 128, :], hfb[i2][:], reads=[k('hfb')], writes=['HF'], sem=k('hfo'))
            for c in range(8):
                op('pe', lambda e, c=c: e.transpose(out=tp4[i2][:, c, :], in_=hfb[i2][:, c * 128:(c + 1) * 128], identity=identb[:]),
                   reads=[k('hfb'), 'identb'], writes=[k('tp4')])

        def S3(t):
            i2, tok, k = K(t)
            op('act', lambda e: e.copy(out=hT[i2][:], in_=tp4[i2][:]), reads=[k('tp4')], writes=[k('hT')])
            for c in range(8):
                op('pe', mm(lgp[:, :], hT[i2][:, c, :], Wr[:, c, :], c == 0, c == 7), reads=[k('hT'), 'Wr'], writes=['lgp'])

        def S4(t):
            i2, tok, k = K(t)
            op('dve', lambda e: e.tensor_tensor(out=lgt[i2][:], in0=lgp[:, :], in1=brb[:], op=ALU.add), reads=['lgp', 'brb'], writes=[k('lgt')])
            op('dve', lambda e: e.max(out=mx8[i2][:], in_=lgt[i2][:]), reads=[k('lgt')], writes=[k('mx8')])
            op('dve', lambda e: e.tensor_scalar(out=sm4[i2][:, 0:1], in0=mx8[i2][:, 0:1], scalar1=-1.0, scalar2=None, op0=ALU.mult), reads=[k('mx8')], writes=[k('nmx')])
            op('act', lambda e: e.activation(out=ex4[i2][:], in_=mx8[i2][:, 0:4], func=AF.Exp, bias=sm4[i2][:, 0:1]), reads=[k('mx8'), k('nmx')], writes=[k('ex4')])
            for kk in range(4):
                op('dve', lambda e, kk=kk: e.tensor_scalar(out=OHall[:, t, kk, :], in0=lgt[i2][:], scalar1=mx8[i2][:, kk:kk + 1], scalar2=None, op0=ALU.is_equal),
                   reads=[k('lgt'), k('mx8')], writes=['OH%d' % t])
            op('dve', lambda e: e.tensor_scalar(out=Mb[i2][:], in0=lgt[i2][:], scalar1=mx8[i2][:, 3:4], scalar2=None, op0=ALU.is_ge),
               reads=[k('lgt'), k('mx8')], writes=[k('Mb')])
            op('pe', mm(rkp[:, 0:32], ustr[:], Mb[i2][:]), reads=['ustr', k('Mb')], writes=['rkp'])
            op('pe', mm(rkp[:, 32:64], onesb[:], Mb[i2][:]), reads=['onesb', k('Mb')], writes=['rkp'])

        def S5(t):
            i2, tok, k = K(t)
            op('dve', lambda e: e.reduce_sum(out=sm4[i2][:, 1:2], in_=ex4[i2][:], axis=AX.X), reads=[k('ex4')], writes=[k('sm1')])
            op('dve', lambda e: e.reciprocal(out=sm4[i2][:, 2:3], in_=sm4[i2][:, 1:2]), reads=[k('sm1')], writes=[k('sm2')])
            op('dve', lambda e: e.tensor_scalar(out=GK[:, t, :], in0=ex4[i2][:], scalar1=sm4[i2][:, 2:3], scalar2=None, op0=ALU.mult),
               reads=[k('ex4'), k('sm2')], writes=['GK'])
            op('dve', lambda e: e.tensor_tensor(out=Rall[:, t, :], in0=rkp[:, 0:32], in1=CUM[:], op=ALU.add), reads=['rkp', 'CUM'], writes=['Rall'])
            op('dve', lambda e: e.tensor_tensor(out=CUM[:], in0=rkp[:, 32:64], in1=CUM[:], op=ALU.add), reads=['rkp', 'CUM', 'Rall'], writes=['CUM'])

        STG = [S0, S1, S2, S3, S4, S5]
        for it in range(32 + len(STG) - 1):
            for kk_ in range(len(STG) - 1, -1, -1):
                t = it - kk_
                if 0 <= t < 32:
                    STG[kk_](t)
        with contextlib.ExitStack() as s2:
            cf = sb("cf", [128, 32], F32, s2)
            ci = sb("ci", [128, 32], I32, s2)
            padT = sb("padT", [32, 128], F32, s2)
            pse = sb("pse", [128, 64], F32, s2)
            cmp3 = sb("cmp3", [128, NBLK, 32], F32, s2)
            Ef = sb("Ef", [128, NBLK], F32, s2)
            eqf = sb("eqf", [128, NBLK], F32, s2)
            ixf = sb("ixf", [128, NBLK], F32, s2)
            dall = sb("dall", [128, 32], F32, s2)
            dtmp = sb("dtmp", [128, 4, 32], F32, s2)
            dstf = sb("dstf", [128, 32, 4], F32, s2)
            op('dve', lambda e: e.tensor_scalar(out=cf[:], in0=CUM[:], scalar1=127.0, scalar2=None, op0=ALU.add), reads=['CUM'], writes=['cf'])
            op('dve', lambda e: e.tensor_copy(out=ci[:], in_=cf[:]), reads=['cf'], writes=['ci'])
            op('dve', lambda e: e.tensor_scalar(out=ci[:], in0=ci[:], scalar1=7, scalar2=7, op0=ALU.arith_shift_right, op1=ALU.arith_shift_left), reads=['ci'], writes=['ci'])
            op('dve', lambda e: e.tensor_copy(out=cf[:], in_=ci[:]), reads=['ci'], writes=['cf'])
            op('pe', lambda e: e.transpose(out=yps[0][0:32, 0:128], in_=cf[:], identity=ident[:]), reads=['cf', 'ident'], writes=['yps0'])
            op('act', lambda e: e.copy(out=padT[:], in_=yps[0][0:32, 0:128]), reads=['yps0'], writes=['padT'])
            op('pe', mm(rkp[:, 0:64], padT[:], tri[:]), reads=['padT', 'tri'], writes=['rkp'])
            op('act', lambda e: e.copy(out=pse[:], in_=rkp[:, 0:64]), reads=['rkp'], writes=['pse'])
            op('dve', lambda e: e.tensor_tensor(out=cmp3[:], in0=pse[:, 32:64].unsqueeze(1).to_broadcast([128, NBLK, 32]),
                                                in1=rtab[:, 0:NBLK].unsqueeze(2).to_broadcast([128, NBLK, 32]), op=ALU.is_le), reads=['pse', 'rtab'], writes=['cmp3'])
            op('dve', lambda e: e.reduce_sum(out=Ef[:], in_=cmp3[:], axis=AX.X), reads=['cmp3'], writes=['Ef'])
            op('dve', lambda e: e.tensor_scalar(out=Ef[:], in0=Ef[:], scalar1=31.0, scalar2=None, op0=ALU.min), reads=['Ef'], writes=['Ef'])
            op('pool', lambda e: e.memset(eqf[:], 0.0), writes=['eqf'])
            op('dve', lambda e: e.tensor_tensor(out=eqf[:, 2:NBLK], in0=Ef[:, 2:NBLK], in1=Ef[:, 0:NBLK - 2], op=ALU.is_equal), reads=['Ef', 'eqf'], writes=['eqf'])
            op('dve', lambda e: e.tensor_scalar(out=eqf[:], in0=eqf[:], scalar1=BIG, scalar2=None, op0=ALU.mult), reads=['eqf'], writes=['eqf'])
            op('dve', lambda e: e.scalar_tensor_tensor(out=ixf[:], in0=Ef[:], scalar=128.0, in1=eqf[:], op0=ALU.mult, op1=ALU.add), reads=['Ef', 'eqf'], writes=['ixf'])
            op('dve', lambda e: e.tensor_scalar(out=ixf[:], in0=ixf[:], scalar1=rtab[:, NBLK:NBLK + 1], scalar2=None, op0=ALU.add), reads=['ixf', 'rtab'], writes=['ixf'])
            op('dve', lambda e: e.tensor_scalar(out=ixf[:], in0=ixf[:], scalar1=0.0, scalar2=2.0e6, op0=ALU.max, op1=ALU.min), reads=['ixf'], writes=['ixf'])
            op('dve', lambda e: e.tensor_copy(out=IDXW[:], in_=ixf[:]), reads=['ixf'], writes=['IDXW'])
            op('dve', lambda e: e.tensor_tensor(out=ixf[:], in0=Ef[:], in1=eqf[:], op=ALU.add), reads=['Ef', 'eqf', 'IDXW'], writes=['ixf'])
            op('dve', lambda e: e.tensor_scalar(out=ixf[:], in0=ixf[:], scalar1=0.0, scalar2=2.0e6, op0=ALU.max, op1=ALU.min), reads=['ixf'], writes=['ixf'])
            op('dve', lambda e: e.tensor_copy(out=IDXB[:], in_=ixf[:]), reads=['ixf'], writes=['IDXB'])
            for t in range(32):
                op('dve', lambda e, t=t: e.tensor_tensor(out=dall[:], in0=Rall[:, t, :], in1=pse[:, 0:32], op=ALU.add), reads=['Rall', 'pse'], writes=['dall'])
                op('dve', lambda e, t=t: e.tensor_tensor(out=dtmp[:], in0=OHall[:, t, :, :], in1=dall[:].unsqueeze(1).to_broadcast([128, 4, 32]), op=ALU.mult),
                   reads=['OH%d' % t, 'dall'], writes=['dtmp'])
                op('dve', lambda e, t=t: e.reduce_sum(out=dstf[:, t, :], in_=dtmp[:], axis=AX.X), reads=['dtmp'], writes=['dstf'])
            op('dve', lambda e: e.tensor_scalar(out=dstf[:], in0=dstf[:], scalar1=0.0, scalar2=float(NPAD - 1), op0=ALU.max, op1=ALU.min), reads=['dstf'], writes=['dstf'])
            op('dve', lambda e: e.tensor_copy(out=DSTi[:], in_=dstf[:].rearrange("p t k -> p (t k)")), reads=['dstf'], writes=['DSTi'])
            kb.barrier()
        kb.barrier()

      with contextlib.ExitStack() as st:
        with contextlib.ExitStack() as s2:
            hfr = [sb("hfr%d" % i, [128, 1024], BF16, s2) for i in range(4)]
            for t in range(32):
                h_, hk = hfr[t % 4], 'hfr%d' % (t % 4)
                dma(h_[:], HF[t * 128:(t + 1) * 128, :], reads=['HF'], writes=[hk], sem=hk)
                for kk in range(4):
                    kb.idma(out=XS[:, :], out_off=DSTi[:, t * 4 + kk:t * 4 + kk + 1], in_=h_[:, :], in_off=None, bounds=NPAD - 1,
                            reads=[hk, 'DSTi', 'XS'], writes=['XSw%d' % kk], sem='xsc')
            kb.barrier()
        with contextlib.ExitStack() as s2:
            W1b = [sb("W1b%d" % i, [128, 8, 2048], BF16, s2) for i in range(2)]
            W2b = [sb("W2b%d" % i, [128, 8, 1024], BF16, s2) for i in range(2)]
            B1b = [sb("B1b%d" % i, [128, 2048], F32, s2) for i in range(2)]
            B2b = [sb("B2b%d" % i, [128, 1024], F32, s2) for i in range(2)]
            xbk = [sb("xbk%d" % i, [128, 1024], BF16, s2) for i in range(4)]
            xTk = [sb("xTk%d" % i, [128, 8, 128], BF16, s2) for i in range(2)]
            t1 = sb("t1", [128, 2048], F32, s2)
            sA = sb("sA", [128, 1024], F32, s2)
            aB = [sb("aB%d" % i, [128, 1024], BF16, s2) for i in range(2)]
            aT = sb("aT", [128, 8, 128], BF16, s2)
            yb = [sb("yb%d" % i, [128, 1024], F32, s2) for i in range(2)]
            up = ps("up", [128, 2048], F32, s2)
            ypm = ps("ypm", [128, 1024], F32, s2)
            tpa = ps("tpa", [128, 8, 128], BF16, s2)
            tpb = ps("tpb", [128, 8, 128], BF16, s2)
            def gatherA(j):
                b = j % 2
                wk = 'wga%d' % b
                kb.idma(out=W1b[b][:].rearrange("p k n -> p (k n)"), out_off=None, in_=W1R[:, :], in_off=IDXW[:, j:j + 1], bounds=4095,
                        reads=['IDXW', 'W1R'], writes=['W1b%d' % b], sem=wk)
                kb.idma(out=B1b[b][:, :], out_off=None, in_=B1R[:, :], in_off=IDXB[:, j:j + 1], bounds=31, reads=['IDXB'], writes=['B1b%d' % b], sem='gb1%d' % b)

            def gatherB(j):
                b = j % 2
                wk = 'wgb%d' % b
                kb.idma(out=W2b[b][:].rearrange("p k n -> p (k n)"), out_off=None, in_=W2R[:, :], in_off=IDXW[:, j:j + 1], bounds=4095,
                        reads=['IDXW', 'W2R'], writes=['W2b%d' % b], sem=wk)
                kb.idma(out=B2b[b][:, :], out_off=None, in_=B2G[:, :], in_off=IDXB[:, j:j + 1], bounds=31, reads=['IDXB', 'B2G'], writes=['B2b%d' % b], sem='gb2%d' % b)

            gatherA(0)
            gatherA(1)
            gatherB(0)
            gatherB(1)

            def loadx(j):
                b4 = j % 4
                dma(xbk[b4][:], XS[j * 128:(j + 1) * 128, :], reads=['XSw0', 'XSw1', 'XSw2', 'XSw3'], writes=['xbk%d' % b4], sem='xbk%d' % b4)

            def TX(j):
                b, b4 = j % 2, j % 4
                xk, xtk = 'xbk%d' % b4, 'xTk%d' % b
                if j + 3 < NBLK:
                    loadx(j + 3)
                for c in range(8):
                    op('pe', lambda e, c=c, b4=b4: e.transpose(out=tpa[:, c, :], in_=xbk[b4][:, c * 128:(c + 1) * 128], identity=identb[:]), reads=[xk, 'identb'], writes=['tpa'])
                op('act', lambda e, b=b: e.copy(out=xTk[b][:], in_=tpa[:]), reads=['tpa'], writes=[xtk])

            def MM1(j, half):
                b = j % 2
                xtk, w1k = 'xTk%d' % b, 'W1b%d' % b
                for n4 in (0, 1) if half == 0 else (2, 3):
                    for kc in range(8):
                        op('pe', mm(up[:, n4 * 512:(n4 + 1) * 512], xTk[b][:, kc, :], W1b[b][:, kc, n4 * 512:(n4 + 1) * 512], kc == 0, kc == 7), reads=[xtk, w1k], writes=['up'])

            def chain(j):
                b = j % 2
                b1k = 'B1b%d' % b
                op('dve', lambda e, b=b: e.tensor_tensor(out=t1[:], in0=up[:, :], in1=B1b[b][:], op=ALU.add), reads=['up', b1k], writes=['t1'])
                if j + 2 < NBLK:
                    gatherA(j + 2)
                op('dve', lambda e: e.tensor_scalar(out=sA[:], in0=t1[:, 0:1024], scalar1=7.0, scalar2=None, op0=ALU.min), reads=['t1'], writes=['sA'])
                op('act', lambda e: e.activation(out=sA[:], in_=sA[:], func=AF.Silu, scale=1.702), reads=['sA'], writes=['sA'])
                op('dve', lambda e: e.tensor_scalar(out=t1[:, 1024:2048], in0=t1[:, 1024:2048], scalar1=-7.0, scalar2=7.0, op0=ALU.max, op1=ALU.min), reads=['t1'], writes=['t1'])
                op('dve', lambda e, b=b: e.scalar_tensor_tensor(out=aB[b][:], in0=t1[:, 1024:2048], scalar=1.0, in1=sA[:], op0=ALU.add, op1=ALU.mult), reads=['t1', 'sA'], writes=['aB%d' % b])

            def TA(j):
                b = j % 2
                for c in range(8):
                    op('pe', lambda e, c=c, b=b: e.transpose(out=tpb[:, c, :], in_=aB[b][:, c * 128:(c + 1) * 128], identity=identb[:]), reads=['aB%d' % b, 'identb'], writes=['tpb'])
                op('act', lambda e: e.copy(out=aT[:], in_=tpb[:]), reads=['tpb'], writes=['aT'])

            def MM2(j):
                b = j % 2
                w2k, b2k = 'W2b%d' % b, 'B2b%d' % b
                for n2 in range(2):
                    for fc in range(8):
                        op('pe', mm(ypm[:, n2 * 512:(n2 + 1) * 512], aT[:, fc, :], W2b[b][:, fc, n2 * 512:(n2 + 1) * 512], fc == 0, fc == 7), reads=['aT', w2k], writes=['ypm'])
                op('dve', lambda e, b=b: e.tensor_tensor(out=yb[b][:], in0=ypm[:, :], in1=B2b[b][:], op=ALU.add), reads=['ypm', b2k], writes=['yb%d' % b])
                dma(YS[j * 128:(j + 1) * 128, :], yb[b][:], reads=['yb%d' % b], writes=['YS'], sem='ybo%d' % b)
                if j + 2 < NBLK:
                    gatherB(j + 2)

            for j0 in range(3):
                loadx(j0)
            TX(0)
            TX(1)
            MM1(0, 0)
            MM1(0, 1)
            chain(0)
            for j in range(NBLK):
                if j + 2 < NBLK:
                    TX(j + 2)
                if j + 1 < NBLK:
                    MM1(j + 1, 0)
                TA(j)
                if j + 1 < NBLK:
                    MM1(j + 1, 1)
                    chain(j + 1)
                MM2(j)
            kb.barrier()
        with contextlib.ExitStack() as s2:
            yg = [[sb("yg%d_%d" % (i, kk), [128, 1024], F32, s2) for kk in range(4)] for i in range(3)]
            x1r = [sb("x1r%d" % i, [128, 1024], F32, s2) for i in range(2)]
            ac = [sb("ac%d" % i, [128, 1024], F32, s2) for i in range(2)]
            for t in range(32):
                i2 = t % 2
                tok = t * 128
                i3 = t % 3
                gk_ = 'yg%d' % i3
                dma(x1r[i2][:], X1[tok:tok + 128, :], reads=['X1'], writes=['x1r%d' % i2], sem='x1r%d' % i2)
                for kk in range(4):
                    kb.idma(out=yg[i3][kk][:, :], out_off=None, in_=YS[:, :], in_off=DSTi[:, t * 4 + kk:t * 4 + kk + 1], bounds=NPAD - 1,
                            reads=['YS', 'DSTi'], writes=[gk_ + 'b%d' % kk], sem=gk_ + '_%d' % kk)
                a_ = ac[i2]
                akk = 'ac%d' % i2
                op('dve', lambda e, a_=a_, i2=i2, t=t: e.scalar_tensor_tensor(out=a_[:], in0=yg[i3][0][:], scalar=GK[:, t, 0:1], in1=x1r[i2][:], op0=ALU.mult, op1=ALU.add),
                   reads=[gk_ + 'b0', 'GK', 'x1r%d' % i2], writes=[akk])
                for kk in range(1, 4):
                    op('dve', lambda e, a_=a_, i2=i2, t=t, kk=kk: e.scalar_tensor_tensor(out=a_[:], in0=yg[i3][kk][:], scalar=GK[:, t, kk:kk + 1], in1=a_[:], op0=ALU.mult, op1=ALU.add),
                       reads=[gk_ + 'b%d' % kk, 'GK', akk], writes=[akk])
                dma(y[tok:tok + 128, :], a_[:], reads=[akk], writes=['y'], sem='yo%d' % i2)
            kb.barrier()
    if DEBUG:
        dma(dbg['mixt'][:, :], MIXT[:, :], reads=['MIXT'], sem='dbg')
        dma(dbg['x1'][:, :], X1[:, :], reads=['X1'], sem='dbg')
        kb.barrier()
    es.close()
    return nc


def _prep_shared(inp):
    f = np.float32
    w_in = np.asarray(inp['w_in'][0], f)
    sw32 = _swap_idx(32)
    sw64 = _swap_idx(64)
    w_ext = np.zeros((D, NCOL), f)
    w_ext[:, C_CQ:C_CQ + 256] = w_in[:, OFF_Q:OFF_Q + 256]
    w_ext[:, C_CKV:C_CKV + 128] = w_in[:, OFF_KV:OFF_KV + 128]
    w_ext[:, C_PE + 64:C_PE + 96] = w_in[:, OFF_PE:OFF_PE + 32]
    w_ext[:, C_PESW + 64:C_PESW + 96] = w_in[:, OFF_PE + sw32]
    for h in range(4):
        b = C_RET + h * 512
        w_ext[:, b + RQ:b + RQ + 64] = w_in[:, OFF_RQ + h * 64:OFF_RQ + (h + 1) * 64]
        w_ext[:, b + RQS:b + RQS + 64] = w_in[:, OFF_RQ + h * 64 + sw64]
        w_ext[:, b + RK:b + RK + 64] = w_in[:, OFF_RK + h * 64:OFF_RK + (h + 1) * 64]
        w_ext[:, b + RKS:b + RKS + 64] = w_in[:, OFF_RK + h * 64 + sw64]
        w_ext[:, b + RGt:b + RGt + 128] = w_in[:, OFF_RG + h * 128:OFF_RG + (h + 1) * 128]
        w_ext[:, b + RVt:b + RVt + 128] = w_in[:, OFF_RV + h * 128:OFF_RV + (h + 1) * 128]
    wqu = np.asarray(inp['w_q_up'][0], f)
    wq_ext = np.zeros((256, 8, 2, 96), f)
    for h in range(8):
        wq_ext[:, h, 0, :] = wqu[:, h * 96:(h + 1) * 96]
        wq_ext[:, h, 1, 64:96] = wqu[:, h * 96 + 64 + sw32]
    wkv = np.asarray(inp['w_kv_up'][0], f).reshape(128, 8, 128)
    smallv = np.zeros((128, 16), f)
    gql = np.asarray(inp['g_q_lora'][0], f)
    smallv[:, 0] = gql[:128]
    smallv[:, 1] = gql[128:]
    smallv[:, 2] = np.asarray(inp['g_kv_lora'][0], f)
    gqh = np.asarray(inp['g_q_head'][0], f)
    gkh = np.asarray(inp['g_k_head'][0], f)
    smallv[:96, 3] = gqh
    smallv[64:96, 4] = gqh[64 + sw32]
    smallv[:96, 5] = gkh
    smallv[64:96, 6] = gkh[64 + sw32]
    gro = np.asarray(inp['g_ret_out'][0], f)
    for h in range(4):
        smallv[:, 7 + h] = gro[h * 128:(h + 1) * 128]
    b1 = np.asarray(inp['b_mlp1'][0], f).reshape(32, 8, 128, 2)
    sh = {
        'w_ada': np.ascontiguousarray(inp['w_ada'][0], f),
        'b_ada': np.ascontiguousarray(np.asarray(inp['b_ada'][0], f).reshape(48, 128).T),
        'gvec': np.ascontiguousarray(np.concatenate([np.asarray(inp['g_attn'][0], f).reshape(8, 128).T,
                                                     np.asarray(inp['g_ffn'][0], f).reshape(8, 128).T], axis=1)),
        'w_ext': w_ext,
        'wq_ext': wq_ext.reshape(256, -1),
        'wkv_k': np.ascontiguousarray(wkv[:, :, :64]).reshape(128, -1),
        'wkv_v': np.ascontiguousarray(wkv[:, :, 64:]).reshape(128, -1),
        'smallv': smallv,
        'lgin': np.ascontiguousarray(np.broadcast_to(np.asarray(inp['ret_decay_logit'][0], f).reshape(1, 8), (128, 8))),
        'w_out': np.ascontiguousarray(inp['w_out'][0], f),
        'w_router': np.ascontiguousarray(inp['w_router'][0], f),
        'br_bc': np.ascontiguousarray(np.broadcast_to(np.asarray(inp['b_router'][0], f).reshape(1, 32), (128, 32))),
        'w1': np.ascontiguousarray(inp['w_mlp1'][0], f),
        'w2': np.ascontiguousarray(inp['w_mlp2'][0], f),
        'b2': np.ascontiguousarray(inp['b_mlp2'][0], f),
        'identf': np.eye(128, dtype=f),
        'ustrict': np.triu(np.ones((128, 128), f), 1),
        'tri32': np.concatenate([np.triu(np.ones((32, 32), f), 1), np.triu(np.ones((32, 32), f), 0)], axis=1),
        'routetab': np.ascontiguousarray(np.concatenate([np.broadcast_to(128.0 * np.arange(160, dtype=f)[None, :], (128, 160)),
                                                         np.arange(128, dtype=f)[:, None]], axis=1)),
        'B1R': np.ascontiguousarray(np.asarray(inp['b_mlp1'][0], f).reshape(32, D, 2).transpose(0, 2, 1)).reshape(32, 2 * D),
    }
    jj = np.arange(128, dtype=f)[:, None]
    ii = np.arange(512, dtype=f)[None, :]
    dg = np.zeros((4, 3, 128, 512), f)
    for r in range(4):
        d = ii - (128 * r + jj)
        dg[r, 0] = np.where(d > 0, d, BIG)
        dg[r, 1] = np.where(d < 0, -d, BIG)
        dg[r, 2] = np.where(d == 0, 2.0, 0.0)
    sh['dgtab'] = dg
    return sh


def _prep_core(core, inp, sh):
    f = np.float32
    b, half = core // 2, core % 2
    x = np.asarray(inp['x'], f)
    own = slice(half * NOWN, (half + 1) * NOWN)
    oth = slice((1 - half) * NOWN, (2 - half) * NOWN)
    m = dict(sh)
    m['xall'] = np.ascontiguousarray(np.concatenate([np.asarray(inp['ctx'][b], f), x[b, oth], x[b, own]], axis=0))
    cv = np.stack([np.asarray(inp['c'][b], f), np.asarray(inp['c_ctx'], f)], axis=-1)
    m['cvec'] = np.ascontiguousarray(cv.reshape(8, 128, 2).transpose(1, 0, 2))
    t = np.arange(2 * NOWN)
    prow, pcol = (t // 64).astype(f), (t % 64).astype(f)
    for dim, kn, qn in ((32, 'kcs', 'qcs'), (64, 'rkcs', 'rqcs')):
        cos, sin = _rope_tables(prow, pcol, dim)
        kc = np.concatenate([np.ones((256, dim), f), cos[oth], cos[own]], axis=0)
        ks = np.concatenate([np.zeros((256, dim), f), sin[oth], sin[own]], axis=0)
        m[kn] = np.ascontiguousarray(np.stack([kc.T, ks.T], axis=0))
        m[qn] = np.ascontiguousarray(np.stack([cos[own].T, sin[own].T], axis=0))
    jj = np.arange(128, dtype=f)[:, None]
    ii = np.arange(512, dtype=f)[None, :]
    s = 1.0 if half == 1 else -1.0
    iq = np.broadcast_to(ii, (128, 512)).astype(f)
    m['utab'] = np.ascontiguousarray(np.stack([iq, -iq, s * iq], axis=0).astype(f))
    base = np.zeros((5, 8, 32), f)
    for qb in range(8):
        for kt in range(32):
            if kt < 2:
                base[0, qb, kt] = half * 4096 + qb * 512 + 256 - kt * 128
                base[4, qb, kt] = 8192 - half * 4096 - qb * 512 + kt * 128
            base[1, qb, kt] = (4096 + qb * 512 - kt * 128) if half == 1 else (4096 + kt * 128 - qb * 512)
            base[2, qb, kt] = qb * 512 - kt * 128
            base[3, qb, kt] = kt * 128 - qb * 512
    sgn = np.array([-1.0, -s, -1.0, 1.0, 1.0], f)
    cw = base.reshape(1, 5, 256) + sgn.reshape(1, 5, 1) * np.arange(128, dtype=f).reshape(128, 1, 1)
    m['cwtab'] = np.ascontiguousarray(cw.reshape(128, 5 * 256).astype(f))
    m['flagv'] = np.ascontiguousarray(np.broadcast_to(np.array([[half, 1 - half]], f), (128, 2)))
    return m


def kernel(**inputs):
    sh = _prep_shared(inputs)
    in_maps = [_prep_core(c, inputs, sh) for c in range(NCORES)]
    nc = build_nc()
    res = run_bass_kernel_spmd(nc, in_maps, core_ids=list(range(NCORES)))
    out = np.zeros((4, 2 * NOWN, D), np.float32)
    for c in range(NCORES):
        b, half = c // 2, c % 2
        out[b, half * NOWN:(half + 1) * NOWN] = res.results[c]["y"]
    if DEBUG:
        kernel.last = res
    return out
```

```python
import contextlib
import numpy as np
import concourse.bass as bass
import concourse.mybir as mybir
from concourse.bass_utils import run_bass_kernel_spmd

F32 = mybir.dt.float32
I32 = mybir.dt.int32
BF16 = mybir.dt.bfloat16
AF = mybir.ActivationFunctionType
ALU = mybir.AluOpType
AX = mybir.AxisListType

D = 1024
NOWN = 4096
NKEY = 8448
NCORES = 8
EPS = 1e-6
BIG = 1.0e6
DEBUG = False
RUN_RET = True
RUN_MLA = True
RUN_REST = True

OFF_Q, OFF_KV, OFF_PE, OFF_RQ, OFF_RK, OFF_RV, OFF_RG = 0, 256, 384, 416, 672, 928, 1440
C_CQ = 0
C_CKV = 256
C_PE = 384
C_PESW = 480
C_RET = 576
NCOL = C_RET + 4 * 512
RQ, RQS, RK, RKS, RGt, RVt = 0, 64, 128, 192, 256, 384


def _swap_idx(dim):
    nf = dim // 4
    idx = np.arange(dim).reshape(2, 2, nf)
    return idx[:, ::-1, :].reshape(dim)


def _rope_tables(pos_row, pos_col, dim):
    nf = dim // 4
    inv = np.power(np.float32(10000.0), -np.arange(nf, dtype=np.float32) / np.float32(nf)).astype(np.float32)
    pos = np.stack([pos_row, pos_col], axis=-1).astype(np.float32)
    ang = pos[:, :, None] * inv
    ang = np.broadcast_to(ang[:, :, None, :], (pos.shape[0], 2, 2, nf)).reshape(pos.shape[0], dim)
    cos = np.cos(ang).astype(np.float32)
    sin = np.sin(ang).astype(np.float32)
    sgn = np.ones((2, 2, nf), np.float32)
    sgn[:, 0, :] = -1.0
    return cos, sin * sgn.reshape(dim)


class KB:
    def __init__(self, nc, es):
        self.nc = nc
        self.es = es
        self.eng = {'pe': nc.tensor, 'act': nc.scalar, 'dve': nc.vector, 'pool': nc.gpsimd, 'sp': nc.sync}
        self.sems = {}
        self.cnt = {}
        for e in ('pe', 'act', 'dve', 'pool'):
            self.sems[e] = es.enter_context(nc.semaphore("c_" + e))
            self.cnt[e] = 0
        self.seen = {e: {} for e in self.eng}
        self.lastw = {}
        self.rds = {}
        self.dcnt = {}
        self.freed = []
        self.uniq = 0
        self.iq = []
        self.bregs = {}

    def _dsem(self, sem):
        if sem not in self.sems:
            if self.freed:
                h, c = self.freed.pop()
            else:
                h, c = self.es.enter_context(self.nc.semaphore("s_" + sem)), 0
            self.sems[sem] = h
            self.dcnt[sem] = c

    def _waits(self, engine, reads, writes):
        need = {}
        for k in list(reads) + list(writes):
            ev = self.lastw.get(k)
            if ev is not None:
                s, v, e = ev
                if not (e == engine and engine == 'pe'):
                    need[s] = max(need.get(s, 0), v)
        for k in writes:
            for (s, v, e) in self.rds.get(k, ()):
                if e == engine:
                    continue
                need[s] = max(need.get(s, 0), v)
        eng = self.eng[engine]
        for s, v in need.items():
            if self.seen[engine].get(s, 0) >= v:
                continue
            eng.wait_ge(self.sems[s], v)
            self.seen[engine][s] = v

    def _record(self, ev, reads, writes):
        for k in reads:
            self.rds.setdefault(k, []).append(ev)
        for k in writes:
            self.lastw[k] = ev
            self.rds[k] = []

    def op(self, engine, fn, reads=(), writes=()):
        self._waits(engine, reads, writes)
        ins = fn(self.eng[engine])
        self.cnt[engine] += 1
        ins.then_inc(self.sems[engine], 1)
        self._record((engine, self.cnt[engine], engine), reads, writes)

    def dma(self, out, in_, reads=(), writes=(), sem='d0', queue='sp'):
        if len(sem) == 2 and sem[0] == 'c' and sem[1].isdigit():
            self.uniq += 1
            sem = '%s_%d' % (sem, self.uniq)
        self._dsem(sem)
        self._waits(queue, reads, writes)
        self.eng[queue].dma_start(out=out, in_=in_).then_inc(self.sems[sem], 16)
        self.dcnt[sem] += 16
        self._record((sem, self.dcnt[sem], 'dma'), reads, writes)

    def idma(self, out, out_off, in_, in_off, bounds, reads=(), writes=(), sem='i0'):
        self._dsem(sem)
        self._waits('pool', reads, writes)
        oo = bass.IndirectOffsetOnAxis(ap=out_off, axis=0) if out_off is not None else None
        io = bass.IndirectOffsetOnAxis(ap=in_off, axis=0) if in_off is not None else None
        if bounds not in self.bregs:
            r = self.nc.gpsimd.alloc_register("bc%d" % bounds)
            self.nc.gpsimd.reg_mov(r, bounds)
            self.bregs[bounds] = r
        self.nc.gpsimd.indirect_dma_start(out=out, out_offset=oo, in_=in_, in_offset=io, bounds_check=self.bregs[bounds],
                                          oob_is_err=False).then_inc(self.sems[sem], 16)
        self.dcnt[sem] += 16
        self._record((sem, self.dcnt[sem], 'dma'), reads, writes)
        self.iq.append((sem, self.dcnt[sem]))
        if len(self.iq) > 24:
            need = {}
            while len(self.iq) > 8:
                s_, v_ = self.iq.pop(0)
                need[s_] = max(need.get(s_, 0), v_)
            for s_, v_ in need.items():
                if s_ in self.sems and self.seen['pool'].get(s_, 0) < v_:
                    self.eng['pool'].wait_ge(self.sems[s_], v_)
                    self.seen['pool'][s_] = v_

    def barrier(self):
        for e in self.eng:
            eng = self.eng[e]
            for s in ('pe', 'act', 'dve', 'pool'):
                if s != e and self.cnt[s] > self.seen[e].get(s, 0):
                    eng.wait_ge(self.sems[s], self.cnt[s])
                    self.seen[e][s] = self.cnt[s]
            for s, v in self.dcnt.items():
                if v > self.seen[e].get(s, 0):
                    eng.wait_ge(self.sems[s], v)
                    self.seen[e][s] = v
        self.lastw = {}
        self.rds = {}
        self.iq = []
        for s in list(self.dcnt):
            self.freed.append((self.sems.pop(s), self.dcnt.pop(s)))
            for e in self.seen:
                self.seen[e].pop(s, None)


def build_nc():
    nc = bass.Bass("TRN2", target_bir_lowering=False)
    es = contextlib.ExitStack()

    def din(name, shape, dt=F32):
        return nc.dram_tensor(name, list(shape), dt, kind="ExternalInput").ap()

    def dscr(name, shape, dt):
        return nc.dram_tensor(name, list(shape), dt).ap()

    xall = din("xall", [NKEY, D])
    cvec = din("cvec", [128, 8, 2])
    w_ada = din("w_ada", [D, 6 * D])
    b_ada = din("b_ada", [128, 48])
    gvec = din("gvec", [128, 16])
    w_ext = din("w_ext", [D, NCOL])
    wq_ext = din("wq_ext", [256, 8 * 2 * 96])
    wkv_k = din("wkv_k", [128, 8 * 64])
    wkv_v = din("wkv_v", [128, 8 * 64])
    smallv = din("smallv", [128, 16])
    lgin = din("lgin", [128, 8])
    kcs = din("kcs", [2, 32, NKEY])
    qcs = din("qcs", [2, 32, NOWN])
    rkcs = din("rkcs", [2, 64, NKEY])
    rqcs = din("rqcs", [2, 64, NOWN])
    utab = din("utab", [3, 128, 512])
    dgtab = din("dgtab", [4, 3, 128, 512])
    cwtab = din("cwtab", [128, 5 * 8 * 32])
    flagv = din("flagv", [128, 2])
    w_out = din("w_out", [D, D])
    w_router = din("w_router", [D, 32])
    br_bc = din("br_bc", [128, 32])
    w1 = din("w1", [32, D, 2 * D])
    w2 = din("w2", [32, D, D])
    b2 = din("b2", [32, D])
    identf = din("identf", [128, 128])
    ustrict = din("ustrict", [128, 128])
    tri32 = din("tri32", [32, 64])
    routetab = din("routetab", [128, 161])
    B1R = din("B1R", [32, 2 * D])
    y = nc.dram_tensor("y", [NOWN, D], F32, kind="ExternalOutput").ap()

    XT = dscr("XT", [128, 8, NKEY], BF16)
    WP = dscr("WP", [128, 8, NCOL], BF16)
    WPC = dscr("WPC", [128, 8, NCOL], BF16)
    MIXT = dscr("MIXT", [D, NOWN], BF16)
    X1 = dscr("X1", [NOWN, D], F32)
    HF = dscr("HF", [NOWN, D], BF16)
    XS = dscr("XS", [160 * 128, D], BF16)
    YS = dscr("YS", [160 * 128, D], F32)
    W1R = dscr("W1R", [32 * 128, 8 * 2048], BF16)
    W2R = dscr("W2R", [32 * 128, 8 * 1024], BF16)
    B2G = dscr("B2G", [32, D], F32)
    dbg = {}
    if DEBUG:
        dbg['mixt'] = nc.dram_tensor("dbg_mixt", [D, NOWN], BF16, kind="ExternalOutput").ap()
        dbg['x1'] = nc.dram_tensor("dbg_x1", [NOWN, D], F32, kind="ExternalOutput").ap()
        dbg['mod'] = nc.dram_tensor("dbg_mod", [128, 96], F32, kind="ExternalOutput").ap()

    kb = KB(nc, es)
    op, dma = kb.op, kb.dma

    def sb(name, shape, dt=F32, stack=None):
        return (stack or es).enter_context(nc.sbuf_tensor(name, list(shape), dt))

    def ps(name, shape, dt=F32, stack=None):
        return (stack or es).enter_context(nc.psum_tensor(name, list(shape), dt))

    ident = sb("ident", [128, 128], F32)
    identb = sb("identb", [128, 128], BF16)
    onesb = sb("onesb", [128, 128], BF16)
    onesf = sb("onesf", [128, 128], F32)
    modv = sb("modv", [128, 96], F32)
    sv = sb("sv", [128, 16], F32)
    gv = sb("gv", [128, 16], F32)
    lg = sb("lg", [128, 16], F32)
    flg = sb("flg", [128, 2], F32)
    rs1 = sb("rs1", [128, 16], F32)
    dma(ident[:], identf[:, :], writes=['ident'], sem='c0')
    dma(sv[:], smallv[:, :], writes=['sv'], sem='c0')
    dma(gv[:], gvec[:, :], writes=['gv'], sem='c0')
    dma(lg[:, 0:8], lgin[:, :], writes=['lg'], sem='c0')
    dma(flg[:], flagv[:, :], writes=['flg'], sem='c0')
    op('dve', lambda e: e.tensor_copy(out=identb[:], in_=ident[:]), reads=['ident'], writes=['identb'])
    op('pool', lambda e: e.memset(onesb[:], 1.0), writes=['onesb'])
    op('pool', lambda e: e.memset(onesf[:], 1.0), writes=['onesf'])
    op('act', lambda e: e.activation(out=lg[:, 12:16], in_=lg[:, 0:4], func=AF.Exp, scale=-1.0), reads=['lg'], writes=['lgs'])
    op('act', lambda e: e.activation(out=lg[:, 8:12], in_=lg[:, 4:8], func=AF.Exp, scale=-1.0), reads=['lg'], writes=['lgs2'])
    op('act', lambda e: e.activation(out=lg[:, 12:16], in_=lg[:, 12:16], func=AF.Ln, bias=1.0), reads=['lgs'], writes=['lgs'])
    op('act', lambda e: e.activation(out=lg[:, 8:12], in_=lg[:, 8:12], func=AF.Ln, bias=1.0), reads=['lgs2'], writes=['lgs2'])
    op('dve', lambda e: e.tensor_scalar(out=lg[:, 0:4], in0=lg[:, 12:16], scalar1=-1.0, scalar2=None, op0=ALU.mult), reads=['lgs', 'lg'], writes=['lgf'])
    op('dve', lambda e: e.tensor_scalar(out=lg[:, 4:8], in0=lg[:, 8:12], scalar1=-1.0, scalar2=None, op0=ALU.mult), reads=['lgs2', 'lg'], writes=['lgb'])
    op('dve', lambda e: e.tensor_scalar(out=lg[:, 12:16], in0=lg[:, 0:4], scalar1=flg[:, 0:1], scalar2=None, op0=ALU.mult), reads=['lgf', 'flg', 'lgs'], writes=['lgt'])
    op('dve', lambda e: e.scalar_tensor_tensor(out=lg[:, 8:12], in0=lg[:, 4:8], scalar=flg[:, 1:2], in1=lg[:, 12:16], op0=ALU.mult, op1=ALU.add), reads=['lgb', 'lgt', 'lgs2'], writes=['lgo'])

    with contextlib.ExitStack() as st:
        cv = sb("cv", [128, 8, 2], F32, st)
        sg = sb("sgm", [128, 8, 2], F32, st)
        wa = [sb("wa%d" % i, [128, 8, 1024], F32, st) for i in range(2)]
        bad = sb("bad", [128, 48], F32, st)
        mps = ps("mps", [128, 96], F32, st)
        dma(cv[:], cvec[:, :, :], writes=['cv'], sem='c0')
        dma(bad[:], b_ada[:, :], writes=['bad'], sem='c0')
        op('act', lambda e: e.activation(out=sg[:], in_=cv[:], func=AF.Sigmoid), reads=['cv'], writes=['sg'])
        op('dve', lambda e: e.tensor_tensor(out=cv[:], in0=cv[:], in1=sg[:], op=ALU.mult), reads=['cv', 'sg'], writes=['cv'])
        wav = w_ada.rearrange("(kc p) n -> p kc n", p=128)
        for mc in range(6):
            w = wa[mc % 2]
            wk = 'wa%d' % (mc % 2)
            for kc in range(8):
                dma(w[:, kc, :], wav[:, kc, mc * 1024:(mc + 1) * 1024], writes=[wk], sem=wk)
            for fc in range(8):
                for kc in range(8):
                    op('pe', lambda e, fc=fc, kc=kc, w=w, mc=mc: e.matmul(
                        mps[:, (mc * 8 + fc) * 2:(mc * 8 + fc) * 2 + 2], lhsT=w[:, kc, fc * 128:(fc + 1) * 128],
                        rhs=cv[:, kc, :], start=(kc == 0), stop=(kc == 7)),
                        reads=[wk, 'cv'], writes=['mps'])
        mv3 = modv[:].rearrange("p (m j) -> p m j", j=2)
        op('dve', lambda e: e.tensor_tensor(out=mv3, in0=mps[:].rearrange("p (m j) -> p m j", j=2),
                                            in1=bad[:].unsqueeze(2).to_broadcast([128, 48, 2]), op=ALU.add),
           reads=['mps', 'bad'], writes=['modv'])
        for j in range(2):
            op('dve', lambda e, j=j: e.scalar_tensor_tensor(out=rs1[:, j * 8:(j + 1) * 8], in0=mv3[:, 8:16, j], scalar=1.0,
                                                          in1=gv[:, 0:8], op0=ALU.add, op1=ALU.mult),
               reads=['modv', 'gv'], writes=['rs1'])
        if DEBUG:
            dma(dbg['mod'][:, :], modv[:], reads=['modv'], sem='dbg')
        kb.barrier()

    g2g_bc = sb("g2g_bc", [128, 1024], F32)
    g2gs_bc = sb("g2gs_bc", [128, 1024], F32)
    mv3 = modv[:].rearrange("p (m j) -> p m j", j=2)
    with contextlib.ExitStack() as st:
        dg0 = [sb("dg0%d" % i, [128, 128], F32, st) for i in range(2)]
        b2t = sb("b2t", [32, 1024], F32, st)
        gps = ps("gps", [128, 1024], F32, st)
        for kc in range(8):
            d_, dk = dg0[kc % 2], 'dg0%d' % (kc % 2)
            op('dve', lambda e, d_=d_, kc=kc: e.tensor_scalar(out=d_[:], in0=ident[:], scalar1=mv3[:, 40 + kc, 0:1], scalar2=None, op0=ALU.mult),
               reads=['ident', 'modv'], writes=[dk])
            op('pe', lambda e, d_=d_, kc=kc: e.matmul(gps[:, kc * 128:(kc + 1) * 128], lhsT=onesf[:], rhs=d_[:], start=True, stop=True), reads=['onesf', dk], writes=['gps'])
        op('act', lambda e: e.copy(out=g2g_bc[:], in_=gps[:, :]), reads=['gps'], writes=['g2g_bc'])
        op('act', lambda e: e.mul(out=g2gs_bc[:], in_=gps[:, :], mul=float(1.0 / 1.702)), reads=['gps'], writes=['g2gs_bc'])
        dma(b2t[:], b2[:, :], writes=['b2t'], sem='c0')
        op('dve', lambda e: e.tensor_tensor(out=b2t[:], in0=b2t[:], in1=g2g_bc[0:32, :], op=ALU.mult), reads=['b2t', 'g2g_bc'], writes=['b2t'])
        dma(B2G[:, :], b2t[:], reads=['b2t'], writes=['B2G'], sem='c0')
        kb.barrier()

    mv3 = modv[:].rearrange("p (m j) -> p m j", j=2)
    FCH = [(C_CQ, 128), (C_CQ + 128, 128), (C_CKV, 128), (C_PE, 96), (C_PESW, 96)]
    for h in range(4):
        b0 = C_RET + h * 512
        FCH += [(b0 + RQ, 64), (b0 + RQS, 64), (b0 + RK, 64), (b0 + RKS, 64), (b0 + RGt, 128)]
    NF = len(FCH)
    FIDX = {c0: i for i, (c0, _) in enumerate(FCH)}
    pbf = sb("pbf", [128, NF, 2], F32)
    pbv = sb("pbv", [128, 2, 512], F32)

    def mm(out, lhsT, rhs, start=True, stop=True):
        return lambda e: e.matmul(out, lhsT=lhsT, rhs=rhs, start=start, stop=stop)

    with contextlib.ExitStack() as st:
        wr = [sb("wr%d" % i, [128, 8, 576], F32, st) for i in range(2)]
        wb = [sb("wb%d" % i, [128, 8, 576], BF16, st) for i in range(2)]
        wc = [sb("wc%d" % i, [128, 8, 576], BF16, st) for i in range(2)]
        shb = sb("shb", [128, 2, 8, 128], F32, st)
        bps = ps("bps", [128, NF * 2], F32, st)
        vps = ps("vps", [128, 2, 512], F32, st)
        for j in range(2):
            for kc in range(8):
                op('dve', lambda e, j=j, kc=kc: e.tensor_copy(out=shb[:, j, kc, :], in_=mv3[:, kc, j:j + 1].to_broadcast([128, 128])),
                   reads=['modv'], writes=['shb'])
        wev = w_ext.rearrange("(kc p) n -> p kc n", p=128)
        pieces = [(0, 576)] + [(C_RET + h * 512, 512) for h in range(4)]
        for pi, (c0, w) in enumerate(pieces):
            r = wr[pi % 2]
            rk = 'wr%d' % (pi % 2)
            for kc in range(8):
                dma(r[:, kc, :w], wev[:, kc, c0:c0 + w], writes=[rk], sem=rk)
            for fi, (fc0, M) in enumerate(FCH):
                if not (c0 <= fc0 < c0 + w):
                    continue
                for kc in range(8):
                    op('pe', mm(bps[0:M, fi * 2:fi * 2 + 2], r[:, kc, fc0 - c0:fc0 - c0 + M], mv3[:, kc, :], kc == 0, kc == 7),
                       reads=[rk, 'modv'], writes=['bps'])
            if pi >= 1:
                h = pi - 1
                for j in range(2):
                    for kc in range(8):
                        op('pe', mm(vps[:, j, h * 128:(h + 1) * 128], shb[:, j, kc, :], r[:, kc, RVt:RVt + 128], kc == 0, kc == 7),
                           reads=[rk, 'shb'], writes=['vps'])
            wbk, wck = 'wb%d' % (pi % 2), 'wc%d' % (pi % 2)
            for kc in range(8):
                op('dve', lambda e, kc=kc, r=r, w=w, pi=pi: e.tensor_scalar(out=wb[pi % 2][:, kc, :w], in0=r[:, kc, :w], scalar1=rs1[:, kc:kc + 1],
                                                                    scalar2=None, op0=ALU.mult), reads=[rk, 'rs1'], writes=[wbk])
                op('act', lambda e, kc=kc, r=r, w=w, pi=pi: e.activation(out=wc[pi % 2][:, kc, :w], in_=r[:, kc, :w], func=AF.Identity, scale=rs1[:, 8 + kc:9 + kc]),
                   reads=[rk, 'rs1'], writes=[wck])
            dma(WP[:, :, c0:c0 + w], wb[pi % 2][:, :, :w], reads=[wbk], writes=['WP'], sem='wpo%d' % (pi % 2))
            dma(WPC[:, :, c0:c0 + w], wc[pi % 2][:, :, :w], reads=[wck], writes=['WPC'], sem='wpc%d' % (pi % 2))
        op('dve', lambda e: e.tensor_copy(out=pbf[:].rearrange("p f j -> p (f j)"), in_=bps[:]), reads=['bps'], writes=['pbf'])
        op('dve', lambda e: e.tensor_copy(out=pbv[:], in_=vps[:]), reads=['vps'], writes=['pbv'])
        kb.barrier()

    GROUPS = [(0, 2)] + [(2 + 4 * g, 4) for g in range(16)]
    with contextlib.ExitStack() as st:
        xt = [sb("xt%d" % i, [128, 1024], F32, st) for i in range(3)]
        sqj = sb("sqj", [128, 1024], F32, st)
        ssq = [sb("ssq%d" % i, [128, 4], F32, st) for i in range(3)]
        xh = [sb("xh%d" % i, [128, 1024], BF16, st) for i in range(2)]
        xTb = [sb("xTb%d" % i, [128, 8, 512], BF16, st) for i in range(2)]
        tps = [ps("tps%d" % i, [128, 8, 128], BF16, st) for i in range(2)]
        tiles = [(gi, t0, nt, t) for gi, (t0, nt) in enumerate(GROUPS) for t in range(nt)]

        def p1_a(ti):
            gi, t0, nt, t = tiles[ti]
            tile = t0 + t
            a, bh = ti % 3, ti % 2
            xk, hk, pk = 'xt%d' % a, 'xh%d' % bh, 'tps%d' % bh
            dma(xt[a][:], xall[tile * 128:(tile + 1) * 128, :], writes=[xk], sem=xk)
            op('act', lambda e, a=a: e.activation(out=sqj[:], in_=xt[a][:], func=AF.Square, accum_out=ssq[a][:, 0:1]),
               reads=[xk], writes=['sqj', 'ss%d' % a])
            op('act', lambda e, a=a: e.activation(out=ssq[a][:, 1:2], in_=ssq[a][:, 0:1], func=AF.Sqrt, scale=1.0 / 1024, bias=EPS),
               reads=['ss%d' % a], writes=['sr%d' % a])
            op('dve', lambda e, a=a: e.reciprocal(out=ssq[a][:, 2:3], in_=ssq[a][:, 1:2]), reads=['sr%d' % a], writes=['rc%d' % a])
            op('dve', lambda e, a=a, bh=bh: e.tensor_scalar(out=xh[bh][:], in0=xt[a][:], scalar1=ssq[a][:, 2:3], scalar2=None, op0=ALU.mult),
               reads=[xk, 'rc%d' % a], writes=[hk])
            for kc in range(8):
                op('pe', lambda e, kc=kc, bh=bh: e.transpose(out=tps[bh][:, kc, :], in_=xh[bh][:, kc * 128:(kc + 1) * 128], identity=identb[:]),
                   reads=[hk, 'identb'], writes=[pk])

        def p1_b(ti):
            gi, t0, nt, t = tiles[ti]
            bh = ti % 2
            xb_ = xTb[gi % 2]
            xbk_ = 'xTb%d' % (gi % 2)
            op('dve', lambda e, bh=bh, t=t, xb_=xb_: e.tensor_copy(out=xb_[:, :, t * 128:(t + 1) * 128], in_=tps[bh][:]), reads=['tps%d' % bh], writes=[xbk_])
            if t == nt - 1:
                dma(XT[:, :, t0 * 128:(t0 + nt) * 128], xb_[:, :, :nt * 128], reads=[xbk_], writes=['XT'], sem='xto%d' % (gi % 2))

        p1_a(0)
        for ti in range(len(tiles)):
            if ti + 1 < len(tiles):
                p1_a(ti + 1)
            p1_b(ti)
        kb.barrier()

    if RUN_RET:
      with contextlib.ExitStack() as st:
        wsl = sb("wsl", [128, 8, 512], BF16, st)
        wslc = sb("wslc", [128, 8, 512], BF16, st)
        kT = sb("kT", [128, NKEY], BF16, st)
        Vr = sb("Vr", [128, 66, 128], BF16, st)
        qT = sb("qT", [128, NOWN], BF16, st)
        qTv = sb("qTv", [128, 3, NOWN], BF16, st)
        sgT = sb("sgT", [128, NOWN], BF16, st)
        xb = [sb("xb%d" % i, [128, 8, 512], BF16, st) for i in range(2)]
        tabc = [sb("tabc%d" % i, [64, 512], F32, st) for i in range(2)]
        tabs = [sb("tabs%d" % i, [64, 512], F32, st) for i in range(2)]
        ta = [sb("ta%d" % i, [64, 512], F32, st) for i in range(2)]
        tb = [sb("tb%d" % i, [64, 512], F32, st) for i in range(2)]
        uts = sb("uts", [128, 3, 512], F32, st)
        UT = sb("UT", [128, 3, 512], F32, st)
        dgs = sb("dgs", [128, 4, 3, 512], F32, st)
        bts = sb("bts", [128, 5, 256], F32, st)
        Bh = sb("Bh", [128, 5, 256], F32, st)
        mk = [sb("mk%d" % i, [128, 512], F32, st) for i in range(3)]
        mkx = sb("mkx", [128, 512], F32, st)
        Am = [sb("Am%d" % i, [128, 512], BF16, st) for i in range(3)]
        osb = sb("osb", [128, 512], F32, st)
        osq = sb("osq", [128, 512], BF16, st)
        orr = sb("orr", [128, 512], F32, st)
        omx = [sb("omx%d" % i, [128, 512], BF16, st) for i in range(2)]
        pk_ps = [ps("pk%d" % i, [64, 512], F32, st) for i in range(2)]
        pg_ps = ps("pg", [128, 512], F32, st)
        st_ps = [ps("stp%d" % i, [128, 512], F32, st) for i in range(3)]
        o_ps = ps("ops", [128, 512], F32, st)
        ss_ps = ps("ssp", [128, 512], F32, st)
        for i in range(3):
            dma(uts[:, i, :], utab[i, :, :], writes=['uts'], sem='c1')
        op('pool', lambda e: e.memset(kT[64:128, :], 0.0), writes=['kTz'])
        op('pool', lambda e: e.memset(qT[64:128, :], 0.0), writes=['qTz'])
        op('pool', lambda e: e.memset(qTv[64:128, :, :], 0.0), writes=['qTvz'])
        for r_ in range(4):
            for i in range(3):
                dma(dgs[:, r_, i, :], dgtab[r_, i, :, :], writes=['dgs'], sem='c1')
        dma(bts[:].rearrange("p c n -> p (c n)"), cwtab[:, :], writes=['bts'], sem='c1')
        LGC = [0, 8, 0, 4, 4]
        for h in range(4):
            b0 = C_RET + h * 512
            dma(wsl[:], WP[:, :, b0:b0 + 512], writes=['wsl'], sem='wsl')
            dma(wslc[:], WPC[:, :, b0:b0 + 512], writes=['wslc'], sem='wslc')
            for cl in range(5):
                op('act', lambda e, cl=cl, h=h: e.activation(out=Bh[:, cl, :], in_=bts[:, cl, :], func=AF.Exp, scale=lg[:, LGC[cl] + h:LGC[cl] + h + 1]),
                   reads=['bts', 'lgf', 'lgb', 'lgo'], writes=['Bh'])
            op('dve', lambda e: e.tensor_scalar(out=Bh[:], in0=Bh[:], scalar1=0.125, scalar2=None, op0=ALU.mult), reads=['Bh'], writes=['Bh'])
            for ti_, lc in enumerate((0, 4, 8)):
                op('act', lambda e, ti_=ti_, lc=lc, h=h: e.activation(out=UT[:, ti_, :], in_=uts[:, ti_, :], func=AF.Exp, scale=lg[:, lc + h:lc + h + 1]),
                   reads=['uts', 'lgf', 'lgb', 'lgo'], writes=['UT'])
            fq, fqs, fk, fks, fg = (FIDX[b0 + RQ], FIDX[b0 + RQS], FIDX[b0 + RK], FIDX[b0 + RKS], FIDX[b0 + RGt])
            for gi, (t0, nt) in enumerate(GROUPS):
                nb = nt * 128
                tok0 = t0 * 128
                j = 1 if gi == 0 else 0
                W = wslc if gi == 0 else wsl
                Wk = 'wslc' if gi == 0 else 'wsl'
                xbb = xb[gi % 2]
                xk = 'xb%d' % (gi % 2)
                dma(xbb[:, :, :nb], XT[:, :, tok0:tok0 + nb], reads=['XT'], writes=[xk], sem=xk)
                tc_, ts_ = tabc[gi % 2], tabs[gi % 2]
                tk = 'tab%d' % (gi % 2)
                dma(tc_[:, :nb], rkcs[0, :, tok0:tok0 + nb], writes=[tk + 'c'], sem=tk + 'c')
                dma(ts_[:, :nb], rkcs[1, :, tok0:tok0 + nb], writes=[tk + 's'], sem=tk + 's')

                def rope_proj(c_a, c_b, f_a, f_b, dst, dkey, q0v=None, gi=gi, nb=nb, W=W, Wk=Wk, xbb=xbb, xk=xk, tc_=tc_, ts_=ts_, tk=tk, j=j):
                    for kc in range(8):
                        op('pe', mm(pk_ps[0][:, :nb], W[:, kc, c_a:c_a + 64], xbb[:, kc, :nb], kc == 0, kc == 7), reads=[Wk, xk], writes=['pk0'])
                    for kc in range(8):
                        op('pe', mm(pk_ps[1][:, :nb], W[:, kc, c_b:c_b + 64], xbb[:, kc, :nb], kc == 0, kc == 7), reads=[Wk, xk], writes=['pk1'])
                    a_, b_ = ta[gi % 2], tb[gi % 2]
                    op('dve', lambda e: e.scalar_tensor_tensor(out=a_[:, :nb], in0=pk_ps[0][:, :nb], scalar=pbf[0:64, f_a, j:j + 1], in1=tc_[:, :nb],
                                                               op0=ALU.add, op1=ALU.mult), reads=['pk0', 'pbf', tk + 'c'], writes=['ta%d' % (gi % 2)])
                    op('dve', lambda e: e.scalar_tensor_tensor(out=b_[:, :nb], in0=pk_ps[1][:, :nb], scalar=pbf[0:64, f_b, j:j + 1], in1=ts_[:, :nb],
                                                               op0=ALU.add, op1=ALU.mult), reads=['pk1', 'pbf', tk + 's'], writes=['tb%d' % (gi % 2)])
                    if q0v is None:
                        op('pool', lambda e: e.tensor_tensor(out=dst, in0=a_[:, :nb], in1=b_[:, :nb], op=ALU.add),
                           reads=['ta%d' % (gi % 2), 'tb%d' % (gi % 2)], writes=[dkey])
                    else:
                        op('dve', lambda e: e.tensor_tensor(out=a_[:, :nb], in0=a_[:, :nb], in1=b_[:, :nb], op=ALU.add),
                           reads=['ta%d' % (gi % 2), 'tb%d' % (gi % 2)], writes=['ta%d' % (gi % 2)])
                        op('pool', lambda e: e.tensor_copy(out=dst, in_=a_[:, :nb]), reads=['ta%d' % (gi % 2)], writes=[dkey])
                        for v in range(3):
                            op('dve', lambda e, v=v: e.tensor_tensor(out=qTv[0:64, v, q0v:q0v + 512], in0=a_[:, :nb], in1=UT[0:64, v, :], op=ALU.mult),
                               reads=['ta%d' % (gi % 2), 'UT'], writes=['qTv'])

                rope_proj(RK, RKS, fk, fks, kT[0:64, tok0:tok0 + nb], 'kT')
                for t in range(nt):
                    for kc in range(8):
                        op('pe', mm(pg_ps[:, t * 128:(t + 1) * 128], xbb[:, kc, t * 128:(t + 1) * 128], W[:, kc, RVt:RVt + 128], kc == 0, kc == 7),
                           reads=[Wk, xk], writes=['pg'])
                op('dve', lambda e, t0=t0, nt=nt, j=j, h=h: e.tensor_tensor(
                    out=Vr[:, t0:t0 + nt, :], in0=pg_ps[:, :nt * 128].rearrange("p (t c) -> p t c", c=128),
                    in1=pbv[:, j, h * 128:(h + 1) * 128].unsqueeze(1).to_broadcast([128, nt, 128]), op=ALU.add),
                    reads=['pg', 'pbv'], writes=['Vr'])
                if gi >= 9:
                    q0 = (gi - 9) * 512
                    rope_proj(RQ, RQS, fq, fqs, qT[0:64, q0:q0 + 512], 'qT', q0v=q0)
                    for kc in range(8):
                        op('pe', mm(pg_ps[:, :], W[:, kc, RGt:RGt + 128], xbb[:, kc, :], kc == 0, kc == 7), reads=[Wk, xk], writes=['pg'])
                    op('act', lambda e, q0=q0, fg=fg: e.activation(out=sgT[:, q0:q0 + 512], in_=pg_ps[:, :], func=AF.Silu, bias=pbf[:, fg, 0:1]),
                       reads=['pg', 'pbf'], writes=['sgT'])
            ui = 0
            rfin = []
            for qb in range(8):
                units = [(0, kt, kt, 0) for kt in range(2)]
                units += [(1, kt, 2 + kt, 2) for kt in range(32)]
                for kt in range(32):
                    if kt < 4 * qb:
                        units.append((2, kt, 34 + kt, 0))
                    elif kt >= 4 * qb + 4:
                        units.append((3, kt, 34 + kt, 1))
                    else:
                        units.append((-1, kt, 34 + kt, kt - 4 * qb))
                units += [(4, kt, kt, 1) for kt in range(2)]
                LA = 2
                pend = []
                for n in range(len(units) + LA):
                    if n < len(units):
                        (cl, kt, ktile, tidx) = units[n]
                        sp_, m_, a_ = st_ps[ui % 3], mk[ui % 3], Am[ui % 3]
                        spk, mkk, ak = 'stp%d' % (ui % 3), 'mk%d' % (ui % 3), 'Am%d' % (ui % 3)
                        qop = qTv[:, tidx, qb * 512:(qb + 1) * 512] if cl >= 0 else qT[:, qb * 512:(qb + 1) * 512]
                        op('pe', mm(sp_[:, :], kT[:, ktile * 128:(ktile + 1) * 128], qop), reads=['kT', 'qT', 'qTv', 'kTz', 'qTz', 'qTvz'], writes=[spk])
                        if cl >= 0:
                            if ui % 2 == 0:
                                op('dve', lambda e, a_=a_, sp_=sp_, cl=cl, qb=qb, kt=kt: e.tensor_scalar(
                                    out=a_[:], in0=sp_[:, :], scalar1=Bh[:, cl, qb * 32 + kt:qb * 32 + kt + 1], scalar2=None, op0=ALU.mult),
                                    reads=[spk, 'Bh'], writes=[ak])
                            else:
                                op('act', lambda e, a_=a_, sp_=sp_, cl=cl, qb=qb, kt=kt: e.activation(
                                    out=a_[:], in_=sp_[:, :], func=AF.Identity, scale=Bh[:, cl, qb * 32 + kt:qb * 32 + kt + 1]),
                                    reads=[spk, 'Bh'], writes=[ak])
                        else:
                            op('act', lambda e, m_=m_, tidx=tidx, h=h: e.activation(out=m_[:], in_=dgs[:, tidx, 0, :], func=AF.Exp, scale=lg[:, h:h + 1]),
                               reads=['dgs', 'lgf'], writes=[mkk])
                            op('act', lambda e, tidx=tidx, h=h: e.activation(out=mkx[:], in_=dgs[:, tidx, 1, :], func=AF.Exp, scale=lg[:, 4 + h:5 + h]),
                               reads=['dgs', 'lgb'], writes=['mkx'])
                            op('pool', lambda e, m_=m_: e.tensor_tensor(out=m_[:], in0=m_[:], in1=mkx[:], op=ALU.add), reads=[mkk, 'mkx'], writes=[mkk])
                            op('pool', lambda e, m_=m_, tidx=tidx: e.tensor_tensor(out=m_[:], in0=m_[:], in1=dgs[:, tidx, 2, :], op=ALU.add),
                               reads=[mkk, 'dgs'], writes=[mkk])
                            op('dve', lambda e, a_=a_, sp_=sp_, m_=m_: e.scalar_tensor_tensor(out=a_[:], in0=sp_[:, :], scalar=0.125, in1=m_[:],
                                                                                             op0=ALU.mult, op1=ALU.mult), reads=[spk, mkk], writes=[ak])
                        pend.append((n, ktile, a_, ak))
                        ui += 1
                        if rfin and n % 6 == 5:
                            rfin.pop(0)()
                    if n >= LA:
                        (n0, ktile0, a0, ak0) = pend.pop(0)
                        op('pe', mm(o_ps[:, :], Vr[:, ktile0, :], a0[:], n0 == 0, n0 == len(units) - 1), reads=['Vr', ak0], writes=['ops'])
                op('act', lambda e: e.copy(out=osb[:], in_=o_ps[:, :]), reads=['ops'], writes=['osb'])

                def fin_steps(h=h, qb=qb):
                    mx = omx[qb % 2]
                    mxk = 'omx%d' % (qb % 2)
                    return [
                        lambda: op('dve', lambda e: e.tensor_tensor(out=osq[:], in0=osb[:], in1=osb[:], op=ALU.mult), reads=['osb'], writes=['osq']),
                        lambda: op('pe', mm(ss_ps[:, :], onesb[:], osq[:]), reads=['onesb', 'osq'], writes=['ssp']),
                        lambda: op('act', lambda e: e.activation(out=orr[:], in_=ss_ps[:, :], func=AF.Sqrt, scale=1.0 / 128, bias=EPS), reads=['ssp'], writes=['orr']),
                        lambda: op('dve', lambda e: e.reciprocal(out=orr[:], in_=orr[:]), reads=['orr'], writes=['orr']),
                        lambda: op('dve', lambda e: e.scalar_tensor_tensor(out=osb[:], in0=osb[:], scalar=sv[:, 7 + h:8 + h], in1=orr[:], op0=ALU.mult, op1=ALU.mult),
                                   reads=['osb', 'orr', 'sv'], writes=['osb']),
                        lambda: (op('dve', lambda e: e.tensor_tensor(out=mx[:], in0=osb[:], in1=sgT[:, qb * 512:(qb + 1) * 512], op=ALU.mult), reads=['osb', 'sgT'], writes=[mxk]),
                                 dma(MIXT[512 + h * 128:512 + (h + 1) * 128, qb * 512:(qb + 1) * 512], mx[:], reads=[mxk], writes=['MIXT'], sem=mxk)),
                    ]
                rfin.extend(fin_steps())
                if qb == 7:
                    while rfin:
                        rfin.pop(0)()
        kb.barrier()
    if RUN_MLA:
      with contextlib.ExitStack() as st:
        ckvT = sb("ckvT", [128, NKEY], BF16, st)
        KT = [sb("KT%d" % i, [96, NKEY], BF16, st) for i in range(2)]
        cqT = sb("cqT", [128, 2, NOWN], BF16, st)
        sspe = sb("sspe", [128, 66], F32, st)
        wqb = sb("wqb", [128, 2, 1536], BF16, st)
        wkb = sb("wkb", [128, 1024], BF16, st)
        fkv, fpe, fpesw, fq0, fq1 = FIDX[C_CKV], FIDX[C_PE], FIDX[C_PESW], FIDX[C_CQ], FIDX[C_CQ + 128]
        with contextlib.ExitStack() as s2:
            wm = sb("wm", [128, 8, 576], BF16, s2)
            wmc = sb("wmc", [128, 8, 576], BF16, s2)
            wqr = sb("wqr", [128, 2, 1536], F32, s2)
            wkr = sb("wkr", [128, 1024], F32, s2)
            xb = [sb("mxb%d" % i, [128, 8, 512], BF16, s2) for i in range(2)]
            pkv = sb("pkv", [128, 512], F32, s2)
            sqv = sb("sqv", [128, 512], BF16, s2)
            srt = sb("srt", [128, 512], F32, s2)
            rawpe = sb("rawpe", [96, 512], F32, s2)
            rawsw = sb("rawsw", [96, 512], F32, s2)
            sqpe = sb("sqpe", [96, 512], BF16, s2)
            tcm = [sb("tcm%d" % i, [96, 512], F32, s2) for i in range(2)]
            tsm = [sb("tsm%d" % i, [96, 512], F32, s2) for i in range(2)]
            pa_ = sb("pa_", [96, 512], F32, s2)
            pb_ = sb("pb_", [96, 512], F32, s2)
            pq = sb("pq", [128, 2, 512], F32, s2)
            sq2 = sb("sq2", [128, 2, 512], BF16, s2)
            pA = ps("pA", [128, 512], F32, s2)
            pB = ps("pB", [96, 512], F32, s2)
            pC = ps("pC", [96, 512], F32, s2)
            ssb = ps("ssb", [128, 512], F32, s2)
            pss = ps("pss", [128, 66], F32, s2)
            pQ = [ps("pQ%d" % i, [128, 512], F32, s2) for i in range(2)]
            dma(wm[:], WP[:, :, 0:576], writes=['wm'], sem='c2')
            dma(wmc[:], WPC[:, :, 0:576], writes=['wmc'], sem='c2')
            dma(wqr[:], wq_ext.rearrange("(c p) n -> p c n", p=128), writes=['wqr'], sem='c2')
            dma(wkr[:, 0:512], wkv_k[:, :], writes=['wkr'], sem='c2')
            dma(wkr[:, 512:1024], wkv_v[:, :], writes=['wkr'], sem='c2')
            for c in range(2):
                op('dve', lambda e, c=c: e.tensor_scalar(out=wqb[:, c, :], in0=wqr[:, c, :], scalar1=sv[:, c:c + 1], scalar2=None, op0=ALU.mult),
                   reads=['wqr', 'sv'], writes=['wqb'])
            op('dve', lambda e: e.tensor_scalar(out=wkb[:], in0=wkr[:], scalar1=sv[:, 2:3], scalar2=None, op0=ALU.mult),
               reads=['wkr', 'sv'], writes=['wkb'])
            for gi, (t0, nt) in enumerate(GROUPS):
                nb, tok0 = nt * 128, t0 * 128
                j = 1 if gi == 0 else 0
                W, Wk = (wmc, 'wmc') if gi == 0 else (wm, 'wm')
                xbb, xk = xb[gi % 2], 'mxb%d' % (gi % 2)
                dma(xbb[:, :, :nb], XT[:, :, tok0:tok0 + nb], reads=['XT'], writes=[xk], sem=xk)
                tc_, ts_, tk = tcm[gi % 2], tsm[gi % 2], 'mtab%d' % (gi % 2)
                dma(tc_[64:96, :nb], kcs[0, :, tok0:tok0 + nb], writes=[tk + 'c'], sem=tk + 'c')
                dma(ts_[64:96, :nb], kcs[1, :, tok0:tok0 + nb], writes=[tk + 's'], sem=tk + 's')
                for kc in range(8):
                    op('pe', mm(pA[:, :nb], W[:, kc, C_CKV:C_CKV + 128], xbb[:, kc, :nb], kc == 0, kc == 7), reads=[Wk, xk], writes=['pA'])
                op('act', lambda e, nb=nb, j=j: e.activation(out=pkv[:, :nb], in_=pA[:, :nb], func=AF.Identity, bias=pbf[:, fkv, j:j + 1]),
                   reads=['pA', 'pbf'], writes=['pkv'])
                op('pool', lambda e, nb=nb: e.tensor_tensor(out=sqv[:, :nb], in0=pkv[:, :nb], in1=pkv[:, :nb], op=ALU.mult), reads=['pkv'], writes=['sqv'])
                op('pe', mm(ssb[:, :nb], onesb[:], sqv[:, :nb]), reads=['onesb', 'sqv'], writes=['ssb'])
                op('act', lambda e, nb=nb: e.activation(out=srt[:, :nb], in_=ssb[:, :nb], func=AF.Sqrt, scale=1.0 / 128, bias=EPS), reads=['ssb'], writes=['srt'])
                op('dve', lambda e, nb=nb: e.reciprocal(out=srt[:, :nb], in_=srt[:, :nb]), reads=['srt'], writes=['srt'])
                op('dve', lambda e, nb=nb, tok0=tok0: e.tensor_tensor(out=ckvT[:, tok0:tok0 + nb], in0=pkv[:, :nb], in1=srt[:, :nb], op=ALU.mult),
                   reads=['pkv', 'srt'], writes=['ckvT'])
                for kc in range(8):
                    op('pe', mm(pB[:, :nb], W[:, kc, C_PE:C_PE + 96], xbb[:, kc, :nb], kc == 0, kc == 7), reads=[Wk, xk], writes=['pB'])
                for kc in range(8):
                    op('pe', mm(pC[:, :nb], W[:, kc, C_PESW:C_PESW + 96], xbb[:, kc, :nb], kc == 0, kc == 7), reads=[Wk, xk], writes=['pC'])
                op('act', lambda e, nb=nb, j=j: e.activation(out=rawpe[64:96, :nb], in_=pB[64:96, :nb], func=AF.Identity, bias=pbf[64:96, fpe, j:j + 1]),
                   reads=['pB', 'pbf'], writes=['rawpe'])
                op('act', lambda e, nb=nb, j=j: e.activation(out=rawsw[64:96, :nb], in_=pC[64:96, :nb], func=AF.Identity, bias=pbf[64:96, fpesw, j:j + 1]),
                   reads=['pC', 'pbf'], writes=['rawsw'])
                op('pool', lambda e, nb=nb: e.tensor_tensor(out=sqpe[64:96, :nb], in0=rawpe[64:96, :nb], in1=rawpe[64:96, :nb], op=ALU.mult),
                   reads=['rawpe'], writes=['sqpe'])
                for t in range(nt):
                    op('pe', mm(pss[:, t0 + t:t0 + t + 1], sqpe[64:96, t * 128:(t + 1) * 128], onesb[64:96, 0:1]), reads=['sqpe', 'onesb'], writes=['pss'])
                op('dve', lambda e, nb=nb, tc_=tc_: e.scalar_tensor_tensor(out=pa_[64:96, :nb], in0=rawpe[64:96, :nb], scalar=sv[64:96, 5:6], in1=tc_[64:96, :nb],
                                                                      op0=ALU.mult, op1=ALU.mult), reads=['rawpe', 'sv', tk + 'c'], writes=['pa_'])
                op('dve', lambda e, nb=nb, ts_=ts_: e.scalar_tensor_tensor(out=pb_[64:96, :nb], in0=rawsw[64:96, :nb], scalar=sv[64:96, 6:7], in1=ts_[64:96, :nb],
                                                                      op0=ALU.mult, op1=ALU.mult), reads=['rawsw', 'sv', tk + 's'], writes=['pb_'])
                op('pool', lambda e, nb=nb, tok0=tok0: e.tensor_tensor(out=KT[0][64:96, tok0:tok0 + nb], in0=pa_[64:96, :nb], in1=pb_[64:96, :nb], op=ALU.add),
                   reads=['pa_', 'pb_'], writes=['KT0pe'])
                op('pool', lambda e, nb=nb, tok0=tok0: e.tensor_copy(out=KT[1][64:96, tok0:tok0 + nb], in_=KT[0][64:96, tok0:tok0 + nb]),
                   reads=['KT0pe'], writes=['KT1pe'])
                if gi >= 9:
                    q0 = (gi - 9) * 512
                    for c in range(2):
                        for kc in range(8):
                            op('pe', mm(pQ[c][:, :], W[:, kc, C_CQ + c * 128:C_CQ + (c + 1) * 128], xbb[:, kc, :], kc == 0, kc == 7),
                               reads=[Wk, xk], writes=['pQ%d' % c])
                        op('act', lambda e, c=c: e.activation(out=pq[:, c, :], in_=pQ[c][:, :], func=AF.Identity, bias=pbf[:, fq0 + c, 0:1]),
                           reads=['pQ%d' % c, 'pbf'], writes=['pq%d' % c])
                        op('pool', lambda e, c=c: e.tensor_tensor(out=sq2[:, c, :], in0=pq[:, c, :], in1=pq[:, c, :], op=ALU.mult),
                           reads=['pq%d' % c], writes=['sq2%d' % c])
                    for c in range(2):
                        op('pe', mm(ssb[:, :], onesb[:], sq2[:, c, :], c == 0, c == 1), reads=['onesb', 'sq2%d' % c], writes=['ssb'])
                    op('act', lambda e: e.activation(out=srt[:, :], in_=ssb[:, :], func=AF.Sqrt, scale=1.0 / 256, bias=EPS), reads=['ssb'], writes=['srt'])
                    op('dve', lambda e: e.reciprocal(out=srt[:, :], in_=srt[:, :]), reads=['srt'], writes=['srt'])
                    for c in range(2):
                        op('dve', lambda e, c=c, q0=q0: e.tensor_tensor(out=cqT[:, c, q0:q0 + 512], in0=pq[:, c, :], in1=srt[:, :], op=ALU.mult),
                           reads=['pq%d' % c, 'srt'], writes=['cqT'])
            op('dve', lambda e: e.tensor_copy(out=sspe[:], in_=pss[:, :]), reads=['pss'], writes=['sspe'])
            kb.barrier()
        with contextlib.ExitStack() as s2:
            QT = [sb("QT%d" % i, [96, NOWN], BF16, s2) for i in range(2)]
            Vh = [sb("Vh%d" % i, [128, 66, 65], BF16, s2) for i in range(2)]
            skh = [sb("skh%d" % i, [128, 66], F32, s2) for i in range(2)]
            sqk = [sb("sqk%d" % i, [64, 512], BF16, s2) for i in range(2)]
            qraw = sb("qraw", [96, 512], F32, s2)
            sqq = sb("sqq", [96, 512], BF16, s2)
            rq = sb("rq", [96, 512], F32, s2)
            qc_ = [sb("qc%d" % i, [96, 512], F32, s2) for i in range(2)]
            qs_ = [sb("qs%d" % i, [96, 512], F32, s2) for i in range(2)]
            qa_ = sb("qa_", [96, 512], F32, s2)
            qb_ = sb("qb_", [96, 512], F32, s2)
            pT = [sb("pT%d" % i, [128, 512], BF16, s2) for i in range(3)]
            ot = sb("ot", [65, 512], F32, s2)
            rec = sb("rec", [65, 512], F32, s2)
            mixh = [sb("mixh%d" % i, [64, 512], BF16, s2) for i in range(2)]
            kn_ps = ps("knp", [64, 512], F32, s2)
            bcp = ps("bcp", [64, 512], F32, s2)
            pvs = ps("pvs", [128, 512], F32, s2)
            pV = pvs[:, 0:256].rearrange("p (t c) -> p t c", c=64)
            pss2 = pvs[:, 256:322]
            qp = ps("qp", [96, 512], F32, s2)
            qsp = ps("qsp", [96, 512], F32, s2)
            stp = [ps("mst%d" % i, [128, 512], F32, s2) for i in range(2)]
            o_ps = ps("mo", [65, 512], F32, s2)
            for i in range(2):
                op('pool', lambda e, i=i: e.memset(Vh[i][:, :, 64:65], 1.0), writes=['Vh%d' % i])
            cs1 = [sb("cs1%d" % i, [128, 2048], F32, s2) for i in range(2)]
            cc1 = [sb("cc1%d" % i, [128, 2, 1024], BF16, s2) for i in range(2)]
            cs2 = [sb("cs2%d" % i, [128, 1024], F32, s2) for i in range(2)]
            cc2 = [sb("cc2%d" % i, [128, 1024], BF16, s2) for i in range(2)]
            zt = sb("zt", [128, 2048], BF16, s2)
            op('pool', lambda e: e.memset(zt[:], 0.0), writes=['zt'])
            XSz = XS.rearrange("(a p r) n -> a p (r n)", p=128, r=2)
            for a in range(160 * 128 // 256):
                dma(XSz[a], zt[:], reads=['zt'], writes=['XS'], sem='xsz')
            cast_ld = [0]
            cast_dn = [0]

            def cast_load(n):
                ex, kc = n // 8, n % 8
                i4 = n % 2
                dma(cs1[i4][:], w1[ex, kc * 128:(kc + 1) * 128, :], writes=['cs1%d' % i4], sem='cs1%d' % i4)
                dma(cs2[i4][:], w2[ex, kc * 128:(kc + 1) * 128, :], writes=['cs2%d' % i4], sem='cs2%d' % i4)

            def cast_do(n):
                ex, kc = n // 8, n % 8
                i4 = n % 2
                a_, ak, b_, bk = cs1[i4], 'cs1%d' % i4, cc1[i4], 'cc1%d' % i4
                c_, ck, d_, dk = cs2[i4], 'cs2%d' % i4, cc2[i4], 'cc2%d' % i4
                op('dve', lambda e: e.tensor_copy(out=b_[:], in_=a_[:].rearrange("p (f g) -> p g f", g=2)), reads=[ak], writes=[bk])
                dma(W1R[ex * 128:(ex + 1) * 128, kc * 2048:(kc + 1) * 2048], b_[:].rearrange("p g f -> p (g f)"), reads=[bk], writes=['W1R'], sem=bk)
                op('dve', lambda e: e.tensor_tensor(out=d_[:], in0=c_[:], in1=g2gs_bc[:], op=ALU.mult), reads=[ck, 'g2gs_bc'], writes=[dk])
                dma(W2R[ex * 128:(ex + 1) * 128, kc * 1024:(kc + 1) * 1024], d_[:], reads=[dk], writes=['W2R'], sem=dk)

            def cast_tick(flush=False):
                if cast_dn[0] < cast_ld[0] and (flush or cast_dn[0] < cast_ld[0] - 0):
                    pass
                if cast_ld[0] < 256:
                    cast_load(cast_ld[0])
                    cast_ld[0] += 1
                    if cast_dn[0] < cast_ld[0] - 1:
                        cast_do(cast_dn[0])
                        cast_dn[0] += 1
                elif cast_dn[0] < 256:
                    cast_do(cast_dn[0])
                    cast_dn[0] += 1

            ui = [0]
            deferred = []

            def gen_steps(h):
                hb = h % 2
                KTh, ktk = KT[hb], 'KT%d' % hb
                vk, sk_k, qtk = 'Vh%d' % hb, 'skh%d' % hb, 'QT%d' % hb
                sk_ = skh[hb]
                steps = []

                def kstep_a(gi, t0, nt):
                    nb, tok0 = nt * 128, t0 * 128
                    kp, kpk = kn_ps, 'knp'
                    sq_, sqkk = sqk[gi % 2], 'sqk%d' % (gi % 2)
                    op('pe', mm(kp[:, :nb], wkb[:, h * 64:(h + 1) * 64], ckvT[:, tok0:tok0 + nb]), reads=['wkb', 'ckvT'], writes=[kpk])
                    for t in range(nt):
                        op('pe', mm(pV[:, t, :], ckvT[:, tok0 + t * 128:tok0 + (t + 1) * 128], wkb[:, 512 + h * 64:512 + (h + 1) * 64]), reads=['ckvT', 'wkb'], writes=['pV'])

                def kstep_b(gi, t0, nt):
                    nb, tok0 = nt * 128, t0 * 128
                    kp, kpk = kn_ps, 'knp'
                    sq_, sqkk = sqk[gi % 2], 'sqk%d' % (gi % 2)
                    op('act', lambda e: e.activation(out=KTh[0:64, tok0:tok0 + nb], in_=kp[:, :nb], func=AF.Identity, scale=sv[0:64, 5:6]), reads=[kpk, 'sv'], writes=[ktk])
                    op('act', lambda e: e.activation(out=sq_[:, :nb], in_=kp[:, :nb], func=AF.Square), reads=[kpk], writes=[sqkk])
                    op('dve', lambda e: e.tensor_copy(out=Vh[hb][:, t0:t0 + nt, 0:64], in_=pV[:, 0:nt, :]), reads=['pV'], writes=[vk])

                def kstep_c(gi, t0, nt):
                    sq_, sqkk = sqk[gi % 2], 'sqk%d' % (gi % 2)
                    for t in range(nt):
                        op('pe', mm(pss2[:, t0 + t:t0 + t + 1], sq_[0:64, t * 128:(t + 1) * 128], onesb[0:64, 0:1]), reads=[sqkk, 'onesb'], writes=['pss2'])

                for gi, (t0, nt) in enumerate(GROUPS):
                    steps.append(lambda gi=gi, t0=t0, nt=nt: kstep_a(gi, t0, nt))
                    steps.append(lambda gi=gi, t0=t0, nt=nt: kstep_b(gi, t0, nt))
                    steps.append(lambda gi=gi, t0=t0, nt=nt: kstep_c(gi, t0, nt))

                steps.append(lambda: op('dve', lambda e: e.tensor_tensor(out=sk_[:], in0=pss2[:, :], in1=sspe[:], op=ALU.add), reads=['pss2', 'sspe'], writes=[sk_k]))
                steps.append(lambda: op('act', lambda e: e.activation(out=sk_[:], in_=sk_[:], func=AF.Sqrt, scale=1.0 / 96, bias=EPS), reads=[sk_k], writes=[sk_k]))

                def scale_c():
                    op('dve', lambda e: e.reciprocal(out=sk_[:], in_=sk_[:]), reads=[sk_k], writes=[sk_k])
                    op('dve', lambda e: e.tensor_scalar(out=sk_[:], in0=sk_[:], scalar1=float(96 ** -0.5), scalar2=None, op0=ALU.mult), reads=[sk_k], writes=[sk_k])
                steps.append(scale_c)

                def q_a(qb):
                    q0 = qb * 512
                    tq = qb % 2
                    dma(qc_[tq][64:96, :], qcs[0, :, q0:q0 + 512], writes=['qc%d' % tq], sem='qtabc%d' % tq)
                    dma(qs_[tq][64:96, :], qcs[1, :, q0:q0 + 512], writes=['qs%d' % tq], sem='qtabs%d' % tq)
                    for c in range(2):
                        op('pe', mm(qp[:, :], wqb[:, c, (h * 2) * 96:(h * 2) * 96 + 96], cqT[:, c, q0:q0 + 512], c == 0, c == 1), reads=['wqb', 'cqT'], writes=['qp'])
                    for c in range(2):
                        op('pe', mm(qsp[:, :], wqb[:, c, (h * 2 + 1) * 96:(h * 2 + 1) * 96 + 96], cqT[:, c, q0:q0 + 512], c == 0, c == 1), reads=['wqb', 'cqT'], writes=['qsp'])

                def q_b(qb):
                    tq = qb % 2
                    op('act', lambda e: e.copy(out=qraw[:], in_=qp[:, :]), reads=['qp'], writes=['qraw'])
                    op('dve', lambda e: e.scalar_tensor_tensor(out=qb_[64:96, :], in0=qsp[64:96, :], scalar=sv[64:96, 4:5], in1=qs_[tq][64:96, :],
                                                               op0=ALU.mult, op1=ALU.mult), reads=['qsp', 'sv', 'qs%d' % tq], writes=['qb_'])

                def q_c(qb):
                    tq = qb % 2
                    op('pool', lambda e: e.tensor_tensor(out=sqq[:], in0=qraw[:], in1=qraw[:], op=ALU.mult), reads=['qraw'], writes=['sqq'])
                    op('dve', lambda e: e.scalar_tensor_tensor(out=qa_[64:96, :], in0=qraw[64:96, :], scalar=sv[64:96, 3:4], in1=qc_[tq][64:96, :],
                                                               op0=ALU.mult, op1=ALU.mult), reads=['qraw', 'sv', 'qc%d' % tq], writes=['qa_'])

                def q_d(qb):
                    op('pe', mm(qp[:, :], onesb[0:96, 0:96], sqq[:]), reads=['onesb', 'sqq', 'qraw'], writes=['qp'])
                    op('pool', lambda e: e.tensor_tensor(out=qa_[64:96, :], in0=qa_[64:96, :], in1=qb_[64:96, :], op=ALU.add), reads=['qa_', 'qb_'], writes=['qa_'])

                def q_e(qb):
                    op('act', lambda e: e.activation(out=rq[:], in_=qp[:, :], func=AF.Sqrt, scale=1.0 / 96, bias=EPS), reads=['qp'], writes=['rq'])

                def q_f(qb):
                    op('dve', lambda e: e.reciprocal(out=rq[:], in_=rq[:]), reads=['rq'], writes=['rq'])

                def q_g(qb):
                    q0 = qb * 512
                    op('dve', lambda e: e.scalar_tensor_tensor(out=QT[hb][0:64, q0:q0 + 512], in0=qraw[0:64, :], scalar=sv[0:64, 3:4], in1=rq[0:64, :],
                                                               op0=ALU.mult, op1=ALU.mult), reads=['qraw', 'sv', 'rq'], writes=[qtk])
                    op('dve', lambda e: e.tensor_tensor(out=QT[hb][64:96, q0:q0 + 512], in0=qa_[64:96, :], in1=rq[64:96, :], op=ALU.mult),
                       reads=['qa_', 'rq'], writes=[qtk])

                for qb in range(8):
                    for f_ in (q_a, q_b, q_c, q_d, q_e, q_f, q_g):
                        steps.append(lambda qb=qb, f_=f_: f_(qb))
                return steps

            def attend(h, nxt):
                hb = h % 2
                KTh, ktk = KT[hb], 'KT%d' % hb
                vk, sk_k, qtk = 'Vh%d' % hb, 'skh%d' % hb, 'QT%d' % hb
                sk_ = skh[hb]
                every = max(1, (8 * 66) // (len(nxt) + 1)) if nxt else 0
                ucount = 0
                for qb in range(8):
                    q0 = qb * 512
                    pend = []
                    for kt in range(66 + 1):
                        if kt < 66:
                            u = ui[0]
                            sp_, spk = stp[u % 2], 'mst%d' % (u % 2)
                            p_, pk = pT[u % 3], 'pT%d' % (u % 3)
                            op('pe', mm(sp_[:, :], KTh[0:96, kt * 128:(kt + 1) * 128], QT[hb][0:96, q0:q0 + 512]), reads=[ktk, 'KT%dpe' % hb, qtk], writes=[spk])
                            op('act', lambda e, p_=p_, sp_=sp_, kt=kt: e.activation(out=p_[:], in_=sp_[:, :], func=AF.Exp, scale=sk_[:, kt:kt + 1]),
                               reads=[spk, sk_k], writes=[pk])
                            pend.append((kt, p_, pk))
                            ui[0] += 1
                            ucount += 1
                            if nxt and ucount % every == 0:
                                nxt.pop(0)()
                            if ucount % 16 == 8:
                                cast_tick()
                            if kt == 8 and deferred:
                                deferred.pop(0)()
                        if kt >= 1:
                            (k0, p0, pk0) = pend.pop(0)
                            op('pe', mm(o_ps[:, :], Vh[hb][:, k0, 0:65], p0[:], k0 == 0, k0 == 65), reads=[vk, pk0], writes=['mo'])
                    op('dve', lambda e: e.tensor_copy(out=ot[:], in_=o_ps[:, :]), reads=['mo'], writes=['ot'])
                    op('dve', lambda e: e.reciprocal(out=rec[64:65, :], in_=ot[64:65, :]), reads=['ot'], writes=['rec'])

                    def fin(h=h, qb=qb, q0=q0):
                        mh, mhk = mixh[qb % 2], 'mixh%d' % (qb % 2)
                        op('pe', mm(bcp[0:64, :], onesf[64:65, 0:64], rec[64:65, :]), reads=['onesf', 'rec'], writes=['bcp'])
                        op('dve', lambda e, mh=mh: e.tensor_tensor(out=mh[:], in0=ot[0:64, :], in1=bcp[0:64, :], op=ALU.mult), reads=['ot', 'bcp'], writes=[mhk])
                        dma(MIXT[h * 64:(h + 1) * 64, q0:q0 + 512], mh[:], reads=[mhk], writes=['MIXT'], sem=mhk)
                    deferred.append(fin)
                while nxt:
                    nxt.pop(0)()
                if h == 7:
                    while deferred:
                        deferred.pop(0)()

            for st_ in gen_steps(0):
                st_()
            for h in range(8):
                attend(h, gen_steps(h + 1) if h + 1 < 8 else [])
            while cast_dn[0] < 256:
                cast_tick()
            kb.barrier()
    if RUN_REST:
      NBLK = 160
      NPAD = NBLK * 128
      GK = sb("GK", [128, 32, 4], F32)
      DSTi = sb("DSTi", [128, 128], I32)
      IDXW = sb("IDXW", [128, NBLK], I32)
      IDXB = sb("IDXB", [128, NBLK], I32)
      XOWN = 256 + NOWN
      with contextlib.ExitStack() as st:
        g1_bc = sb("g1_bc", [128, 1024], F32, st)
        g2s_bc = sb("g2s_bc", [128, 1024], F32, st)
        sh2_bc = sb("sh2_bc", [128, 1024], F32, st)
        g2sv = sb("g2sv", [128, 8], F32, st)
        dg_ = [sb("dg_%d" % i, [128, 128], F32, st) for i in range(2)]
        Wo = sb("Wo", [128, 8, 1024], BF16, st)
        Wr = sb("Wr", [128, 8, 32], BF16, st)
        Wrf = sb("Wrf", [128, 8, 32], F32, st)
        brb = sb("brb", [128, 32], F32, st)
        OHall = sb("OHall", [128, 32, 4, 32], BF16, st)
        Rall = sb("Rall", [128, 32, 32], F32, st)
        CUM = sb("CUM", [128, 32], F32, st)
        ustr = sb("ustr", [128, 128], BF16, st)
        ustrf = sb("ustrf", [128, 128], F32, st)
        tri = sb("tri", [32, 64], F32, st)
        rtab = sb("rtab", [128, NBLK + 1], F32, st)
        yps = [ps("yps%d" % i, [128, 1024], F32, st) for i in range(2)]
        tp4 = [ps("tp4%d" % i, [128, 8, 128], BF16, st) for i in range(2)]
        lgp = ps("lgp", [128, 32], F32, st)
        rkp = ps("rkp", [128, 64], F32, st)
        with contextlib.ExitStack() as s2:
            wof = sb("wof", [128, 8, 1024], F32, s2)
            dma(wof[:], w_out.rearrange("(c p) n -> p c n", p=128), writes=['wof'], sem='c3')
            for c in range(8):
                op('pool' if c % 2 else 'dve', lambda e, c=c: e.tensor_copy(out=Wo[:, c, :], in_=wof[:, c, :]), reads=['wof'], writes=['Wo'])
            dma(Wrf[:], w_router.rearrange("(c p) n -> p c n", p=128), writes=['Wrf'], sem='c3')
            op('dve', lambda e: e.tensor_copy(out=Wr[:], in_=Wrf[:]), reads=['Wrf'], writes=['Wr'])
            dma(brb[:], br_bc[:, :], writes=['brb'], sem='c3')
            dma(ustrf[:], ustrict[:, :], writes=['ustrf'], sem='c3')
            op('dve', lambda e: e.tensor_copy(out=ustr[:], in_=ustrf[:]), reads=['ustrf'], writes=['ustr'])
            dma(tri[:], tri32[:, :], writes=['tri'], sem='c3')
            dma(rtab[:], routetab[:, :], writes=['rtab'], sem='c3')
            op('pool', lambda e: e.memset(CUM[:], 0.0), writes=['CUM'])
            op('dve', lambda e: e.scalar_tensor_tensor(out=g2sv[:], in0=mv3[:, 32:40, 0], scalar=1.0, in1=gv[:, 8:16], op0=ALU.add, op1=ALU.mult),
               reads=['modv', 'gv'], writes=['g2sv'])
            di = 0
            for (dst, dkey, vec) in ((g1_bc, 'g1_bc', lambda kc: mv3[:, 16 + kc, 0:1]), (g2s_bc, 'g2s_bc', lambda kc: g2sv[:, kc:kc + 1]),
                                     (sh2_bc, 'sh2_bc', lambda kc: mv3[:, 24 + kc, 0:1])):
                for kc in range(8):
                    d_, dk = dg_[di % 2], 'dg_%d' % (di % 2)
                    op('dve', lambda e, d_=d_, vec=vec, kc=kc: e.tensor_scalar(out=d_[:], in0=ident[:], scalar1=vec(kc), scalar2=None, op0=ALU.mult),
                       reads=['ident', 'modv', 'g2sv'], writes=[dk])
                    op('pe', mm(yps[0][:, kc * 128:(kc + 1) * 128], onesf[:], d_[:]), reads=['onesf', dk], writes=['yps0'])
                    di += 1
                op('act', lambda e, dst=dst: e.copy(out=dst[:], in_=yps[0][:, :]), reads=['yps0'], writes=[dkey])
            kb.barrier()
        mt = [sb("mt%d" % i, [128, 8, 128], BF16, st) for i in range(2)]
        xo = [sb("xo%d" % i, [128, 1024], F32, st) for i in range(2)]
        x1t = [sb("x1t%d" % i, [128, 1024], F32, st) for i in range(2)]
        tmp4 = sb("tmp4", [128, 1024], F32, st)
        sq4 = sb("sq4", [128, 1024], F32, st)
        st4 = [sb("st4%d" % i, [128, 4], F32, st) for i in range(2)]
        hfb = [sb("hfb%d" % i, [128, 1024], BF16, st) for i in range(2)]
        hT = [sb("hT%d" % i, [128, 8, 128], BF16, st) for i in range(2)]
        lgt = [sb("lgt%d" % i, [128, 32], F32, st) for i in range(2)]
        mx8 = [sb("mx8%d" % i, [128, 8], F32, st) for i in range(2)]
        ex4 = [sb("ex4%d" % i, [128, 4], F32, st) for i in range(2)]
        Mb = [sb("Mb%d" % i, [128, 32], BF16, st) for i in range(2)]
        sm4 = [sb("sm4%d" % i, [128, 4], F32, st) for i in range(2)]
        MIXv = MIXT.rearrange("(c p) n -> p c n", p=128)
        for t in range(32):
            i2 = t % 2
            tok = t * 128
            k = lambda s: '%s%d' % (s, i2)
            dma(mt[i2][:], MIXv[:, :, tok:tok + 128], reads=['MIXT'], writes=[k('mt')], sem=k('mt'))
            dma(xo[i2][:], xall[XOWN + tok:XOWN + tok + 128, :], writes=[k('xo')], sem=k('xo'))
            for n2 in range(2):
                for c in range(8):
                    op('pe', mm(yps[i2][:, n2 * 512:(n2 + 1) * 512], mt[i2][:, c, :], Wo[:, c, n2 * 512:(n2 + 1) * 512], c == 0, c == 7),
                       reads=[k('mt'), 'Wo'], writes=[k('yps')])
            op('dve', lambda e, i2=i2: e.tensor_tensor(out=tmp4[:], in0=yps[i2][:, :], in1=g1_bc[:], op=ALU.mult), reads=[k('yps'), 'g1_bc'], writes=['tmp4'])
            op('dve', lambda e, i2=i2: e.tensor_tensor(out=x1t[i2][:], in0=tmp4[:], in1=xo[i2][:], op=ALU.add), reads=['tmp4', k('xo')], writes=[k('x1t')])
            dma(X1[tok:tok + 128, :], x1t[i2][:], reads=[k('x1t')], writes=['X1'], sem=k('x1o'))
            op('act', lambda e, i2=i2: e.activation(out=sq4[:], in_=x1t[i2][:], func=AF.Square, accum_out=st4[i2][:, 0:1]), reads=[k('x1t')], writes=['sq4', k('s4a')])
            op('act', lambda e, i2=i2: e.activation(out=st4[i2][:, 1:2], in_=st4[i2][:, 0:1], func=AF.Sqrt, scale=1.0 / 1024, bias=EPS), reads=[k('s4a')], writes=[k('s4b')])
            op('dve', lambda e, i2=i2: e.reciprocal(out=st4[i2][:, 2:3], in_=st4[i2][:, 1:2]), reads=[k('s4b')], writes=[k('s4c')])
            op('dve', lambda e, i2=i2: e.scalar_tensor_tensor(out=tmp4[:], in0=x1t[i2][:], scalar=st4[i2][:, 2:3], in1=g2s_bc[:], op0=ALU.mult, op1=ALU.mult),
               reads=[k('x1t'), k('s4c'), 'g2s_bc'], writes=['tmp4'])
            op('dve', lambda e, i2=i2: e.tensor_tensor(out=hfb[i2][:], in0=tmp4[:], in1=sh2_bc[:], op=ALU.add), reads=['tmp4', 'sh2_bc'], writes=[k('hfb')])
            dma(HF[tok:tok + 128, :], hfb[i2][:], reads=[k('hfb')], writes=['HF'], sem=k('hfo'))
            for c in range(8):
                op('pe', lambda e, c=c, i2=i2: e.transpose(out=tp4[i2][:, c, :], in_=hfb[i2][:, c * 128:(c + 1) * 128], identity=identb[:]),
                   reads=[k('hfb'), 'identb'], writes=[k('tp4')])
            op('act', lambda e, i2=i2: e.copy(out=hT[i2][:], in_=tp4[i2][:]), reads=[k('tp4')], writes=[k('hT')])
            for c in range(8):
                op('pe', mm(lgp[:, :], hT[i2][:, c, :], Wr[:, c, :], c == 0, c == 7), reads=[k('hT'), 'Wr'], writes=['lgp'])
            op('dve', lambda e, i2=i2: e.tensor_tensor(out=lgt[i2][:], in0=lgp[:, :], in1=brb[:], op=ALU.add), reads=['lgp', 'brb'], writes=[k('lgt')])
            op('dve', lambda e, i2=i2: e.max(out=mx8[i2][:], in_=lgt[i2][:]), reads=[k('lgt')], writes=[k('mx8')])
            op('dve', lambda e, i2=i2: e.tensor_scalar(out=sm4[i2][:, 0:1], in0=mx8[i2][:, 0:1], scalar1=-1.0, scalar2=None, op0=ALU.mult), reads=[k('mx8')], writes=[k('nmx')])
            op('act', lambda e, i2=i2: e.activation(out=ex4[i2][:], in_=mx8[i2][:, 0:4], func=AF.Exp, bias=sm4[i2][:, 0:1]), reads=[k('mx8'), k('nmx')], writes=[k('ex4')])
            op('dve', lambda e, i2=i2: e.reduce_sum(out=sm4[i2][:, 1:2], in_=ex4[i2][:], axis=AX.X), reads=[k('ex4')], writes=[k('sm1')])
            op('dve', lambda e, i2=i2: e.reciprocal(out=sm4[i2][:, 2:3], in_=sm4[i2][:, 1:2]), reads=[k('sm1')], writes=[k('sm2')])
            op('dve', lambda e, i2=i2, t=t: e.tensor_scalar(out=GK[:, t, :], in0=ex4[i2][:], scalar1=sm4[i2][:, 2:3], scalar2=None, op0=ALU.mult),
               reads=[k('ex4'), k('sm2')], writes=['GK'])
            for kk in range(4):
                op('dve', lambda e, i2=i2, t=t, kk=kk: e.tensor_scalar(out=OHall[:, t, kk, :], in0=lgt[i2][:], scalar1=mx8[i2][:, kk:kk + 1], scalar2=None, op0=ALU.is_equal),
                   reads=[k('lgt'), k('mx8')], writes=['OH%d' % t])
            op('dve', lambda e, i2=i2, t=t: e.tensor_scalar(out=Mb[i2][:], in0=lgt[i2][:], scalar1=mx8[i2][:, 3:4], scalar2=None, op0=ALU.is_ge),
               reads=[k('lgt'), k('mx8')], writes=[k('Mb')])
            op('pe', mm(rkp[:, 0:32], ustr[:], Mb[i2][:]), reads=['ustr', k('Mb')], writes=['rkp'])
            op('pe', mm(rkp[:, 32:64], onesb[:], Mb[i2][:]), reads=['onesb', k('Mb')], writes=['rkp'])
            op('dve', lambda e, t=t: e.tensor_tensor(out=Rall[:, t, :], in0=rkp[:, 0:32], in1=CUM[:], op=ALU.add), reads=['rkp', 'CUM'], writes=['Rall'])
            op('dve', lambda e: e.tensor_tensor(out=CUM[:], in0=rkp[:, 32:64], in1=CUM[:], op=ALU.add), reads=['rkp', 'CUM', 'Rall'], writes=['CUM'])
        with contextlib.ExitStack() as s2:
            cf = sb("cf", [128, 32], F32, s2)
            ci = sb("ci", [128, 32], I32, s2)
            padT = sb("padT", [32, 128], F32, s2)
            pse = sb("pse", [128, 64], F32, s2)
            cmp3 = sb("cmp3", [128, NBLK, 32], F32, s2)
            Ef = sb("Ef", [128, NBLK], F32, s2)
            eqf = sb("eqf", [128, NBLK], F32, s2)
            ixf = sb("ixf", [128, NBLK], F32, s2)
            dall = sb("dall", [128, 32], F32, s2)
            dtmp = sb("dtmp", [128, 4, 32], F32, s2)
            dstf = sb("dstf", [128, 32, 4], F32, s2)
            op('dve', lambda e: e.tensor_scalar(out=cf[:], in0=CUM[:], scalar1=127.0, scalar2=None, op0=ALU.add), reads=['CUM'], writes=['cf'])
            op('dve', lambda e: e.tensor_copy(out=ci[:], in_=cf[:]), reads=['cf'], writes=['ci'])
            op('dve', lambda e: e.tensor_scalar(out=ci[:], in0=ci[:], scalar1=7, scalar2=7, op0=ALU.arith_shift_right, op1=ALU.arith_shift_left), reads=['ci'], writes=['ci'])
            op('dve', lambda e: e.tensor_copy(out=cf[:], in_=ci[:]), reads=['ci'], writes=['cf'])
            op('pe', lambda e: e.transpose(out=yps[0][0:32, 0:128], in_=cf[:], identity=ident[:]), reads=['cf', 'ident'], writes=['yps0'])
            op('act', lambda e: e.copy(out=padT[:], in_=yps[0][0:32, 0:128]), reads=['yps0'], writes=['padT'])
            op('pe', mm(rkp[:, 0:64], padT[:], tri[:]), reads=['padT', 'tri'], writes=['rkp'])
            op('act', lambda e: e.copy(out=pse[:], in_=rkp[:, 0:64]), reads=['rkp'], writes=['pse'])
            op('dve', lambda e: e.tensor_tensor(out=cmp3[:], in0=pse[:, 32:64].unsqueeze(1).to_broadcast([128, NBLK, 32]),
                                                in1=rtab[:, 0:NBLK].unsqueeze(2).to_broadcast([128, NBLK, 32]), op=ALU.is_le), reads=['pse', 'rtab'], writes=['cmp3'])
            op('dve', lambda e: e.reduce_sum(out=Ef[:], in_=cmp3[:], axis=AX.X), reads=['cmp3'], writes=['Ef'])
            op('dve', lambda e: e.tensor_scalar(out=Ef[:], in0=Ef[:], scalar1=31.0, scalar2=None, op0=ALU.min), reads=['Ef'], writes=['Ef'])
            op('pool', lambda e: e.memset(eqf[:], 0.0), writes=['eqf'])
            op('dve', lambda e: e.tensor_tensor(out=eqf[:, 2:NBLK], in0=Ef[:, 2:NBLK], in1=Ef[:, 0:NBLK - 2], op=ALU.is_equal), reads=['Ef', 'eqf'], writes=['eqf'])
            op('dve', lambda e: e.tensor_scalar(out=eqf[:], in0=eqf[:], scalar1=BIG, scalar2=None, op0=ALU.mult), reads=['eqf'], writes=['eqf'])
            op('dve', lambda e: e.scalar_tensor_tensor(out=ixf[:], in0=Ef[:], scalar=128.0, in1=eqf[:], op0=ALU.mult, op1=ALU.add), reads=['Ef', 'eqf'], writes=['ixf'])
            op('dve', lambda e: e.tensor_scalar(out=ixf[:], in0=ixf[:], scalar1=rtab[:, NBLK:NBLK + 1], scalar2=None, op0=ALU.add), reads=['ixf', 'rtab'], writes=['ixf'])
            op('dve', lambda e: e.tensor_scalar(out=ixf[:], in0=ixf[:], scalar1=0.0, scalar2=2.0e6, op0=ALU.max, op1=ALU.min), reads=['ixf'], writes=['ixf'])
            op('dve', lambda e: e.tensor_copy(out=IDXW[:], in_=ixf[:]), reads=['ixf'], writes=['IDXW'])
            op('dve', lambda e: e.tensor_tensor(out=ixf[:], in0=Ef[:], in1=eqf[:], op=ALU.add), reads=['Ef', 'eqf', 'IDXW'], writes=['ixf'])
            op('dve', lambda e: e.tensor_scalar(out=ixf[:], in0=ixf[:], scalar1=0.0, scalar2=2.0e6, op0=ALU.max, op1=ALU.min), reads=['ixf'], writes=['ixf'])
            op('dve', lambda e: e.tensor_copy(out=IDXB[:], in_=ixf[:]), reads=['ixf'], writes=['IDXB'])
            for t in range(32):
                op('dve', lambda e, t=t: e.tensor_tensor(out=dall[:], in0=Rall[:, t, :], in1=pse[:, 0:32], op=ALU.add), reads=['Rall', 'pse'], writes=['dall'])
                op('dve', lambda e, t=t: e.tensor_tensor(out=dtmp[:], in0=OHall[:, t, :, :], in1=dall[:].unsqueeze(1).to_broadcast([128, 4, 32]), op=ALU.mult),
                   reads=['OH%d' % t, 'dall'], writes=['dtmp'])
                op('dve', lambda e, t=t: e.reduce_sum(out=dstf[:, t, :], in_=dtmp[:], axis=AX.X), reads=['dtmp'], writes=['dstf'])
            op('dve', lambda e: e.tensor_scalar(out=dstf[:], in0=dstf[:], scalar1=0.0, scalar2=float(NPAD - 1), op0=ALU.max, op1=ALU.min), reads=['dstf'], writes=['dstf'])
            op('dve', lambda e: e.tensor_copy(out=DSTi[:], in_=dstf[:].rearrange("p t k -> p (t k)")), reads=['dstf'], writes=['DSTi'])
            kb.barrier()
        kb.barrier()

      with contextlib.ExitStack() as st:
        with contextlib.ExitStack() as s2:
            W1b = [sb("W1b%d" % i, [128, 8, 2048], BF16, s2) for i in range(2)]
            W2b = [sb("W2b%d" % i, [128, 8, 1024], BF16, s2) for i in range(2)]
            B1b = [sb("B1b%d" % i, [128, 2048], F32, s2) for i in range(2)]
            B2b = [sb("B2b%d" % i, [128, 1024], F32, s2) for i in range(2)]
            xbk = [sb("xbk%d" % i, [128, 1024], BF16, s2) for i in range(4)]
            xTk = [sb("xTk%d" % i, [128, 8, 128], BF16, s2) for i in range(2)]
            t1 = sb("t1", [128, 2048], F32, s2)
            sA = sb("sA", [128, 1024], F32, s2)
            aB = [sb("aB%d" % i, [128, 1024], BF16, s2) for i in range(2)]
            aT = sb("aT", [128, 8, 128], BF16, s2)
            yb = [sb("yb%d" % i, [128, 1024], F32, s2) for i in range(2)]
            up = ps("up", [128, 2048], F32, s2)
            ypm = ps("ypm", [128, 1024], F32, s2)
            tpa = ps("tpa", [128, 8, 128], BF16, s2)
            tpb = ps("tpb", [128, 8, 128], BF16, s2)
            def gatherA(j):
                b = j % 2
                wk = 'wga%d' % b
                kb.idma(out=W1b[b][:].rearrange("p k n -> p (k n)"), out_off=None, in_=W1R[:, :], in_off=IDXW[:, j:j + 1], bounds=4095,
                        reads=['IDXW', 'W1R'], writes=['W1b%d' % b], sem=wk)
                kb.idma(out=B1b[b][:, :], out_off=None, in_=B1R[:, :], in_off=IDXB[:, j:j + 1], bounds=31, reads=['IDXB'], writes=['B1b%d' % b], sem='gb1%d' % b)

            def gatherB(j):
                b = j % 2
                wk = 'wgb%d' % b
                kb.idma(out=W2b[b][:].rearrange("p k n -> p (k n)"), out_off=None, in_=W2R[:, :], in_off=IDXW[:, j:j + 1], bounds=4095,
                        reads=['IDXW', 'W2R'], writes=['W2b%d' % b], sem=wk)
                kb.idma(out=B2b[b][:, :], out_off=None, in_=B2G[:, :], in_off=IDXB[:, j:j + 1], bounds=31, reads=['IDXB', 'B2G'], writes=['B2b%d' % b], sem='gb2%d' % b)

            gatherA(0)
            gatherA(1)
            gatherB(0)
            gatherB(1)
            hfr = [sb("hfr%d" % i, [128, 1024], BF16, s2) for i in range(4)]
            for t in range(32):
                h_, hk = hfr[t % 4], 'hfr%d' % (t % 4)
                dma(h_[:], HF[t * 128:(t + 1) * 128, :], reads=['HF'], writes=[hk], sem=hk)
                for kk in range(4):
                    kb.idma(out=XS[:, :], out_off=DSTi[:, t * 4 + kk:t * 4 + kk + 1], in_=h_[:, :], in_off=None, bounds=NPAD - 1,
                            reads=[hk, 'DSTi', 'XS'], writes=['XSw%d' % kk], sem='xsc')

            def loadx(j):
                b4 = j % 4
                dma(xbk[b4][:], XS[j * 128:(j + 1) * 128, :], reads=['XSw0', 'XSw1', 'XSw2', 'XSw3'], writes=['xbk%d' % b4], sem='xbk%d' % b4)

            def TX(j):
                b, b4 = j % 2, j % 4
                xk, xtk = 'xbk%d' % b4, 'xTk%d' % b
                if j + 3 < NBLK:
                    loadx(j + 3)
                for c in range(8):
                    op('pe', lambda e, c=c, b4=b4: e.transpose(out=tpa[:, c, :], in_=xbk[b4][:, c * 128:(c + 1) * 128], identity=identb[:]), reads=[xk, 'identb'], writes=['tpa'])
                op('act', lambda e, b=b: e.copy(out=xTk[b][:], in_=tpa[:]), reads=['tpa'], writes=[xtk])

            def MM1(j, half):
                b = j % 2
                xtk, w1k = 'xTk%d' % b, 'W1b%d' % b
                for n4 in (0, 1) if half == 0 else (2, 3):
                    for kc in range(8):
                        op('pe', mm(up[:, n4 * 512:(n4 + 1) * 512], xTk[b][:, kc, :], W1b[b][:, kc, n4 * 512:(n4 + 1) * 512], kc == 0, kc == 7), reads=[xtk, w1k], writes=['up'])

            def chain(j):
                b = j % 2
                b1k = 'B1b%d' % b
                op('dve', lambda e, b=b: e.tensor_tensor(out=t1[:], in0=up[:, :], in1=B1b[b][:], op=ALU.add), reads=['up', b1k], writes=['t1'])
                if j + 2 < NBLK:
                    gatherA(j + 2)
                op('dve', lambda e: e.tensor_scalar(out=sA[:], in0=t1[:, 0:1024], scalar1=7.0, scalar2=None, op0=ALU.min), reads=['t1'], writes=['sA'])
                op('act', lambda e: e.activation(out=sA[:], in_=sA[:], func=AF.Silu, scale=1.702), reads=['sA'], writes=['sA'])
                op('dve', lambda e: e.tensor_scalar(out=t1[:, 1024:2048], in0=t1[:, 1024:2048], scalar1=-7.0, scalar2=7.0, op0=ALU.max, op1=ALU.min), reads=['t1'], writes=['t1'])
                op('dve', lambda e, b=b: e.scalar_tensor_tensor(out=aB[b][:], in0=t1[:, 1024:2048], scalar=1.0, in1=sA[:], op0=ALU.add, op1=ALU.mult), reads=['t1', 'sA'], writes=['aB%d' % b])

            def TA(j):
                b = j % 2
                for c in range(8):
                    op('pe', lambda e, c=c, b=b: e.transpose(out=tpb[:, c, :], in_=aB[b][:, c * 128:(c + 1) * 128], identity=identb[:]), reads=['aB%d' % b, 'identb'], writes=['tpb'])
                op('act', lambda e: e.copy(out=aT[:], in_=tpb[:]), reads=['tpb'], writes=['aT'])

            def MM2(j):
                b = j % 2
                w2k, b2k = 'W2b%d' % b, 'B2b%d' % b
                for n2 in range(2):
                    for fc in range(8):
                        op('pe', mm(ypm[:, n2 * 512:(n2 + 1) * 512], aT[:, fc, :], W2b[b][:, fc, n2 * 512:(n2 + 1) * 512], fc == 0, fc == 7), reads=['aT', w2k], writes=['ypm'])
                op('dve', lambda e, b=b: e.tensor_tensor(out=yb[b][:], in0=ypm[:, :], in1=B2b[b][:], op=ALU.add), reads=['ypm', b2k], writes=['yb%d' % b])
                dma(YS[j * 128:(j + 1) * 128, :], yb[b][:], reads=['yb%d' % b], writes=['YS'], sem='ybo%d' % b)
                if j + 2 < NBLK:
                    gatherB(j + 2)

            for j0 in range(3):
                loadx(j0)
            TX(0)
            TX(1)
            MM1(0, 0)
            MM1(0, 1)
            chain(0)
            for j in range(NBLK):
                if j + 2 < NBLK:
                    TX(j + 2)
                if j + 1 < NBLK:
                    MM1(j + 1, 0)
                TA(j)
                if j + 1 < NBLK:
                    MM1(j + 1, 1)
                    chain(j + 1)
                MM2(j)
            kb.barrier()
        with contextlib.ExitStack() as s2:
            yg = [[sb("yg%d_%d" % (i, kk), [128, 1024], F32, s2) for kk in range(4)] for i in range(3)]
            x1r = [sb("x1r%d" % i, [128, 1024], F32, s2) for i in range(2)]
            ac = [sb("ac%d" % i, [128, 1024], F32, s2) for i in range(2)]
            for t in range(32):
                i2 = t % 2
                tok = t * 128
                i3 = t % 3
                gk_ = 'yg%d' % i3
                dma(x1r[i2][:], X1[tok:tok + 128, :], reads=['X1'], writes=['x1r%d' % i2], sem='x1r%d' % i2)
                for kk in range(4):
                    kb.idma(out=yg[i3][kk][:, :], out_off=None, in_=YS[:, :], in_off=DSTi[:, t * 4 + kk:t * 4 + kk + 1], bounds=NPAD - 1,
                            reads=['YS', 'DSTi'], writes=[gk_ + 'b%d' % kk], sem=gk_ + '_%d' % kk)
                a_ = ac[i2]
                akk = 'ac%d' % i2
                op('dve', lambda e, a_=a_, i2=i2, t=t: e.scalar_tensor_tensor(out=a_[:], in0=yg[i3][0][:], scalar=GK[:, t, 0:1], in1=x1r[i2][:], op0=ALU.mult, op1=ALU.add),
                   reads=[gk_ + 'b0', 'GK', 'x1r%d' % i2], writes=[akk])
                for kk in range(1, 4):
                    op('dve', lambda e, a_=a_, i2=i2, t=t, kk=kk: e.scalar_tensor_tensor(out=a_[:], in0=yg[i3][kk][:], scalar=GK[:, t, kk:kk + 1], in1=a_[:], op0=ALU.mult, op1=ALU.add),
                       reads=[gk_ + 'b%d' % kk, 'GK', akk], writes=[akk])
                dma(y[tok:tok + 128, :], a_[:], reads=[akk], writes=['y'], sem='yo%d' % i2)
            kb.barrier()
    if DEBUG:
        dma(dbg['mixt'][:, :], MIXT[:, :], reads=['MIXT'], sem='dbg')
        dma(dbg['x1'][:, :], X1[:, :], reads=['X1'], sem='dbg')
        kb.barrier()
    es.close()
    return nc


def _prep_shared(inp):
    f = np.float32
    w_in = np.asarray(inp['w_in'][0], f)
    sw32 = _swap_idx(32)
    sw64 = _swap_idx(64)
    w_ext = np.zeros((D, NCOL), f)
    w_ext[:, C_CQ:C_CQ + 256] = w_in[:, OFF_Q:OFF_Q + 256]
    w_ext[:, C_CKV:C_CKV + 128] = w_in[:, OFF_KV:OFF_KV + 128]
    w_ext[:, C_PE + 64:C_PE + 96] = w_in[:, OFF_PE:OFF_PE + 32]
    w_ext[:, C_PESW + 64:C_PESW + 96] = w_in[:, OFF_PE + sw32]
    for h in range(4):
        b = C_RET + h * 512
        w_ext[:, b + RQ:b + RQ + 64] = w_in[:, OFF_RQ + h * 64:OFF_RQ + (h + 1) * 64]
        w_ext[:, b + RQS:b + RQS + 64] = w_in[:, OFF_RQ + h * 64 + sw64]
        w_ext[:, b + RK:b + RK + 64] = w_in[:, OFF_RK + h * 64:OFF_RK + (h + 1) * 64]
        w_ext[:, b + RKS:b + RKS + 64] = w_in[:, OFF_RK + h * 64 + sw64]
        w_ext[:, b + RGt:b + RGt + 128] = w_in[:, OFF_RG + h * 128:OFF_RG + (h + 1) * 128]
        w_ext[:, b + RVt:b + RVt + 128] = w_in[:, OFF_RV + h * 128:OFF_RV + (h + 1) * 128]
    wqu = np.asarray(inp['w_q_up'][0], f)
    wq_ext = np.zeros((256, 8, 2, 96), f)
    for h in range(8):
        wq_ext[:, h, 0, :] = wqu[:, h * 96:(h + 1) * 96]
        wq_ext[:, h, 1, 64:96] = wqu[:, h * 96 + 64 + sw32]
    wkv = np.asarray(inp['w_kv_up'][0], f).reshape(128, 8, 128)
    smallv = np.zeros((128, 16), f)
    gql = np.asarray(inp['g_q_lora'][0], f)
    smallv[:, 0] = gql[:128]
    smallv[:, 1] = gql[128:]
    smallv[:, 2] = np.asarray(inp['g_kv_lora'][0], f)
    gqh = np.asarray(inp['g_q_head'][0], f)
    gkh = np.asarray(inp['g_k_head'][0], f)
    smallv[:96, 3] = gqh
    smallv[64:96, 4] = gqh[64 + sw32]
    smallv[:96, 5] = gkh
    smallv[64:96, 6] = gkh[64 + sw32]
    gro = np.asarray(inp['g_ret_out'][0], f)
    for h in range(4):
        smallv[:, 7 + h] = gro[h * 128:(h + 1) * 128]
    b1 = np.asarray(inp['b_mlp1'][0], f).reshape(32, 8, 128, 2)
    sh = {
        'w_ada': np.ascontiguousarray(inp['w_ada'][0], f),
        'b_ada': np.ascontiguousarray(np.asarray(inp['b_ada'][0], f).reshape(48, 128).T),
        'gvec': np.ascontiguousarray(np.concatenate([np.asarray(inp['g_attn'][0], f).reshape(8, 128).T,
                                                     np.asarray(inp['g_ffn'][0], f).reshape(8, 128).T], axis=1)),
        'w_ext': w_ext,
        'wq_ext': wq_ext.reshape(256, -1),
        'wkv_k': np.ascontiguousarray(wkv[:, :, :64]).reshape(128, -1),
        'wkv_v': np.ascontiguousarray(wkv[:, :, 64:]).reshape(128, -1),
        'smallv': smallv,
        'lgin': np.ascontiguousarray(np.broadcast_to(np.asarray(inp['ret_decay_logit'][0], f).reshape(1, 8), (128, 8))),
        'w_out': np.ascontiguousarray(inp['w_out'][0], f),
        'w_router': np.ascontiguousarray(inp['w_router'][0], f),
        'br_bc': np.ascontiguousarray(np.broadcast_to(np.asarray(inp['b_router'][0], f).reshape(1, 32), (128, 32))),
        'w1': np.ascontiguousarray(inp['w_mlp1'][0], f),
        'w2': np.ascontiguousarray(inp['w_mlp2'][0], f),
        'b2': np.ascontiguousarray(inp['b_mlp2'][0], f),
        'identf': np.eye(128, dtype=f),
        'ustrict': np.triu(np.ones((128, 128), f), 1),
        'tri32': np.concatenate([np.triu(np.ones((32, 32), f), 1), np.triu(np.ones((32, 32), f), 0)], axis=1),
        'routetab': np.ascontiguousarray(np.concatenate([np.broadcast_to(128.0 * np.arange(160, dtype=f)[None, :], (128, 160)),
                                                         np.arange(128, dtype=f)[:, None]], axis=1)),
        'B1R': np.ascontiguousarray(np.asarray(inp['b_mlp1'][0], f).reshape(32, D, 2).transpose(0, 2, 1)).reshape(32, 2 * D),
    }
    jj = np.arange(128, dtype=f)[:, None]
    ii = np.arange(512, dtype=f)[None, :]
    dg = np.zeros((4, 3, 128, 512), f)
    for r in range(4):
        d = ii - (128 * r + jj)
        dg[r, 0] = np.where(d > 0, d, BIG)
        dg[r, 1] = np.where(d < 0, -d, BIG)
        dg[r, 2] = np.where(d == 0, 2.0, 0.0)
    sh['dgtab'] = dg
    return sh


def _prep_core(core, inp, sh):
    f = np.float32
    b, half = core // 2, core % 2
    x = np.asarray(inp['x'], f)
    own = slice(half * NOWN, (half + 1) * NOWN)
    oth = slice((1 - half) * NOWN, (2 - half) * NOWN)
    m = dict(sh)
    m['xall'] = np.ascontiguousarray(np.concatenate([np.asarray(inp['ctx'][b], f), x[b, oth], x[b, own]], axis=0))
    cv = np.stack([np.asarray(inp['c'][b], f), np.asarray(inp['c_ctx'], f)], axis=-1)
    m['cvec'] = np.ascontiguousarray(cv.reshape(8, 128, 2).transpose(1, 0, 2))
    t = np.arange(2 * NOWN)
    prow, pcol = (t // 64).astype(f), (t % 64).astype(f)
    for dim, kn, qn in ((32, 'kcs', 'qcs'), (64, 'rkcs', 'rqcs')):
        cos, sin = _rope_tables(prow, pcol, dim)
        kc = np.concatenate([np.ones((256, dim), f), cos[oth], cos[own]], axis=0)
        ks = np.concatenate([np.zeros((256, dim), f), sin[oth], sin[own]], axis=0)
        m[kn] = np.ascontiguousarray(np.stack([kc.T, ks.T], axis=0))
        m[qn] = np.ascontiguousarray(np.stack([cos[own].T, sin[own].T], axis=0))
    jj = np.arange(128, dtype=f)[:, None]
    ii = np.arange(512, dtype=f)[None, :]
    s = 1.0 if half == 1 else -1.0
    iq = np.broadcast_to(ii, (128, 512)).astype(f)
    m['utab'] = np.ascontiguousarray(np.stack([iq, -iq, s * iq], axis=0).astype(f))
    base = np.zeros((5, 8, 32), f)
    for qb in range(8):
        for kt in range(32):
            if kt < 2:
                base[0, qb, kt] = half * 4096 + qb * 512 + 256 - kt * 128
                base[4, qb, kt] = 8192 - half * 4096 - qb * 512 + kt * 128
            base[1, qb, kt] = (4096 + qb * 512 - kt * 128) if half == 1 else (4096 + kt * 128 - qb * 512)
            base[2, qb, kt] = qb * 512 - kt * 128
            base[3, qb, kt] = kt * 128 - qb * 512
    sgn = np.array([-1.0, -s, -1.0, 1.0, 1.0], f)
    cw = base.reshape(1, 5, 256) + sgn.reshape(1, 5, 1) * np.arange(128, dtype=f).reshape(128, 1, 1)
    m['cwtab'] = np.ascontiguousarray(cw.reshape(128, 5 * 256).astype(f))
    m['flagv'] = np.ascontiguousarray(np.broadcast_to(np.array([[half, 1 - half]], f), (128, 2)))
    return m


def kernel(**inputs):
    sh = _prep_shared(inputs)
    in_maps = [_prep_core(c, inputs, sh) for c in range(NCORES)]
    nc = build_nc()
    res = run_bass_kernel_spmd(nc, in_maps, core_ids=list(range(NCORES)))
    out = np.zeros((4, 2 * NOWN, D), np.float32)
    for c in range(NCORES):
        b, half = c // 2, c % 2
        out[b, half * NOWN:(half + 1) * NOWN] = res.results[c]["y"]
    if DEBUG:
        kernel.last = res
    return out
```

```python
import contextlib
import numpy as np
import concourse.bass as bass
import concourse.mybir as mybir
from concourse.bass_utils import run_bass_kernel_spmd

F32 = mybir.dt.float32
I32 = mybir.dt.int32
BF16 = mybir.dt.bfloat16
AF = mybir.ActivationFunctionType
ALU = mybir.AluOpType
AX = mybir.AxisListType

D = 1024
NOWN = 4096
NKEY = 8448
NCORES = 8
EPS = 1e-6
BIG = 1.0e6
DEBUG = False
RUN_RET = True
RUN_MLA = True
RUN_REST = True

OFF_Q, OFF_KV, OFF_PE, OFF_RQ, OFF_RK, OFF_RV, OFF_RG = 0, 256, 384, 416, 672, 928, 1440
C_CQ = 0
C_CKV = 256
C_PE = 384
C_PESW = 480
C_RET = 576
NCOL = C_RET + 4 * 512
RQ, RQS, RK, RKS, RGt, RVt = 0, 64, 128, 192, 256, 384


def _swap_idx(dim):
    nf = dim // 4
    idx = np.arange(dim).reshape(2, 2, nf)
    return idx[:, ::-1, :].reshape(dim)


def _rope_tables(pos_row, pos_col, dim):
    nf = dim // 4
    inv = np.power(np.float32(10000.0), -np.arange(nf, dtype=np.float32) / np.float32(nf)).astype(np.float32)
    pos = np.stack([pos_row, pos_col], axis=-1).astype(np.float32)
    ang = pos[:, :, None] * inv
    ang = np.broadcast_to(ang[:, :, None, :], (pos.shape[0], 2, 2, nf)).reshape(pos.shape[0], dim)
    cos = np.cos(ang).astype(np.float32)
    sin = np.sin(ang).astype(np.float32)
    sgn = np.ones((2, 2, nf), np.float32)
    sgn[:, 0, :] = -1.0
    return cos, sin * sgn.reshape(dim)


class KB:
    def __init__(self, nc, es):
        self.nc = nc
        self.es = es
        self.eng = {'pe': nc.tensor, 'act': nc.scalar, 'dve': nc.vector, 'pool': nc.gpsimd, 'sp': nc.sync}
        self.sems = {}
        self.cnt = {}
        for e in ('pe', 'act', 'dve', 'pool'):
            self.sems[e] = es.enter_context(nc.semaphore("c_" + e))
            self.cnt[e] = 0
        self.seen = {e: {} for e in self.eng}
        self.lastw = {}
        self.rds = {}
        self.dcnt = {}
        self.freed = []
        self.uniq = 0
        self.iq = []
        self.bregs = {}

    def _dsem(self, sem):
        if sem not in self.sems:
            if self.freed:
                h, c = self.freed.pop()
            else:
                h, c = self.es.enter_context(self.nc.semaphore("s_" + sem)), 0
            self.sems[sem] = h
            self.dcnt[sem] = c

    def _waits(self, engine, reads, writes):
        need = {}
        for k in list(reads) + list(writes):
            ev = self.lastw.get(k)
            if ev is not None:
                s, v, e = ev
                if not (e == engine and engine == 'pe'):
                    need[s] = max(need.get(s, 0), v)
        for k in writes:
            for (s, v, e) in self.rds.get(k, ()):
                if e == engine:
                    continue
                need[s] = max(need.get(s, 0), v)
        eng = self.eng[engine]
        for s, v in need.items():
            if self.seen[engine].get(s, 0) >= v:
                continue
            eng.wait_ge(self.sems[s], v)
            self.seen[engine][s] = v

    def _record(self, ev, reads, writes):
        for k in reads:
            self.rds.setdefault(k, []).append(ev)
        for k in writes:
            self.lastw[k] = ev
            self.rds[k] = []

    def op(self, engine, fn, reads=(), writes=()):
        self._waits(engine, reads, writes)
        ins = fn(self.eng[engine])
        self.cnt[engine] += 1
        ins.then_inc(self.sems[engine], 1)
        self._record((engine, self.cnt[engine], engine), reads, writes)

    def dma(self, out, in_, reads=(), writes=(), sem='d0', queue='sp'):
        if len(sem) == 2 and sem[0] == 'c' and sem[1].isdigit():
            self.uniq += 1
            sem = '%s_%d' % (sem, self.uniq)
        self._dsem(sem)
        self._waits(queue, reads, writes)
        self.eng[queue].dma_start(out=out, in_=in_).then_inc(self.sems[sem], 16)
        self.dcnt[sem] += 16
        self._record((sem, self.dcnt[sem], 'dma'), reads, writes)

    def idma(self, out, out_off, in_, in_off, bounds, reads=(), writes=(), sem='i0'):
        self._dsem(sem)
        self._waits('pool', reads, writes)
        oo = bass.IndirectOffsetOnAxis(ap=out_off, axis=0) if out_off is not None else None
        io = bass.IndirectOffsetOnAxis(ap=in_off, axis=0) if in_off is not None else None
        if bounds not in self.bregs:
            r = self.nc.gpsimd.alloc_register("bc%d" % bounds)
            self.nc.gpsimd.reg_mov(r, bounds)
            self.bregs[bounds] = r
        self.nc.gpsimd.indirect_dma_start(out=out, out_offset=oo, in_=in_, in_offset=io, bounds_check=self.bregs[bounds],
                                          oob_is_err=False).then_inc(self.sems[sem], 16)
        self.dcnt[sem] += 16
        self._record((sem, self.dcnt[sem], 'dma'), reads, writes)
        self.iq.append((sem, self.dcnt[sem]))
        if len(self.iq) > 24:
            need = {}
            while len(self.iq) > 8:
                s_, v_ = self.iq.pop(0)
                need[s_] = max(need.get(s_, 0), v_)
            for s_, v_ in need.items():
                if s_ in self.sems and self.seen['pool'].get(s_, 0) < v_:
                    self.eng['pool'].wait_ge(self.sems[s_], v_)
                    self.seen['pool'][s_] = v_

    def barrier(self):
        for e in self.eng:
            eng = self.eng[e]
            for s in ('pe', 'act', 'dve', 'pool'):
                if s != e and self.cnt[s] > self.seen[e].get(s, 0):
                    eng.wait_ge(self.sems[s], self.cnt[s])
                    self.seen[e][s] = self.cnt[s]
            for s, v in self.dcnt.items():
                if v > self.seen[e].get(s, 0):
                    eng.wait_ge(self.sems[s], v)
                    self.seen[e][s] = v
        self.lastw = {}
        self.rds = {}
        self.iq = []
        for s in list(self.dcnt):
            self.freed.append((self.sems.pop(s), self.dcnt.pop(s)))
            for e in self.seen:
                self.seen[e].pop(s, None)


def build_nc():
    nc = bass.Bass("TRN2", target_bir_lowering=False)
    es = contextlib.ExitStack()

    def din(name, shape, dt=F32):
        return nc.dram_tensor(name, list(shape), dt, kind="ExternalInput").ap()

    def dscr(name, shape, dt):
        return nc.dram_tensor(name, list(shape), dt).ap()

    xall = din("xall", [NKEY, D])
    cvec = din("cvec", [128, 8, 2])
    w_ada = din("w_ada", [D, 6 * D])
    b_ada = din("b_ada", [128, 48])
    gvec = din("gvec", [128, 16])
    w_ext = din("w_ext", [D, NCOL])
    wq_ext = din("wq_ext", [256, 8 * 2 * 96])
    wkv_k = din("wkv_k", [128, 8 * 64])
    wkv_v = din("wkv_v", [128, 8 * 64])
    smallv = din("smallv", [128, 16])
    lgin = din("lgin", [128, 8])
    kcs = din("kcs", [2, 32, NKEY])
    qcs = din("qcs", [2, 32, NOWN])
    rkcs = din("rkcs", [2, 64, NKEY])
    rqcs = din("rqcs", [2, 64, NOWN])
    utab = din("utab", [3, 128, 512])
    dgtab = din("dgtab", [4, 3, 128, 512])
    cwtab = din("cwtab", [128, 5 * 8 * 32])
    flagv = din("flagv", [128, 2])
    w_out = din("w_out", [D, D])
    w_router = din("w_router", [D, 32])
    br_bc = din("br_bc", [128, 32])
    w1 = din("w1", [32, D, 2 * D])
    w2 = din("w2", [32, D, D])
    b2 = din("b2", [32, D])
    identf = din("identf", [128, 128])
    ustrict = din("ustrict", [128, 128])
    tri32 = din("tri32", [32, 64])
    routetab = din("routetab", [128, 161])
    B1R = din("B1R", [32, 2 * D])
    y = nc.dram_tensor("y", [NOWN, D], F32, kind="ExternalOutput").ap()

    XT = dscr("XT", [128, 8, NKEY], BF16)
    WP = dscr("WP", [128, 8, NCOL], BF16)
    WPC = dscr("WPC", [128, 8, NCOL], BF16)
    MIXT = dscr("MIXT", [D, NOWN], BF16)
    X1 = dscr("X1", [NOWN, D], F32)
    HF = dscr("HF", [NOWN, D], BF16)
    XS = dscr("XS", [160 * 128, D], BF16)
    YS = dscr("YS", [160 * 128, D], F32)
    W1R = dscr("W1R", [32 * 128, 8 * 2048], BF16)
    W2R = dscr("W2R", [32 * 128, 8 * 1024], BF16)
    B2G = dscr("B2G", [32, D], F32)
    dbg = {}
    if DEBUG:
        dbg['mixt'] = nc.dram_tensor("dbg_mixt", [D, NOWN], BF16, kind="ExternalOutput").ap()
        dbg['x1'] = nc.dram_tensor("dbg_x1", [NOWN, D], F32, kind="ExternalOutput").ap()
        dbg['mod'] = nc.dram_tensor("dbg_mod", [128, 96], F32, kind="ExternalOutput").ap()

    kb = KB(nc, es)
    op, dma = kb.op, kb.dma

    def sb(name, shape, dt=F32, stack=None):
        return (stack or es).enter_context(nc.sbuf_tensor(name, list(shape), dt))

    def ps(name, shape, dt=F32, stack=None):
        return (stack or es).enter_context(nc.psum_tensor(name, list(shape), dt))

    ident = sb("ident", [128, 128], F32)
    identb = sb("identb", [128, 128], BF16)
    onesb = sb("onesb", [128, 128], BF16)
    onesf = sb("onesf", [128, 128], F32)
    modv = sb("modv", [128, 96], F32)
    sv = sb("sv", [128, 16], F32)
    gv = sb("gv", [128, 16], F32)
    lg = sb("lg", [128, 16], F32)
    flg = sb("flg", [128, 2], F32)
    rs1 = sb("rs1", [128, 16], F32)
    dma(ident[:], identf[:, :], writes=['ident'], sem='c0')
    dma(sv[:], smallv[:, :], writes=['sv'], sem='c0')
    dma(gv[:], gvec[:, :], writes=['gv'], sem='c0')
    dma(lg[:, 0:8], lgin[:, :], writes=['lg'], sem='c0')
    dma(flg[:], flagv[:, :], writes=['flg'], sem='c0')
    op('dve', lambda e: e.tensor_copy(out=identb[:], in_=ident[:]), reads=['ident'], writes=['identb'])
    op('pool', lambda e: e.memset(onesb[:], 1.0), writes=['onesb'])
    op('pool', lambda e: e.memset(onesf[:], 1.0), writes=['onesf'])
    op('act', lambda e: e.activation(out=lg[:, 12:16], in_=lg[:, 0:4], func=AF.Exp, scale=-1.0), reads=['lg'], writes=['lgs'])
    op('act', lambda e: e.activation(out=lg[:, 8:12], in_=lg[:, 4:8], func=AF.Exp, scale=-1.0), reads=['lg'], writes=['lgs2'])
    op('act', lambda e: e.activation(out=lg[:, 12:16], in_=lg[:, 12:16], func=AF.Ln, bias=1.0), reads=['lgs'], writes=['lgs'])
    op('act', lambda e: e.activation(out=lg[:, 8:12], in_=lg[:, 8:12], func=AF.Ln, bias=1.0), reads=['lgs2'], writes=['lgs2'])
    op('dve', lambda e: e.tensor_scalar(out=lg[:, 0:4], in0=lg[:, 12:16], scalar1=-1.0, scalar2=None, op0=ALU.mult), reads=['lgs', 'lg'], writes=['lgf'])
    op('dve', lambda e: e.tensor_scalar(out=lg[:, 4:8], in0=lg[:, 8:12], scalar1=-1.0, scalar2=None, op0=ALU.mult), reads=['lgs2', 'lg'], writes=['lgb'])
    op('dve', lambda e: e.tensor_scalar(out=lg[:, 12:16], in0=lg[:, 0:4], scalar1=flg[:, 0:1], scalar2=None, op0=ALU.mult), reads=['lgf', 'flg', 'lgs'], writes=['lgt'])
    op('dve', lambda e: e.scalar_tensor_tensor(out=lg[:, 8:12], in0=lg[:, 4:8], scalar=flg[:, 1:2], in1=lg[:, 12:16], op0=ALU.mult, op1=ALU.add), reads=['lgb', 'lgt', 'lgs2'], writes=['lgo'])

    with contextlib.ExitStack() as st:
        cv = sb("cv", [128, 8, 2], F32, st)
        sg = sb("sgm", [128, 8, 2], F32, st)
        wa = [sb("wa%d" % i, [128, 8, 1024], F32, st) for i in range(2)]
        bad = sb("bad", [128, 48], F32, st)
        mps = ps("mps", [128, 96], F32, st)
        dma(cv[:], cvec[:, :, :], writes=['cv'], sem='c0')
        dma(bad[:], b_ada[:, :], writes=['bad'], sem='c0')
        op('act', lambda e: e.activation(out=sg[:], in_=cv[:], func=AF.Sigmoid), reads=['cv'], writes=['sg'])
        op('dve', lambda e: e.tensor_tensor(out=cv[:], in0=cv[:], in1=sg[:], op=ALU.mult), reads=['cv', 'sg'], writes=['cv'])
        wav = w_ada.rearrange("(kc p) n -> p kc n", p=128)
        for mc in range(6):
            w = wa[mc % 2]
            wk = 'wa%d' % (mc % 2)
            for kc in range(8):
                dma(w[:, kc, :], wav[:, kc, mc * 1024:(mc + 1) * 1024], writes=[wk], sem=wk)
            for fc in range(8):
                for kc in range(8):
                    op('pe', lambda e, fc=fc, kc=kc, w=w, mc=mc: e.matmul(
                        mps[:, (mc * 8 + fc) * 2:(mc * 8 + fc) * 2 + 2], lhsT=w[:, kc, fc * 128:(fc + 1) * 128],
                        rhs=cv[:, kc, :], start=(kc == 0), stop=(kc == 7)),
                        reads=[wk, 'cv'], writes=['mps'])
        mv3 = modv[:].rearrange("p (m j) -> p m j", j=2)
        op('dve', lambda e: e.tensor_tensor(out=mv3, in0=mps[:].rearrange("p (m j) -> p m j", j=2),
                                            in1=bad[:].unsqueeze(2).to_broadcast([128, 48, 2]), op=ALU.add),
           reads=['mps', 'bad'], writes=['modv'])
        for j in range(2):
            op('dve', lambda e, j=j: e.scalar_tensor_tensor(out=rs1[:, j * 8:(j + 1) * 8], in0=mv3[:, 8:16, j], scalar=1.0,
                                                          in1=gv[:, 0:8], op0=ALU.add, op1=ALU.mult),
               reads=['modv', 'gv'], writes=['rs1'])
        if DEBUG:
            dma(dbg['mod'][:, :], modv[:], reads=['modv'], sem='dbg')
        kb.barrier()

    g2g_bc = sb("g2g_bc", [128, 1024], F32)
    g2gs_bc = sb("g2gs_bc", [128, 1024], F32)
    mv3 = modv[:].rearrange("p (m j) -> p m j", j=2)
    with contextlib.ExitStack() as st:
        dg0 = [sb("dg0%d" % i, [128, 128], F32, st) for i in range(2)]
        b2t = sb("b2t", [32, 1024], F32, st)
        gps = ps("gps", [128, 1024], F32, st)
        for kc in range(8):
            d_, dk = dg0[kc % 2], 'dg0%d' % (kc % 2)
            op('dve', lambda e, d_=d_, kc=kc: e.tensor_scalar(out=d_[:], in0=ident[:], scalar1=mv3[:, 40 + kc, 0:1], scalar2=None, op0=ALU.mult),
               reads=['ident', 'modv'], writes=[dk])
            op('pe', lambda e, d_=d_, kc=kc: e.matmul(gps[:, kc * 128:(kc + 1) * 128], lhsT=onesf[:], rhs=d_[:], start=True, stop=True), reads=['onesf', dk], writes=['gps'])
        op('act', lambda e: e.copy(out=g2g_bc[:], in_=gps[:, :]), reads=['gps'], writes=['g2g_bc'])
        op('act', lambda e: e.mul(out=g2gs_bc[:], in_=gps[:, :], mul=float(1.0 / 1.702)), reads=['gps'], writes=['g2gs_bc'])
        dma(b2t[:], b2[:, :], writes=['b2t'], sem='c0')
        op('dve', lambda e: e.tensor_tensor(out=b2t[:], in0=b2t[:], in1=g2g_bc[0:32, :], op=ALU.mult), reads=['b2t', 'g2g_bc'], writes=['b2t'])
        dma(B2G[:, :], b2t[:], reads=['b2t'], writes=['B2G'], sem='c0')
        kb.barrier()

    mv3 = modv[:].rearrange("p (m j) -> p m j", j=2)
    FCH = [(C_CQ, 128), (C_CQ + 128, 128), (C_CKV, 128), (C_PE, 96), (C_PESW, 96)]
    for h in range(4):
        b0 = C_RET + h * 512
        FCH += [(b0 + RQ, 64), (b0 + RQS, 64), (b0 + RK, 64), (b0 + RKS, 64), (b0 + RGt, 128)]
    NF = len(FCH)
    FIDX = {c0: i for i, (c0, _) in enumerate(FCH)}
    pbf = sb("pbf", [128, NF, 2], F32)
    pbv = sb("pbv", [128, 2, 512], F32)

    def mm(out, lhsT, rhs, start=True, stop=True):
        return lambda e: e.matmul(out, lhsT=lhsT, rhs=rhs, start=start, stop=stop)

    with contextlib.ExitStack() as st:
        wr = [sb("wr%d" % i, [128, 8, 576], F32, st) for i in range(2)]
        wb = [sb("wb%d" % i, [128, 8, 576], BF16, st) for i in range(2)]
        wc = [sb("wc%d" % i, [128, 8, 576], BF16, st) for i in range(2)]
        shb = sb("shb", [128, 2, 8, 128], F32, st)
        bps = ps("bps", [128, NF * 2], F32, st)
        vps = ps("vps", [128, 2, 512], F32, st)
        for j in range(2):
            for kc in range(8):
                op('dve', lambda e, j=j, kc=kc: e.tensor_copy(out=shb[:, j, kc, :], in_=mv3[:, kc, j:j + 1].to_broadcast([128, 128])),
                   reads=['modv'], writes=['shb'])
        wev = w_ext.rearrange("(kc p) n -> p kc n", p=128)
        pieces = [(0, 576)] + [(C_RET + h * 512, 512) for h in range(4)]
        for pi, (c0, w) in enumerate(pieces):
            r = wr[pi % 2]
            rk = 'wr%d' % (pi % 2)
            for kc in range(8):
                dma(r[:, kc, :w], wev[:, kc, c0:c0 + w], writes=[rk], sem=rk)
            for fi, (fc0, M) in enumerate(FCH):
                if not (c0 <= fc0 < c0 + w):
                    continue
                for kc in range(8):
                    op('pe', mm(bps[0:M, fi * 2:fi * 2 + 2], r[:, kc, fc0 - c0:fc0 - c0 + M], mv3[:, kc, :], kc == 0, kc == 7),
                       reads=[rk, 'modv'], writes=['bps'])
            if pi >= 1:
                h = pi - 1
                for j in range(2):
                    for kc in range(8):
                        op('pe', mm(vps[:, j, h * 128:(h + 1) * 128], shb[:, j, kc, :], r[:, kc, RVt:RVt + 128], kc == 0, kc == 7),
                           reads=[rk, 'shb'], writes=['vps'])
            wbk, wck = 'wb%d' % (pi % 2), 'wc%d' % (pi % 2)
            for kc in range(8):
                op('dve', lambda e, kc=kc, r=r, w=w, pi=pi: e.tensor_scalar(out=wb[pi % 2][:, kc, :w], in0=r[:, kc, :w], scalar1=rs1[:, kc:kc + 1],
                                                                    scalar2=None, op0=ALU.mult), reads=[rk, 'rs1'], writes=[wbk])
                op('act', lambda e, kc=kc, r=r, w=w, pi=pi: e.activation(out=wc[pi % 2][:, kc, :w], in_=r[:, kc, :w], func=AF.Identity, scale=rs1[:, 8 + kc:9 + kc]),
                   reads=[rk, 'rs1'], writes=[wck])
            dma(WP[:, :, c0:c0 + w], wb[pi % 2][:, :, :w], reads=[wbk], writes=['WP'], sem='wpo%d' % (pi % 2))
            dma(WPC[:, :, c0:c0 + w], wc[pi % 2][:, :, :w], reads=[wck], writes=['WPC'], sem='wpc%d' % (pi % 2))
        op('dve', lambda e: e.tensor_copy(out=pbf[:].rearrange("p f j -> p (f j)"), in_=bps[:]), reads=['bps'], writes=['pbf'])
        op('dve', lambda e: e.tensor_copy(out=pbv[:], in_=vps[:]), reads=['vps'], writes=['pbv'])
        kb.barrier()

    GROUPS = [(0, 2)] + [(2 + 4 * g, 4) for g in range(16)]
    with contextlib.ExitStack() as st:
        xt = [sb("xt%d" % i, [128, 1024], F32, st) for i in range(3)]
        sqj = sb("sqj", [128, 1024], F32, st)
        ssq = [sb("ssq%d" % i, [128, 4], F32, st) for i in range(3)]
        xh = [sb("xh%d" % i, [128, 1024], BF16, st) for i in range(2)]
        xTb = [sb("xTb%d" % i, [128, 8, 512], BF16, st) for i in range(2)]
        tps = [ps("tps%d" % i, [128, 8, 128], BF16, st) for i in range(2)]
        tiles = [(gi, t0, nt, t) for gi, (t0, nt) in enumerate(GROUPS) for t in range(nt)]

        def p1_a(ti):
            gi, t0, nt, t = tiles[ti]
            tile = t0 + t
            a, bh = ti % 3, ti % 2
            xk, hk, pk = 'xt%d' % a, 'xh%d' % bh, 'tps%d' % bh
            dma(xt[a][:], xall[tile * 128:(tile + 1) * 128, :], writes=[xk], sem=xk)
            op('act', lambda e, a=a: e.activation(out=sqj[:], in_=xt[a][:], func=AF.Square, accum_out=ssq[a][:, 0:1]),
               reads=[xk], writes=['sqj', 'ss%d' % a])
            op('act', lambda e, a=a: e.activation(out=ssq[a][:, 1:2], in_=ssq[a][:, 0:1], func=AF.Sqrt, scale=1.0 / 1024, bias=EPS),
               reads=['ss%d' % a], writes=['sr%d' % a])
            op('dve', lambda e, a=a: e.reciprocal(out=ssq[a][:, 2:3], in_=ssq[a][:, 1:2]), reads=['sr%d' % a], writes=['rc%d' % a])
            op('dve', lambda e, a=a, bh=bh: e.tensor_scalar(out=xh[bh][:], in0=xt[a][:], scalar1=ssq[a][:, 2:3], scalar2=None, op0=ALU.mult),
               reads=[xk, 'rc%d' % a], writes=[hk])
            for kc in range(8):
                op('pe', lambda e, kc=kc, bh=bh: e.transpose(out=tps[bh][:, kc, :], in_=xh[bh][:, kc * 128:(kc + 1) * 128], identity=identb[:]),
                   reads=[hk, 'identb'], writes=[pk])

        def p1_b(ti):
            gi, t0, nt, t = tiles[ti]
            bh = ti % 2
            xb_ = xTb[gi % 2]
            xbk_ = 'xTb%d' % (gi % 2)
            op('dve', lambda e, bh=bh, t=t, xb_=xb_: e.tensor_copy(out=xb_[:, :, t * 128:(t + 1) * 128], in_=tps[bh][:]), reads=['tps%d' % bh], writes=[xbk_])
            if t == nt - 1:
                dma(XT[:, :, t0 * 128:(t0 + nt) * 128], xb_[:, :, :nt * 128], reads=[xbk_], writes=['XT'], sem='xto%d' % (gi % 2))

        p1_a(0)
        for ti in range(len(tiles)):
            if ti + 1 < len(tiles):
                p1_a(ti + 1)
            p1_b(ti)
        kb.barrier()

    if RUN_RET:
      with contextlib.ExitStack() as st:
        wsl = sb("wsl", [128, 8, 512], BF16, st)
        wslc = sb("wslc", [128, 8, 512], BF16, st)
        kT = sb("kT", [128, NKEY], BF16, st)
        Vr = sb("Vr", [128, 66, 128], BF16, st)
        qT = sb("qT", [128, NOWN], BF16, st)
        qTv = sb("qTv", [128, 3, NOWN], BF16, st)
        sgT = sb("sgT", [128, NOWN], BF16, st)
        xb = [sb("xb%d" % i, [128, 8, 512], BF16, st) for i in range(2)]
        tabc = [sb("tabc%d" % i, [64, 512], F32, st) for i in range(2)]
        tabs = [sb("tabs%d" % i, [64, 512], F32, st) for i in range(2)]
        ta = [sb("ta%d" % i, [64, 512], F32, st) for i in range(2)]
        tb = [sb("tb%d" % i, [64, 512], F32, st) for i in range(2)]
        uts = sb("uts", [128, 3, 512], F32, st)
        UT = sb("UT", [128, 3, 512], F32, st)
        dgs = sb("dgs", [128, 4, 3, 512], F32, st)
        bts = sb("bts", [128, 5, 256], F32, st)
        Bh = sb("Bh", [128, 5, 256], F32, st)
        mk = [sb("mk%d" % i, [128, 512], F32, st) for i in range(3)]
        mkx = sb("mkx", [128, 512], F32, st)
        Am = [sb("Am%d" % i, [128, 512], BF16, st) for i in range(3)]
        osb = sb("osb", [128, 512], F32, st)
        osq = sb("osq", [128, 512], BF16, st)
        orr = sb("orr", [128, 512], F32, st)
        omx = [sb("omx%d" % i, [128, 512], BF16, st) for i in range(2)]
        pk_ps = [ps("pk%d" % i, [64, 512], F32, st) for i in range(2)]
        pg_ps = ps("pg", [128, 512], F32, st)
        st_ps = [ps("stp%d" % i, [128, 512], F32, st) for i in range(3)]
        o_ps = ps("ops", [128, 512], F32, st)
        ss_ps = ps("ssp", [128, 512], F32, st)
        for i in range(3):
            dma(uts[:, i, :], utab[i, :, :], writes=['uts'], sem='c1')
        op('pool', lambda e: e.memset(kT[64:128, :], 0.0), writes=['kTz'])
        op('pool', lambda e: e.memset(qT[64:128, :], 0.0), writes=['qTz'])
        op('pool', lambda e: e.memset(qTv[64:128, :, :], 0.0), writes=['qTvz'])
        for r_ in range(4):
            for i in range(3):
                dma(dgs[:, r_, i, :], dgtab[r_, i, :, :], writes=['dgs'], sem='c1')
        dma(bts[:].rearrange("p c n -> p (c n)"), cwtab[:, :], writes=['bts'], sem='c1')
        LGC = [0, 8, 0, 4, 4]
        for h in range(4):
            b0 = C_RET + h * 512
            dma(wsl[:], WP[:, :, b0:b0 + 512], writes=['wsl'], sem='wsl')
            dma(wslc[:], WPC[:, :, b0:b0 + 512], writes=['wslc'], sem='wslc')
            for cl in range(5):
                op('act', lambda e, cl=cl, h=h: e.activation(out=Bh[:, cl, :], in_=bts[:, cl, :], func=AF.Exp, scale=lg[:, LGC[cl] + h:LGC[cl] + h + 1]),
                   reads=['bts', 'lgf', 'lgb', 'lgo'], writes=['Bh'])
            op('dve', lambda e: e.tensor_scalar(out=Bh[:], in0=Bh[:], scalar1=0.125, scalar2=None, op0=ALU.mult), reads=['Bh'], writes=['Bh'])
            for ti_, lc in enumerate((0, 4, 8)):
                op('act', lambda e, ti_=ti_, lc=lc, h=h: e.activation(out=UT[:, ti_, :], in_=uts[:, ti_, :], func=AF.Exp, scale=lg[:, lc + h:lc + h + 1]),
                   reads=['uts', 'lgf', 'lgb', 'lgo'], writes=['UT'])
            fq, fqs, fk, fks, fg = (FIDX[b0 + RQ], FIDX[b0 + RQS], FIDX[b0 + RK], FIDX[b0 + RKS], FIDX[b0 + RGt])
            for gi, (t0, nt) in enumerate(GROUPS):
                nb = nt * 128
                tok0 = t0 * 128
                j = 1 if gi == 0 else 0
                W = wslc if gi == 0 else wsl
                Wk = 'wslc' if gi == 0 else 'wsl'
                xbb = xb[gi % 2]
                xk = 'xb%d' % (gi % 2)
                dma(xbb[:, :, :nb], XT[:, :, tok0:tok0 + nb], reads=['XT'], writes=[xk], sem=xk)
                tc_, ts_ = tabc[gi % 2], tabs[gi % 2]
                tk = 'tab%d' % (gi % 2)
                dma(tc_[:, :nb], rkcs[0, :, tok0:tok0 + nb], writes=[tk + 'c'], sem=tk + 'c')
                dma(ts_[:, :nb], rkcs[1, :, tok0:tok0 + nb], writes=[tk + 's'], sem=tk + 's')

                def rope_proj(c_a, c_b, f_a, f_b, dst, dkey, q0v=None, gi=gi, nb=nb, W=W, Wk=Wk, xbb=xbb, xk=xk, tc_=tc_, ts_=ts_, tk=tk, j=j):
                    for kc in range(8):
                        op('pe', mm(pk_ps[0][:, :nb], W[:, kc, c_a:c_a + 64], xbb[:, kc, :nb], kc == 0, kc == 7), reads=[Wk, xk], writes=['pk0'])
                    for kc in range(8):
                        op('pe', mm(pk_ps[1][:, :nb], W[:, kc, c_b:c_b + 64], xbb[:, kc, :nb], kc == 0, kc == 7), reads=[Wk, xk], writes=['pk1'])
                    a_, b_ = ta[gi % 2], tb[gi % 2]
                    op('dve', lambda e: e.scalar_tensor_tensor(out=a_[:, :nb], in0=pk_ps[0][:, :nb], scalar=pbf[0:64, f_a, j:j + 1], in1=tc_[:, :nb],
                                                               op0=ALU.add, op1=ALU.mult), reads=['pk0', 'pbf', tk + 'c'], writes=['ta%d' % (gi % 2)])
                    op('dve', lambda e: e.scalar_tensor_tensor(out=b_[:, :nb], in0=pk_ps[1][:, :nb], scalar=pbf[0:64, f_b, j:j + 1], in1=ts_[:, :nb],
                                                               op0=ALU.add, op1=ALU.mult), reads=['pk1', 'pbf', tk + 's'], writes=['tb%d' % (gi % 2)])
                    if q0v is None:
                        op('pool', lambda e: e.tensor_tensor(out=dst, in0=a_[:, :nb], in1=b_[:, :nb], op=ALU.add),
                           reads=['ta%d' % (gi % 2), 'tb%d' % (gi % 2)], writes=[dkey])
                    else:
                        op('dve', lambda e: e.tensor_tensor(out=a_[:, :nb], in0=a_[:, :nb], in1=b_[:, :nb], op=ALU.add),
                           reads=['ta%d' % (gi % 2), 'tb%d' % (gi % 2)], writes=['ta%d' % (gi % 2)])
                        op('pool', lambda e: e.tensor_copy(out=dst, in_=a_[:, :nb]), reads=['ta%d' % (gi % 2)], writes=[dkey])
                        for v in range(3):
                            op('dve', lambda e, v=v: e.tensor_tensor(out=qTv[0:64, v, q0v:q0v + 512], in0=a_[:, :nb], in1=UT[0:64, v, :], op=ALU.mult),
                               reads=['ta%d' % (gi % 2), 'UT'], writes=['qTv'])

                rope_proj(RK, RKS, fk, fks, kT[0:64, tok0:tok0 + nb], 'kT')
                for t in range(nt):
                    for kc in range(8):
                        op('pe', mm(pg_ps[:, t * 128:(t + 1) * 128], xbb[:, kc, t * 128:(t + 1) * 128], W[:, kc, RVt:RVt + 128], kc == 0, kc == 7),
                           reads=[Wk, xk], writes=['pg'])
                op('dve', lambda e, t0=t0, nt=nt, j=j, h=h: e.tensor_tensor(
                    out=Vr[:, t0:t0 + nt, :], in0=pg_ps[:, :nt * 128].rearrange("p (t c) -> p t c", c=128),
                    in1=pbv[:, j, h * 128:(h + 1) * 128].unsqueeze(1).to_broadcast([128, nt, 128]), op=ALU.add),
                    reads=['pg', 'pbv'], writes=['Vr'])
                if gi >= 9:
                    q0 = (gi - 9) * 512
                    rope_proj(RQ, RQS, fq, fqs, qT[0:64, q0:q0 + 512], 'qT', q0v=q0)
                    for kc in range(8):
                        op('pe', mm(pg_ps[:, :], W[:, kc, RGt:RGt + 128], xbb[:, kc, :], kc == 0, kc == 7), reads=[Wk, xk], writes=['pg'])
                    op('act', lambda e, q0=q0, fg=fg: e.activation(out=sgT[:, q0:q0 + 512], in_=pg_ps[:, :], func=AF.Silu, bias=pbf[:, fg, 0:1]),
                       reads=['pg', 'pbf'], writes=['sgT'])
            ui = 0
            rfin = []
            for qb in range(8):
                units = [(0, kt, kt, 0) for kt in range(2)]
                units += [(1, kt, 2 + kt, 2) for kt in range(32)]
                for kt in range(32):
                    if kt < 4 * qb:
                        units.append((2, kt, 34 + kt, 0))
                    elif kt >= 4 * qb + 4:
                        units.append((3, kt, 34 + kt, 1))
                    else:
                        units.append((-1, kt, 34 + kt, kt - 4 * qb))
                units += [(4, kt, kt, 1) for kt in range(2)]
                LA = 2
                pend = []
                for n in range(len(units) + LA):
                    if n < len(units):
                        (cl, kt, ktile, tidx) = units[n]
                        sp_, m_, a_ = st_ps[ui % 3], mk[ui % 3], Am[ui % 3]
                        spk, mkk, ak = 'stp%d' % (ui % 3), 'mk%d' % (ui % 3), 'Am%d' % (ui % 3)
                        qop = qTv[:, tidx, qb * 512:(qb + 1) * 512] if cl >= 0 else qT[:, qb * 512:(qb + 1) * 512]
                        op('pe', mm(sp_[:, :], kT[:, ktile * 128:(ktile + 1) * 128], qop), reads=['kT', 'qT', 'qTv', 'kTz', 'qTz', 'qTvz'], writes=[spk])
                        if cl >= 0:
                            if ui % 2 == 0:
                                op('dve', lambda e, a_=a_, sp_=sp_, cl=cl, qb=qb, kt=kt: e.tensor_scalar(
                                    out=a_[:], in0=sp_[:, :], scalar1=Bh[:, cl, qb * 32 + kt:qb * 32 + kt + 1], scalar2=None, op0=ALU.mult),
                                    reads=[spk, 'Bh'], writes=[ak])
                            else:
                                op('act', lambda e, a_=a_, sp_=sp_, cl=cl, qb=qb, kt=kt: e.activation(
                                    out=a_[:], in_=sp_[:, :], func=AF.Identity, scale=Bh[:, cl, qb * 32 + kt:qb * 32 + kt + 1]),
                                    reads=[spk, 'Bh'], writes=[ak])
                        else:
                            op('act', lambda e, m_=m_, tidx=tidx, h=h: e.activation(out=m_[:], in_=dgs[:, tidx, 0, :], func=AF.Exp, scale=lg[:, h:h + 1]),
                               reads=['dgs', 'lgf'], writes=[mkk])
                            op('act', lambda e, tidx=tidx, h=h: e.activation(out=mkx[:], in_=dgs[:, tidx, 1, :], func=AF.Exp, scale=lg[:, 4 + h:5 + h]),
                               reads=['dgs', 'lgb'], writes=['mkx'])
                            op('pool', lambda e, m_=m_: e.tensor_tensor(out=m_[:], in0=m_[:], in1=mkx[:], op=ALU.add), reads=[mkk, 'mkx'], writes=[mkk])
                            op('pool', lambda e, m_=m_, tidx=tidx: e.tensor_tensor(out=m_[:], in0=m_[:], in1=dgs[:, tidx, 2, :], op=ALU.add),
                               reads=[mkk, 'dgs'], writes=[mkk])
                            op('dve', lambda e, a_=a_, sp_=sp_, m_=m_: e.scalar_tensor_tensor(out=a_[:], in0=sp_[:, :], scalar=0.125, in1=m_[:],
                                                                                             op0=ALU.mult, op1=ALU.mult), reads=[spk, mkk], writes=[ak])
                        pend.append((n, ktile, a_, ak))
                        ui += 1
                        if rfin and n % 6 == 5:
                            rfin.pop(0)()
                    if n >= LA:
                        (n0, ktile0, a0, ak0) = pend.pop(0)
                        op('pe', mm(o_ps[:, :], Vr[:, ktile0, :], a0[:], n0 == 0, n0 == len(units) - 1), reads=['Vr', ak0], writes=['ops'])
                op('act', lambda e: e.copy(out=osb[:], in_=o_ps[:, :]), reads=['ops'], writes=['osb'])

                def fin_steps(h=h, qb=qb):
                    mx = omx[qb % 2]
                    mxk = 'omx%d' % (qb % 2)
                    return [
                        lambda: op('dve', lambda e: e.tensor_tensor(out=osq[:], in0=osb[:], in1=osb[:], op=ALU.mult), reads=['osb'], writes=['osq']),
                        lambda: op('pe', mm(ss_ps[:, :], onesb[:], osq[:]), reads=['onesb', 'osq'], writes=['ssp']),
                        lambda: op('act', lambda e: e.activation(out=orr[:], in_=ss_ps[:, :], func=AF.Sqrt, scale=1.0 / 128, bias=EPS), reads=['ssp'], writes=['orr']),
                        lambda: op('dve', lambda e: e.reciprocal(out=orr[:], in_=orr[:]), reads=['orr'], writes=['orr']),
                        lambda: op('dve', lambda e: e.scalar_tensor_tensor(out=osb[:], in0=osb[:], scalar=sv[:, 7 + h:8 + h], in1=orr[:], op0=ALU.mult, op1=ALU.mult),
                                   reads=['osb', 'orr', 'sv'], writes=['osb']),
                        lambda: (op('dve', lambda e: e.tensor_tensor(out=mx[:], in0=osb[:], in1=sgT[:, qb * 512:(qb + 1) * 512], op=ALU.mult), reads=['osb', 'sgT'], writes=[mxk]),
                                 dma(MIXT[512 + h * 128:512 + (h + 1) * 128, qb * 512:(qb + 1) * 512], mx[:], reads=[mxk], writes=['MIXT'], sem=mxk)),
                    ]
                rfin.extend(fin_steps())
                if qb == 7:
                    while rfin:
                        rfin.pop(0)()
        kb.barrier()
    if RUN_MLA:
      with contextlib.ExitStack() as st:
        ckvT = sb("ckvT", [128, NKEY], BF16, st)
        KT = [sb("KT%d" % i, [96, NKEY], BF16, st) for i in range(2)]
        cqT = sb("cqT", [128, 2, NOWN], BF16, st)
        sspe = sb("sspe", [128, 66], F32, st)
        wqb = sb("wqb", [128, 2, 1536], BF16, st)
        wkb = sb("wkb", [128, 1024], BF16, st)
        fkv, fpe, fpesw, fq0, fq1 = FIDX[C_CKV], FIDX[C_PE], FIDX[C_PESW], FIDX[C_CQ], FIDX[C_CQ + 128]
        with contextlib.ExitStack() as s2:
            wm = sb("wm", [128, 8, 576], BF16, s2)
            wmc = sb("wmc", [128, 8, 576], BF16, s2)
            wqr = sb("wqr", [128, 2, 1536], F32, s2)
            wkr = sb("wkr", [128, 1024], F32, s2)
            xb = [sb("mxb%d" % i, [128, 8, 512], BF16, s2) for i in range(2)]
            pkv = sb("pkv", [128, 512], F32, s2)
            sqv = sb("sqv", [128, 512], BF16, s2)
            srt = sb("srt", [128, 512], F32, s2)
            rawpe = sb("rawpe", [96, 512], F32, s2)
            rawsw = sb("rawsw", [96, 512], F32, s2)
            sqpe = sb("sqpe", [96, 512], BF16, s2)
            tcm = [sb("tcm%d" % i, [96, 512], F32, s2) for i in range(2)]
            tsm = [sb("tsm%d" % i, [96, 512], F32, s2) for i in range(2)]
            pa_ = sb("pa_", [96, 512], F32, s2)
            pb_ = sb("pb_", [96, 512], F32, s2)
            pq = sb("pq", [128, 2, 512], F32, s2)
            sq2 = sb("sq2", [128, 2, 512], BF16, s2)
            pA = ps("pA", [128, 512], F32, s2)
            pB = ps("pB", [96, 512], F32, s2)
            pC = ps("pC", [96, 512], F32, s2)
            ssb = ps("ssb", [128, 512], F32, s2)
            pss = ps("pss", [128, 66], F32, s2)
            pQ = [ps("pQ%d" % i, [128, 512], F32, s2) for i in range(2)]
            dma(wm[:], WP[:, :, 0:576], writes=['wm'], sem='c2')
            dma(wmc[:], WPC[:, :, 0:576], writes=['wmc'], sem='c2')
            dma(wqr[:], wq_ext.rearrange("(c p) n -> p c n", p=128), writes=['wqr'], sem='c2')
            dma(wkr[:, 0:512], wkv_k[:, :], writes=['wkr'], sem='c2')
            dma(wkr[:, 512:1024], wkv_v[:, :], writes=['wkr'], sem='c2')
            for c in range(2):
                op('dve', lambda e, c=c: e.tensor_scalar(out=wqb[:, c, :], in0=wqr[:, c, :], scalar1=sv[:, c:c + 1], scalar2=None, op0=ALU.mult),
                   reads=['wqr', 'sv'], writes=['wqb'])
            op('dve', lambda e: e.tensor_scalar(out=wkb[:], in0=wkr[:], scalar1=sv[:, 2:3], scalar2=None, op0=ALU.mult),
               reads=['wkr', 'sv'], writes=['wkb'])
            for gi, (t0, nt) in enumerate(GROUPS):
                nb, tok0 = nt * 128, t0 * 128
                j = 1 if gi == 0 else 0
                W, Wk = (wmc, 'wmc') if gi == 0 else (wm, 'wm')
                xbb, xk = xb[gi % 2], 'mxb%d' % (gi % 2)
                dma(xbb[:, :, :nb], XT[:, :, tok0:tok0 + nb], reads=['XT'], writes=[xk], sem=xk)
                tc_, ts_, tk = tcm[gi % 2], tsm[gi % 2], 'mtab%d' % (gi % 2)
                dma(tc_[64:96, :nb], kcs[0, :, tok0:tok0 + nb], writes=[tk + 'c'], sem=tk + 'c')
                dma(ts_[64:96, :nb], kcs[1, :, tok0:tok0 + nb], writes=[tk + 's'], sem=tk + 's')
                for kc in range(8):
                    op('pe', mm(pA[:, :nb], W[:, kc, C_CKV:C_CKV + 128], xbb[:, kc, :nb], kc == 0, kc == 7), reads=[Wk, xk], writes=['pA'])
                op('act', lambda e, nb=nb, j=j: e.activation(out=pkv[:, :nb], in_=pA[:, :nb], func=AF.Identity, bias=pbf[:, fkv, j:j + 1]),
                   reads=['pA', 'pbf'], writes=['pkv'])
                op('pool', lambda e, nb=nb: e.tensor_tensor(out=sqv[:, :nb], in0=pkv[:, :nb], in1=pkv[:, :nb], op=ALU.mult), reads=['pkv'], writes=['sqv'])
                op('pe', mm(ssb[:, :nb], onesb[:], sqv[:, :nb]), reads=['onesb', 'sqv'], writes=['ssb'])
                op('act', lambda e, nb=nb: e.activation(out=srt[:, :nb], in_=ssb[:, :nb], func=AF.Sqrt, scale=1.0 / 128, bias=EPS), reads=['ssb'], writes=['srt'])
                op('dve', lambda e, nb=nb: e.reciprocal(out=srt[:, :nb], in_=srt[:, :nb]), reads=['srt'], writes=['srt'])
                op('dve', lambda e, nb=nb, tok0=tok0: e.tensor_tensor(out=ckvT[:, tok0:tok0 + nb], in0=pkv[:, :nb], in1=srt[:, :nb], op=ALU.mult),
                   reads=['pkv', 'srt'], writes=['ckvT'])
                for kc in range(8):
                    op('pe', mm(pB[:, :nb], W[:, kc, C_PE:C_PE + 96], xbb[:, kc, :nb], kc == 0, kc == 7), reads=[Wk, xk], writes=['pB'])
                for kc in range(8):
                    op('pe', mm(pC[:, :nb], W[:, kc, C_PESW:C_PESW + 96], xbb[:, kc, :nb], kc == 0, kc == 7), reads=[Wk, xk], writes=['pC'])
                op('act', lambda e, nb=nb, j=j: e.activation(out=rawpe[64:96, :nb], in_=pB[64:96, :nb], func=AF.Identity, bias=pbf[64:96, fpe, j:j + 1]),
                   reads=['pB', 'pbf'], writes=['rawpe'])
                op('act', lambda e, nb=nb, j=j: e.activation(out=rawsw[64:96, :nb], in_=pC[64:96, :nb], func=AF.Identity, bias=pbf[64:96, fpesw, j:j + 1]),
                   reads=['pC', 'pbf'], writes=['rawsw'])
                op('pool', lambda e, nb=nb: e.tensor_tensor(out=sqpe[64:96, :nb], in0=rawpe[64:96, :nb], in1=rawpe[64:96, :nb], op=ALU.mult),
                   reads=['rawpe'], writes=['sqpe'])
                for t in range(nt):
                    op('pe', mm(pss[:, t0 + t:t0 + t + 1], sqpe[64:96, t * 128:(t + 1) * 128], onesb[64:96, 0:1]), reads=['sqpe', 'onesb'], writes=['pss'])
                op('dve', lambda e, nb=nb, tc_=tc_: e.scalar_tensor_tensor(out=pa_[64:96, :nb], in0=rawpe[64:96, :nb], scalar=sv[64:96, 5:6], in1=tc_[64:96, :nb],
                                                                      op0=ALU.mult, op1=ALU.mult), reads=['rawpe', 'sv', tk + 'c'], writes=['pa_'])
                op('dve', lambda e, nb=nb, ts_=ts_: e.scalar_tensor_tensor(out=pb_[64:96, :nb], in0=rawsw[64:96, :nb], scalar=sv[64:96, 6:7], in1=ts_[64:96, :nb],
                                                                      op0=ALU.mult, op1=ALU.mult), reads=['rawsw', 'sv', tk + 's'], writes=['pb_'])
                op('pool', lambda e, nb=nb, tok0=tok0: e.tensor_tensor(out=KT[0][64:96, tok0:tok0 + nb], in0=pa_[64:96, :nb], in1=pb_[64:96, :nb], op=ALU.add),
                   reads=['pa_', 'pb_'], writes=['KT0pe'])
                op('pool', lambda e, nb=nb, tok0=tok0: e.tensor_copy(out=KT[1][64:96, tok0:tok0 + nb], in_=KT[0][64:96, tok0:tok0 + nb]),
                   reads=['KT0pe'], writes=['KT1pe'])
                if gi >= 9:
                    q0 = (gi - 9) * 512
                    for c in range(2):
                        for kc in range(8):
                            op('pe', mm(pQ[c][:, :], W[:, kc, C_CQ + c * 128:C_CQ + (c + 1) * 128], xbb[:, kc, :], kc == 0, kc == 7),
                               reads=[Wk, xk], writes=['pQ%d' % c])
                        op('act', lambda e, c=c: e.activation(out=pq[:, c, :], in_=pQ[c][:, :], func=AF.Identity, bias=pbf[:, fq0 + c, 0:1]),
                           reads=['pQ%d' % c, 'pbf'], writes=['pq%d' % c])
                        op('pool', lambda e, c=c: e.tensor_tensor(out=sq2[:, c, :], in0=pq[:, c, :], in1=pq[:, c, :], op=ALU.mult),
                           reads=['pq%d' % c], writes=['sq2%d' % c])
                    for c in range(2):
                        op('pe', mm(ssb[:, :], onesb[:], sq2[:, c, :], c == 0, c == 1), reads=['onesb', 'sq2%d' % c], writes=['ssb'])
                    op('act', lambda e: e.activation(out=srt[:, :], in_=ssb[:, :], func=AF.Sqrt, scale=1.0 / 256, bias=EPS), reads=['ssb'], writes=['srt'])
                    op('dve', lambda e: e.reciprocal(out=srt[:, :], in_=srt[:, :]), reads=['srt'], writes=['srt'])
                    for c in range(2):
                        op('dve', lambda e, c=c, q0=q0: e.tensor_tensor(out=cqT[:, c, q0:q0 + 512], in0=pq[:, c, :], in1=srt[:, :], op=ALU.mult),
                           reads=['pq%d' % c, 'srt'], writes=['cqT'])
            op('dve', lambda e: e.tensor_copy(out=sspe[:], in_=pss[:, :]), reads=['pss'], writes=['sspe'])
            kb.barrier()
        with contextlib.ExitStack() as s2:
            QT = [sb("QT%d" % i, [96, NOWN], BF16, s2) for i in range(2)]
            Vh = [sb("Vh%d" % i, [128, 66, 65], BF16, s2) for i in range(2)]
            skh = [sb("skh%d" % i, [128, 66], F32, s2) for i in range(2)]
            sqk = [sb("sqk%d" % i, [64, 512], BF16, s2) for i in range(2)]
            qraw = sb("qraw", [96, 512], F32, s2)
            sqq = sb("sqq", [96, 512], BF16, s2)
            rq = sb("rq", [96, 512], F32, s2)
            qc_ = [sb("qc%d" % i, [96, 512], F32, s2) for i in range(2)]
            qs_ = [sb("qs%d" % i, [96, 512], F32, s2) for i in range(2)]
            qa_ = sb("qa_", [96, 512], F32, s2)
            qb_ = sb("qb_", [96, 512], F32, s2)
            pT = [sb("pT%d" % i, [128, 512], BF16, s2) for i in range(3)]
            ot = sb("ot", [65, 512], F32, s2)
            rec = sb("rec", [65, 512], F32, s2)
            mixh = [sb("mixh%d" % i, [64, 512], BF16, s2) for i in range(2)]
            kn_ps = ps("knp", [64, 512], F32, s2)
            bcp = ps("bcp", [64, 512], F32, s2)
            pvs = ps("pvs", [128, 512], F32, s2)
            pV = pvs[:, 0:256].rearrange("p (t c) -> p t c", c=64)
            pss2 = pvs[:, 256:322]
            qp = ps("qp", [96, 512], F32, s2)
            qsp = ps("qsp", [96, 512], F32, s2)
            stp = [ps("mst%d" % i, [128, 512], F32, s2) for i in range(2)]
            o_ps = ps("mo", [65, 512], F32, s2)
            for i in range(2):
                op('pool', lambda e, i=i: e.memset(Vh[i][:, :, 64:65], 1.0), writes=['Vh%d' % i])
            cs1 = [sb("cs1%d" % i, [128, 2048], F32, s2) for i in range(2)]
            cc1 = [sb("cc1%d" % i, [128, 2, 1024], BF16, s2) for i in range(2)]
            cs2 = [sb("cs2%d" % i, [128, 1024], F32, s2) for i in range(2)]
            cc2 = [sb("cc2%d" % i, [128, 1024], BF16, s2) for i in range(2)]
            zt = sb("zt", [128, 2048], BF16, s2)
            op('pool', lambda e: e.memset(zt[:], 0.0), writes=['zt'])
            XSz = XS.rearrange("(a p r) n -> a p (r n)", p=128, r=2)
            for a in range(160 * 128 // 256):
                dma(XSz[a], zt[:], reads=['zt'], writes=['XS'], sem='xsz')
            cast_ld = [0]
            cast_dn = [0]

            def cast_load(n):
                ex, kc = n // 8, n % 8
                i4 = n % 2
                dma(cs1[i4][:], w1[ex, kc * 128:(kc + 1) * 128, :], writes=['cs1%d' % i4], sem='cs1%d' % i4)
                dma(cs2[i4][:], w2[ex, kc * 128:(kc + 1) * 128, :], writes=['cs2%d' % i4], sem='cs2%d' % i4)

            def cast_do(n):
                ex, kc = n // 8, n % 8
                i4 = n % 2
                a_, ak, b_, bk = cs1[i4], 'cs1%d' % i4, cc1[i4], 'cc1%d' % i4
                c_, ck, d_, dk = cs2[i4], 'cs2%d' % i4, cc2[i4], 'cc2%d' % i4
                op('dve', lambda e: e.tensor_copy(out=b_[:], in_=a_[:].rearrange("p (f g) -> p g f", g=2)), reads=[ak], writes=[bk])
                dma(W1R[ex * 128:(ex + 1) * 128, kc * 2048:(kc + 1) * 2048], b_[:].rearrange("p g f -> p (g f)"), reads=[bk], writes=['W1R'], sem=bk)
                op('dve', lambda e: e.tensor_tensor(out=d_[:], in0=c_[:], in1=g2gs_bc[:], op=ALU.mult), reads=[ck, 'g2gs_bc'], writes=[dk])
                dma(W2R[ex * 128:(ex + 1) * 128, kc * 1024:(kc + 1) * 1024], d_[:], reads=[dk], writes=['W2R'], sem=dk)

            def cast_tick(flush=False):
                if cast_dn[0] < cast_ld[0] and (flush or cast_dn[0] < cast_ld[0] - 0):
                    pass
                if cast_ld[0] < 256:
                    cast_load(cast_ld[0])
                    cast_ld[0] += 1
                    if cast_dn[0] < cast_ld[0] - 1:
                        cast_do(cast_dn[0])
                        cast_dn[0] += 1
                elif cast_dn[0] < 256:
                    cast_do(cast_dn[0])
                    cast_dn[0] += 1

            ui = [0]
            deferred = []

            def gen_steps(h):
                hb = h % 2
                KTh, ktk = KT[hb], 'KT%d' % hb
                vk, sk_k, qtk = 'Vh%d' % hb, 'skh%d' % hb, 'QT%d' % hb
                sk_ = skh[hb]
                steps = []

                def kstep_a(gi, t0, nt):
                    nb, tok0 = nt * 128, t0 * 128
                    kp, kpk = kn_ps, 'knp'
                    sq_, sqkk = sqk[gi % 2], 'sqk%d' % (gi % 2)
                    op('pe', mm(kp[:, :nb], wkb[:, h * 64:(h + 1) * 64], ckvT[:, tok0:tok0 + nb]), reads=['wkb', 'ckvT'], writes=[kpk])
                    for t in range(nt):
                        op('pe', mm(pV[:, t, :], ckvT[:, tok0 + t * 128:tok0 + (t + 1) * 128], wkb[:, 512 + h * 64:512 + (h + 1) * 64]), reads=['ckvT', 'wkb'], writes=['pV'])

                def kstep_b(gi, t0, nt):
                    nb, tok0 = nt * 128, t0 * 128
                    kp, kpk = kn_ps, 'knp'
                    sq_, sqkk = sqk[gi % 2], 'sqk%d' % (gi % 2)
                    op('act', lambda e: e.activation(out=KTh[0:64, tok0:tok0 + nb], in_=kp[:, :nb], func=AF.Identity, scale=sv[0:64, 5:6]), reads=[kpk, 'sv'], writes=[ktk])
                    op('act', lambda e: e.activation(out=sq_[:, :nb], in_=kp[:, :nb], func=AF.Square), reads=[kpk], writes=[sqkk])
                    op('dve', lambda e: e.tensor_copy(out=Vh[hb][:, t0:t0 + nt, 0:64], in_=pV[:, 0:nt, :]), reads=['pV'], writes=[vk])

                def kstep_c(gi, t0, nt):
                    sq_, sqkk = sqk[gi % 2], 'sqk%d' % (gi % 2)
                    for t in range(nt):
                        op('pe', mm(pss2[:, t0 + t:t0 + t + 1], sq_[0:64, t * 128:(t + 1) * 128], onesb[0:64, 0:1]), reads=[sqkk, 'onesb'], writes=['pss2'])

                for gi, (t0, nt) in enumerate(GROUPS):
                    steps.append(lambda gi=gi, t0=t0, nt=nt: kstep_a(gi, t0, nt))
                    steps.append(lambda gi=gi, t0=t0, nt=nt: kstep_b(gi, t0, nt))
                    steps.append(lambda gi=gi, t0=t0, nt=nt: kstep_c(gi, t0, nt))

                steps.append(lambda: op('dve', lambda e: e.tensor_tensor(out=sk_[:], in0=pss2[:, :], in1=sspe[:], op=ALU.add), reads=['pss2', 'sspe'], writes=[sk_k]))
                steps.append(lambda: op('act', lambda e: e.activation(out=sk_[:], in_=sk_[:], func=AF.Sqrt, scale=1.0 / 96, bias=EPS), reads=[sk_k], writes=[sk_k]))

                def scale_c():
                    op('dve', lambda e: e.reciprocal(out=sk_[:], in_=sk_[:]), reads=[sk_k], writes=[sk_k])
                    op('dve', lambda e: e.tensor_scalar(out=sk_[:], in0=sk_[:], scalar1=float(96 ** -0.5), scalar2=None, op0=ALU.mult), reads=[sk_k], writes=[sk_k])
                steps.append(scale_c)

                def q_a(qb):
                    q0 = qb * 512
                    tq = qb % 2
                    dma(qc_[tq][64:96, :], qcs[0, :, q0:q0 + 512], writes=['qc%d' % tq], sem='qtabc%d' % tq)
                    dma(qs_[tq][64:96, :], qcs[1, :, q0:q0 + 512], writes=['qs%d' % tq], sem='qtabs%d' % tq)
                    for c in range(2):
                        op('pe', mm(qp[:, :], wqb[:, c, (h * 2) * 96:(h * 2) * 96 + 96], cqT[:, c, q0:q0 + 512], c == 0, c == 1), reads=['wqb', 'cqT'], writes=['qp'])
                    for c in range(2):
                        op('pe', mm(qsp[:, :], wqb[:, c, (h * 2 + 1) * 96:(h * 2 + 1) * 96 + 96], cqT[:, c, q0:q0 + 512], c == 0, c == 1), reads=['wqb', 'cqT'], writes=['qsp'])

                def q_b(qb):
                    tq = qb % 2
                    op('act', lambda e: e.copy(out=qraw[:], in_=qp[:, :]), reads=['qp'], writes=['qraw'])
                    op('dve', lambda e: e.scalar_tensor_tensor(out=qb_[64:96, :], in0=qsp[64:96, :], scalar=sv[64:96, 4:5], in1=qs_[tq][64:96, :],
                                                               op0=ALU.mult, op1=ALU.mult), reads=['qsp', 'sv', 'qs%d' % tq], writes=['qb_'])

                def q_c(qb):
                    tq = qb % 2
                    op('pool', lambda e: e.tensor_tensor(out=sqq[:], in0=qraw[:], in1=qraw[:], op=ALU.mult), reads=['qraw'], writes=['sqq'])
                    op('dve', lambda e: e.scalar_tensor_tensor(out=qa_[64:96, :], in0=qraw[64:96, :], scalar=sv[64:96, 3:4], in1=qc_[tq][64:96, :],
                                                               op0=ALU.mult, op1=ALU.mult), reads=['qraw', 'sv', 'qc%d' % tq], writes=['qa_'])

                def q_d(qb):
                    op('pe', mm(qp[:, :], onesb[0:96, 0:96], sqq[:]), reads=['onesb', 'sqq', 'qraw'], writes=['qp'])
                    op('pool', lambda e: e.tensor_tensor(out=qa_[64:96, :], in0=qa_[64:96, :], in1=qb_[64:96, :], op=ALU.add), reads=['qa_', 'qb_'], writes=['qa_'])

                def q_e(qb):
                    op('act', lambda e: e.activation(out=rq[:], in_=qp[:, :], func=AF.Sqrt, scale=1.0 / 96, bias=EPS), reads=['qp'], writes=['rq'])

                def q_f(qb):
                    op('dve', lambda e: e.reciprocal(out=rq[:], in_=rq[:]), reads=['rq'], writes=['rq'])

                def q_g(qb):
                    q0 = qb * 512
                    op('dve', lambda e: e.scalar_tensor_tensor(out=QT[hb][0:64, q0:q0 + 512], in0=qraw[0:64, :], scalar=sv[0:64, 3:4], in1=rq[0:64, :],
                                                               op0=ALU.mult, op1=ALU.mult), reads=['qraw', 'sv', 'rq'], writes=[qtk])
                    op('dve', lambda e: e.tensor_tensor(out=QT[hb][64:96, q0:q0 + 512], in0=qa_[64:96, :], in1=rq[64:96, :], op=ALU.mult),
                       reads=['qa_', 'rq'], writes=[qtk])

                for qb in range(8):
                    for f_ in (q_a, q_b, q_c, q_d, q_e, q_f, q_g):
                        steps.append(lambda qb=qb, f_=f_: f_(qb))
                return steps

            def attend(h, nxt):
                hb = h % 2
                KTh, ktk = KT[hb], 'KT%d' % hb
                vk, sk_k, qtk = 'Vh%d' % hb, 'skh%d' % hb, 'QT%d' % hb
                sk_ = skh[hb]
                every = max(1, (8 * 66) // (len(nxt) + 1)) if nxt else 0
                ucount = 0
                for qb in range(8):
                    q0 = qb * 512
                    pend = []
                    for kt in range(66 + 1):
                        if kt < 66:
                            u = ui[0]
                            sp_, spk = stp[u % 2], 'mst%d' % (u % 2)
                            p_, pk = pT[u % 3], 'pT%d' % (u % 3)
                            op('pe', mm(sp_[:, :], KTh[0:96, kt * 128:(kt + 1) * 128], QT[hb][0:96, q0:q0 + 512]), reads=[ktk, 'KT%dpe' % hb, qtk], writes=[spk])
                            op('act', lambda e, p_=p_, sp_=sp_, kt=kt: e.activation(out=p_[:], in_=sp_[:, :], func=AF.Exp, scale=sk_[:, kt:kt + 1]),
                               reads=[spk, sk_k], writes=[pk])
                            pend.append((kt, p_, pk))
                            ui[0] += 1
                            ucount += 1
                            if nxt and ucount % every == 0:
                                nxt.pop(0)()
                            if ucount % 16 == 8:
                                cast_tick()
                            if kt == 8 and deferred:
                                deferred.pop(0)()
                        if kt >= 1:
                            (k0, p0, pk0) = pend.pop(0)
                            op('pe', mm(o_ps[:, :], Vh[hb][:, k0, 0:65], p0[:], k0 == 0, k0 == 65), reads=[vk, pk0], writes=['mo'])
                    op('dve', lambda e: e.tensor_copy(out=ot[:], in_=o_ps[:, :]), reads=['mo'], writes=['ot'])
                    op('dve', lambda e: e.reciprocal(out=rec[64:65, :], in_=ot[64:65, :]), reads=['ot'], writes=['rec'])

                    def fin(h=h, qb=qb, q0=q0):
                        mh, mhk = mixh[qb % 2], 'mixh%d' % (qb % 2)
                        op('pe', mm(bcp[0:64, :], onesf[64:65, 0:64], rec[64:65, :]), reads=['onesf', 'rec'], writes=['bcp'])
                        op('dve', lambda e, mh=mh: e.tensor_tensor(out=mh[:], in0=ot[0:64, :], in1=bcp[0:64, :], op=ALU.mult), reads=['ot', 'bcp'], writes=[mhk])
                        dma(MIXT[h * 64:(h + 1) * 64, q0:q0 + 512], mh[:], reads=[mhk], writes=['MIXT'], sem=mhk)
                    deferred.append(fin)
                while nxt:
                    nxt.pop(0)()
                if h == 7:
                    while deferred:
                        deferred.pop(0)()

            for st_ in gen_steps(0):
                st_()
            for h in range(8):
                attend(h, gen_steps(h + 1) if h + 1 < 8 else [])
            while cast_dn[0] < 256:
                cast_tick()
            kb.barrier()
    if RUN_REST:
      NBLK = 160
      NPAD = NBLK * 128
      GK = sb("GK", [128, 32, 4], F32)
      DSTi = sb("DSTi", [128, 128], I32)
      IDXW = sb("IDXW", [128, NBLK], I32)
      IDXB = sb("IDXB", [128, NBLK], I32)
      XOWN = 256 + NOWN
      with contextlib.ExitStack() as st:
        g1_bc = sb("g1_bc", [128, 1024], F32, st)
        g2s_bc = sb("g2s_bc", [128, 1024], F32, st)
        sh2_bc = sb("sh2_bc", [128, 1024], F32, st)
        g2sv = sb("g2sv", [128, 8], F32, st)
        dg_ = [sb("dg_%d" % i, [128, 128], F32, st) for i in range(2)]
        Wo = sb("Wo", [128, 8, 1024], BF16, st)
        Wr = sb("Wr", [128, 8, 32], BF16, st)
        Wrf = sb("Wrf", [128, 8, 32], F32, st)
        brb = sb("brb", [128, 32], F32, st)
        OHall = sb("OHall", [128, 32, 4, 32], BF16, st)
        Rall = sb("Rall", [128, 32, 32], F32, st)
        CUM = sb("CUM", [128, 32], F32, st)
        ustr = sb("ustr", [128, 128], BF16, st)
        ustrf = sb("ustrf", [128, 128], F32, st)
        tri = sb("tri", [32, 64], F32, st)
        rtab = sb("rtab", [128, NBLK + 1], F32, st)
        yps = [ps("yps%d" % i, [128, 1024], F32, st) for i in range(2)]
        tp4 = [ps("tp4%d" % i, [128, 8, 128], BF16, st) for i in range(2)]
        lgp = ps("lgp", [128, 32], F32, st)
        rkp = ps("rkp", [128, 64], F32, st)
        with contextlib.ExitStack() as s2:
            wof = sb("wof", [128, 8, 1024], F32, s2)
            dma(wof[:], w_out.rearrange("(c p) n -> p c n", p=128), writes=['wof'], sem='c3')
            for c in range(8):
                op('pool' if c % 2 else 'dve', lambda e, c=c: e.tensor_copy(out=Wo[:, c, :], in_=wof[:, c, :]), reads=['wof'], writes=['Wo'])
            dma(Wrf[:], w_router.rearrange("(c p) n -> p c n", p=128), writes=['Wrf'], sem='c3')
            op('dve', lambda e: e.tensor_copy(out=Wr[:], in_=Wrf[:]), reads=['Wrf'], writes=['Wr'])
            dma(brb[:], br_bc[:, :], writes=['brb'], sem='c3')
            dma(ustrf[:], ustrict[:, :], writes=['ustrf'], sem='c3')
            op('dve', lambda e: e.tensor_copy(out=ustr[:], in_=ustrf[:]), reads=['ustrf'], writes=['ustr'])
            dma(tri[:], tri32[:, :], writes=['tri'], sem='c3')
            dma(rtab[:], routetab[:, :], writes=['rtab'], sem='c3')
            op('pool', lambda e: e.memset(CUM[:], 0.0), writes=['CUM'])
            op('dve', lambda e: e.scalar_tensor_tensor(out=g2sv[:], in0=mv3[:, 32:40, 0], scalar=1.0, in1=gv[:, 8:16], op0=ALU.add, op1=ALU.mult),
               reads=['modv', 'gv'], writes=['g2sv'])
            di = 0
            for (dst, dkey, vec) in ((g1_bc, 'g1_bc', lambda kc: mv3[:, 16 + kc, 0:1]), (g2s_bc, 'g2s_bc', lambda kc: g2sv[:, kc:kc + 1]),
                                     (sh2_bc, 'sh2_bc', lambda kc: mv3[:, 24 + kc, 0:1])):
                for kc in range(8):
                    d_, dk = dg_[di % 2], 'dg_%d' % (di % 2)
                    op('dve', lambda e, d_=d_, vec=vec, kc=kc: e.tensor_scalar(out=d_[:], in0=ident[:], scalar1=vec(kc), scalar2=None, op0=ALU.mult),
                       reads=['ident', 'modv', 'g2sv'], writes=[dk])
                    op('pe', mm(yps[0][:, kc * 128:(kc + 1) * 128], onesf[:], d_[:]), reads=['onesf', dk], writes=['yps0'])
                    di += 1
                op('act', lambda e, dst=dst: e.copy(out=dst[:], in_=yps[0][:, :]), reads=['yps0'], writes=[dkey])
            kb.barrier()
        mt = [sb("mt%d" % i, [128, 8, 128], BF16, st) for i in range(2)]
        xo = [sb("xo%d" % i, [128, 1024], F32, st) for i in range(2)]
        x1t = [sb("x1t%d" % i, [128, 1024], F32, st) for i in range(2)]
        tmp4 = sb("tmp4", [128, 1024], F32, st)
        sq4 = sb("sq4", [128, 1024], F32, st)
        st4 = [sb("st4%d" % i, [128, 4], F32, st) for i in range(2)]
        hfb = [sb("hfb%d" % i, [128, 1024], BF16, st) for i in range(2)]
        hT = [sb("hT%d" % i, [128, 8, 128], BF16, st) for i in range(2)]
        lgt = [sb("lgt%d" % i, [128, 32], F32, st) for i in range(2)]
        mx8 = [sb("mx8%d" % i, [128, 8], F32, st) for i in range(2)]
        ex4 = [sb("ex4%d" % i, [128, 4], F32, st) for i in range(2)]
        Mb = [sb("Mb%d" % i, [128, 32], BF16, st) for i in range(2)]
        sm4 = [sb("sm4%d" % i, [128, 4], F32, st) for i in range(2)]
        MIXv = MIXT.rearrange("(c p) n -> p c n", p=128)
        def K(t):
            i2 = t % 2
            return i2, t * 128, (lambda nm: '%s%d' % (nm, i2))

        def S0(t):
            i2, tok, k = K(t)
            dma(mt[i2][:], MIXv[:, :, tok:tok + 128], reads=['MIXT'], writes=[k('mt')], sem=k('mt'))
            dma(xo[i2][:], xall[XOWN + tok:XOWN + tok + 128, :], writes=[k('xo')], sem=k('xo'))

        def S1(t):
            i2, tok, k = K(t)
            for n2 in range(2):
                for c in range(8):
                    op('pe', mm(yps[i2][:, n2 * 512:(n2 + 1) * 512], mt[i2][:, c, :], Wo[:, c, n2 * 512:(n2 + 1) * 512], c == 0, c == 7),
                       reads=[k('mt'), 'Wo'], writes=[k('yps')])
            op('dve', lambda e: e.tensor_tensor(out=tmp4[:], in0=yps[i2][:, :], in1=g1_bc[:], op=ALU.mult), reads=[k('yps'), 'g1_bc'], writes=['tmp4'])
            op('dve', lambda e: e.tensor_tensor(out=x1t[i2][:], in0=tmp4[:], in1=xo[i2][:], op=ALU.add), reads=['tmp4', k('xo')], writes=[k('x1t')])
            dma(X1[tok:tok + 128, :], x1t[i2][:], reads=[k('x1t')], writes=['X1'], sem=k('x1o'))
            op('act', lambda e: e.activation(out=sq4[:], in_=x1t[i2][:], func=AF.Square, accum_out=st4[i2][:, 0:1]), reads=[k('x1t')], writes=['sq4', k('s4a')])

        def S2(t):
            i2, tok, k = K(t)
            op('act', lambda e: e.activation(out=st4[i2][:, 1:2], in_=st4[i2][:, 0:1], func=AF.Sqrt, scale=1.0 / 1024, bias=EPS), reads=[k('s4a')], writes=[k('s4b')])
            op('dve', lambda e: e.reciprocal(out=st4[i2][:, 2:3], in_=st4[i2][:, 1:2]), reads=[k('s4b')], writes=[k('s4c')])
            op('dve', lambda e: e.scalar_tensor_tensor(out=tmp4[:], in0=x1t[i2][:], scalar=st4[i2][:, 2:3], in1=g2s_bc[:], op0=ALU.mult, op1=ALU.mult),
               reads=[k('x1t'), k('s4c'), 'g2s_bc'], writes=['tmp4'])
            op('dve', lambda e: e.tensor_tensor(out=hfb[i2][:], in0=tmp4[:], in1=sh2_bc[:], op=ALU.add), reads=['tmp4', 'sh2_bc'], writes=[k('hfb')])
            dma(HF[tok:tok + 128, :], hfb[i2][:], reads=[k('hfb')], writes=['HF'], sem=k('hfo'))
            for c in range(8):
                op('pe', lambda e, c=c: e.transpose(out=tp4[i2][:, c, :], in_=hfb[i2][:, c * 128:(c + 1) * 128], identity=identb[:]),
                   reads=[k('hfb'), 'identb'], writes=[k('tp4')])

        def S3(t):
            i2, tok, k = K(t)
            op('act', lambda e: e.copy(out=hT[i2][:], in_=tp4[i2][:]), reads=[k('tp4')], writes=[k('hT')])
            for c in range(8):
                op('pe', mm(lgp[:, :], hT[i2][:, c, :], Wr[:, c, :], c == 0, c == 7), reads=[k('hT'), 'Wr'], writes=['lgp'])

        def S4(t):
            i2, tok, k = K(t)
            op('dve', lambda e: e.tensor_tensor(out=lgt[i2][:], in0=lgp[:, :], in1=brb[:], op=ALU.add), reads=['lgp', 'brb'], writes=[k('lgt')])
            op('dve', lambda e: e.max(out=mx8[i2][:], in_=lgt[i2][:]), reads=[k('lgt')], writes=[k('mx8')])
            op('dve', lambda e: e.tensor_scalar(out=sm4[i2][:, 0:1], in0=mx8[i2][:, 0:1], scalar1=-1.0, scalar2=None, op0=ALU.mult), reads=[k('mx8')], writes=[k('nmx')])
            op('act', lambda e: e.activation(out=ex4[i2][:], in_=mx8[i2][:, 0:4], func=AF.Exp, bias=sm4[i2][:, 0:1]), reads=[k('mx8'), k('nmx')], writes=[k('ex4')])
            for kk in range(4):
                op('dve', lambda e, kk=kk: e.tensor_scalar(out=OHall[:, t, kk, :], in0=lgt[i2][:], scalar1=mx8[i2][:, kk:kk + 1], scalar2=None, op0=ALU.is_equal),
                   reads=[k('lgt'), k('mx8')], writes=['OH%d' % t])
            op('dve', lambda e: e.tensor_scalar(out=Mb[i2][:], in0=lgt[i2][:], scalar1=mx8[i2][:, 3:4], scalar2=None, op0=ALU.is_ge),
               reads=[k('lgt'), k('mx8')], writes=[k('Mb')])
            op('pe', mm(rkp[:, 0:32], ustr[:], Mb[i2][:]), reads=['ustr', k('Mb')], writes=['rkp'])
            op('pe', mm(rkp[:, 32:64], onesb[:], Mb[i2][:]), reads=['onesb', k('Mb')], writes=['rkp'])

        def S5(t):
            i2, tok, k = K(t)
            op('dve', lambda e: e.reduce_sum(out=sm4[i2][:, 1:2], in_=ex4[i2][:], axis=AX.X), reads=[k('ex4')], writes=[k('sm1')])
            op('dve', lambda e: e.reciprocal(out=sm4[i2][:, 2:3], in_=sm4[i2][:, 1:2]), reads=[k('sm1')], writes=[k('sm2')])
            op('dve', lambda e: e.tensor_scalar(out=GK[:, t, :], in0=ex4[i2][:], scalar1=sm4[i2][:, 2:3], scalar2=None, op0=ALU.mult),
               reads=[k('ex4'), k('sm2')], writes=['GK'])
            op('dve', lambda e: e.tensor_tensor(out=Rall[:, t, :], in0=rkp[:, 0:32], in1=CUM[:], op=ALU.add), reads=['rkp', 'CUM'], writes=['Rall'])
            op('dve', lambda e: e.tensor_tensor(out=CUM[:], in0=rkp[:, 32:64], in1=CUM[:], op=ALU.add), reads=['rkp', 'CUM', 'Rall'], writes=['CUM'])

        STG = [S0, S1, S2, S3, S4, S5]
        for it in range(32 + len(STG) - 1):
            for kk_ in range(len(STG) - 1, -1, -1):
                t = it - kk_
                if 0 <= t < 32:
                    STG[kk_](t)
        with contextlib.ExitStack() as s2:
            cf = sb("cf", [128, 32], F32, s2)
            ci = sb("ci", [128, 32], I32, s2)
            padT = sb("padT", [32, 128], F32, s2)
            pse = sb("pse", [128, 64], F32, s2)
            cmp3 = sb("cmp3", [128, NBLK, 32], F32, s2)
            Ef = sb("Ef", [128, NBLK], F32, s2)
            eqf = sb("eqf", [128, NBLK], F32, s2)
            ixf = sb("ixf", [128, NBLK], F32, s2)
            dall = sb("dall", [128, 32], F32, s2)
            dtmp = sb("dtmp", [128, 4, 32], F32, s2)
            dstf = sb("dstf", [128, 32, 4], F32, s2)
            op('dve', lambda e: e.tensor_scalar(out=cf[:], in0=CUM[:], scalar1=127.0, scalar2=None, op0=ALU.add), reads=['CUM'], writes=['cf'])
            op('dve', lambda e: e.tensor_copy(out=ci[:], in_=cf[:]), reads=['cf'], writes=['ci'])
            op('dve', lambda e: e.tensor_scalar(out=ci[:], in0=ci[:], scalar1=7, scalar2=7, op0=ALU.arith_shift_right, op1=ALU.arith_shift_left), reads=['ci'], writes=['ci'])
            op('dve', lambda e: e.tensor_copy(out=cf[:], in_=ci[:]), reads=['ci'], writes=['cf'])
            op('pe', lambda e: e.transpose(out=yps[0][0:32, 0:128], in_=cf[:], identity=ident[:]), reads=['cf', 'ident'], writes=['yps0'])
            op('act', lambda e: e.copy(out=padT[:], in_=yps[0][0:32, 0:128]), reads=['yps0'], writes=['padT'])
            op('pe', mm(rkp[:, 0:64], padT[:], tri[:]), reads=['padT', 'tri'], writes=['rkp'])
            op('act', lambda e: e.copy(out=pse[:], in_=rkp[:, 0:64]), reads=['rkp'], writes=['pse'])
            op('dve', lambda e: e.tensor_tensor(out=cmp3[:], in0=pse[:, 32:64].unsqueeze(1).to_broadcast([128, NBLK, 32]),
                                                in1=rtab[:, 0:NBLK].unsqueeze(2).to_broadcast([128, NBLK, 32]), op=ALU.is_le), reads=['pse', 'rtab'], writes=['cmp3'])
            op('dve', lambda e: e.reduce_sum(out=Ef[:], in_=cmp3[:], axis=AX.X), reads=['cmp3'], writes=['Ef'])
            op('dve', lambda e: e.tensor_scalar(out=Ef[:], in0=Ef[:], scalar1=31.0, scalar2=None, op0=ALU.min), reads=['Ef'], writes=['Ef'])
            op('pool', lambda e: e.memset(eqf[:], 0.0), writes=['eqf'])
            op('dve', lambda e: e.tensor_tensor(out=eqf[:, 2:NBLK], in0=Ef[:, 2:NBLK], in1=Ef[:, 0:NBLK - 2], op=ALU.is_equal), reads=['Ef', 'eqf'], writes=['eqf'])
            op('dve', lambda e: e.tensor_scalar(out=eqf[:], in0=eqf[:], scalar1=BIG, scalar2=None, op0=ALU.mult), reads=['eqf'], writes=['eqf'])
            op('dve', lambda e: e.scalar_tensor_tensor(out=ixf[:], in0=Ef[:], scalar=128.0, in1=eqf[:], op0=ALU.mult, op1=ALU.add), reads=['Ef', 'eqf'], writes=['ixf'])
            op('dve', lambda e: e.tensor_scalar(out=ixf[:], in0=ixf[:], scalar1=rtab[:, NBLK:NBLK + 1], scalar2=None, op0=ALU.add), reads=['ixf', 'rtab'], writes=['ixf'])
            op('dve', lambda e: e.tensor_scalar(out=ixf[:], in0=ixf[:], scalar1=0.0, scalar2=2.0e6, op0=ALU.max, op1=ALU.min), reads=['ixf'], writes=['ixf'])
            op('dve', lambda e: e.tensor_copy(out=IDXW[:], in_=ixf[:]), reads=['ixf'], writes=['IDXW'])
            op('dve', lambda e: e.tensor_tensor(out=ixf[:], in0=Ef[:], in1=eqf[:], op=ALU.add), reads=['Ef', 'eqf', 'IDXW'], writes=['ixf'])
            op('dve', lambda e: e.tensor_scalar(out=ixf[:], in0=ixf[:], scalar1=0.0, scalar2=2.0e6, op0=ALU.max, op1=ALU.min), reads=['ixf'], writes=['ixf'])
            op('dve', lambda e: e.tensor_copy(out=IDXB[:], in_=ixf[:]), reads=['ixf'], writes=['IDXB'])
            for t in range(32):
                op('dve', lambda e, t=t: e.tensor_tensor(out=dall[:], in0=Rall[:, t, :], in1=pse[:, 0:32], op=ALU.add), reads=['Rall', 'pse'], writes=['dall'])
                op('dve', lambda e, t=t: e.tensor_tensor(out=dtmp[:], in0=OHall[:, t, :, :], in1=dall[:].unsqueeze(1).to_broadcast([128, 4, 32]), op=ALU.mult),
                   reads=['OH%d' % t, 'dall'], writes=['dtmp'])
                op('dve', lambda e, t=t: e.reduce_sum(out=dstf[:, t, :], in_=dtmp[:], axis=AX.X), reads=['dtmp'], writes=['dstf'])
            op('dve', lambda e: e.tensor_scalar(out=dstf[:], in0=dstf[:], scalar1=0.0, scalar2=float(NPAD - 1), op0=ALU.max, op1=ALU.min), reads=['dstf'], writes=['dstf'])
            op('dve', lambda e: e.tensor_copy(out=DSTi[:], in_=dstf[:].rearrange("p t k -> p (t k)")), reads=['dstf'], writes=['DSTi'])
            kb.barrier()
        kb.barrier()

      with contextlib.ExitStack() as st:
        with contextlib.ExitStack() as s2:
            W1b = [sb("W1b%d" % i, [128, 8, 2048], BF16, s2) for i in range(2)]
            W2b = [sb("W2b%d" % i, [128, 8, 1024], BF16, s2) for i in range(2)]
            B1b = [sb("B1b%d" % i, [128, 2048], F32, s2) for i in range(2)]
            B2b = [sb("B2b%d" % i, [128, 1024], F32, s2) for i in range(2)]
            xbk = [sb("xbk%d" % i, [128, 1024], BF16, s2) for i in range(4)]
            xTk = [sb("xTk%d" % i, [128, 8, 128], BF16, s2) for i in range(2)]
            t1 = sb("t1", [128, 2048], F32, s2)
            sA = sb("sA", [128, 1024], F32, s2)
            aB = [sb("aB%d" % i, [128, 1024], BF16, s2) for i in range(2)]
            aT = sb("aT", [128, 8, 128], BF16, s2)
            yb = [sb("yb%d" % i, [128, 1024], F32, s2) for i in range(2)]
            up = ps("up", [128, 2048], F32, s2)
            ypm = ps("ypm", [128, 1024], F32, s2)
            tpa = ps("tpa", [128, 8, 128], BF16, s2)
            tpb = ps("tpb", [128, 8, 128], BF16, s2)
            def gatherA(j):
                b = j % 2
                wk = 'wga%d' % b
                kb.idma(out=W1b[b][:].rearrange("p k n -> p (k n)"), out_off=None, in_=W1R[:, :], in_off=IDXW[:, j:j + 1], bounds=4095,
                        reads=['IDXW', 'W1R'], writes=['W1b%d' % b], sem=wk)
                kb.idma(out=B1b[b][:, :], out_off=None, in_=B1R[:, :], in_off=IDXB[:, j:j + 1], bounds=31, reads=['IDXB'], writes=['B1b%d' % b], sem='gb1%d' % b)

            def gatherB(j):
                b = j % 2
                wk = 'wgb%d' % b
                kb.idma(out=W2b[b][:].rearrange("p k n -> p (k n)"), out_off=None, in_=W2R[:, :], in_off=IDXW[:, j:j + 1], bounds=4095,
                        reads=['IDXW', 'W2R'], writes=['W2b%d' % b], sem=wk)
                kb.idma(out=B2b[b][:, :], out_off=None, in_=B2G[:, :], in_off=IDXB[:, j:j + 1], bounds=31, reads=['IDXB', 'B2G'], writes=['B2b%d' % b], sem='gb2%d' % b)

            gatherA(0)
            gatherA(1)
            gatherB(0)
            gatherB(1)
            hfr = [sb("hfr%d" % i, [128, 1024], BF16, s2) for i in range(4)]
            for t in range(32):
                h_, hk = hfr[t % 4], 'hfr%d' % (t % 4)
                dma(h_[:], HF[t * 128:(t + 1) * 128, :], reads=['HF'], writes=[hk], sem=hk)
                for kk in range(4):
                    kb.idma(out=XS[:, :], out_off=DSTi[:, t * 4 + kk:t * 4 + kk + 1], in_=h_[:, :], in_off=None, bounds=NPAD - 1,
                            reads=[hk, 'DSTi', 'XS'], writes=['XSw%d' % kk], sem='xsc')

            def loadx(j):
                b4 = j % 4
                dma(xbk[b4][:], XS[j * 128:(j + 1) * 128, :], reads=['XSw0', 'XSw1', 'XSw2', 'XSw3'], writes=['xbk%d' % b4], sem='xbk%d' % b4)

            def TX(j):
                b, b4 = j % 2, j % 4
                xk, xtk = 'xbk%d' % b4, 'xTk%d' % b
                if j + 3 < NBLK:
                    loadx(j + 3)
                for c in range(8):
                    op('pe', lambda e, c=c, b4=b4: e.transpose(out=tpa[:, c, :], in_=xbk[b4][:, c * 128:(c + 1) * 128], identity=identb[:]), reads=[xk, 'identb'], writes=['tpa'])
                op('act', lambda e, b=b: e.copy(out=xTk[b][:], in_=tpa[:]), reads=['tpa'], writes=[xtk])

            def MM1(j, half):
                b = j % 2
                xtk, w1k = 'xTk%d' % b, 'W1b%d' % b
                for n4 in (0, 1) if half == 0 else (2, 3):
                    for kc in range(8):
                        op('pe', mm(up[:, n4 * 512:(n4 + 1) * 512], xTk[b][:, kc, :], W1b[b][:, kc, n4 * 512:(n4 + 1) * 512], kc == 0, kc == 7), reads=[xtk, w1k], writes=['up'])

            def chain(j):
                b = j % 2
                b1k = 'B1b%d' % b
                op('dve', lambda e, b=b: e.tensor_tensor(out=t1[:], in0=up[:, :], in1=B1b[b][:], op=ALU.add), reads=['up', b1k], writes=['t1'])
                if j + 2 < NBLK:
                    gatherA(j + 2)
                op('dve', lambda e: e.tensor_scalar(out=sA[:], in0=t1[:, 0:1024], scalar1=7.0, scalar2=None, op0=ALU.min), reads=['t1'], writes=['sA'])
                op('act', lambda e: e.activation(out=sA[:], in_=sA[:], func=AF.Silu, scale=1.702), reads=['sA'], writes=['sA'])
                op('dve', lambda e: e.tensor_scalar(out=t1[:, 1024:2048], in0=t1[:, 1024:2048], scalar1=-7.0, scalar2=7.0, op0=ALU.max, op1=ALU.min), reads=['t1'], writes=['t1'])
                op('dve', lambda e, b=b: e.scalar_tensor_tensor(out=aB[b][:], in0=t1[:, 1024:2048], scalar=1.0, in1=sA[:], op0=ALU.add, op1=ALU.mult), reads=['t1', 'sA'], writes=['aB%d' % b])

            def TA(j):
                b = j % 2
                for c in range(8):
                    op('pe', lambda e, c=c, b=b: e.transpose(out=tpb[:, c, :], in_=aB[b][:, c * 128:(c + 1) * 128], identity=identb[:]), reads=['aB%d' % b, 'identb'], writes=['tpb'])
                op('act', lambda e: e.copy(out=aT[:], in_=tpb[:]), reads=['tpb'], writes=['aT'])

            def MM2(j):
                b = j % 2
                w2k, b2k = 'W2b%d' % b, 'B2b%d' % b
                for n2 in range(2):
                    for fc in range(8):
                        op('pe', mm(ypm[:, n2 * 512:(n2 + 1) * 512], aT[:, fc, :], W2b[b][:, fc, n2 * 512:(n2 + 1) * 512], fc == 0, fc == 7), reads=['aT', w2k], writes=['ypm'])
                op('dve', lambda e, b=b: e.tensor_tensor(out=yb[b][:], in0=ypm[:, :], in1=B2b[b][:], op=ALU.add), reads=['ypm', b2k], writes=['yb%d' % b])
                dma(YS[j * 128:(j + 1) * 128, :], yb[b][:], reads=['yb%d' % b], writes=['YS'], sem='ybo%d' % b)
                if j + 2 < NBLK:
                    gatherB(j + 2)

            for j0 in range(3):
                loadx(j0)
            TX(0)
            TX(1)
            MM1(0, 0)
            MM1(0, 1)
            chain(0)
            for j in range(NBLK):
                if j + 2 < NBLK:
                    TX(j + 2)
                if j + 1 < NBLK:
                    MM1(j + 1, 0)
                TA(j)
                if j + 1 < NBLK:
                    MM1(j + 1, 1)
                    chain(j + 1)
                MM2(j)
            kb.barrier()
        with contextlib.ExitStack() as s2:
            yg = [[sb("yg%d_%d" % (i, kk), [128, 1024], F32, s2) for kk in range(4)] for i in range(3)]
            x1r = [sb("x1r%d" % i, [128, 1024], F32, s2) for i in range(2)]
            ac = [sb("ac%d" % i, [128, 1024], F32, s2) for i in range(2)]
            for t in range(32):
                i2 = t % 2
                tok = t * 128
                i3 = t % 3
                gk_ = 'yg%d' % i3
                dma(x1r[i2][:], X1[tok:tok + 128, :], reads=['X1'], writes=['x1r%d' % i2], sem='x1r%d' % i2)
                for kk in range(4):
                    kb.idma(out=yg[i3][kk][:, :], out_off=None, in_=YS[:, :], in_off=DSTi[:, t * 4 + kk:t * 4 + kk + 1], bounds=NPAD - 1,
                            reads=['YS', 'DSTi'], writes=[gk_ + 'b%d' % kk], sem=gk_ + '_%d' % kk)
                a_ = ac[i2]
                akk = 'ac%d' % i2
                op('dve', lambda e, a_=a_, i2=i2, t=t: e.scalar_tensor_tensor(out=a_[:], in0=yg[i3][0][:], scalar=GK[:, t, 0:1], in1=x1r[i2][:], op0=ALU.mult, op1=ALU.add),
                   reads=[gk_ + 'b0', 'GK', 'x1r%d' % i2], writes=[akk])
                for kk in range(1, 4):
                    op('dve', lambda e, a_=a_, i2=i2, t=t, kk=kk: e.scalar_tensor_tensor(out=a_[:], in0=yg[i3][kk][:], scalar=GK[:, t, kk:kk + 1], in1=a_[:], op0=ALU.mult, op1=ALU.add),
                       reads=[gk_ + 'b%d' % kk, 'GK', akk], writes=[akk])
                dma(y[tok:tok + 128, :], a_[:], reads=[akk], writes=['y'], sem='yo%d' % i2)
            kb.barrier()
    if DEBUG:
        dma(dbg['mixt'][:, :], MIXT[:, :], reads=['MIXT'], sem='dbg')
        dma(dbg['x1'][:, :], X1[:, :], reads=['X1'], sem='dbg')
        kb.barrier()
    es.close()
    return nc


def _prep_shared(inp):
    f = np.float32
    w_in = np.asarray(inp['w_in'][0], f)
    sw32 = _swap_idx(32)
    sw64 = _swap_idx(64)
    w_ext = np.zeros((D, NCOL), f)
    w_ext[:, C_CQ:C_CQ + 256] = w_in[:, OFF_Q:OFF_Q + 256]
    w_ext[:, C_CKV:C_CKV + 128] = w_in[:, OFF_KV:OFF_KV + 128]
    w_ext[:, C_PE + 64:C_PE + 96] = w_in[:, OFF_PE:OFF_PE + 32]
    w_ext[:, C_PESW + 64:C_PESW + 96] = w_in[:, OFF_PE + sw32]
    for h in range(4):
        b = C_RET + h * 512
        w_ext[:, b + RQ:b + RQ + 64] = w_in[:, OFF_RQ + h * 64:OFF_RQ + (h + 1) * 64]
        w_ext[:, b + RQS:b + RQS + 64] = w_in[:, OFF_RQ + h * 64 + sw64]
        w_ext[:, b + RK:b + RK + 64] = w_in[:, OFF_RK + h * 64:OFF_RK + (h + 1) * 64]
        w_ext[:, b + RKS:b + RKS + 64] = w_in[:, OFF_RK + h * 64 + sw64]
        w_ext[:, b + RGt:b + RGt + 128] = w_in[:, OFF_RG + h * 128:OFF_RG + (h + 1) * 128]
        w_ext[:, b + RVt:b + RVt + 128] = w_in[:, OFF_RV + h * 128:OFF_RV + (h + 1) * 128]
    wqu = np.asarray(inp['w_q_up'][0], f)
    wq_ext = np.zeros((256, 8, 2, 96), f)
    for h in range(8):
        wq_ext[:, h, 0, :] = wqu[:, h * 96:(h + 1) * 96]
        wq_ext[:, h, 1, 64:96] = wqu[:, h * 96 + 64 + sw32]
    wkv = np.asarray(inp['w_kv_up'][0], f).reshape(128, 8, 128)
    smallv = np.zeros((128, 16), f)
    gql = np.asarray(inp['g_q_lora'][0], f)
    smallv[:, 0] = gql[:128]
    smallv[:, 1] = gql[128:]
    smallv[:, 2] = np.asarray(inp['g_kv_lora'][0], f)
    gqh = np.asarray(inp['g_q_head'][0], f)
    gkh = np.asarray(inp['g_k_head'][0], f)
    smallv[:96, 3] = gqh
    smallv[64:96, 4] = gqh[64 + sw32]
    smallv[:96, 5] = gkh
    smallv[64:96, 6] = gkh[64 + sw32]
    gro = np.asarray(inp['g_ret_out'][0], f)
    for h in range(4):
        smallv[:, 7 + h] = gro[h * 128:(h + 1) * 128]
    b1 = np.asarray(inp['b_mlp1'][0], f).reshape(32, 8, 128, 2)
    sh = {
        'w_ada': np.ascontiguousarray(inp['w_ada'][0], f),
        'b_ada': np.ascontiguousarray(np.asarray(inp['b_ada'][0], f).reshape(48, 128).T),
        'gvec': np.ascontiguousarray(np.concatenate([np.asarray(inp['g_attn'][0], f).reshape(8, 128).T,
                                                     np.asarray(inp['g_ffn'][0], f).reshape(8, 128).T], axis=1)),
        'w_ext': w_ext,
        'wq_ext': wq_ext.reshape(256, -1),
        'wkv_k': np.ascontiguousarray(wkv[:, :, :64]).reshape(128, -1),
        'wkv_v': np.ascontiguousarray(wkv[:, :, 64:]).reshape(128, -1),
        'smallv': smallv,
        'lgin': np.ascontiguousarray(np.broadcast_to(np.asarray(inp['ret_decay_logit'][0], f).reshape(1, 8), (128, 8))),
        'w_out': np.ascontiguousarray(inp['w_out'][0], f),
        'w_router': np.ascontiguousarray(inp['w_router'][0], f),
        'br_bc': np.ascontiguousarray(np.broadcast_to(np.asarray(inp['b_router'][0], f).reshape(1, 32), (128, 32))),
        'w1': np.ascontiguousarray(inp['w_mlp1'][0], f),
        'w2': np.ascontiguousarray(inp['w_mlp2'][0], f),
        'b2': np.ascontiguousarray(inp['b_mlp2'][0], f),
        'identf': np.eye(128, dtype=f),
        'ustrict': np.triu(np.ones((128, 128), f), 1),
        'tri32': np.concatenate([np.triu(np.ones((32, 32), f), 1), np.triu(np.ones((32, 32), f), 0)], axis=1),
        'routetab': np.ascontiguousarray(np.concatenate([np.broadcast_to(128.0 * np.arange(160, dtype=f)[None, :], (128, 160)),
                                                         np.arange(128, dtype=f)[:, None]], axis=1)),
        'B1R': np.ascontiguousarray(np.asarray(inp['b_mlp1'][0], f).reshape(32, D, 2).transpose(0, 2, 1)).reshape(32, 2 * D),
    }
    jj = np.arange(128, dtype=f)[:, None]
    ii = np.arange(512, dtype=f)[None, :]
    dg = np.zeros((4, 3, 128, 512), f)
    for r in range(4):
        d = ii - (128 * r + jj)
        dg[r, 0] = np.where(d > 0, d, BIG)
        dg[r, 1] = np.where(d < 0, -d, BIG)
        dg[r, 2] = np.where(d == 0, 2.0, 0.0)
    sh['dgtab'] = dg
    return sh


def _prep_core(core, inp, sh):
    f = np.float32
    b, half = core // 2, core % 2
    x = np.asarray(inp['x'], f)
    own = slice(half * NOWN, (half + 1) * NOWN)
    oth = slice((1 - half) * NOWN, (2 - half) * NOWN)
    m = dict(sh)
    m['xall'] = np.ascontiguousarray(np.concatenate([np.asarray(inp['ctx'][b], f), x[b, oth], x[b, own]], axis=0))
    cv = np.stack([np.asarray(inp['c'][b], f), np.asarray(inp['c_ctx'], f)], axis=-1)
    m['cvec'] = np.ascontiguousarray(cv.reshape(8, 128, 2).transpose(1, 0, 2))
    t = np.arange(2 * NOWN)
    prow, pcol = (t // 64).astype(f), (t % 64).astype(f)
    for dim, kn, qn in ((32, 'kcs', 'qcs'), (64, 'rkcs', 'rqcs')):
        cos, sin = _rope_tables(prow, pcol, dim)
        kc = np.concatenate([np.ones((256, dim), f), cos[oth], cos[own]], axis=0)
        ks = np.concatenate([np.zeros((256, dim), f), sin[oth], sin[own]], axis=0)
        m[kn] = np.ascontiguousarray(np.stack([kc.T, ks.T], axis=0))
        m[qn] = np.ascontiguousarray(np.stack([cos[own].T, sin[own].T], axis=0))
    jj = np.arange(128, dtype=f)[:, None]
    ii = np.arange(512, dtype=f)[None, :]
    s = 1.0 if half == 1 else -1.0
    iq = np.broadcast_to(ii, (128, 512)).astype(f)
    m['utab'] = np.ascontiguousarray(np.stack([iq, -iq, s * iq], axis=0).astype(f))
    base = np.zeros((5, 8, 32), f)
    for qb in range(8):
        for kt in range(32):
            if kt < 2:
                base[0, qb, kt] = half * 4096 + qb * 512 + 256 - kt * 128
                base[4, qb, kt] = 8192 - half * 4096 - qb * 512 + kt * 128
            base[1, qb, kt] = (4096 + qb * 512 - kt * 128) if half == 1 else (4096 + kt * 128 - qb * 512)
            base[2, qb, kt] = qb * 512 - kt * 128
            base[3, qb, kt] = kt * 128 - qb * 512
    sgn = np.array([-1.0, -s, -1.0, 1.0, 1.0], f)
    cw = base.reshape(1, 5, 256) + sgn.reshape(1, 5, 1) * np.arange(128, dtype=f).reshape(128, 1, 1)
    m['cwtab'] = np.ascontiguousarray(cw.reshape(128, 5 * 256).astype(f))
    m['flagv'] = np.ascontiguousarray(np.broadcast_to(np.array([[half, 1 - half]], f), (128, 2)))
    return m


def kernel(**inputs):
    sh = _prep_shared(inputs)
    in_maps = [_prep_core(c, inputs, sh) for c in range(NCORES)]
    nc = build_nc()
    res = run_bass_kernel_spmd(nc, in_maps, core_ids=list(range(NCORES)))
    out = np.zeros((4, 2 * NOWN, D), np.float32)
    for c in range(NCORES):
        b, half = c // 2, c % 2
        out[b, half * NOWN:(half + 1) * NOWN] = res.results[c]["y"]
    if DEBUG:
        kernel.last = res
    return out
```

```python
import contextlib
import numpy as np
import concourse.bass as bass
import concourse.mybir as mybir
from concourse.bass_utils import run_bass_kernel_spmd

F32 = mybir.dt.float32
I32 = mybir.dt.int32
BF16 = mybir.dt.bfloat16
AF = mybir.ActivationFunctionType
ALU = mybir.AluOpType
AX = mybir.AxisListType

D = 1024
NOWN = 4096
NKEY = 8448
NCORES = 8
EPS = 1e-6
BIG = 1.0e6
DEBUG = False
RUN_RET = True
RUN_MLA = True
RUN_REST = True

OFF_Q, OFF_KV, OFF_PE, OFF_RQ, OFF_RK, OFF_RV, OFF_RG = 0, 256, 384, 416, 672, 928, 1440
C_CQ = 0
C_CKV = 256
C_PE = 384
C_PESW = 480
C_RET = 576
NCOL = C_RET + 4 * 512
RQ, RQS, RK, RKS, RGt, RVt = 0, 64, 128, 192, 256, 384


def _swap_idx(dim):
    nf = dim // 4
    idx = np.arange(dim).reshape(2, 2, nf)
    return idx[:, ::-1, :].reshape(dim)


def _rope_tables(pos_row, pos_col, dim):
    nf = dim // 4
    inv = np.power(np.float32(10000.0), -np.arange(nf, dtype=np.float32) / np.float32(nf)).astype(np.float32)
    pos = np.stack([pos_row, pos_col], axis=-1).astype(np.float32)
    ang = pos[:, :, None] * inv
    ang = np.broadcast_to(ang[:, :, None, :], (pos.shape[0], 2, 2, nf)).reshape(pos.shape[0], dim)
    cos = np.cos(ang).astype(np.float32)
    sin = np.sin(ang).astype(np.float32)
    sgn = np.ones((2, 2, nf), np.float32)
    sgn[:, 0, :] = -1.0
    return cos, sin * sgn.reshape(dim)


class KB:
    def __init__(self, nc, es):
        self.nc = nc
        self.es = es
        self.eng = {'pe': nc.tensor, 'act': nc.scalar, 'dve': nc.vector, 'pool': nc.gpsimd, 'sp': nc.sync}
        self.sems = {}
        self.cnt = {}
        for e in ('pe', 'act', 'dve', 'pool'):
            self.sems[e] = es.enter_context(nc.semaphore("c_" + e))
            self.cnt[e] = 0
        self.seen = {e: {} for e in self.eng}
        self.lastw = {}
        self.rds = {}
        self.dcnt = {}
        self.freed = []
        self.uniq = 0
        self.iq = []
        self.bregs = {}

    def _dsem(self, sem):
        if sem not in self.sems:
            if self.freed:
                h, c = self.freed.pop()
            else:
                h, c = self.es.enter_context(self.nc.semaphore("s_" + sem)), 0
            self.sems[sem] = h
            self.dcnt[sem] = c

    def _waits(self, engine, reads, writes):
        need = {}
        for k in list(reads) + list(writes):
            ev = self.lastw.get(k)
            if ev is not None:
                s, v, e = ev
                if not (e == engine and engine == 'pe'):
                    need[s] = max(need.get(s, 0), v)
        for k in writes:
            for (s, v, e) in self.rds.get(k, ()):
                if e == engine:
                    continue
                need[s] = max(need.get(s, 0), v)
        eng = self.eng[engine]
        for s, v in need.items():
            if self.seen[engine].get(s, 0) >= v:
                continue
            eng.wait_ge(self.sems[s], v)
            self.seen[engine][s] = v

    def _record(self, ev, reads, writes):
        for k in reads:
            self.rds.setdefault(k, []).append(ev)
        for k in writes:
            self.lastw[k] = ev
            self.rds[k] = []

    def op(self, engine, fn, reads=(), writes=()):
        self._waits(engine, reads, writes)
        ins = fn(self.eng[engine])
        self.cnt[engine] += 1
        ins.then_inc(self.sems[engine], 1)
        self._record((engine, self.cnt[engine], engine), reads, writes)

    def dma(self, out, in_, reads=(), writes=(), sem='d0', queue='sp'):
        if len(sem) == 2 and sem[0] == 'c' and sem[1].isdigit():
            self.uniq += 1
            sem = '%s_%d' % (sem, self.uniq)
        self._dsem(sem)
        self._waits(queue, reads, writes)
        self.eng[queue].dma_start(out=out, in_=in_).then_inc(self.sems[sem], 16)
        self.dcnt[sem] += 16
        self._record((sem, self.dcnt[sem], 'dma'), reads, writes)

    def idma(self, out, out_off, in_, in_off, bounds, reads=(), writes=(), sem='i0'):
        self._dsem(sem)
        self._waits('pool', reads, writes)
        oo = bass.IndirectOffsetOnAxis(ap=out_off, axis=0) if out_off is not None else None
        io = bass.IndirectOffsetOnAxis(ap=in_off, axis=0) if in_off is not None else None
        if bounds not in self.bregs:
            r = self.nc.gpsimd.alloc_register("bc%d" % bounds)
            self.nc.gpsimd.reg_mov(r, bounds)
            self.bregs[bounds] = r
        self.nc.gpsimd.indirect_dma_start(out=out, out_offset=oo, in_=in_, in_offset=io, bounds_check=self.bregs[bounds],
                                          oob_is_err=False).then_inc(self.sems[sem], 16)
        self.dcnt[sem] += 16
        self._record((sem, self.dcnt[sem], 'dma'), reads, writes)
        self.iq.append((sem, self.dcnt[sem]))
        if len(self.iq) > 24:
            need = {}
            while len(self.iq) > 8:
                s_, v_ = self.iq.pop(0)
                need[s_] = max(need.get(s_, 0), v_)
            for s_, v_ in need.items():
                if s_ in self.sems and self.seen['pool'].get(s_, 0) < v_:
                    self.eng['pool'].wait_ge(self.sems[s_], v_)
                    self.seen['pool'][s_] = v_

    def barrier(self):
        for e in self.eng:
            eng = self.eng[e]
            for s in ('pe', 'act', 'dve', 'pool'):
                if s != e and self.cnt[s] > self.seen[e].get(s, 0):
                    eng.wait_ge(self.sems[s], self.cnt[s])
                    self.seen[e][s] = self.cnt[s]
            for s, v in self.dcnt.items():
                if v > self.seen[e].get(s, 0):
                    eng.wait_ge(self.sems[s], v)
                    self.seen[e][s] = v
        self.lastw = {}
        self.rds = {}
        self.iq = []
        for s in list(self.dcnt):
            self.freed.append((self.sems.pop(s), self.dcnt.pop(s)))
            for e in self.seen:
                self.seen[e].pop(s, None)


def build_nc():
    nc = bass.Bass("TRN2", target_bir_lowering=False)
    es = contextlib.ExitStack()

    def din(name, shape, dt=F32):
        return nc.dram_tensor(name, list(shape), dt, kind="ExternalInput").ap()

    def dscr(name, shape, dt):
        return nc.dram_tensor(name, list(shape), dt).ap()

    xall = din("xall", [NKEY, D])
    cvec = din("cvec", [128, 8, 2])
    w_ada = din("w_ada", [D, 6 * D])
    b_ada = din("b_ada", [128, 48])
    gvec = din("gvec", [128, 16])
    w_ext = din("w_ext", [D, NCOL])
    wq_ext = din("wq_ext", [256, 8 * 2 * 96])
    wkv_k = din("wkv_k", [128, 8 * 64])
    wkv_v = din("wkv_v", [128, 8 * 64])
    smallv = din("smallv", [128, 16])
    lgin = din("lgin", [128, 8])
    kcs = din("kcs", [2, 32, NKEY])
    qcs = din("qcs", [2, 32, NOWN])
    rkcs = din("rkcs", [2, 64, NKEY])
    rqcs = din("rqcs", [2, 64, NOWN])
    utab = din("utab", [3, 128, 512])
    dgtab = din("dgtab", [4, 3, 128, 512])
    cwtab = din("cwtab", [128, 5 * 8 * 32])
    flagv = din("flagv", [128, 2])
    w_out = din("w_out", [D, D])
    w_router = din("w_router", [D, 32])
    br_bc = din("br_bc", [128, 32])
    w1 = din("w1", [32, D, 2 * D])
    w2 = din("w2", [32, D, D])
    b2 = din("b2", [32, D])
    identf = din("identf", [128, 128])
    ustrict = din("ustrict", [128, 128])
    tri32 = din("tri32", [32, 64])
    routetab = din("routetab", [128, 161])
    B1R = din("B1R", [32, 2 * D])
    y = nc.dram_tensor("y", [NOWN, D], F32, kind="ExternalOutput").ap()

    XT = dscr("XT", [128, 8, NKEY], BF16)
    WP = dscr("WP", [128, 8, NCOL], BF16)
    WPC = dscr("WPC", [128, 8, NCOL], BF16)
    MIXT = dscr("MIXT", [D, NOWN], BF16)
    X1 = dscr("X1", [NOWN, D], F32)
    HF = dscr("HF", [NOWN, D], BF16)
    XS = dscr("XS", [160 * 128, D], BF16)
    YS = dscr("YS", [160 * 128, D], F32)
    W1R = dscr("W1R", [32 * 128, 8 * 2048], BF16)
    W2R = dscr("W2R", [32 * 128, 8 * 1024], BF16)
    B2G = dscr("B2G", [32, D], F32)
    dbg = {}
    if DEBUG:
        dbg['mixt'] = nc.dram_tensor("dbg_mixt", [D, NOWN], BF16, kind="ExternalOutput").ap()
        dbg['x1'] = nc.dram_tensor("dbg_x1", [NOWN, D], F32, kind="ExternalOutput").ap()
        dbg['mod'] = nc.dram_tensor("dbg_mod", [128, 96], F32, kind="ExternalOutput").ap()

    kb = KB(nc, es)
    op, dma = kb.op, kb.dma

    def sb(name, shape, dt=F32, stack=None):
        return (stack or es).enter_context(nc.sbuf_tensor(name, list(shape), dt))

    def ps(name, shape, dt=F32, stack=None):
        return (stack or es).enter_context(nc.psum_tensor(name, list(shape), dt))

    ident = sb("ident", [128, 128], F32)
    identb = sb("identb", [128, 128], BF16)
    onesb = sb("onesb", [128, 128], BF16)
    onesf = sb("onesf", [128, 128], F32)
    modv = sb("modv", [128, 96], F32)
    sv = sb("sv", [128, 16], F32)
    gv = sb("gv", [128, 16], F32)
    lg = sb("lg", [128, 16], F32)
    flg = sb("flg", [128, 2], F32)
    rs1 = sb("rs1", [128, 16], F32)
    dma(ident[:], identf[:, :], writes=['ident'], sem='c0')
    dma(sv[:], smallv[:, :], writes=['sv'], sem='c0')
    dma(gv[:], gvec[:, :], writes=['gv'], sem='c0')
    dma(lg[:, 0:8], lgin[:, :], writes=['lg'], sem='c0')
    dma(flg[:], flagv[:, :], writes=['flg'], sem='c0')
    op('dve', lambda e: e.tensor_copy(out=identb[:], in_=ident[:]), reads=['ident'], writes=['identb'])
    op('pool', lambda e: e.memset(onesb[:], 1.0), writes=['onesb'])
    op('pool', lambda e: e.memset(onesf[:], 1.0), writes=['onesf'])
    op('act', lambda e: e.activation(out=lg[:, 12:16], in_=lg[:, 0:4], func=AF.Exp, scale=-1.0), reads=['lg'], writes=['lgs'])
    op('act', lambda e: e.activation(out=lg[:, 8:12], in_=lg[:, 4:8], func=AF.Exp, scale=-1.0), reads=['lg'], writes=['lgs2'])
    op('act', lambda e: e.activation(out=lg[:, 12:16], in_=lg[:, 12:16], func=AF.Ln, bias=1.0), reads=['lgs'], writes=['lgs'])
    op('act', lambda e: e.activation(out=lg[:, 8:12], in_=lg[:, 8:12], func=AF.Ln, bias=1.0), reads=['lgs2'], writes=['lgs2'])
    op('dve', lambda e: e.tensor_scalar(out=lg[:, 0:4], in0=lg[:, 12:16], scalar1=-1.0, scalar2=None, op0=ALU.mult), reads=['lgs', 'lg'], writes=['lgf'])
    op('dve', lambda e: e.tensor_scalar(out=lg[:, 4:8], in0=lg[:, 8:12], scalar1=-1.0, scalar2=None, op0=ALU.mult), reads=['lgs2', 'lg'], writes=['lgb'])
    op('dve', lambda e: e.tensor_scalar(out=lg[:, 12:16], in0=lg[:, 0:4], scalar1=flg[:, 0:1], scalar2=None, op0=ALU.mult), reads=['lgf', 'flg', 'lgs'], writes=['lgt'])
    op('dve', lambda e: e.scalar_tensor_tensor(out=lg[:, 8:12], in0=lg[:, 4:8], scalar=flg[:, 1:2], in1=lg[:, 12:16], op0=ALU.mult, op1=ALU.add), reads=['lgb', 'lgt', 'lgs2'], writes=['lgo'])

    with contextlib.ExitStack() as st:
        cv = sb("cv", [128, 8, 2], F32, st)
        sg = sb("sgm", [128, 8, 2], F32, st)
        wa = [sb("wa%d" % i, [128, 8, 1024], F32, st) for i in range(2)]
        bad = sb("bad", [128, 48], F32, st)
        mps = ps("mps", [128, 96], F32, st)
        dma(cv[:], cvec[:, :, :], writes=['cv'], sem='c0')
        dma(bad[:], b_ada[:, :], writes=['bad'], sem='c0')
        op('act', lambda e: e.activation(out=sg[:], in_=cv[:], func=AF.Sigmoid), reads=['cv'], writes=['sg'])
        op('dve', lambda e: e.tensor_tensor(out=cv[:], in0=cv[:], in1=sg[:], op=ALU.mult), reads=['cv', 'sg'], writes=['cv'])
        wav = w_ada.rearrange("(kc p) n -> p kc n", p=128)
        for mc in range(6):
            w = wa[mc % 2]
            wk = 'wa%d' % (mc % 2)
            for kc in range(8):
                dma(w[:, kc, :], wav[:, kc, mc * 1024:(mc + 1) * 1024], writes=[wk], sem=wk)
            for fc in range(8):
                for kc in range(8):
                    op('pe', lambda e, fc=fc, kc=kc, w=w, mc=mc: e.matmul(
                        mps[:, (mc * 8 + fc) * 2:(mc * 8 + fc) * 2 + 2], lhsT=w[:, kc, fc * 128:(fc + 1) * 128],
                        rhs=cv[:, kc, :], start=(kc == 0), stop=(kc == 7)),
                        reads=[wk, 'cv'], writes=['mps'])
        mv3 = modv[:].rearrange("p (m j) -> p m j", j=2)
        op('dve', lambda e: e.tensor_tensor(out=mv3, in0=mps[:].rearrange("p (m j) -> p m j", j=2),
                                            in1=bad[:].unsqueeze(2).to_broadcast([128, 48, 2]), op=ALU.add),
           reads=['mps', 'bad'], writes=['modv'])
        for j in range(2):
            op('dve', lambda e, j=j: e.scalar_tensor_tensor(out=rs1[:, j * 8:(j + 1) * 8], in0=mv3[:, 8:16, j], scalar=1.0,
                                                          in1=gv[:, 0:8], op0=ALU.add, op1=ALU.mult),
               reads=['modv', 'gv'], writes=['rs1'])
        if DEBUG:
            dma(dbg['mod'][:, :], modv[:], reads=['modv'], sem='dbg')
        kb.barrier()

    g2g_bc = sb("g2g_bc", [128, 1024], F32)
    g2gs_bc = sb("g2gs_bc", [128, 1024], F32)
    mv3 = modv[:].rearrange("p (m j) -> p m j", j=2)
    with contextlib.ExitStack() as st:
        dg0 = [sb("dg0%d" % i, [128, 128], F32, st) for i in range(2)]
        b2t = sb("b2t", [32, 1024], F32, st)
        gps = ps("gps", [128, 1024], F32, st)
        for kc in range(8):
            d_, dk = dg0[kc % 2], 'dg0%d' % (kc % 2)
            op('dve', lambda e, d_=d_, kc=kc: e.tensor_scalar(out=d_[:], in0=ident[:], scalar1=mv3[:, 40 + kc, 0:1], scalar2=None, op0=ALU.mult),
               reads=['ident', 'modv'], writes=[dk])
            op('pe', lambda e, d_=d_, kc=kc: e.matmul(gps[:, kc * 128:(kc + 1) * 128], lhsT=onesf[:], rhs=d_[:], start=True, stop=True), reads=['onesf', dk], writes=['gps'])
        op('act', lambda e: e.copy(out=g2g_bc[:], in_=gps[:, :]), reads=['gps'], writes=['g2g_bc'])
        op('act', lambda e: e.mul(out=g2gs_bc[:], in_=gps[:, :], mul=float(1.0 / 1.702)), reads=['gps'], writes=['g2gs_bc'])
        dma(b2t[:], b2[:, :], writes=['b2t'], sem='c0')
        op('dve', lambda e: e.tensor_tensor(out=b2t[:], in0=b2t[:], in1=g2g_bc[0:32, :], op=ALU.mult), reads=['b2t', 'g2g_bc'], writes=['b2t'])
        dma(B2G[:, :], b2t[:], reads=['b2t'], writes=['B2G'], sem='c0')
        kb.barrier()

    mv3 = modv[:].rearrange("p (m j) -> p m j", j=2)
    FCH = [(C_CQ, 128), (C_CQ + 128, 128), (C_CKV, 128), (C_PE, 96), (C_PESW, 96)]
    for h in range(4):
        b0 = C_RET + h * 512
        FCH += [(b0 + RQ, 64), (b0 + RQS, 64), (b0 + RK, 64), (b0 + RKS, 64), (b0 + RGt, 128)]
    NF = len(FCH)
    FIDX = {c0: i for i, (c0, _) in enumerate(FCH)}
    pbf = sb("pbf", [128, NF, 2], F32)
    pbv = sb("pbv", [128, 2, 512], F32)

    def mm(out, lhsT, rhs, start=True, stop=True):
        return lambda e: e.matmul(out, lhsT=lhsT, rhs=rhs, start=start, stop=stop)

    with contextlib.ExitStack() as st:
        wr = [sb("wr%d" % i, [128, 8, 576], F32, st) for i in range(2)]
        wb = [sb("wb%d" % i, [128, 8, 576], BF16, st) for i in range(2)]
        wc = [sb("wc%d" % i, [128, 8, 576], BF16, st) for i in range(2)]
        shb = sb("shb", [128, 2, 8, 128], F32, st)
        bps = ps("bps", [128, NF * 2], F32, st)
        vps = ps("vps", [128, 2, 512], F32, st)
        for j in range(2):
            for kc in range(8):
                op('dve', lambda e, j=j, kc=kc: e.tensor_copy(out=shb[:, j, kc, :], in_=mv3[:, kc, j:j + 1].to_broadcast([128, 128])),
                   reads=['modv'], writes=['shb'])
        wev = w_ext.rearrange("(kc p) n -> p kc n", p=128)
        pieces = [(0, 576)] + [(C_RET + h * 512, 512) for h in range(4)]
        for pi, (c0, w) in enumerate(pieces):
            r = wr[pi % 2]
            rk = 'wr%d' % (pi % 2)
            for kc in range(8):
                dma(r[:, kc, :w], wev[:, kc, c0:c0 + w], writes=[rk], sem=rk)
            for fi, (fc0, M) in enumerate(FCH):
                if not (c0 <= fc0 < c0 + w):
                    continue
                for kc in range(8):
                    op('pe', mm(bps[0:M, fi * 2:fi * 2 + 2], r[:, kc, fc0 - c0:fc0 - c0 + M], mv3[:, kc, :], kc == 0, kc == 7),
                       reads=[rk, 'modv'], writes=['bps'])
            if pi >= 1:
                h = pi - 1
                for j in range(2):
                    for kc in range(8):
                        op('pe', mm(vps[:, j, h * 128:(h + 1) * 128], shb[:, j, kc, :], r[:, kc, RVt:RVt + 128], kc == 0, kc == 7),
                           reads=[rk, 'shb'], writes=['vps'])
            wbk, wck = 'wb%d' % (pi % 2), 'wc%d' % (pi % 2)
            for kc in range(8):
                op('dve', lambda e, kc=kc, r=r, w=w, pi=pi: e.tensor_scalar(out=wb[pi % 2][:, kc, :w], in0=r[:, kc, :w], scalar1=rs1[:, kc:kc + 1],
                                                                    scalar2=None, op0=ALU.mult), reads=[rk, 'rs1'], writes=[wbk])
                op('act', lambda e, kc=kc, r=r, w=w, pi=pi: e.activation(out=wc[pi % 2][:, kc, :w], in_=r[:, kc, :w], func=AF.Identity, scale=rs1[:, 8 + kc:9 + kc]),
                   reads=[rk, 'rs1'], writes=[wck])
            dma(WP[:, :, c0:c0 + w], wb[pi % 2][:, :, :w], reads=[wbk], writes=['WP'], sem='wpo%d' % (pi % 2))
            dma(WPC[:, :, c0:c0 + w], wc[pi % 2][:, :, :w], reads=[wck], writes=['WPC'], sem='wpc%d' % (pi % 2))
        op('dve', lambda e: e.tensor_copy(out=pbf[:].rearrange("p f j -> p (f j)"), in_=bps[:]), reads=['bps'], writes=['pbf'])
        op('dve', lambda e: e.tensor_copy(out=pbv[:], in_=vps[:]), reads=['vps'], writes=['pbv'])
        kb.barrier()

    GROUPS = [(0, 2)] + [(2 + 4 * g, 4) for g in range(16)]
    with contextlib.ExitStack() as st:
        xt = [sb("xt%d" % i, [128, 1024], F32, st) for i in range(3)]
        sqj = sb("sqj", [128, 1024], F32, st)
        ssq = [sb("ssq%d" % i, [128, 4], F32, st) for i in range(3)]
        xh = [sb("xh%d" % i, [128, 1024], BF16, st) for i in range(2)]
        xTb = [sb("xTb%d" % i, [128, 8, 512], BF16, st) for i in range(2)]
        tps = [ps("tps%d" % i, [128, 8, 128], BF16, st) for i in range(2)]
        tiles = [(gi, t0, nt, t) for gi, (t0, nt) in enumerate(GROUPS) for t in range(nt)]

        def p1_a(ti):
            gi, t0, nt, t = tiles[ti]
            tile = t0 + t
            a, bh = ti % 3, ti % 2
            xk, hk, pk = 'xt%d' % a, 'xh%d' % bh, 'tps%d' % bh
            dma(xt[a][:], xall[tile * 128:(tile + 1) * 128, :], writes=[xk], sem=xk)
            op('act', lambda e, a=a: e.activation(out=sqj[:], in_=xt[a][:], func=AF.Square, accum_out=ssq[a][:, 0:1]),
               reads=[xk], writes=['sqj', 'ss%d' % a])
            op('act', lambda e, a=a: e.activation(out=ssq[a][:, 1:2], in_=ssq[a][:, 0:1], func=AF.Sqrt, scale=1.0 / 1024, bias=EPS),
               reads=['ss%d' % a], writes=['sr%d' % a])
            op('dve', lambda e, a=a: e.reciprocal(out=ssq[a][:, 2:3], in_=ssq[a][:, 1:2]), reads=['sr%d' % a], writes=['rc%d' % a])
            op('dve', lambda e, a=a, bh=bh: e.tensor_scalar(out=xh[bh][:], in0=xt[a][:], scalar1=ssq[a][:, 2:3], scalar2=None, op0=ALU.mult),
               reads=[xk, 'rc%d' % a], writes=[hk])
            for kc in range(8):
                op('pe', lambda e, kc=kc, bh=bh: e.transpose(out=tps[bh][:, kc, :], in_=xh[bh][:, kc * 128:(kc + 1) * 128], identity=identb[:]),
                   reads=[hk, 'identb'], writes=[pk])

        def p1_b(ti):
            gi, t0, nt, t = tiles[ti]
            bh = ti % 2
            xb_ = xTb[gi % 2]
            xbk_ = 'xTb%d' % (gi % 2)
            op('dve', lambda e, bh=bh, t=t, xb_=xb_: e.tensor_copy(out=xb_[:, :, t * 128:(t + 1) * 128], in_=tps[bh][:]), reads=['tps%d' % bh], writes=[xbk_])
            if t == nt - 1:
                dma(XT[:, :, t0 * 128:(t0 + nt) * 128], xb_[:, :, :nt * 128], reads=[xbk_], writes=['XT'], sem='xto%d' % (gi % 2))

        p1_a(0)
        for ti in range(len(tiles)):
            if ti + 1 < len(tiles):
                p1_a(ti + 1)
            p1_b(ti)
        kb.barrier()

    if RUN_RET:
      with contextlib.ExitStack() as st:
        wsl = sb("wsl", [128, 8, 512], BF16, st)
        wslc = sb("wslc", [128, 8, 512], BF16, st)
        kT = sb("kT", [128, NKEY], BF16, st)
        Vr = sb("Vr", [128, 66, 128], BF16, st)
        qT = sb("qT", [128, NOWN], BF16, st)
        qTv = sb("qTv", [128, 3, NOWN], BF16, st)
        sgT = sb("sgT", [128, NOWN], BF16, st)
        xb = [sb("xb%d" % i, [128, 8, 512], BF16, st) for i in range(2)]
        tabc = [sb("tabc%d" % i, [64, 512], F32, st) for i in range(2)]
        tabs = [sb("tabs%d" % i, [64, 512], F32, st) for i in range(2)]
        ta = [sb("ta%d" % i, [64, 512], F32, st) for i in range(2)]
        tb = [sb("tb%d" % i, [64, 512], F32, st) for i in range(2)]
        uts = sb("uts", [128, 3, 512], F32, st)
        UT = sb("UT", [128, 3, 512], F32, st)
        dgs = sb("dgs", [128, 4, 3, 512], F32, st)
        bts = sb("bts", [128, 5, 256], F32, st)
        Bh = sb("Bh", [128, 5, 256], F32, st)
        mk = [sb("mk%d" % i, [128, 512], F32, st) for i in range(3)]
        mkx = sb("mkx", [128, 512], F32, st)
        Am = [sb("Am%d" % i, [128, 512], BF16, st) for i in range(3)]
        osb = sb("osb", [128, 512], F32, st)
        osq = sb("osq", [128, 512], BF16, st)
        orr = sb("orr", [128, 512], F32, st)
        omx = [sb("omx%d" % i, [128, 512], BF16, st) for i in range(2)]
        pk_ps = [ps("pk%d" % i, [64, 512], F32, st) for i in range(2)]
        pg_ps = ps("pg", [128, 512], F32, st)
        st_ps = [ps("stp%d" % i, [128, 512], F32, st) for i in range(3)]
        o_ps = ps("ops", [128, 512], F32, st)
        ss_ps = ps("ssp", [128, 512], F32, st)
        for i in range(3):
            dma(uts[:, i, :], utab[i, :, :], writes=['uts'], sem='c1')
        op('pool', lambda e: e.memset(kT[64:128, :], 0.0), writes=['kTz'])
        op('pool', lambda e: e.memset(qT[64:128, :], 0.0), writes=['qTz'])
        op('pool', lambda e: e.memset(qTv[64:128, :, :], 0.0), writes=['qTvz'])
        for r_ in range(4):
            for i in range(3):
                dma(dgs[:, r_, i, :], dgtab[r_, i, :, :], writes=['dgs'], sem='c1')
        dma(bts[:].rearrange("p c n -> p (c n)"), cwtab[:, :], writes=['bts'], sem='c1')
        pkc = [0]
        LGC = [0, 8, 0, 4, 4]
        for h in range(4):
            b0 = C_RET + h * 512
            dma(wsl[:], WP[:, :, b0:b0 + 512], writes=['wsl'], sem='wsl')
            dma(wslc[:], WPC[:, :, b0:b0 + 512], writes=['wslc'], sem='wslc')
            for cl in range(5):
                op('act', lambda e, cl=cl, h=h: e.activation(out=Bh[:, cl, :], in_=bts[:, cl, :], func=AF.Exp, scale=lg[:, LGC[cl] + h:LGC[cl] + h + 1]),
                   reads=['bts', 'lgf', 'lgb', 'lgo'], writes=['Bh'])
            op('dve', lambda e: e.tensor_scalar(out=Bh[:], in0=Bh[:], scalar1=0.125, scalar2=None, op0=ALU.mult), reads=['Bh'], writes=['Bh'])
            for ti_, lc in enumerate((0, 4, 8)):
                op('act', lambda e, ti_=ti_, lc=lc, h=h: e.activation(out=UT[:, ti_, :], in_=uts[:, ti_, :], func=AF.Exp, scale=lg[:, lc + h:lc + h + 1]),
                   reads=['uts', 'lgf', 'lgb', 'lgo'], writes=['UT'])
            fq, fqs, fk, fks, fg = (FIDX[b0 + RQ], FIDX[b0 + RQS], FIDX[b0 + RK], FIDX[b0 + RKS], FIDX[b0 + RGt])
            for gi, (t0, nt) in enumerate(GROUPS):
                nb = nt * 128
                tok0 = t0 * 128
                j = 1 if gi == 0 else 0
                W = wslc if gi == 0 else wsl
                Wk = 'wslc' if gi == 0 else 'wsl'
                xbb = xb[gi % 2]
                xk = 'xb%d' % (gi % 2)
                dma(xbb[:, :, :nb], XT[:, :, tok0:tok0 + nb], reads=['XT'], writes=[xk], sem=xk)
                tc_, ts_ = tabc[gi % 2], tabs[gi % 2]
                tk = 'tab%d' % (gi % 2)
                dma(tc_[:, :nb], rkcs[0, :, tok0:tok0 + nb], writes=[tk + 'c'], sem=tk + 'c')
                dma(ts_[:, :nb], rkcs[1, :, tok0:tok0 + nb], writes=[tk + 's'], sem=tk + 's')

                def rope_proj(c_a, c_b, f_a, f_b, dst, dkey, q0v=None, gi=gi, nb=nb, W=W, Wk=Wk, xbb=xbb, xk=xk, tc_=tc_, ts_=ts_, tk=tk, j=j):
                    if pkc[0] % 2 == 0:
                        pa, pak, pb, pbk = pk_ps[0], 'pk0', pk_ps[1], 'pk1'
                    else:
                        pa, pak, pb, pbk = st_ps[1], 'stp1', st_ps[2], 'stp2'
                    pkc[0] += 1
                    for kc in range(8):
                        op('pe', mm(pa[0:64, :nb], W[:, kc, c_a:c_a + 64], xbb[:, kc, :nb], kc == 0, kc == 7), reads=[Wk, xk], writes=[pak])
                    for kc in range(8):
                        op('pe', mm(pb[0:64, :nb], W[:, kc, c_b:c_b + 64], xbb[:, kc, :nb], kc == 0, kc == 7), reads=[Wk, xk], writes=[pbk])
                    a_, b_ = ta[gi % 2], tb[gi % 2]
                    op('dve', lambda e: e.scalar_tensor_tensor(out=a_[:, :nb], in0=pa[0:64, :nb], scalar=pbf[0:64, f_a, j:j + 1], in1=tc_[:, :nb],
                                                               op0=ALU.add, op1=ALU.mult), reads=[pak, 'pbf', tk + 'c'], writes=['ta%d' % (gi % 2)])
                    op('dve', lambda e: e.scalar_tensor_tensor(out=b_[:, :nb], in0=pb[0:64, :nb], scalar=pbf[0:64, f_b, j:j + 1], in1=ts_[:, :nb],
                                                               op0=ALU.add, op1=ALU.mult), reads=[pbk, 'pbf', tk + 's'], writes=['tb%d' % (gi % 2)])
                    if q0v is None:
                        op('pool', lambda e: e.tensor_tensor(out=dst, in0=a_[:, :nb], in1=b_[:, :nb], op=ALU.add),
                           reads=['ta%d' % (gi % 2), 'tb%d' % (gi % 2)], writes=[dkey])
                    else:
                        op('dve', lambda e: e.tensor_tensor(out=a_[:, :nb], in0=a_[:, :nb], in1=b_[:, :nb], op=ALU.add),
                           reads=['ta%d' % (gi % 2), 'tb%d' % (gi % 2)], writes=['ta%d' % (gi % 2)])
                        op('pool', lambda e: e.tensor_copy(out=dst, in_=a_[:, :nb]), reads=['ta%d' % (gi % 2)], writes=[dkey])
                        for v in range(3):
                            op('dve', lambda e, v=v: e.tensor_tensor(out=qTv[0:64, v, q0v:q0v + 512], in0=a_[:, :nb], in1=UT[0:64, v, :], op=ALU.mult),
                               reads=['ta%d' % (gi % 2), 'UT'], writes=['qTv'])

                rope_proj(RK, RKS, fk, fks, kT[0:64, tok0:tok0 + nb], 'kT')
                for t in range(nt):
                    for kc in range(8):
                        op('pe', mm(pg_ps[:, t * 128:(t + 1) * 128], xbb[:, kc, t * 128:(t + 1) * 128], W[:, kc, RVt:RVt + 128], kc == 0, kc == 7),
                           reads=[Wk, xk], writes=['pg'])
                op('dve', lambda e, t0=t0, nt=nt, j=j, h=h: e.tensor_tensor(
                    out=Vr[:, t0:t0 + nt, :], in0=pg_ps[:, :nt * 128].rearrange("p (t c) -> p t c", c=128),
                    in1=pbv[:, j, h * 128:(h + 1) * 128].unsqueeze(1).to_broadcast([128, nt, 128]), op=ALU.add),
                    reads=['pg', 'pbv'], writes=['Vr'])
                if gi >= 9:
                    q0 = (gi - 9) * 512
                    rope_proj(RQ, RQS, fq, fqs, qT[0:64, q0:q0 + 512], 'qT', q0v=q0)
                    for kc in range(8):
                        op('pe', mm(st_ps[0][:, :], W[:, kc, RGt:RGt + 128], xbb[:, kc, :], kc == 0, kc == 7), reads=[Wk, xk], writes=['stp0'])
                    op('act', lambda e, q0=q0, fg=fg: e.activation(out=sgT[:, q0:q0 + 512], in_=st_ps[0][:, :], func=AF.Silu, bias=pbf[:, fg, 0:1]),
                       reads=['stp0', 'pbf'], writes=['sgT'])
            ui = 0
            rfin = []
            for qb in range(8):
                units = [(0, kt, kt, 0) for kt in range(2)]
                units += [(1, kt, 2 + kt, 2) for kt in range(32)]
                for kt in range(32):
                    if kt < 4 * qb:
                        units.append((2, kt, 34 + kt, 0))
                    elif kt >= 4 * qb + 4:
                        units.append((3, kt, 34 + kt, 1))
                    else:
                        units.append((-1, kt, 34 + kt, kt - 4 * qb))
                units += [(4, kt, kt, 1) for kt in range(2)]
                LA = 2
                pend = []
                for n in range(len(units) + LA):
                    if n < len(units):
                        (cl, kt, ktile, tidx) = units[n]
                        sp_, m_, a_ = st_ps[ui % 3], mk[ui % 3], Am[ui % 3]
                        spk, mkk, ak = 'stp%d' % (ui % 3), 'mk%d' % (ui % 3), 'Am%d' % (ui % 3)
                        qop = qTv[:, tidx, qb * 512:(qb + 1) * 512] if cl >= 0 else qT[:, qb * 512:(qb + 1) * 512]
                        op('pe', mm(sp_[:, :], kT[:, ktile * 128:(ktile + 1) * 128], qop), reads=['kT', 'qT', 'qTv', 'kTz', 'qTz', 'qTvz'], writes=[spk])
                        if cl >= 0:
                            if ui % 2 == 0:
                                op('dve', lambda e, a_=a_, sp_=sp_, cl=cl, qb=qb, kt=kt: e.tensor_scalar(
                                    out=a_[:], in0=sp_[:, :], scalar1=Bh[:, cl, qb * 32 + kt:qb * 32 + kt + 1], scalar2=None, op0=ALU.mult),
                                    reads=[spk, 'Bh'], writes=[ak])
                            else:
                                op('act', lambda e, a_=a_, sp_=sp_, cl=cl, qb=qb, kt=kt: e.activation(
                                    out=a_[:], in_=sp_[:, :], func=AF.Identity, scale=Bh[:, cl, qb * 32 + kt:qb * 32 + kt + 1]),
                                    reads=[spk, 'Bh'], writes=[ak])
                        else:
                            op('act', lambda e, m_=m_, tidx=tidx, h=h: e.activation(out=m_[:], in_=dgs[:, tidx, 0, :], func=AF.Exp, scale=lg[:, h:h + 1]),
                               reads=['dgs', 'lgf'], writes=[mkk])
                            op('act', lambda e, tidx=tidx, h=h: e.activation(out=mkx[:], in_=dgs[:, tidx, 1, :], func=AF.Exp, scale=lg[:, 4 + h:5 + h]),
                               reads=['dgs', 'lgb'], writes=['mkx'])
                            op('pool', lambda e, m_=m_: e.tensor_tensor(out=m_[:], in0=m_[:], in1=mkx[:], op=ALU.add), reads=[mkk, 'mkx'], writes=[mkk])
                            op('pool', lambda e, m_=m_, tidx=tidx: e.tensor_tensor(out=m_[:], in0=m_[:], in1=dgs[:, tidx, 2, :], op=ALU.add),
                               reads=[mkk, 'dgs'], writes=[mkk])
                            op('dve', lambda e, a_=a_, sp_=sp_, m_=m_: e.scalar_tensor_tensor(out=a_[:], in0=sp_[:, :], scalar=0.125, in1=m_[:],
                                                                                             op0=ALU.mult, op1=ALU.mult), reads=[spk, mkk], writes=[ak])
                        pend.append((n, ktile, a_, ak))
                        ui += 1
                        if rfin and n % 6 == 5:
                            rfin.pop(0)()
                    if n >= LA:
                        (n0, ktile0, a0, ak0) = pend.pop(0)
                        op('pe', mm(o_ps[:, :], Vr[:, ktile0, :], a0[:], n0 == 0, n0 == len(units) - 1), reads=['Vr', ak0], writes=['ops'])
                op('act', lambda e: e.copy(out=osb[:], in_=o_ps[:, :]), reads=['ops'], writes=['osb'])

                def fin_steps(h=h, qb=qb):
                    mx = omx[qb % 2]
                    mxk = 'omx%d' % (qb % 2)
                    return [
                        lambda: op('dve', lambda e: e.tensor_tensor(out=osq[:], in0=osb[:], in1=osb[:], op=ALU.mult), reads=['osb'], writes=['osq']),
                        lambda: op('pe', mm(ss_ps[:, :], onesb[:], osq[:]), reads=['onesb', 'osq'], writes=['ssp']),
                        lambda: op('act', lambda e: e.activation(out=orr[:], in_=ss_ps[:, :], func=AF.Sqrt, scale=1.0 / 128, bias=EPS), reads=['ssp'], writes=['orr']),
                        lambda: op('dve', lambda e: e.reciprocal(out=orr[:], in_=orr[:]), reads=['orr'], writes=['orr']),
                        lambda: op('dve', lambda e: e.scalar_tensor_tensor(out=osb[:], in0=osb[:], scalar=sv[:, 7 + h:8 + h], in1=orr[:], op0=ALU.mult, op1=ALU.mult),
                                   reads=['osb', 'orr', 'sv'], writes=['osb']),
                        lambda: (op('dve', lambda e: e.tensor_tensor(out=mx[:], in0=osb[:], in1=sgT[:, qb * 512:(qb + 1) * 512], op=ALU.mult), reads=['osb', 'sgT'], writes=[mxk]),
                                 dma(MIXT[512 + h * 128:512 + (h + 1) * 128, qb * 512:(qb + 1) * 512], mx[:], reads=[mxk], writes=['MIXT'], sem=mxk)),
                    ]
                rfin.extend(fin_steps())
                if qb == 7:
                    while rfin:
                        rfin.pop(0)()
        kb.barrier()
    if RUN_MLA:
      with contextlib.ExitStack() as st:
        ckvT = sb("ckvT", [128, NKEY], BF16, st)
        KT = [sb("KT%d" % i, [96, NKEY], BF16, st) for i in range(2)]
        cqT = sb("cqT", [128, 2, NOWN], BF16, st)
        sspe = sb("sspe", [128, 66], F32, st)
        wqb = sb("wqb", [128, 2, 1536], BF16, st)
        wkb = sb("wkb", [128, 1024], BF16, st)
        fkv, fpe, fpesw, fq0, fq1 = FIDX[C_CKV], FIDX[C_PE], FIDX[C_PESW], FIDX[C_CQ], FIDX[C_CQ + 128]
        with contextlib.ExitStack() as s2:
            wm = sb("wm", [128, 8, 576], BF16, s2)
            wmc = sb("wmc", [128, 8, 576], BF16, s2)
            wqr = sb("wqr", [128, 2, 1536], F32, s2)
            wkr = sb("wkr", [128, 1024], F32, s2)
            xb = [sb("mxb%d" % i, [128, 8, 512], BF16, s2) for i in range(2)]
            pkv = sb("pkv", [128, 512], F32, s2)
            sqv = sb("sqv", [128, 512], BF16, s2)
            srt = sb("srt", [128, 512], F32, s2)
            rawpe = sb("rawpe", [96, 512], F32, s2)
            rawsw = sb("rawsw", [96, 512], F32, s2)
            sqpe = sb("sqpe", [96, 512], BF16, s2)
            tcm = [sb("tcm%d" % i, [96, 512], F32, s2) for i in range(2)]
            tsm = [sb("tsm%d" % i, [96, 512], F32, s2) for i in range(2)]
            pa_ = sb("pa_", [96, 512], F32, s2)
            pb_ = sb("pb_", [96, 512], F32, s2)
            pq = sb("pq", [128, 2, 512], F32, s2)
            sq2 = sb("sq2", [128, 2, 512], BF16, s2)
            pA = ps("pA", [128, 512], F32, s2)
            pB = ps("pB", [96, 512], F32, s2)
            pC = ps("pC", [96, 512], F32, s2)
            ssb = ps("ssb", [128, 512], F32, s2)
            pss = ps("pss", [128, 66], F32, s2)
            pQ = [ps("pQ%d" % i, [128, 512], F32, s2) for i in range(2)]
            dma(wm[:], WP[:, :, 0:576], writes=['wm'], sem='c2')
            dma(wmc[:], WPC[:, :, 0:576], writes=['wmc'], sem='c2')
            dma(wqr[:], wq_ext.rearrange("(c p) n -> p c n", p=128), writes=['wqr'], sem='c2')
            dma(wkr[:, 0:512], wkv_k[:, :], writes=['wkr'], sem='c2')
            dma(wkr[:, 512:1024], wkv_v[:, :], writes=['wkr'], sem='c2')
            for c in range(2):
                op('dve', lambda e, c=c: e.tensor_scalar(out=wqb[:, c, :], in0=wqr[:, c, :], scalar1=sv[:, c:c + 1], scalar2=None, op0=ALU.mult),
                   reads=['wqr', 'sv'], writes=['wqb'])
            op('dve', lambda e: e.tensor_scalar(out=wkb[:], in0=wkr[:], scalar1=sv[:, 2:3], scalar2=None, op0=ALU.mult),
               reads=['wkr', 'sv'], writes=['wkb'])
            for gi, (t0, nt) in enumerate(GROUPS):
                nb, tok0 = nt * 128, t0 * 128
                j = 1 if gi == 0 else 0
                W, Wk = (wmc, 'wmc') if gi == 0 else (wm, 'wm')
                xbb, xk = xb[gi % 2], 'mxb%d' % (gi % 2)
                dma(xbb[:, :, :nb], XT[:, :, tok0:tok0 + nb], reads=['XT'], writes=[xk], sem=xk)
                tc_, ts_, tk = tcm[gi % 2], tsm[gi % 2], 'mtab%d' % (gi % 2)
                dma(tc_[64:96, :nb], kcs[0, :, tok0:tok0 + nb], writes=[tk + 'c'], sem=tk + 'c')
                dma(ts_[64:96, :nb], kcs[1, :, tok0:tok0 + nb], writes=[tk + 's'], sem=tk + 's')
                for kc in range(8):
                    op('pe', mm(pA[:, :nb], W[:, kc, C_CKV:C_CKV + 128], xbb[:, kc, :nb], kc == 0, kc == 7), reads=[Wk, xk], writes=['pA'])
                op('act', lambda e, nb=nb, j=j: e.activation(out=pkv[:, :nb], in_=pA[:, :nb], func=AF.Identity, bias=pbf[:, fkv, j:j + 1]),
                   reads=['pA', 'pbf'], writes=['pkv'])
                op('pool', lambda e, nb=nb: e.tensor_tensor(out=sqv[:, :nb], in0=pkv[:, :nb], in1=pkv[:, :nb], op=ALU.mult), reads=['pkv'], writes=['sqv'])
                op('pe', mm(ssb[:, :nb], onesb[:], sqv[:, :nb]), reads=['onesb', 'sqv'], writes=['ssb'])
                op('act', lambda e, nb=nb: e.activation(out=srt[:, :nb], in_=ssb[:, :nb], func=AF.Sqrt, scale=1.0 / 128, bias=EPS), reads=['ssb'], writes=['srt'])
                op('dve', lambda e, nb=nb: e.reciprocal(out=srt[:, :nb], in_=srt[:, :nb]), reads=['srt'], writes=['srt'])
                op('dve', lambda e, nb=nb, tok0=tok0: e.tensor_tensor(out=ckvT[:, tok0:tok0 + nb], in0=pkv[:, :nb], in1=srt[:, :nb], op=ALU.mult),
                   reads=['pkv', 'srt'], writes=['ckvT'])
                for kc in range(8):
                    op('pe', mm(pB[:, :nb], W[:, kc, C_PE:C_PE + 96], xbb[:, kc, :nb], kc == 0, kc == 7), reads=[Wk, xk], writes=['pB'])
                for kc in range(8):
                    op('pe', mm(pC[:, :nb], W[:, kc, C_PESW:C_PESW + 96], xbb[:, kc, :nb], kc == 0, kc == 7), reads=[Wk, xk], writes=['pC'])
                op('act', lambda e, nb=nb, j=j: e.activation(out=rawpe[64:96, :nb], in_=pB[64:96, :nb], func=AF.Identity, bias=pbf[64:96, fpe, j:j + 1]),
                   reads=['pB', 'pbf'], writes=['rawpe'])
                op('act', lambda e, nb=nb, j=j: e.activation(out=rawsw[64:96, :nb], in_=pC[64:96, :nb], func=AF.Identity, bias=pbf[64:96, fpesw, j:j + 1]),
                   reads=['pC', 'pbf'], writes=['rawsw'])
                op('pool', lambda e, nb=nb: e.tensor_tensor(out=sqpe[64:96, :nb], in0=rawpe[64:96, :nb], in1=rawpe[64:96, :nb], op=ALU.mult),
                   reads=['rawpe'], writes=['sqpe'])
                for t in range(nt):
                    op('pe', mm(pss[:, t0 + t:t0 + t + 1], sqpe[64:96, t * 128:(t + 1) * 128], onesb[64:96, 0:1]), reads=['sqpe', 'onesb'], writes=['pss'])
                op('dve', lambda e, nb=nb, tc_=tc_: e.scalar_tensor_tensor(out=pa_[64:96, :nb], in0=rawpe[64:96, :nb], scalar=sv[64:96, 5:6], in1=tc_[64:96, :nb],
                                                                      op0=ALU.mult, op1=ALU.mult), reads=['rawpe', 'sv', tk + 'c'], writes=['pa_'])
                op('dve', lambda e, nb=nb, ts_=ts_: e.scalar_tensor_tensor(out=pb_[64:96, :nb], in0=rawsw[64:96, :nb], scalar=sv[64:96, 6:7], in1=ts_[64:96, :nb],
                                                                      op0=ALU.mult, op1=ALU.mult), reads=['rawsw', 'sv', tk + 's'], writes=['pb_'])
                op('pool', lambda e, nb=nb, tok0=tok0: e.tensor_tensor(out=KT[0][64:96, tok0:tok0 + nb], in0=pa_[64:96, :nb], in1=pb_[64:96, :nb], op=ALU.add),
                   reads=['pa_', 'pb_'], writes=['KT0pe'])
                op('pool', lambda e, nb=nb, tok0=tok0: e.tensor_copy(out=KT[1][64:96, tok0:tok0 + nb], in_=KT[0][64:96, tok0:tok0 + nb]),
                   reads=['KT0pe'], writes=['KT1pe'])
                if gi >= 9:
                    q0 = (gi - 9) * 512
                    for c in range(2):
                        for kc in range(8):
                            op('pe', mm(pQ[c][:, :], W[:, kc, C_CQ + c * 128:C_CQ + (c + 1) * 128], xbb[:, kc, :], kc == 0, kc == 7),
                               reads=[Wk, xk], writes=['pQ%d' % c])
                        op('act', lambda e, c=c: e.activation(out=pq[:, c, :], in_=pQ[c][:, :], func=AF.Identity, bias=pbf[:, fq0 + c, 0:1]),
                           reads=['pQ%d' % c, 'pbf'], writes=['pq%d' % c])
                        op('pool', lambda e, c=c: e.tensor_tensor(out=sq2[:, c, :], in0=pq[:, c, :], in1=pq[:, c, :], op=ALU.mult),
                           reads=['pq%d' % c], writes=['sq2%d' % c])
                    for c in range(2):
                        op('pe', mm(ssb[:, :], onesb[:], sq2[:, c, :], c == 0, c == 1), reads=['onesb', 'sq2%d' % c], writes=['ssb'])
                    op('act', lambda e: e.activation(out=srt[:, :], in_=ssb[:, :], func=AF.Sqrt, scale=1.0 / 256, bias=EPS), reads=['ssb'], writes=['srt'])
                    op('dve', lambda e: e.reciprocal(out=srt[:, :], in_=srt[:, :]), reads=['srt'], writes=['srt'])
                    for c in range(2):
                        op('dve', lambda e, c=c, q0=q0: e.tensor_tensor(out=cqT[:, c, q0:q0 + 512], in0=pq[:, c, :], in1=srt[:, :], op=ALU.mult),
                           reads=['pq%d' % c, 'srt'], writes=['cqT'])
            op('dve', lambda e: e.tensor_copy(out=sspe[:], in_=pss[:, :]), reads=['pss'], writes=['sspe'])
            kb.barrier()
        with contextlib.ExitStack() as s2:
            QT = [sb("QT%d" % i, [96, NOWN], BF16, s2) for i in range(2)]
            Vh = [sb("Vh%d" % i, [128, 66, 65], BF16, s2) for i in range(2)]
            skh = [sb("skh%d" % i, [128, 66], F32, s2) for i in range(2)]
            sqk = [sb("sqk%d" % i, [64, 512], BF16, s2) for i in range(2)]
            qraw = sb("qraw", [96, 512], F32, s2)
            sqq = sb("sqq", [96, 512], BF16, s2)
            rq = sb("rq", [96, 512], F32, s2)
            qc_ = [sb("qc%d" % i, [96, 512], F32, s2) for i in range(2)]
            qs_ = [sb("qs%d" % i, [96, 512], F32, s2) for i in range(2)]
            qa_ = sb("qa_", [96, 512], F32, s2)
            qb_ = sb("qb_", [96, 512], F32, s2)
            pT = [sb("pT%d" % i, [128, 512], BF16, s2) for i in range(3)]
            ot = sb("ot", [65, 512], F32, s2)
            rec = sb("rec", [65, 512], F32, s2)
            mixh = [sb("mixh%d" % i, [64, 512], BF16, s2) for i in range(2)]
            kn_ps = ps("knp", [64, 512], F32, s2)
            bcp = ps("bcp", [64, 512], F32, s2)
            pvs = ps("pvs", [128, 512], F32, s2)
            pV = pvs[:, 0:256].rearrange("p (t c) -> p t c", c=64)
            pss2 = pvs[:, 256:322]
            qp = ps("qp", [96, 512], F32, s2)
            qsp = ps("qsp", [96, 512], F32, s2)
            stp = [ps("mst%d" % i, [128, 512], F32, s2) for i in range(2)]
            o_ps = ps("mo", [65, 512], F32, s2)
            for i in range(2):
                op('pool', lambda e, i=i: e.memset(Vh[i][:, :, 64:65], 1.0), writes=['Vh%d' % i])
            cs1 = [sb("cs1%d" % i, [128, 2048], F32, s2) for i in range(2)]
            cc1 = [sb("cc1%d" % i, [128, 2, 1024], BF16, s2) for i in range(2)]
            cs2 = [sb("cs2%d" % i, [128, 1024], F32, s2) for i in range(2)]
            cc2 = [sb("cc2%d" % i, [128, 1024], BF16, s2) for i in range(2)]
            zt = sb("zt", [128, 2048], BF16, s2)
            op('pool', lambda e: e.memset(zt[:], 0.0), writes=['zt'])
            XSz = XS.rearrange("(a p r) n -> a p (r n)", p=128, r=2)
            for a in range(160 * 128 // 256):
                dma(XSz[a], zt[:], reads=['zt'], writes=['XS'], sem='xsz')
            cast_ld = [0]
            cast_dn = [0]

            def cast_load(n):
                ex, kc = n // 8, n % 8
                i4 = n % 2
                dma(cs1[i4][:], w1[ex, kc * 128:(kc + 1) * 128, :], writes=['cs1%d' % i4], sem='cs1%d' % i4)
                dma(cs2[i4][:], w2[ex, kc * 128:(kc + 1) * 128, :], writes=['cs2%d' % i4], sem='cs2%d' % i4)

            def cast_do(n):
                ex, kc = n // 8, n % 8
                i4 = n % 2
                a_, ak, b_, bk = cs1[i4], 'cs1%d' % i4, cc1[i4], 'cc1%d' % i4
                c_, ck, d_, dk = cs2[i4], 'cs2%d' % i4, cc2[i4], 'cc2%d' % i4
                op('dve', lambda e: e.tensor_copy(out=b_[:], in_=a_[:].rearrange("p (f g) -> p g f", g=2)), reads=[ak], writes=[bk])
                dma(W1R[ex * 128:(ex + 1) * 128, kc * 2048:(kc + 1) * 2048], b_[:].rearrange("p g f -> p (g f)"), reads=[bk], writes=['W1R'], sem=bk)
                op('dve', lambda e: e.tensor_tensor(out=d_[:], in0=c_[:], in1=g2gs_bc[:], op=ALU.mult), reads=[ck, 'g2gs_bc'], writes=[dk])
                dma(W2R[ex * 128:(ex + 1) * 128, kc * 1024:(kc + 1) * 1024], d_[:], reads=[dk], writes=['W2R'], sem=dk)

            def cast_tick(flush=False):
                if cast_dn[0] < cast_ld[0] and (flush or cast_dn[0] < cast_ld[0] - 0):
                    pass
                if cast_ld[0] < 256:
                    cast_load(cast_ld[0])
                    cast_ld[0] += 1
                    if cast_dn[0] < cast_ld[0] - 1:
                        cast_do(cast_dn[0])
                        cast_dn[0] += 1
                elif cast_dn[0] < 256:
                    cast_do(cast_dn[0])
                    cast_dn[0] += 1

            ui = [0]
            deferred = []

            def gen_steps(h):
                hb = h % 2
                KTh, ktk = KT[hb], 'KT%d' % hb
                vk, sk_k, qtk = 'Vh%d' % hb, 'skh%d' % hb, 'QT%d' % hb
                sk_ = skh[hb]
                steps = []

                def kstep_a(gi, t0, nt):
                    nb, tok0 = nt * 128, t0 * 128
                    kp, kpk = kn_ps, 'knp'
                    sq_, sqkk = sqk[gi % 2], 'sqk%d' % (gi % 2)
                    op('pe', mm(kp[:, :nb], wkb[:, h * 64:(h + 1) * 64], ckvT[:, tok0:tok0 + nb]), reads=['wkb', 'ckvT'], writes=[kpk])
                    for t in range(nt):
                        op('pe', mm(pV[:, t, :], ckvT[:, tok0 + t * 128:tok0 + (t + 1) * 128], wkb[:, 512 + h * 64:512 + (h + 1) * 64]), reads=['ckvT', 'wkb'], writes=['pV'])

                def kstep_b(gi, t0, nt):
                    nb, tok0 = nt * 128, t0 * 128
                    kp, kpk = kn_ps, 'knp'
                    sq_, sqkk = sqk[gi % 2], 'sqk%d' % (gi % 2)
                    op('act', lambda e: e.activation(out=KTh[0:64, tok0:tok0 + nb], in_=kp[:, :nb], func=AF.Identity, scale=sv[0:64, 5:6]), reads=[kpk, 'sv'], writes=[ktk])
                    op('act', lambda e: e.activation(out=sq_[:, :nb], in_=kp[:, :nb], func=AF.Square), reads=[kpk], writes=[sqkk])
                    op('dve', lambda e: e.tensor_copy(out=Vh[hb][:, t0:t0 + nt, 0:64], in_=pV[:, 0:nt, :]), reads=['pV'], writes=[vk])

                def kstep_c(gi, t0, nt):
                    sq_, sqkk = sqk[gi % 2], 'sqk%d' % (gi % 2)
                    for t in range(nt):
                        op('pe', mm(pss2[:, t0 + t:t0 + t + 1], sq_[0:64, t * 128:(t + 1) * 128], onesb[0:64, 0:1]), reads=[sqkk, 'onesb'], writes=['pss2'])

                for gi, (t0, nt) in enumerate(GROUPS):
                    steps.append(lambda gi=gi, t0=t0, nt=nt: kstep_a(gi, t0, nt))
                    steps.append(lambda gi=gi, t0=t0, nt=nt: kstep_b(gi, t0, nt))
                    steps.append(lambda gi=gi, t0=t0, nt=nt: kstep_c(gi, t0, nt))

                steps.append(lambda: op('dve', lambda e: e.tensor_tensor(out=sk_[:], in0=pss2[:, :], in1=sspe[:], op=ALU.add), reads=['pss2', 'sspe'], writes=[sk_k]))
                steps.append(lambda: op('act', lambda e: e.activation(out=sk_[:], in_=sk_[:], func=AF.Sqrt, scale=1.0 / 96, bias=EPS), reads=[sk_k], writes=[sk_k]))

                def scale_c():
                    op('dve', lambda e: e.reciprocal(out=sk_[:], in_=sk_[:]), reads=[sk_k], writes=[sk_k])
                    op('dve', lambda e: e.tensor_scalar(out=sk_[:], in0=sk_[:], scalar1=float(96 ** -0.5), scalar2=None, op0=ALU.mult), reads=[sk_k], writes=[sk_k])
                steps.append(scale_c)

                def q_a(qb):
                    q0 = qb * 512
                    tq = qb % 2
                    dma(qc_[tq][64:96, :], qcs[0, :, q0:q0 + 512], writes=['qc%d' % tq], sem='qtabc%d' % tq)
                    dma(qs_[tq][64:96, :], qcs[1, :, q0:q0 + 512], writes=['qs%d' % tq], sem='qtabs%d' % tq)
                    for c in range(2):
                        op('pe', mm(qp[:, :], wqb[:, c, (h * 2) * 96:(h * 2) * 96 + 96], cqT[:, c, q0:q0 + 512], c == 0, c == 1), reads=['wqb', 'cqT'], writes=['qp'])
                    for c in range(2):
                        op('pe', mm(qsp[:, :], wqb[:, c, (h * 2 + 1) * 96:(h * 2 + 1) * 96 + 96], cqT[:, c, q0:q0 + 512], c == 0, c == 1), reads=['wqb', 'cqT'], writes=['qsp'])

                def q_b(qb):
                    tq = qb % 2
                    op('act', lambda e: e.copy(out=qraw[:], in_=qp[:, :]), reads=['qp'], writes=['qraw'])
                    op('dve', lambda e: e.scalar_tensor_tensor(out=qb_[64:96, :], in0=qsp[64:96, :], scalar=sv[64:96, 4:5], in1=qs_[tq][64:96, :],
                                                               op0=ALU.mult, op1=ALU.mult), reads=['qsp', 'sv', 'qs%d' % tq], writes=['qb_'])

                def q_c(qb):
                    tq = qb % 2
                    op('pool', lambda e: e.tensor_tensor(out=sqq[:], in0=qraw[:], in1=qraw[:], op=ALU.mult), reads=['qraw'], writes=['sqq'])
                    op('dve', lambda e: e.scalar_tensor_tensor(out=qa_[64:96, :], in0=qraw[64:96, :], scalar=sv[64:96, 3:4], in1=qc_[tq][64:96, :],
                                                               op0=ALU.mult, op1=ALU.mult), reads=['qraw', 'sv', 'qc%d' % tq], writes=['qa_'])

                def q_d(qb):
                    op('pe', mm(qp[:, :], onesb[0:96, 0:96], sqq[:]), reads=['onesb', 'sqq', 'qraw'], writes=['qp'])
                    op('pool', lambda e: e.tensor_tensor(out=qa_[64:96, :], in0=qa_[64:96, :], in1=qb_[64:96, :], op=ALU.add), reads=['qa_', 'qb_'], writes=['qa_'])

                def q_e(qb):
                    op('act', lambda e: e.activation(out=rq[:], in_=qp[:, :], func=AF.Sqrt, scale=1.0 / 96, bias=EPS), reads=['qp'], writes=['rq'])

                def q_f(qb):
                    op('dve', lambda e: e.reciprocal(out=rq[:], in_=rq[:]), reads=['rq'], writes=['rq'])

                def q_g(qb):
                    q0 = qb * 512
                    op('dve', lambda e: e.scalar_tensor_tensor(out=QT[hb][0:64, q0:q0 + 512], in0=qraw[0:64, :], scalar=sv[0:64, 3:4], in1=rq[0:64, :],
                                                               op0=ALU.mult, op1=ALU.mult), reads=['qraw', 'sv', 'rq'], writes=[qtk])
                    op('dve', lambda e: e.tensor_tensor(out=QT[hb][64:96, q0:q0 + 512], in0=qa_[64:96, :], in1=rq[64:96, :], op=ALU.mult),
                       reads=['qa_', 'rq'], writes=[qtk])

                for qb in range(8):
                    for f_ in (q_a, q_b, q_c, q_d, q_e, q_f, q_g):
                        steps.append(lambda qb=qb, f_=f_: f_(qb))
                return steps

            def attend(h, nxt):
                hb = h % 2
                KTh, ktk = KT[hb], 'KT%d' % hb
                vk, sk_k, qtk = 'Vh%d' % hb, 'skh%d' % hb, 'QT%d' % hb
                sk_ = skh[hb]
                every = max(1, (8 * 66) // (len(nxt) + 1)) if nxt else 0
                ucount = 0
                for qb in range(8):
                    q0 = qb * 512
                    pend = []
                    for kt in range(66 + 1):
                        if kt < 66:
                            u = ui[0]
                            sp_, spk = stp[u % 2], 'mst%d' % (u % 2)
                            p_, pk = pT[u % 3], 'pT%d' % (u % 3)
                            op('pe', mm(sp_[:, :], KTh[0:96, kt * 128:(kt + 1) * 128], QT[hb][0:96, q0:q0 + 512]), reads=[ktk, 'KT%dpe' % hb, qtk], writes=[spk])
                            op('act', lambda e, p_=p_, sp_=sp_, kt=kt: e.activation(out=p_[:], in_=sp_[:, :], func=AF.Exp, scale=sk_[:, kt:kt + 1]),
                               reads=[spk, sk_k], writes=[pk])
                            pend.append((kt, p_, pk))
                            ui[0] += 1
                            ucount += 1
                            if nxt and ucount % every == 0:
                                nxt.pop(0)()
                            if ucount % 16 == 8:
                                cast_tick()
                            if kt == 8 and deferred:
                                deferred.pop(0)()
                        if kt >= 1:
                            (k0, p0, pk0) = pend.pop(0)
                            op('pe', mm(o_ps[:, :], Vh[hb][:, k0, 0:65], p0[:], k0 == 0, k0 == 65), reads=[vk, pk0], writes=['mo'])
                    op('dve', lambda e: e.tensor_copy(out=ot[:], in_=o_ps[:, :]), reads=['mo'], writes=['ot'])
                    op('dve', lambda e: e.reciprocal(out=rec[64:65, :], in_=ot[64:65, :]), reads=['ot'], writes=['rec'])

                    def fin(h=h, qb=qb, q0=q0):
                        mh, mhk = mixh[qb % 2], 'mixh%d' % (qb % 2)
                        op('pe', mm(bcp[0:64, :], onesf[64:65, 0:64], rec[64:65, :]), reads=['onesf', 'rec'], writes=['bcp'])
                        op('dve', lambda e, mh=mh: e.tensor_tensor(out=mh[:], in0=ot[0:64, :], in1=bcp[0:64, :], op=ALU.mult), reads=['ot', 'bcp'], writes=[mhk])
                        dma(MIXT[h * 64:(h + 1) * 64, q0:q0 + 512], mh[:], reads=[mhk], writes=['MIXT'], sem=mhk)
                    deferred.append(fin)
                while nxt:
                    nxt.pop(0)()
                if h == 7:
                    while deferred:
                        deferred.pop(0)()

            for st_ in gen_steps(0):
                st_()
            for h in range(8):
                attend(h, gen_steps(h + 1) if h + 1 < 8 else [])
            while cast_dn[0] < 256:
                cast_tick()
            kb.barrier()
    if RUN_REST:
      NBLK = 160
      NPAD = NBLK * 128
      GK = sb("GK", [128, 32, 4], F32)
      DSTi = sb("DSTi", [128, 128], I32)
      IDXW = sb("IDXW", [128, NBLK], I32)
      IDXB = sb("IDXB", [128, NBLK], I32)
      XOWN = 256 + NOWN
      with contextlib.ExitStack() as st:
        g1_bc = sb("g1_bc", [128, 1024], F32, st)
        g2s_bc = sb("g2s_bc", [128, 1024], F32, st)
        sh2_bc = sb("sh2_bc", [128, 1024], F32, st)
        g2sv = sb("g2sv", [128, 8], F32, st)
        dg_ = [sb("dg_%d" % i, [128, 128], F32, st) for i in range(2)]
        Wo = sb("Wo", [128, 8, 1024], BF16, st)
        Wr = sb("Wr", [128, 8, 32], BF16, st)
        Wrf = sb("Wrf", [128, 8, 32], F32, st)
        brb = sb("brb", [128, 32], F32, st)
        OHall = sb("OHall", [128, 32, 4, 32], BF16, st)
        Rall = sb("Rall", [128, 32, 32], F32, st)
        CUM = sb("CUM", [128, 32], F32, st)
        ustr = sb("ustr", [128, 128], BF16, st)
        ustrf = sb("ustrf", [128, 128], F32, st)
        tri = sb("tri", [32, 64], F32, st)
        rtab = sb("rtab", [128, NBLK + 1], F32, st)
        yps = [ps("yps%d" % i, [128, 1024], F32, st) for i in range(2)]
        tp4 = [ps("tp4%d" % i, [128, 8, 128], BF16, st) for i in range(2)]
        lgp = ps("lgp", [128, 32], F32, st)
        rkp = ps("rkp", [128, 64], F32, st)
        with contextlib.ExitStack() as s2:
            wof = sb("wof", [128, 8, 1024], F32, s2)
            dma(wof[:], w_out.rearrange("(c p) n -> p c n", p=128), writes=['wof'], sem='c3')
            for c in range(8):
                op('pool' if c % 2 else 'dve', lambda e, c=c: e.tensor_copy(out=Wo[:, c, :], in_=wof[:, c, :]), reads=['wof'], writes=['Wo'])
            dma(Wrf[:], w_router.rearrange("(c p) n -> p c n", p=128), writes=['Wrf'], sem='c3')
            op('dve', lambda e: e.tensor_copy(out=Wr[:], in_=Wrf[:]), reads=['Wrf'], writes=['Wr'])
            dma(brb[:], br_bc[:, :], writes=['brb'], sem='c3')
            dma(ustrf[:], ustrict[:, :], writes=['ustrf'], sem='c3')
            op('dve', lambda e: e.tensor_copy(out=ustr[:], in_=ustrf[:]), reads=['ustrf'], writes=['ustr'])
            dma(tri[:], tri32[:, :], writes=['tri'], sem='c3')
            dma(rtab[:], routetab[:, :], writes=['rtab'], sem='c3')
            op('pool', lambda e: e.memset(CUM[:], 0.0), writes=['CUM'])
            op('dve', lambda e: e.scalar_tensor_tensor(out=g2sv[:], in0=mv3[:, 32:40, 0], scalar=1.0, in1=gv[:, 8:16], op0=ALU.add, op1=ALU.mult),
               reads=['modv', 'gv'], writes=['g2sv'])
            di = 0
            for (dst, dkey, vec) in ((g1_bc, 'g1_bc', lambda kc: mv3[:, 16 + kc, 0:1]), (g2s_bc, 'g2s_bc', lambda kc: g2sv[:, kc:kc + 1]),
                                     (sh2_bc, 'sh2_bc', lambda kc: mv3[:, 24 + kc, 0:1])):
                for kc in range(8):
                    d_, dk = dg_[di % 2], 'dg_%d' % (di % 2)
                    op('dve', lambda e, d_=d_, vec=vec, kc=kc: e.tensor_scalar(out=d_[:], in0=ident[:], scalar1=vec(kc), scalar2=None, op0=ALU.mult),
                       reads=['ident', 'modv', 'g2sv'], writes=[dk])
                    op('pe', mm(yps[0][:, kc * 128:(kc + 1) * 128], onesf[:], d_[:]), reads=['onesf', dk], writes=['yps0'])
                    di += 1
                op('act', lambda e, dst=dst: e.copy(out=dst[:], in_=yps[0][:, :]), reads=['yps0'], writes=[dkey])
            kb.barrier()
        mt = [sb("mt%d" % i, [128, 8, 128], BF16, st) for i in range(2)]
        xo = [sb("xo%d" % i, [128, 1024], F32, st) for i in range(2)]
        x1t = [sb("x1t%d" % i, [128, 1024], F32, st) for i in range(2)]
        tmp4 = sb("tmp4", [128, 1024], F32, st)
        sq4 = sb("sq4", [128, 1024], F32, st)
        st4 = [sb("st4%d" % i, [128, 4], F32, st) for i in range(2)]
        hfb = [sb("hfb%d" % i, [128, 1024], BF16, st) for i in range(2)]
        hT = [sb("hT%d" % i, [128, 8, 128], BF16, st) for i in range(2)]
        lgt = [sb("lgt%d" % i, [128, 32], F32, st) for i in range(2)]
        mx8 = [sb("mx8%d" % i, [128, 8], F32, st) for i in range(2)]
        ex4 = [sb("ex4%d" % i, [128, 4], F32, st) for i in range(2)]
        Mb = [sb("Mb%d" % i, [128, 32], BF16, st) for i in range(2)]
        sm4 = [sb("sm4%d" % i, [128, 4], F32, st) for i in range(2)]
        MIXv = MIXT.rearrange("(c p) n -> p c n", p=128)
        for t in range(32):
            i2 = t % 2
            tok = t * 128
            k = lambda s: '%s%d' % (s, i2)
            dma(mt[i2][:], MIXv[:, :, tok:tok + 128], reads=['MIXT'], writes=[k('mt')], sem=k('mt'))
            dma(xo[i2][:], xall[XOWN + tok:XOWN + tok + 128, :], writes=[k('xo')], sem=k('xo'))
            for n2 in range(2):
                for c in range(8):
                    op('pe', mm(yps[i2][:, n2 * 512:(n2 + 1) * 512], mt[i2][:, c, :], Wo[:, c, n2 * 512:(n2 + 1) * 512], c == 0, c == 7),
                       reads=[k('mt'), 'Wo'], writes=[k('yps')])
            op('dve', lambda e, i2=i2: e.tensor_tensor(out=tmp4[:], in0=yps[i2][:, :], in1=g1_bc[:], op=ALU.mult), reads=[k('yps'), 'g1_bc'], writes=['tmp4'])
            op('dve', lambda e, i2=i2: e.tensor_tensor(out=x1t[i2][:], in0=tmp4[:], in1=xo[i2][:], op=ALU.add), reads=['tmp4', k('xo')], writes=[k('x1t')])
            dma(X1[tok:tok + 128, :], x1t[i2][:], reads=[k('x1t')], writes=['X1'], sem=k('x1o'))
            op('act', lambda e, i2=i2: e.activation(out=sq4[:], in_=x1t[i2][:], func=AF.Square, accum_out=st4[i2][:, 0:1]), reads=[k('x1t')], writes=['sq4', k('s4a')])
            op('act', lambda e, i2=i2: e.activation(out=st4[i2][:, 1:2], in_=st4[i2][:, 0:1], func=AF.Sqrt, scale=1.0 / 1024, bias=EPS), reads=[k('s4a')], writes=[k('s4b')])
            op('dve', lambda e, i2=i2: e.reciprocal(out=st4[i2][:, 2:3], in_=st4[i2][:, 1:2]), reads=[k('s4b')], writes=[k('s4c')])
            op('dve', lambda e, i2=i2: e.scalar_tensor_tensor(out=tmp4[:], in0=x1t[i2][:], scalar=st4[i2][:, 2:3], in1=g2s_bc[:], op0=ALU.mult, op1=ALU.mult),
               reads=[k('x1t'), k('s4c'), 'g2s_bc'], writes=['tmp4'])
            op('dve', lambda e, i2=i2: e.tensor_tensor(out=hfb[i2][:], in0=tmp4[:], in1=sh2_bc[:], op=ALU.add), reads=['tmp4', 'sh2_bc'], writes=[k('hfb')])
            dma(HF[tok:tok + 128, :], hfb[i2][:], reads=[k('hfb')], writes=['HF'], sem=k('hfo'))
            for c in range(8):
                op('pe', lambda e, c=c, i2=i2: e.transpose(out=tp4[i2][:, c, :], in_=hfb[i2][:, c * 128:(c + 1) * 128], identity=identb[:]),
                   reads=[k('hfb'), 'identb'], writes=[k('tp4')])
            op('act', lambda e, i2=i2: e.copy(out=hT[i2][:], in_=tp4[i2][:]), reads=[k('tp4')], writes=[k('hT')])
            for c in range(8):
                op('pe', mm(lgp[:, :], hT[i2][:, c, :], Wr[:, c, :], c == 0, c == 7), reads=[k('hT'), 'Wr'], writes=['lgp'])
            op('dve', lambda e, i2=i2: e.tensor_tensor(out=lgt[i2][:], in0=lgp[:, :], in1=brb[:], op=ALU.add), reads=['lgp', 'brb'], writes=[k('lgt')])
            op('dve', lambda e, i2=i2: e.max(out=mx8[i2][:], in_=lgt[i2][:]), reads=[k('lgt')], writes=[k('mx8')])
            op('dve', lambda e, i2=i2: e.tensor_scalar(out=sm4[i2][:, 0:1], in0=mx8[i2][:, 0:1], scalar1=-1.0, scalar2=None, op0=ALU.mult), reads=[k('mx8')], writes=[k('nmx')])
            op('act', lambda e, i2=i2: e.activation(out=ex4[i2][:], in_=mx8[i2][:, 0:4], func=AF.Exp, bias=sm4[i2][:, 0:1]), reads=[k('mx8'), k('nmx')], writes=[k('ex4')])
            op('dve', lambda e, i2=i2: e.reduce_sum(out=sm4[i2][:, 1:2], in_=ex4[i2][:], axis=AX.X), reads=[k('ex4')], writes=[k('sm1')])
            op('dve', lambda e, i2=i2: e.reciprocal(out=sm4[i2][:, 2:3], in_=sm4[i2][:, 1:2]), reads=[k('sm1')], writes=[k('sm2')])
            op('dve', lambda e, i2=i2, t=t: e.tensor_scalar(out=GK[:, t, :], in0=ex4[i2][:], scalar1=sm4[i2][:, 2:3], scalar2=None, op0=ALU.mult),
               reads=[k('ex4'), k('sm2')], writes=['GK'])
            for kk in range(4):
                op('dve', lambda e, i2=i2, t=t, kk=kk: e.tensor_scalar(out=OHall[:, t, kk, :], in0=lgt[i2][:], scalar1=mx8[i2][:, kk:kk + 1], scalar2=None, op0=ALU.is_equal),
                   reads=[k('lgt'), k('mx8')], writes=['OH%d' % t])
            op('dve', lambda e, i2=i2, t=t: e.tensor_scalar(out=Mb[i2][:], in0=lgt[i2][:], scalar1=mx8[i2][:, 3:4], scalar2=None, op0=ALU.is_ge),
               reads=[k('lgt'), k('mx8')], writes=[k('Mb')])
            op('pe', mm(rkp[:, 0:32], ustr[:], Mb[i2][:]), reads=['ustr', k('Mb')], writes=['rkp'])
            op('pe', mm(rkp[:, 32:64], onesb[:], Mb[i2][:]), reads=['onesb', k('Mb')], writes=['rkp'])
            op('dve', lambda e, t=t: e.tensor_tensor(out=Rall[:, t, :], in0=rkp[:, 0:32], in1=CUM[:], op=ALU.add), reads=['rkp', 'CUM'], writes=['Rall'])
            op('dve', lambda e: e.tensor_tensor(out=CUM[:], in0=rkp[:, 32:64], in1=CUM[:], op=ALU.add), reads=['rkp', 'CUM', 'Rall'], writes=['CUM'])
        with contextlib.ExitStack() as s2:
            cf = sb("cf", [128, 32], F32, s2)
            ci = sb("ci", [128, 32], I32, s2)
            padT = sb("padT", [32, 128], F32, s2)
            pse = sb("pse", [128, 64], F32, s2)
            cmp3 = sb("cmp3", [128, NBLK, 32], F32, s2)
            Ef = sb("Ef", [128, NBLK], F32, s2)
            eqf = sb("eqf", [128, NBLK], F32, s2)
            ixf = sb("ixf", [128, NBLK], F32, s2)
            dall = sb("dall", [128, 32], F32, s2)
            dtmp = sb("dtmp", [128, 4, 32], F32, s2)
            dstf = sb("dstf", [128, 32, 4], F32, s2)
            op('dve', lambda e: e.tensor_scalar(out=cf[:], in0=CUM[:], scalar1=127.0, scalar2=None, op0=ALU.add), reads=['CUM'], writes=['cf'])
            op('dve', lambda e: e.tensor_copy(out=ci[:], in_=cf[:]), reads=['cf'], writes=['ci'])
            op('dve', lambda e: e.tensor_scalar(out=ci[:], in0=ci[:], scalar1=7, scalar2=7, op0=ALU.arith_shift_right, op1=ALU.arith_shift_left), reads=['ci'], writes=['ci'])
            op('dve', lambda e: e.tensor_copy(out=cf[:], in_=ci[:]), reads=['ci'], writes=['cf'])
            op('pe', lambda e: e.transpose(out=yps[0][0:32, 0:128], in_=cf[:], identity=ident[:]), reads=['cf', 'ident'], writes=['yps0'])
            op('act', lambda e: e.copy(out=padT[:], in_=yps[0][0:32, 0:128]), reads=['yps0'], writes=['padT'])
            op('pe', mm(rkp[:, 0:64], padT[:], tri[:]), reads=['padT', 'tri'], writes=['rkp'])
            op('act', lambda e: e.copy(out=pse[:], in_=rkp[:, 0:64]), reads=['rkp'], writes=['pse'])
            op('dve', lambda e: e.tensor_tensor(out=cmp3[:], in0=pse[:, 32:64].unsqueeze(1).to_broadcast([128, NBLK, 32]),
                                                in1=rtab[:, 0:NBLK].unsqueeze(2).to_broadcast([128, NBLK, 32]), op=ALU.is_le), reads=['pse', 'rtab'], writes=['cmp3'])
            op('dve', lambda e: e.reduce_sum(out=Ef[:], in_=cmp3[:], axis=AX.X), reads=['cmp3'], writes=['Ef'])
            op('dve', lambda e: e.tensor_scalar(out=Ef[:], in0=Ef[:], scalar1=31.0, scalar2=None, op0=ALU.min), reads=['Ef'], writes=['Ef'])
            op('pool', lambda e: e.memset(eqf[:], 0.0), writes=['eqf'])
            op('dve', lambda e: e.tensor_tensor(out=eqf[:, 2:NBLK], in0=Ef[:, 2:NBLK], in1=Ef[:, 0:NBLK - 2], op=ALU.is_equal), reads=['Ef', 'eqf'], writes=['eqf'])
            op('dve', lambda e: e.tensor_scalar(out=eqf[:], in0=eqf[:], scalar1=BIG, scalar2=None, op0=ALU.mult), reads=['eqf'], writes=['eqf'])
            op('dve', lambda e: e.scalar_tensor_tensor(out=ixf[:], in0=Ef[:], scalar=128.0, in1=eqf[:], op0=ALU.mult, op1=ALU.add), reads=['Ef', 'eqf'], writes=['ixf'])
            op('dve', lambda e: e.tensor_scalar(out=ixf[:], in0=ixf[:], scalar1=rtab[:, NBLK:NBLK + 1], scalar2=None, op0=ALU.add), reads=['ixf', 'rtab'], writes=['ixf'])
            op('dve', lambda e: e.tensor_scalar(out=ixf[:], in0=ixf[:], scalar1=0.0, scalar2=2.0e6, op0=ALU.max, op1=ALU.min), reads=['ixf'], writes=['ixf'])
            op('dve', lambda e: e.tensor_copy(out=IDXW[:], in_=ixf[:]), reads=['ixf'], writes=['IDXW'])
            op('dve', lambda e: e.tensor_tensor(out=ixf[:], in0=Ef[:], in1=eqf[:], op=ALU.add), reads=['Ef', 'eqf', 'IDXW'], writes=['ixf'])
            op('dve', lambda e: e.tensor_scalar(out=ixf[:], in0=ixf[:], scalar1=0.0, scalar2=2.0e6, op0=ALU.max, op1=ALU.min), reads=['ixf'], writes=['ixf'])
            op('dve', lambda e: e.tensor_copy(out=IDXB[:], in_=ixf[:]), reads=['ixf'], writes=['IDXB'])
            for t in range(32):
                op('dve', lambda e, t=t: e.tensor_tensor(out=dall[:], in0=Rall[:, t, :], in1=pse[:, 0:32], op=ALU.add), reads=['Rall', 'pse'], writes=['dall'])
                op('dve', lambda e, t=t: e.tensor_tensor(out=dtmp[:], in0=OHall[:, t, :, :], in1=dall[:].unsqueeze(1).to_broadcast([128, 4, 32]), op=ALU.mult),
                   reads=['OH%d' % t, 'dall'], writes=['dtmp'])
                op('dve', lambda e, t=t: e.reduce_sum(out=dstf[:, t, :], in_=dtmp[:], axis=AX.X), reads=['dtmp'], writes=['dstf'])
            op('dve', lambda e: e.tensor_scalar(out=dstf[:], in0=dstf[:], scalar1=0.0, scalar2=float(NPAD - 1), op0=ALU.max, op1=ALU.min), reads=['dstf'], writes=['dstf'])
            op('dve', lambda e: e.tensor_copy(out=DSTi[:], in_=dstf[:].rearrange("p t k -> p (t k)")), reads=['dstf'], writes=['DSTi'])
            kb.barrier()
        kb.barrier()

      with contextlib.ExitStack() as st:
        with contextlib.ExitStack() as s2:
            W1b = [sb("W1b%d" % i, [128, 8, 2048], BF16, s2) for i in range(2)]
            W2b = [sb("W2b%d" % i, [128, 8, 1024], BF16, s2) for i in range(2)]
            B1b = [sb("B1b%d" % i, [128, 2048], F32, s2) for i in range(2)]
            B2b = [sb("B2b%d" % i, [128, 1024], F32, s2) for i in range(2)]
            xbk = [sb("xbk%d" % i, [128, 1024], BF16, s2) for i in range(4)]
            xTk = [sb("xTk%d" % i, [128, 8, 128], BF16, s2) for i in range(2)]
            t1 = sb("t1", [128, 2048], F32, s2)
            sA = sb("sA", [128, 1024], F32, s2)
            aB = [sb("aB%d" % i, [128, 1024], BF16, s2) for i in range(2)]
            aT = sb("aT", [128, 8, 128], BF16, s2)
            yb = [sb("yb%d" % i, [128, 1024], F32, s2) for i in range(2)]
            up = ps("up", [128, 2048], F32, s2)
            ypm = ps("ypm", [128, 1024], F32, s2)
            tpa = ps("tpa", [128, 8, 128], BF16, s2)
            tpb = ps("tpb", [128, 8, 128], BF16, s2)
            def gatherA(j):
                b = j % 2
                wk = 'wga%d' % b
                kb.idma(out=W1b[b][:].rearrange("p k n -> p (k n)"), out_off=None, in_=W1R[:, :], in_off=IDXW[:, j:j + 1], bounds=4095,
                        reads=['IDXW', 'W1R'], writes=['W1b%d' % b], sem=wk)
                kb.idma(out=B1b[b][:, :], out_off=None, in_=B1R[:, :], in_off=IDXB[:, j:j + 1], bounds=31, reads=['IDXB'], writes=['B1b%d' % b], sem='gb1%d' % b)

            def gatherB(j):
                b = j % 2
                wk = 'wgb%d' % b
                kb.idma(out=W2b[b][:].rearrange("p k n -> p (k n)"), out_off=None, in_=W2R[:, :], in_off=IDXW[:, j:j + 1], bounds=4095,
                        reads=['IDXW', 'W2R'], writes=['W2b%d' % b], sem=wk)
                kb.idma(out=B2b[b][:, :], out_off=None, in_=B2G[:, :], in_off=IDXB[:, j:j + 1], bounds=31, reads=['IDXB', 'B2G'], writes=['B2b%d' % b], sem='gb2%d' % b)

            gatherA(0)
            gatherA(1)
            gatherB(0)
            gatherB(1)
            hfr = [sb("hfr%d" % i, [128, 1024], BF16, s2) for i in range(4)]
            for t in range(32):
                h_, hk = hfr[t % 4], 'hfr%d' % (t % 4)
                dma(h_[:], HF[t * 128:(t + 1) * 128, :], reads=['HF'], writes=[hk], sem=hk)
                for kk in range(4):
                    kb.idma(out=XS[:, :], out_off=DSTi[:, t * 4 + kk:t * 4 + kk + 1], in_=h_[:, :], in_off=None, bounds=NPAD - 1,
                            reads=[hk, 'DSTi', 'XS'], writes=['XSw%d' % kk], sem='xsc')

            def loadx(j):
                b4 = j % 4
                dma(xbk[b4][:], XS[j * 128:(j + 1) * 128, :], reads=['XSw0', 'XSw1', 'XSw2', 'XSw3'], writes=['xbk%d' % b4], sem='xbk%d' % b4)

            def TX(j):
                b, b4 = j % 2, j % 4
                xk, xtk = 'xbk%d' % b4, 'xTk%d' % b
                if j + 3 < NBLK:
                    loadx(j + 3)
                for c in range(8):
                    op('pe', lambda e, c=c, b4=b4: e.transpose(out=tpa[:, c, :], in_=xbk[b4][:, c * 128:(c + 1) * 128], identity=identb[:]), reads=[xk, 'identb'], writes=['tpa'])
                op('act', lambda e, b=b: e.copy(out=xTk[b][:], in_=tpa[:]), reads=['tpa'], writes=[xtk])

            def MM1(j, half):
                b = j % 2
                xtk, w1k = 'xTk%d' % b, 'W1b%d' % b
                for n4 in (0, 1) if half == 0 else (2, 3):
                    for kc in range(8):
                        op('pe', mm(up[:, n4 * 512:(n4 + 1) * 512], xTk[b][:, kc, :], W1b[b][:, kc, n4 * 512:(n4 + 1) * 512], kc == 0, kc == 7), reads=[xtk, w1k], writes=['up'])

            def chain(j):
                b = j % 2
                b1k = 'B1b%d' % b
                op('dve', lambda e, b=b: e.tensor_tensor(out=t1[:], in0=up[:, :], in1=B1b[b][:], op=ALU.add), reads=['up', b1k], writes=['t1'])
                if j + 2 < NBLK:
                    gatherA(j + 2)
                op('dve', lambda e: e.tensor_scalar(out=sA[:], in0=t1[:, 0:1024], scalar1=7.0, scalar2=None, op0=ALU.min), reads=['t1'], writes=['sA'])
                op('act', lambda e: e.activation(out=sA[:], in_=sA[:], func=AF.Silu, scale=1.702), reads=['sA'], writes=['sA'])
                op('dve', lambda e: e.tensor_scalar(out=t1[:, 1024:2048], in0=t1[:, 1024:2048], scalar1=-7.0, scalar2=7.0, op0=ALU.max, op1=ALU.min), reads=['t1'], writes=['t1'])
                op('dve', lambda e, b=b: e.scalar_tensor_tensor(out=aB[b][:], in0=t1[:, 1024:2048], scalar=1.0, in1=sA[:], op0=ALU.add, op1=ALU.mult), reads=['t1', 'sA'], writes=['aB%d' % b])

            def TA(j):
                b = j % 2
                for c in range(8):
                    op('pe', lambda e, c=c, b=b: e.transpose(out=tpb[:, c, :], in_=aB[b][:, c * 128:(c + 1) * 128], identity=identb[:]), reads=['aB%d' % b, 'identb'], writes=['tpb'])
                op('act', lambda e: e.copy(out=aT[:], in_=tpb[:]), reads=['tpb'], writes=['aT'])

            def MM2(j):
                b = j % 2
                w2k, b2k = 'W2b%d' % b, 'B2b%d' % b
                for n2 in range(2):
                    for fc in range(8):
                        op('pe', mm(ypm[:, n2 * 512:(n2 + 1) * 512], aT[:, fc, :], W2b[b][:, fc, n2 * 512:(n2 + 1) * 512], fc == 0, fc == 7), reads=['aT', w2k], writes=['ypm'])
                op('dve', lambda e, b=b: e.tensor_tensor(out=yb[b][:], in0=ypm[:, :], in1=B2b[b][:], op=ALU.add), reads=['ypm', b2k], writes=['yb%d' % b])
                dma(YS[j * 128:(j + 1) * 128, :], yb[b][:], reads=['yb%d' % b], writes=['YS'], sem='ybo%d' % b)
                if j + 2 < NBLK:
                    gatherB(j + 2)

            for j0 in range(3):
                loadx(j0)
            TX(0)
            TX(1)
            MM1(0, 0)
            MM1(0, 1)
            chain(0)
            for j in range(NBLK):
                if j + 2 < NBLK:
                    TX(j + 2)
                if j + 1 < NBLK:
                    MM1(j + 1, 0)
                TA(j)
                if j + 1 < NBLK:
                    MM1(j + 1, 1)
                    chain(j + 1)
                MM2(j)
            kb.barrier()
        with contextlib.ExitStack() as s2:
            yg = [[sb("yg%d_%d" % (i, kk), [128, 1024], F32, s2) for kk in range(4)] for i in range(3)]
            x1r = [sb("x1r%d" % i, [128, 1024], F32, s2) for i in range(2)]
            ac = [sb("ac%d" % i, [128, 1024], F32, s2) for i in range(2)]
            for t in range(32):
                i2 = t % 2
                tok = t * 128
                i3 = t % 3
                gk_ = 'yg%d' % i3
                dma(x1r[i2][:], X1[tok:tok + 128, :], reads=['X1'], writes=['x1r%d' % i2], sem='x1r%d' % i2)
                for kk in range(4):
                    kb.idma(out=yg[i3][kk][:, :], out_off=None, in_=YS[:, :], in_off=DSTi[:, t * 4 + kk:t * 4 + kk + 1], bounds=NPAD - 1,
                            reads=['YS', 'DSTi'], writes=[gk_ + 'b%d' % kk], sem=gk_ + '_%d' % kk)
                a_ = ac[i2]
                akk = 'ac%d' % i2
                op('dve', lambda e, a_=a_, i2=i2, t=t: e.scalar_tensor_tensor(out=a_[:], in0=yg[i3][0][:], scalar=GK[:, t, 0:1], in1=x1r[i2][:], op0=ALU.mult, op1=ALU.add),
                   reads=[gk_ + 'b0', 'GK', 'x1r%d' % i2], writes=[akk])
                for kk in range(1, 4):
                    op('dve', lambda e, a_=a_, i2=i2, t=t, kk=kk: e.scalar_tensor_tensor(out=a_[:], in0=yg[i3][kk][:], scalar=GK[:, t, kk:kk + 1], in1=a_[:], op0=ALU.mult, op1=ALU.add),
                       reads=[gk_ + 'b%d' % kk, 'GK', akk], writes=[akk])
                dma(y[tok:tok + 128, :], a_[:], reads=[akk], writes=['y'], sem='yo%d' % i2)
            kb.barrier()
    if DEBUG:
        dma(dbg['mixt'][:, :], MIXT[:, :], reads=['MIXT'], sem='dbg')
        dma(dbg['x1'][:, :], X1[:, :], reads=['X1'], sem='dbg')
        kb.barrier()
    es.close()
    return nc


def _prep_shared(inp):
    f = np.float32
    w_in = np.asarray(inp['w_in'][0], f)
    sw32 = _swap_idx(32)
    sw64 = _swap_idx(64)
    w_ext = np.zeros((D, NCOL), f)
    w_ext[:, C_CQ:C_CQ + 256] = w_in[:, OFF_Q:OFF_Q + 256]
    w_ext[:, C_CKV:C_CKV + 128] = w_in[:, OFF_KV:OFF_KV + 128]
    w_ext[:, C_PE + 64:C_PE + 96] = w_in[:, OFF_PE:OFF_PE + 32]
    w_ext[:, C_PESW + 64:C_PESW + 96] = w_in[:, OFF_PE + sw32]
    for h in range(4):
        b = C_RET + h * 512
        w_ext[:, b + RQ:b + RQ + 64] = w_in[:, OFF_RQ + h * 64:OFF_RQ + (h + 1) * 64]
        w_ext[:, b + RQS:b + RQS + 64] = w_in[:, OFF_RQ + h * 64 + sw64]
        w_ext[:, b + RK:b + RK + 64] = w_in[:, OFF_RK + h * 64:OFF_RK + (h + 1) * 64]
        w_ext[:, b + RKS:b + RKS + 64] = w_in[:, OFF_RK + h * 64 + sw64]
        w_ext[:, b + RGt:b + RGt + 128] = w_in[:, OFF_RG + h * 128:OFF_RG + (h + 1) * 128]
        w_ext[:, b + RVt:b + RVt + 128] = w_in[:, OFF_RV + h * 128:OFF_RV + (h + 1) * 128]
    wqu = np.asarray(inp['w_q_up'][0], f)
    wq_ext = np.zeros((256, 8, 2, 96), f)
    for h in range(8):
        wq_ext[:, h, 0, :] = wqu[:, h * 96:(h + 1) * 96]
        wq_ext[:, h, 1, 64:96] = wqu[:, h * 96 + 64 + sw32]
    wkv = np.asarray(inp['w_kv_up'][0], f).reshape(128, 8, 128)
    smallv = np.zeros((128, 16), f)
    gql = np.asarray(inp['g_q_lora'][0], f)
    smallv[:, 0] = gql[:128]
    smallv[:, 1] = gql[128:]
    smallv[:, 2] = np.asarray(inp['g_kv_lora'][0], f)
    gqh = np.asarray(inp['g_q_head'][0], f)
    gkh = np.asarray(inp['g_k_head'][0], f)
    smallv[:96, 3] = gqh
    smallv[64:96, 4] = gqh[64 + sw32]
    smallv[:96, 5] = gkh
    smallv[64:96, 6] = gkh[64 + sw32]
    gro = np.asarray(inp['g_ret_out'][0], f)
    for h in range(4):
        smallv[:, 7 + h] = gro[h * 128:(h + 1) * 128]
    b1 = np.asarray(inp['b_mlp1'][0], f).reshape(32, 8, 128, 2)
    sh = {
        'w_ada': np.ascontiguousarray(inp['w_ada'][0], f),
        'b_ada': np.ascontiguousarray(np.asarray(inp['b_ada'][0], f).reshape(48, 128).T),
        'gvec': np.ascontiguousarray(np.concatenate([np.asarray(inp['g_attn'][0], f).reshape(8, 128).T,
                                                     np.asarray(inp['g_ffn'][0], f).reshape(8, 128).T], axis=1)),
        'w_ext': w_ext,
        'wq_ext': wq_ext.reshape(256, -1),
        'wkv_k': np.ascontiguousarray(wkv[:, :, :64]).reshape(128, -1),
        'wkv_v': np.ascontiguousarray(wkv[:, :, 64:]).reshape(128, -1),
        'smallv': smallv,
        'lgin': np.ascontiguousarray(np.broadcast_to(np.asarray(inp['ret_decay_logit'][0], f).reshape(1, 8), (128, 8))),
        'w_out': np.ascontiguousarray(inp['w_out'][0], f),
        'w_router': np.ascontiguousarray(inp['w_router'][0], f),
        'br_bc': np.ascontiguousarray(np.broadcast_to(np.asarray(inp['b_router'][0], f).reshape(1, 32), (128, 32))),
        'w1': np.ascontiguousarray(inp['w_mlp1'][0], f),
        'w2': np.ascontiguousarray(inp['w_mlp2'][0], f),
        'b2': np.ascontiguousarray(inp['b_mlp2'][0], f),
        'identf': np.eye(128, dtype=f),
        'ustrict': np.triu(np.ones((128, 128), f), 1),
        'tri32': np.concatenate([np.triu(np.ones((32, 32), f), 1), np.triu(np.ones((32, 32), f), 0)], axis=1),
        'routetab': np.ascontiguousarray(np.concatenate([np.broadcast_to(128.0 * np.arange(160, dtype=f)[None, :], (128, 160)),
                                                         np.arange(128, dtype=f)[:, None]], axis=1)),
        'B1R': np.ascontiguousarray(np.asarray(inp['b_mlp1'][0], f).reshape(32, D, 2).transpose(0, 2, 1)).reshape(32, 2 * D),
    }
    jj = np.arange(128, dtype=f)[:, None]
    ii = np.arange(512, dtype=f)[None, :]
    dg = np.zeros((4, 3, 128, 512), f)
    for r in range(4):
        d = ii - (128 * r + jj)
        dg[r, 0] = np.where(d > 0, d, BIG)
        dg[r, 1] = np.where(d < 0, -d, BIG)
        dg[r, 2] = np.where(d == 0, 2.0, 0.0)
    sh['dgtab'] = dg
    return sh


def _prep_core(core, inp, sh):
    f = np.float32
    b, half = core // 2, core % 2
    x = np.asarray(inp['x'], f)
    own = slice(half * NOWN, (half + 1) * NOWN)
    oth = slice((1 - half) * NOWN, (2 - half) * NOWN)
    m = dict(sh)
    m['xall'] = np.ascontiguousarray(np.concatenate([np.asarray(inp['ctx'][b], f), x[b, oth], x[b, own]], axis=0))
    cv = np.stack([np.asarray(inp['c'][b], f), np.asarray(inp['c_ctx'], f)], axis=-1)
    m['cvec'] = np.ascontiguousarray(cv.reshape(8, 128, 2).transpose(1, 0, 2))
    t = np.arange(2 * NOWN)
    prow, pcol = (t // 64).astype(f), (t % 64).astype(f)
    for dim, kn, qn in ((32, 'kcs', 'qcs'), (64, 'rkcs', 'rqcs')):
        cos, sin = _rope_tables(prow, pcol, dim)
        kc = np.concatenate([np.ones((256, dim), f), cos[oth], cos[own]], axis=0)
        ks = np.concatenate([np.zeros((256, dim), f), sin[oth], sin[own]], axis=0)
        m[kn] = np.ascontiguousarray(np.stack([kc.T, ks.T], axis=0))
        m[qn] = np.ascontiguousarray(np.stack([cos[own].T, sin[own].T], axis=0))
    jj = np.arange(128, dtype=f)[:, None]
    ii = np.arange(512, dtype=f)[None, :]
    s = 1.0 if half == 1 else -1.0
    iq = np.broadcast_to(ii, (128, 512)).astype(f)
    m['utab'] = np.ascontiguousarray(np.stack([iq, -iq, s * iq], axis=0).astype(f))
    base = np.zeros((5, 8, 32), f)
    for qb in range(8):
        for kt in range(32):
            if kt < 2:
                base[0, qb, kt] = half * 4096 + qb * 512 + 256 - kt * 128
                base[4, qb, kt] = 8192 - half * 4096 - qb * 512 + kt * 128
            base[1, qb, kt] = (4096 + qb * 512 - kt * 128) if half == 1 else (4096 + kt * 128 - qb * 512)
            base[2, qb, kt] = qb * 512 - kt * 128
            base[3, qb, kt] = kt * 128 - qb * 512
    sgn = np.array([-1.0, -s, -1.0, 1.0, 1.0], f)
    cw = base.reshape(1, 5, 256) + sgn.reshape(1, 5, 1) * np.arange(128, dtype=f).reshape(128, 1, 1)
    m['cwtab'] = np.ascontiguousarray(cw.reshape(128, 5 * 256).astype(f))
    m['flagv'] = np.ascontiguousarray(np.broadcast_to(np.array([[half, 1 - half]], f), (128, 2)))
    return m


def kernel(**inputs):
    sh = _prep_shared(inputs)
    in_maps = [_prep_core(c, inputs, sh) for c in range(NCORES)]
    nc = build_nc()
    res = run_bass_kernel_spmd(nc, in_maps, core_ids=list(range(NCORES)))
    out = np.zeros((4, 2 * NOWN, D), np.float32)
    for c in range(NCORES):
        b, half = c // 2, c % 2
        out[b, half * NOWN:(half + 1) * NOWN] = res.results[c]["y"]
    if DEBUG:
        kernel.last = res
    return out
```
